# Optimizing a Trainium2 kernel written in Bass

```python
import jax
import jax.numpy as jnp
from jax import lax
import numpy as np

D_MODEL = 2048
BATCH = 4
SEQ = 2048
DEPTH = 2

GRID_W = 64
CTX_LEN = 256

NA_HEADS = 16
NA_HEAD_DIM = 64
NA_WIN_R_MAX = 8
NA_WIN_C = 16
NA_W = NA_HEADS * NA_HEAD_DIM

GLA_HEADS = 4
GLA_DK = 128
GLA_DV = 256
GLA_QK = GLA_HEADS * GLA_DK
GLA_V = GLA_HEADS * GLA_DV
GLA_GATE_RANK = 16
GLA_GATE_TAU = 16.0
GLA_CHUNK = 64

RW_HEADS = 16
RW_HEAD = 64
RW_W = RW_HEADS * RW_HEAD
RW_DECAY_RANK = 96
RW_A_RANK = 96
RW_GATE_RANK = 256
RW_SPLITS = (RW_W, RW_W, RW_W, 2 * RW_DECAY_RANK, 2 * RW_A_RANK, RW_GATE_RANK)
RW_IN = sum(RW_SPLITS)
RW_GN_EPS = 64e-5

N_BRANCH = 3
BRANCH_W = 1024
IN_SPLITS = (NA_W, NA_W, NA_W, GLA_QK, GLA_QK, GLA_V, GLA_V, 2 * GLA_GATE_RANK, RW_IN, N_BRANCH * D_MODEL)
D_IN = sum(IN_SPLITS)

N_EXPERTS = 16
N_GROUPS = 4
EXPERTS_PER_GROUP = N_EXPERTS // N_GROUPS
TOP_K = 2
D_EXPERT = 512

ROPE_BASE = 10000.0
EPS = 1e-6
F32 = jnp.float32

kernel_name = 'hybrid_na_gla_rwkv7_grouped_moe_dit'


def split_at(z, sizes):
    return jnp.split(z, np.cumsum(sizes)[:-1].tolist(), axis=-1)


def rms_norm(x, g):
    xf = x.astype(F32)
    y = xf * lax.rsqrt(jnp.mean(xf * xf, axis=-1, keepdims=True) + EPS)
    return (y * g.astype(F32)).astype(x.dtype)


def axial_angles(pos, dim):
    inv = ROPE_BASE ** (-jnp.arange(0, dim, 2, dtype=F32) / dim)
    ang = pos.astype(F32)[:, None] * inv[None, :]
    return jnp.cos(ang), jnp.sin(ang)


def rotate_half(x, cos, sin):
    x1, x2 = jnp.split(x, 2, axis=-1)
    cos = cos[None, :, None, :]
    sin = sin[None, :, None, :]
    return jnp.concatenate([x1 * cos - x2 * sin, x1 * sin + x2 * cos], axis=-1)


def rope_2d(x, rows, cols):
    half = x.shape[-1] // 2
    xr, xc = jnp.split(x.astype(F32), 2, axis=-1)
    xr = rotate_half(xr, *axial_angles(rows, half))
    xc = rotate_half(xc, *axial_angles(cols, half))
    return jnp.concatenate([xr, xc], axis=-1).astype(x.dtype)


def flip(t):
    return jnp.flip(t, axis=1)


def na_mixer(zl, zc, q_norm, k_norm, rpb, with_ctx):
    B, S, _ = zl[0].shape
    scale = NA_HEAD_DIM ** -0.5

    def heads(t):
        return t.reshape(t.shape[0], t.shape[1], NA_HEADS, NA_HEAD_DIM)

    ql = rms_norm(heads(zl[0]), q_norm) * scale
    kl = rms_norm(heads(zl[1]), k_norm)
    vl = heads(zl[2])
    qc = rms_norm(heads(zc[0]), q_norm) * scale
    kc = rms_norm(heads(zc[1]), k_norm)
    vc = heads(zc[2])

    n_rows = S // GRID_W
    win_r = min(NA_WIN_R_MAX, n_rows)
    n_loc = win_r * NA_WIN_C

    def grid(t):
        return t.reshape(B, n_rows, GRID_W, NA_HEADS, NA_HEAD_DIM)

    qg, kg, vg = grid(ql), grid(kl), grid(vl)
    col = np.arange(GRID_W)
    col_start = np.clip(col - NA_WIN_C // 2, 0, GRID_W - NA_WIN_C)
    col_idx = col_start[:, None] + np.arange(NA_WIN_C)[None, :]
    dc_idx = col_idx - col[:, None] + NA_WIN_C - 1
    rpb_c = rpb[:, :, dc_idx]

    def row_block(r):
        rs = jnp.clip(r - win_r // 2, 0, n_rows - win_r)
        q_r = lax.dynamic_index_in_dim(qg, r, axis=1, keepdims=False)
        k_win = lax.dynamic_slice_in_dim(kg, rs, win_r, axis=1)[:, :, col_idx]
        v_win = lax.dynamic_slice_in_dim(vg, rs, win_r, axis=1)[:, :, col_idx]
        dr_idx = rs + jnp.arange(win_r) - r + NA_WIN_R_MAX - 1
        bias = jnp.take(rpb_c, dr_idx, axis=1).transpose(0, 2, 1, 3)
        s_loc = jnp.einsum('bqhd,bnqjhd->bhqnj', q_r, k_win).astype(F32) + bias.astype(F32)[None]
        s_ctx = jnp.einsum('bqhd,blhd->bhql', q_r, kc).astype(F32)
        scores = jnp.concatenate([s_loc.reshape(B, NA_HEADS, GRID_W, n_loc), s_ctx], axis=-1)
        p = jax.nn.softmax(scores, axis=-1).astype(vl.dtype)
        p_loc = p[..., :n_loc].reshape(B, NA_HEADS, GRID_W, win_r, NA_WIN_C)
        return (jnp.einsum('bhqnj,bnqjhd->bqhd', p_loc, v_win)
                + jnp.einsum('bhql,blhd->bqhd', p[..., n_loc:], vc))

    o = lax.map(row_block, jnp.arange(n_rows))
    y = o.transpose(1, 0, 2, 3, 4).reshape(B, S, NA_W)
    yc = None
    if with_ctx:
        pc = jax.nn.softmax(jnp.einsum('bqhd,blhd->bhql', qc, kc).astype(F32), axis=-1).astype(vc.dtype)
        yc = jnp.einsum('bhql,blhd->bqhd', pc, vc).reshape(B, -1, NA_W)
    return y, yc


def gla_scan(q, k, v, log_a, s0):
    q, k, v, log_a = (t.astype(F32) for t in (q, k, v, log_a))
    B, T, H, dk = q.shape
    dv = v.shape[-1]
    C = GLA_CHUNK
    N = T // C

    def chunks(t):
        return t.reshape(B, N, C, H, t.shape[-1])

    q, k, v, log_a = chunks(q), chunks(k), chunks(v), chunks(log_a)
    b = jnp.cumsum(log_a, axis=2)
    b_last = b[:, :, -1:]
    b_ref = b[:, :, C // 2:C // 2 + 1]
    qi = q * jnp.exp(b - b_ref)
    kj = k * jnp.exp(b_ref - b)
    att = jnp.einsum('bnihd,bnjhd->bnhij', qi, kj)
    att = jnp.where(jnp.tril(jnp.ones((C, C), dtype=bool)), att, 0.0)
    y = jnp.einsum('bnhij,bnjhv->bnihv', att, v)
    kv = jnp.einsum('bnjhd,bnjhv->bnhdv', k * jnp.exp(b_last - b), v)
    decay = jnp.exp(b_last[:, :, 0])

    def step(s, inp):
        dec, kv_n = inp
        return dec[..., None] * s + kv_n, s

    s_fin, s_prev = lax.scan(step, s0, (jnp.moveaxis(decay, 1, 0), jnp.moveaxis(kv, 1, 0)))
    s_prev = jnp.moveaxis(s_prev, 0, 1)
    y = y + jnp.einsum('bnihd,bnhdv->bnihv', q * jnp.exp(b), s_prev)
    return y.reshape(B, T, H, dv), s_fin


def gla_mixer(zl, zc, rows, cols, gate_w2, gate_b, norm_g, with_ctx):
    def prep(z, pos):
        q, k, v, r, gd = z
        B, T, _ = q.shape
        q = q.reshape(B, T, GLA_HEADS, GLA_DK) * GLA_DK ** -0.5
        k = k.reshape(B, T, GLA_HEADS, GLA_DK)
        if pos is not None:
            q = rope_2d(q, pos[0], pos[1])
            k = rope_2d(k, pos[0], pos[1])
        v = v.reshape(B, T, GLA_HEADS, GLA_DV)
        gds = jnp.split(gd, 2, axis=-1)
        log_a = []
        for d in range(2):
            pre = (gds[d] @ gate_w2[d] + gate_b[d]).astype(F32)
            log_a.append((jax.nn.log_sigmoid(pre) / GLA_GATE_TAU).reshape(B, T, GLA_HEADS, GLA_DK))
        return q, k, v, r, log_a

    ql, kl, vl, rl, lal = prep(zl, (rows, cols))
    qc, kc, vc, rc, lac = prep(zc, None)
    B = ql.shape[0]
    s0 = jnp.zeros((B, GLA_HEADS, GLA_DK, GLA_DV), F32)
    yc_f, sc_f = gla_scan(qc, kc, vc, lac[0], s0)
    yc_b, sc_b = gla_scan(flip(qc), flip(kc), flip(vc), flip(lac[1]), s0)
    yl_f, _ = gla_scan(ql, kl, vl, lal[0], sc_f)
    yl_b, _ = gla_scan(flip(ql), flip(kl), flip(vl), flip(lal[1]), sc_b)

    def finish(y_f, y_b_rev, r):
        y = rms_norm(y_f + flip(y_b_rev), norm_g)
        return (y.reshape(r.shape) * jax.nn.silu(r.astype(F32))).astype(r.dtype)

    y = finish(yl_f, yl_b, rl)
    yc = finish(yc_f, yc_b, rc) if with_ctx else None
    return y, yc


def token_shift(z, mu):
    prev = jnp.pad(z[:, :-1], ((0, 0), (1, 0), (0, 0)))
    nxt = jnp.pad(z[:, 1:], ((0, 0), (0, 1), (0, 0)))
    return z + mu * (0.5 * (prev + nxt) - z)


def rwkv_scan(r, w, k, v, kk, a, s0):
    def step(s, inp):
        r_t, w_t, k_t, v_t, kk_t, a_t = inp
        sa = jnp.einsum('bhvk,bhk->bhv', s, kk_t)
        s = (s * w_t[:, :, None, :] - sa[..., None] * (kk_t * a_t)[:, :, None, :]
             + v_t[..., None] * k_t[:, :, None, :])
        return s, jnp.einsum('bhvk,bhk->bhv', s, r_t)

    xs = tuple(jnp.moveaxis(t, 1, 0) for t in (r, w, k, v, kk, a))
    s_fin, ys = lax.scan(step, s0, xs)
    return jnp.moveaxis(ys, 0, 1), s_fin


def rwkv_prep(z, mu, w0, w2, a0, a2, g2, k_k, k_a):
    B, T, _ = z.shape
    z = token_shift(z, mu)
    r, k, v, wd, ad, gd = split_at(z, RW_SPLITS)

    def heads(t):
        return t.reshape(B, T, RW_HEADS, RW_HEAD).astype(F32)

    kk = heads(k * k_k)
    kk = kk * lax.rsqrt(jnp.sum(kk * kk, axis=-1, keepdims=True) + EPS)
    wds = jnp.split(wd, 2, axis=-1)
    ads = jnp.split(ad, 2, axis=-1)
    per_dir = []
    for d in range(2):
        w_log = (-jax.nn.softplus(-(w0[d] + jnp.tanh(wds[d]) @ w2[d])) - 0.5).astype(F32)
        decay = jnp.exp(-jnp.exp(w_log))
        a = jax.nn.sigmoid(a0[d] + ads[d] @ a2[d])
        k_d = k * (1.0 + (a - 1.0) * k_a)
        per_dir.append((heads(decay), heads(k_d), heads(a)))
    g = jax.nn.sigmoid(gd) @ g2
    return heads(r), heads(k), heads(v), kk, per_dir, g


def rwkv_mixer(zl, zc, mu, w0, w2, a0, a2, g2, k_k, k_a, r_k, gn_g, gn_b, with_ctx):
    rl, kl, vl, kkl, dl, gl = rwkv_prep(zl, mu, w0, w2, a0, a2, g2, k_k, k_a)
    rc, kc, vc, kkc, dcx, gc = rwkv_prep(zc, mu, w0, w2, a0, a2, g2, k_k, k_a)
    B = rl.shape[0]
    s0 = jnp.zeros((B, RW_HEADS, RW_HEAD, RW_HEAD), F32)
    yc_f, sc_f = rwkv_scan(rc, dcx[0][0], dcx[0][1], vc, kkc, dcx[0][2], s0)
    yc_b, sc_b = rwkv_scan(flip(rc), flip(dcx[1][0]), flip(dcx[1][1]), flip(vc), flip(kkc), flip(dcx[1][2]), s0)
    yl_f, _ = rwkv_scan(rl, dl[0][0], dl[0][1], vl, kkl, dl[0][2], sc_f)
    yl_b, _ = rwkv_scan(flip(rl), flip(dl[1][0]), flip(dl[1][1]), flip(vl), flip(kkl), flip(dl[1][2]), sc_b)
    gamma = gn_g.astype(F32).reshape(RW_HEADS, RW_HEAD)
    beta = gn_b.astype(F32).reshape(RW_HEADS, RW_HEAD)

    def finish(y_f, y_b_rev, r, k, v, g):
        y = y_f + flip(y_b_rev)
        mean = jnp.mean(y, axis=-1, keepdims=True)
        var = jnp.mean(jnp.square(y - mean), axis=-1, keepdims=True)
        y = (y - mean) * lax.rsqrt(var + RW_GN_EPS) * gamma + beta
        y = y + jnp.sum(r * k * r_k.astype(F32), axis=-1, keepdims=True) * v
        return (y.reshape(g.shape) * g.astype(F32)).astype(g.dtype)

    y = finish(yl_f, yl_b, rl, kl, vl, gl)
    yc = finish(yc_f, yc_b, rc, kc, vc, gc) if with_ctx else None
    return y, yc


def merge_branches(ys, gates, w_branch, w_out):
    y = jnp.stack(ys, axis=-2)
    proj = jnp.einsum('btiw,iwd->btid', y, w_branch)
    g = jax.nn.sigmoid(gates.reshape(gates.shape[:-1] + (N_BRANCH, D_MODEL)))
    return jnp.sum(g * proj, axis=-2) @ w_out


def token_mixers(h, hc, rows, cols, p, with_ctx):
    zl = split_at(h @ p['w_in'], IN_SPLITS)
    zc = split_at(hc @ p['w_in'], IN_SPLITS)
    y_na, yc_na = na_mixer(zl[0:3], zc[0:3], p['na_q_norm'], p['na_k_norm'], p['na_rpb'], with_ctx)
    y_gla, yc_gla = gla_mixer(zl[3:8], zc[3:8], rows, cols, p['gla_gate_w2'], p['gla_gate_b'],
                              p['gla_norm_g'], with_ctx)
    y_rw, yc_rw = rwkv_mixer(zl[8], zc[8], p['rw_mu'], p['rw_w0'], p['rw_w2'], p['rw_a0'], p['rw_a2'],
                             p['rw_g2'], p['rw_k_k'], p['rw_k_a'], p['rw_r_k'], p['rw_gn_g'],
                             p['rw_gn_b'], with_ctx)
    y = merge_branches((y_na, y_gla, y_rw), zl[9], p['w_branch'], p['w_out'])
    yc = merge_branches((yc_na, yc_gla, yc_rw), zc[9], p['w_branch'], p['w_out']) if with_ctx else None
    return y, yc


def moe_ffn(h, router_w, router_bias, w_gate, w_up, w_down):
    shp = h.shape
    t = h.reshape(-1, shp[-1])
    s = jax.nn.sigmoid(t.astype(F32) @ router_w.astype(F32))
    biased = s + router_bias.astype(F32)
    grp = biased.reshape(-1, N_GROUPS, EXPERTS_PER_GROUP)
    grp_score = jnp.sum(lax.top_k(grp, TOP_K)[0], axis=-1)
    g_sel = jnp.argmax(grp_score, axis=-1)
    in_group = (jnp.arange(N_EXPERTS) // EXPERTS_PER_GROUP)[None, :] == g_sel[:, None]
    _, idx = lax.top_k(jnp.where(in_group, biased, -jnp.inf), TOP_K)
    w_sel = jnp.take_along_axis(s, idx, axis=-1)
    w_sel = w_sel / jnp.sum(w_sel, axis=-1, keepdims=True)
    comb = jnp.sum(jax.nn.one_hot(idx, N_EXPERTS, dtype=F32) * w_sel[..., None], axis=1)
    act = jax.nn.silu(jnp.einsum('td,edf->tef', t, w_gate)) * jnp.einsum('td,edf->tef', t, w_up)
    y = jnp.einsum('tef,efd->td', act * comb[..., None].astype(act.dtype), w_down)
    return y.reshape(shp)


def setup_inputs(seed: int = 0) -> dict:
    key = jax.random.key(seed)
    ks = iter(jax.random.split(key, 48))
    L, D = DEPTH, D_MODEL

    def nrm(shape, scale):
        return scale * jax.random.normal(next(ks), shape, F32)

    def gain(shape):
        return 1.0 + nrm(shape, 0.02)

    return {
        'x': nrm((BATCH, SEQ, D), 1.0),
        'c': nrm((BATCH, D), 1.0),
        'ctx': nrm((BATCH, CTX_LEN, D), 1.0),
        'c_ctx': nrm((D,), 1.0),
        'ada_w': nrm((L, D, 6 * D), 0.5 * D ** -0.5),
        'ada_b': nrm((L, 6 * D), 0.02),
        'norm1_g': gain((L, D)),
        'norm2_g': gain((L, D)),
        'w_in': nrm((L, D, D_IN), D ** -0.5),
        'na_q_norm': gain((L, NA_HEAD_DIM)),
        'na_k_norm': gain((L, NA_HEAD_DIM)),
        'na_rpb': nrm((L, NA_HEADS, 2 * NA_WIN_R_MAX - 1, 2 * NA_WIN_C - 1), 0.1),
        'gla_gate_w2': nrm((L, 2, GLA_GATE_RANK, GLA_QK), GLA_GATE_RANK ** -0.5),
        'gla_gate_b': nrm((L, 2, GLA_QK), 0.1),
        'gla_norm_g': gain((L, GLA_DV)),
        'rw_mu': jax.random.uniform(next(ks), (L, RW_IN), F32),
        'rw_w0': jax.random.uniform(next(ks), (L, 2, RW_W), F32, -6.0, -1.0),
        'rw_w2': nrm((L, 2, RW_DECAY_RANK, RW_W), 0.1 * RW_DECAY_RANK ** -0.5),
        'rw_a0': nrm((L, 2, RW_W), 0.1),
        'rw_a2': nrm((L, 2, RW_A_RANK, RW_W), 0.3 * RW_A_RANK ** -0.5),
        'rw_g2': nrm((L, RW_GATE_RANK, RW_W), RW_GATE_RANK ** -0.5),
        'rw_k_k': 0.85 + nrm((L, RW_W), 0.05),
        'rw_k_a': 1.0 + nrm((L, RW_W), 0.05),
        'rw_r_k': nrm((L, RW_HEADS, RW_HEAD), 0.1),
        'rw_gn_g': gain((L, RW_W)),
        'rw_gn_b': nrm((L, RW_W), 0.02),
        'w_branch': nrm((L, N_BRANCH, BRANCH_W, D), BRANCH_W ** -0.5),
        'w_out': nrm((L, D, D), D ** -0.5),
        'router_w': nrm((D, N_EXPERTS), D ** -0.5),
        'router_bias': nrm((N_EXPERTS,), 0.01),
        'moe_w_gate': nrm((L, N_EXPERTS, D, D_EXPERT), D ** -0.5),
        'moe_w_up': nrm((L, N_EXPERTS, D, D_EXPERT), D ** -0.5),
        'moe_w_down': nrm((L, N_EXPERTS, D_EXPERT, D), D_EXPERT ** -0.5),
    }


def reference(x, c, ctx, c_ctx, ada_w, ada_b, norm1_g, norm2_g, w_in, na_q_norm, na_k_norm, na_rpb,
              gla_gate_w2, gla_gate_b, gla_norm_g, rw_mu, rw_w0, rw_w2, rw_a0, rw_a2, rw_g2,
              rw_k_k, rw_k_a, rw_r_k, rw_gn_g, rw_gn_b, w_branch, w_out, router_w, router_bias,
              moe_w_gate, moe_w_up, moe_w_down):
    S = x.shape[1]
    t = jnp.arange(S, dtype=jnp.int32)
    rows = t // GRID_W
    cols = t % GRID_W
    for l in range(DEPTH):
        with_ctx = l < DEPTH - 1
        p = {
            'w_in': w_in[l], 'na_q_norm': na_q_norm[l], 'na_k_norm': na_k_norm[l], 'na_rpb': na_rpb[l],
            'gla_gate_w2': gla_gate_w2[l], 'gla_gate_b': gla_gate_b[l], 'gla_norm_g': gla_norm_g[l],
            'rw_mu': rw_mu[l], 'rw_w0': rw_w0[l], 'rw_w2': rw_w2[l], 'rw_a0': rw_a0[l], 'rw_a2': rw_a2[l],
            'rw_g2': rw_g2[l], 'rw_k_k': rw_k_k[l], 'rw_k_a': rw_k_a[l], 'rw_r_k': rw_r_k[l],
            'rw_gn_g': rw_gn_g[l], 'rw_gn_b': rw_gn_b[l], 'w_branch': w_branch[l], 'w_out': w_out[l],
        }
        mod = jax.nn.silu(c) @ ada_w[l] + ada_b[l]
        mod_c = jax.nn.silu(c_ctx) @ ada_w[l] + ada_b[l]
        sh1, sc1, g1, sh2, sc2, g2 = jnp.split(mod[:, None, :], 6, axis=-1)
        csh1, csc1, cg1, csh2, csc2, cg2 = jnp.split(mod_c, 6)

        h = rms_norm(x, norm1_g[l]) * (1.0 + sc1) + sh1
        hc = rms_norm(ctx, norm1_g[l]) * (1.0 + csc1) + csh1
        y, yc = token_mixers(h, hc, rows, cols, p, with_ctx)
        x = x + g1 * y
        h2 = rms_norm(x, norm2_g[l]) * (1.0 + sc2) + sh2
        x = x + g2 * moe_ffn(h2, router_w, router_bias, moe_w_gate[l], moe_w_up[l], moe_w_down[l])

        if with_ctx:
            ctx = ctx + cg1 * yc
            hc2 = rms_norm(ctx, norm2_g[l]) * (1.0 + csc2) + csh2
            ctx = ctx + cg2 * moe_ffn(hc2, router_w, router_bias, moe_w_gate[l], moe_w_up[l], moe_w_down[l])
    return x
```

```python
import contextlib
import numpy as np
import concourse.bass as bass
import concourse.mybir as mybir
from concourse.bass_utils import run_bass_kernel_spmd

F32 = mybir.dt.float32
BF16 = mybir.dt.bfloat16
AF = mybir.ActivationFunctionType
ALU = mybir.AluOpType
AX = mybir.AxisListType

D = 2048
KC = 16
NCTX = 256
SEQ = 2048
TT = NCTX + SEQ
D_IN = 16032
EPS = 1e-6
NEG = -30000.0
DEPTH = 2

C_NAQ, C_NAK, C_NAV = 0, 1024, 2048
C_GQ, C_GK, C_GV, C_GR, C_GGD = 3072, 3584, 4096, 5120, 6144
C_RW = 6176
C_RR, C_RK, C_RV, C_RWD, C_RAD, C_RGD = C_RW, C_RW + 1024, C_RW + 2048, C_RW + 3072, C_RW + 3264, C_RW + 3456
C_GATE = 9888

TILES = [(0, 256), (256, 512), (768, 512), (1280, 512), (1792, 512)]

NDS = 12


class Prog:
    def __init__(self, nc, es):
        self.nc = nc
        self.e = dict(pe=nc.tensor, act=nc.scalar, dve=nc.vector, pool=nc.gpsimd, sp=nc.sync)
        self.sem = {k: es.enter_context(nc.semaphore("s_" + k)) for k in self.e}
        self.cnt = {k: 0 for k in self.e}
        self.seen = {k: {} for k in self.e}
        self.lw = {}
        self.rd = {}
        self.dsem = [es.enter_context(nc.semaphore(f"dq{i}")) for i in range(NDS)]
        self.dcnt = [0] * NDS
        self.dnext = 0

    def semh(self, k):
        return self.dsem[k[1]] if isinstance(k, tuple) else self.sem[k]

    def _wait(self, eng, k, v):
        if self.seen[eng].get(k, 0) < v:
            self.e[eng].wait_ge(self.semh(k), v)
            self.seen[eng][k] = v

    def _deps(self, eng, reads, writes):
        deps = {}
        for b in reads:
            lw = self.lw.get(b)
            if lw:
                deps[lw[0]] = max(deps.get(lw[0], 0), lw[1])
        for b in writes:
            lw = self.lw.get(b)
            if lw:
                deps[lw[0]] = max(deps.get(lw[0], 0), lw[1])
            for k, v in self.rd.get(b, {}).items():
                deps[k] = max(deps.get(k, 0), v)
        for k, v in deps.items():
            if k == 'pe' and eng == 'pe':
                continue
            self._wait(eng, k, v)

    def _mark(self, pt, reads, writes):
        for b in writes:
            self.lw[b] = pt
            self.rd[b] = {}
        for b in reads:
            d = self.rd.setdefault(b, {})
            d[pt[0]] = max(d.get(pt[0], 0), pt[1])

    halt = False

    pe_rg = None
    dummy = None

    def op(self, eng, reads, writes, fn, rg=None):
        if self.halt:
            return None
        if eng == 'pe':
            if rg is not None and self.pe_rg is not None and self.pe_rg != rg:
                self.cnt['pe'] += 1
                self.dummy(self.e['pe']).then_inc(self.sem['pe'], 1)
            self.pe_rg = rg
        self._deps(eng, reads, writes)
        ins = fn(self.e[eng])
        self.cnt[eng] += 1
        ins.then_inc(self.sem[eng], 1)
        self._mark((eng, self.cnt[eng]), reads, writes)
        return ins

    def dma(self, out, in_, reads, writes, q='sp', **kw):
        if self.halt:
            return None
        i = self.dnext % NDS
        self.dnext += 1
        k = ('d', i)
        if self.dcnt[i]:
            self._wait(q, k, self.dcnt[i])
        self._deps(q, reads, writes)
        ins = self.e[q].dma_start(out=out, in_=in_, **kw)
        self.dcnt[i] += 16
        ins.then_inc(self.dsem[i], 16)
        self._mark((k, self.dcnt[i]), reads, writes)
        return ins

    def barrier(self):
        for q in self.e:
            for i in range(NDS):
                if self.dcnt[i]:
                    self._wait(q, ('d', i), self.dcnt[i])
            for k in self.e:
                if k != q and self.cnt[k]:
                    self._wait(q, k, self.cnt[k])
        self.lw = {}
        self.rd = {}

    def finish(self, q='sp'):
        for i in range(NDS):
            if self.dcnt[i]:
                self.e[q].wait_ge(self.dsem[i], self.dcnt[i])
        for k in self.e:
            if k != q and self.cnt[k]:
                self.e[q].wait_ge(self.sem[k], self.cnt[k])


def fm(v):
    v = np.asarray(v, np.float32)
    return np.ascontiguousarray(v.reshape(-1, 128).T)


VEC_SLOTS = {}


def _vec_layout():
    off = 0
    def add(name, n):
        nonlocal off
        VEC_SLOTS[name] = (off, n)
        off += n
    add('n1g', 16); add('n2g', 16); add('adab', 96)
    add('naq', 1); add('nak', 1)
    add('ggb', 8)
    add('gng', 2)
    add('mu_r', 8); add('mu_k', 8); add('mu_v', 8); add('mu_wd', 2); add('mu_ad', 2); add('mu_gd', 2)
    add('w0', 16); add('a0', 16); add('kk', 8); add('ka', 8); add('rk', 8); add('gng_rw', 8); add('gnb_rw', 8)
    return off


NV = _vec_layout()


def pack_vecs(inp, l):
    v = np.zeros((128, NV), np.float32)
    def put(name, arr):
        o, n = VEC_SLOTS[name]
        assert arr.shape == (128, n), (name, arr.shape)
        v[:, o:o + n] = arr
    put('n1g', fm(inp['norm1_g'][l])); put('n2g', fm(inp['norm2_g'][l])); put('adab', fm(inp['ada_b'][l]))
    put('naq', np.tile(inp['na_q_norm'][l], 2)[:, None]); put('nak', np.tile(inp['na_k_norm'][l], 2)[:, None])
    put('ggb', fm(inp['gla_gate_b'][l].reshape(-1)))
    put('gng', fm(inp['gla_norm_g'][l]))
    mu = inp['rw_mu'][l]
    put('mu_r', fm(mu[0:1024])); put('mu_k', fm(mu[1024:2048])); put('mu_v', fm(mu[2048:3072]))
    def pad96(a):
        o = np.zeros((128, 2), np.float32); o[:96, 0] = a[:96]; o[:96, 1] = a[96:192]; return o
    put('mu_wd', pad96(mu[3072:3264])); put('mu_ad', pad96(mu[3264:3456])); put('mu_gd', fm(mu[3456:3712]))
    put('w0', fm(inp['rw_w0'][l].reshape(-1))); put('a0', fm(inp['rw_a0'][l].reshape(-1)))
    put('kk', fm(inp['rw_k_k'][l])); put('ka', fm(inp['rw_k_a'][l])); put('rk', fm(inp['rw_r_k'][l].reshape(-1)))
    put('gng_rw', fm(inp['rw_gn_g'][l])); put('gnb_rw', fm(inp['rw_gn_b'][l]))
    return v


class _Stop(Exception):
    pass


class Builder:
    rw_stop = 0

    def stopat(self, k):
        if self.rw_stop == k:
            self.P.halt = True

    def __init__(self, layers=(0, 1), upto='all', debug=()):
        self.layers = layers
        self.upto = upto
        self.debug = set(debug)
        self.nc = bass.Bass("TRN2", target_bir_lowering=False)
        self.es = contextlib.ExitStack()
        self.dbg_outs = {}

    def din(self, name, shape, dt=F32):
        return self.nc.dram_tensor(name, list(shape), dt, kind="ExternalInput").ap()

    def dout(self, name, shape, dt=F32):
        return self.nc.dram_tensor(name, list(shape), dt, kind="ExternalOutput").ap()

    def dscr(self, name, shape, dt=F32):
        return self.nc.dram_tensor(name, list(shape), dt, kind="Internal").ap()

    def sb(self, st, name, shape, dt=F32):
        self._uid = getattr(self, '_uid', 0) + 1
        return st.enter_context(self.nc.sbuf_tensor(f"{name}_u{self._uid}", list(shape), dt))

    def vec(self, l, name):
        o, n = VEC_SLOTS[name]
        return self.vecs[l][:, o:o + n]

    def build(self):
        nc = self.nc
        es = self.es
        with es:
            self.P = P = Prog(nc, es)
            self.xT0 = self.din("xT0", [D, TT])
            self.cT = self.din("cT", [128, 32])
            self.ada_w = self.din("ada_w", [DEPTH, D, 6 * D])
            self.w_in = self.din("w_in", [DEPTH, D, D_IN])
            self.vecs_d = self.din("vecs", [DEPTH, 128, NV])
            self.outT = self.dout("outT", [D, SEQ])
            self.xT = self.dscr("xT_s", [D, TT])
            self.zT = self.dscr("zT_s", [C_GATE, TT])
            self.gT = self.dscr("gT_s", [3 * D, TT], BF16)
            self.vtm = self.dscr("vtm_s", [TT, 2048], BF16)
            self.yT = self.dscr("yT_s", [3, 1024, TT], BF16)
            self.natab = self.din("natab", [DEPTH, 16, 128, 14 * 64])
            self.consts = self.din("consts", [128, NCONST])
            self.gla_w2 = self.din("gla_gate_w2", [DEPTH, 2, 16, 512])
            self.rw_w2 = self.din("rw_w2", [DEPTH, 2, 96, 1024])
            self.rw_a2 = self.din("rw_a2", [DEPTH, 2, 96, 1024])
            self.rw_g2 = self.din("rw_g2", [DEPTH, 256, 1024])
            self.w_branch = self.din("w_branch", [DEPTH, 3, 1024, D])
            self.w_out = self.din("w_out", [DEPTH, D, D])
            self.router_w = self.din("router_w", [D, 16])
            self.router_b = self.din("router_bias", [1, 16])
            self.moe_g = self.din("moe_w_gate", [DEPTH, 16, D, 512])
            self.moe_u = self.din("moe_w_up", [DEPTH, 16, D, 512])
            self.moe_d = self.din("moe_w_down", [DEPTH, 16, 512, D])
            self.ldT = self.dscr("ldT_s", [2, 1024, TT])
            self.aT = self.dscr("aT_s", [2, 1024, TT])
            self.g2T = self.dscr("g2T_s", [1024, TT], BF16)
            self.ones_bf = self.sb(es, "ones_bf", [128, 128], BF16)
            self.vecs = [self.sb(es, f"vecs{l}", [128, NV]) for l in range(DEPTH)]
            self.mod = [self.sb(es, f"mod{l}", [128, 96, 2]) for l in range(DEPTH)]
            self.gm1 = [self.sb(es, f"gm1_{l}", [128, 16, 2]) for l in range(DEPTH)]
            self.gm2 = [self.sb(es, f"gm2_{l}", [128, 16, 2]) for l in range(DEPTH)]
            self.ps = [es.enter_context(nc.psum_tensor(f"ps{i}", [128, 512], F32)) for i in range(7)]
            self.psb = es.enter_context(nc.psum_tensor("psb", [128, 1024], BF16))
            psd = self.psb[:, 512:1024].bitcast(F32)
            P.dummy = lambda e: e.matmul(psd[:, 0:1], lhsT=self.ones_bf[:], rhs=self.ones_bf[:, 0:1], start=True, stop=True)
            cst = self.sb(es, "cst_f", [128, 4 * 128])
            self.cst_bf = self.sb(es, "cst_bf", [128, 4 * 128], BF16)
            P.dma(cst[:], self.consts[:, 0:512], [], ['cst_f'])
            P.op('dve', ['cst_f'], ['cst_bf'], lambda e: e.tensor_copy(out=self.cst_bf[:], in_=cst[:]))
            self.ident_bf = self.cst_bf[:, 0:128]
            self.perm_bf = self.cst_bf[:, 128:256]
            self.mask_f = cst[:, 256:384]
            self.mask_b = cst[:, 384:512]
            self.ident32 = cst[:, 0:128]
            cst2 = self.sb(es, "cst2", [128, 384])
            P.dma(cst2[:], self.consts[:, 512 + 2 * SEQ:512 + 2 * SEQ + 384], [], ['cst_f'])
            self.mask_fs = cst2[:, 0:128]
            self.mask_bs = cst2[:, 128:256]
            self.mask_bd = cst2[:, 256:384]
            P.op('pool', [], ['ones_bf'], lambda e: e.memset(self.ones_bf[:], 1.0))
            self.bd_bf = self.sb(es, "bd_bf", [128, 128], BF16)
            P.op('pool', [], ['bd_bf'], lambda e: e.memset(self.bd_bf[:], 0.0))
            P.op('pool', ['bd_bf'], ['bd_bf'], lambda e: e.memset(self.bd_bf[0:64, 0:64], 1.0))
            P.op('pool', ['bd_bf'], ['bd_bf'], lambda e: e.memset(self.bd_bf[64:128, 64:128], 1.0))
            self.eps_t = self.sb(es, "eps_t", [128, 1])
            P.op('pool', [], ['eps_t'], lambda e: e.memset(self.eps_t[:], EPS))
            for l in range(DEPTH):
                P.dma(self.vecs[l][:], self.vecs_d[l], [], [f'vecs{l}'])

            self.prologue()
            for l in self.layers:
                self.layer(l)
                if self.upto != 'all':
                    break
            P.finish()
        return nc

    def dbg(self, name, src_ap_fn, shape, dt=F32, reads=()):
        o = self.dout("dbg_" + name, shape, dt)
        self.dbg_outs[name] = o
        src_ap_fn(o)

    def prologue(self):
        nc, P = self.nc, self.P
        with contextlib.ExitStack() as st:
            sc = self.sb(st, "sc", [128, 32])
            sc2 = self.sb(st, "sc2", [128, 32])
            awb = [self.sb(st, f"awb{i}", [128, 16, 512]) for i in range(2)]
            P.dma(sc[:], self.cT, [], ['sc'])
            P.op('act', ['sc'], ['sc2'], lambda e: e.activation(out=sc2[:], in_=sc[:], func=AF.Silu))
            for l in range(DEPTH):
                aw = self.ada_w[l].rearrange("(kc p) c -> p kc c", p=128)
                adab = self.vec(l, 'adab')
                for g in range(24):
                    wt = awb[g % 2]
                    wk = f'awb{g % 2}'
                    P.dma(wt[:], aw[:, :, g * 512:(g + 1) * 512], [], [wk], q=('sp' if g % 2 == 0 else 'act'))
                    pst = self.ps[g % 2]
                    pk = f'ps{g % 2}'
                    for f in range(4):
                        for kc in range(KC):
                            P.op('pe', [wk, 'sc2'], [pk], lambda e, f=f, kc=kc: e.matmul(
                                pst[:, f * 2:(f + 1) * 2], lhsT=wt[:, kc, f * 128:(f + 1) * 128],
                                rhs=sc2[:, kc * 2:(kc + 1) * 2], start=(kc == 0), stop=(kc == KC - 1)))
                    P.op('dve', [pk, f'vecs{l}'], [f'mod{l}'], lambda e, g=g: e.tensor_tensor(
                        out=self.mod[l][:, g * 4:(g + 1) * 4, :],
                        in0=pst[:, 0:8].rearrange("p (f j) -> p f j", j=2),
                        in1=adab[:, g * 4:(g + 1) * 4].unsqueeze(2).to_broadcast([128, 4, 2]), op=ALU.add))
                for (gm, nm, sco, key) in ((self.gm1[l], 'n1g', 16, f'gm1_{l}'), (self.gm2[l], 'n2g', 64, f'gm2_{l}')):
                    P.op('dve', [f'mod{l}'], [key], lambda e, gm=gm, sco=sco: e.tensor_scalar(
                        out=gm[:], in0=self.mod[l][:, sco:sco + 16, :], scalar1=1.0, scalar2=None, op0=ALU.add))
                    P.op('dve', [key, f'vecs{l}'], [key], lambda e, gm=gm, nm=nm: e.tensor_tensor(
                        out=gm[:], in0=gm[:], in1=self.vec(l, nm).unsqueeze(2).to_broadcast([128, 16, 2]), op=ALU.mult))
            if 'mod' in self.debug:
                for l in range(DEPTH):
                    self.dbg(f'mod{l}', lambda o, l=l: P.dma(o, self.mod[l][:].rearrange("p a b -> p (a b)"), [f'mod{l}'], []), [128, 192])
            P.barrier()

    def norm_tile(self, st_bufs, x, xk, n, j, gm, gmk, shmod, shoff, modk, out_fn, outk, ps_i=6):
        P = self.P
        sq, rt = st_bufs
        pst = self.ps[ps_i]
        pk = f'ps{ps_i}'
        P.op('act', [xk], ['nsq'], lambda e: e.activation(out=sq[:, :, :n], in_=x, func=AF.Square))
        for kc in range(KC):
            P.op('pe', ['nsq', 'ones_bf'], [pk], lambda e, kc=kc: e.matmul(
                pst[:, :n], lhsT=self.ones_bf[:], rhs=sq[:, kc, :n], start=(kc == 0), stop=(kc == KC - 1)))
        P.op('act', [pk], ['nrt'], lambda e: e.activation(out=rt[:, :n], in_=pst[:, :n], func=AF.Sqrt,
                                                         scale=1.0 / D, bias=self.eps_t[:, 0:1]))
        P.op('dve', ['nrt'], ['nrt'], lambda e: e.reciprocal(out=rt[:, :n], in_=rt[:, :n]))
        P.op('dve', [xk, 'nrt'], [xk], lambda e: e.tensor_tensor(
            out=x, in0=x, in1=rt[:, :n].unsqueeze(1).to_broadcast([128, KC, n]), op=ALU.mult))
        for kc in range(KC):
            P.op('act', [xk, gmk, modk], [outk], lambda e, kc=kc: e.activation(
                out=out_fn(kc), in_=x[:, kc, :], func=AF.Identity,
                scale=gm[:, kc, j:j + 1], bias=shmod[:, shoff + kc, j:j + 1]))

    def layer(self, l):
        nc, P = self.nc, self.P
        src = self.xT0 if l == 0 else self.xT
        with contextlib.ExitStack() as st:
            hT = self.sb(st, "hT", [128, KC, TT], BF16)
            with contextlib.ExitStack() as st2:
                xb = [self.sb(st2, f"xb{i}", [128, KC, 512]) for i in range(2)]
                sq = self.sb(st2, "nsq", [128, KC, 512], BF16)
                rt = self.sb(st2, "nrt", [128, 512])
                for ti, (t0, n) in enumerate(TILES):
                    x = xb[ti % 2]
                    xk = f'xb{ti % 2}'
                    j = 1 if t0 < NCTX else 0
                    P.dma(x[:, :, :n], src.rearrange("(kc p) t -> p kc t", p=128)[:, :, t0:t0 + n], ['xT'], [xk])
                    self.norm_tile((sq, rt), x[:, :, :n], xk, n, j, self.gm1[l], f'gm1_{l}', self.mod[l], 0, f'mod{l}',
                                   lambda kc, t0=t0, n=n: hT[:, kc, t0:t0 + n], 'hT')
                if 'hT' in self.debug:
                    self.dbg(f'hT{l}', lambda o: P.dma(o.rearrange("(kc p) t -> p kc t", p=128), hT[:], ['hT'], []), [D, TT], BF16)
                P.barrier()
            if self.upto == 'norm':
                return
            self.inproj(l, hT)
            P.barrier()
        if self.upto == 'inproj':
            return
        self.na_mixer(l)
        P.barrier()
        if self.upto == 'na':
            return
        self.gla_mixer(l)
        P.barrier()
        if self.upto == 'gla':
            return
        self.rwkv_pre(l)
        P.barrier()
        if self.upto == 'rwpre':
            self.dbg(f'ld{l}', lambda o: P.dma(o, self.ldT, ['ldT'], []), [2, 1024, TT])
            self.dbg(f'a{l}', lambda o: P.dma(o, self.aT, ['aT'], []), [2, 1024, TT])
            self.dbg(f'g2{l}', lambda o: P.dma(o, self.g2T, ['g2T'], []), [1024, TT], BF16)
            return
        self.rwkv_mixer(l)
        P.halt = False
        P.barrier()
        if self.upto == 'rwkv':
            return
        self.merge_moe(l)
        P.barrier()

    def inproj(self, l, hT):
        nc, P = self.nc, self.P
        w = self.w_in[l].rearrange("(kc p) c -> p kc c", p=128)
        with contextlib.ExitStack() as st:
            wsl = [self.sb(st, f"wsl{i}", [128, KC, 512], BF16) for i in range(3)]
            zst = [self.sb(st, f"zst{i}", [128, 512]) for i in range(4)]
            gst = [self.sb(st, f"gst{i}", [128, 512], BF16) for i in range(4)]
            segs = [(0, 2048), (C_GQ, C_GV), (C_GR, C_GGD), (C_GGD, C_GGD + 16), (C_GGD + 16, C_RW),
                    (C_RR, C_RWD), (C_RWD, C_RWD + 96), (C_RWD + 96, C_RAD), (C_RAD, C_RAD + 96), (C_RAD + 96, C_RGD),
                    (C_RGD, C_GATE), (C_GATE, D_IN)]
            nload = 0
            nev = 0
            npsum = 0
            for (s0, s1) in segs:
                for b0 in range(s0, s1, 512):
                    bw = min(512, s1 - b0)
                    si = nload % 3
                    nload += 1
                    wk = f'wsl{si}'
                    P.dma(wsl[si][:, :, :bw], w[:, :, b0:b0 + bw], [], [wk], q='pool')
                    for c0 in range(b0, b0 + bw, 128):
                        m = min(128, b0 + bw - c0)
                        for (t0, n) in TILES:
                            pi = npsum % 4
                            npsum += 1
                            pst = self.ps[pi]
                            pk = f'ps{pi}'
                            for kc in range(KC):
                                P.op('pe', [wk, 'hT'], [pk], lambda e, kc=kc, c0=c0, m=m, t0=t0, n=n, si=si, pst=pst: e.matmul(
                                    pst[:m, :n], lhsT=wsl[si][:, kc, c0 - b0:c0 - b0 + m], rhs=hT[:, kc, t0:t0 + n],
                                    start=(kc == 0), stop=(kc == KC - 1)))
                            ei = nev % 4
                            eng = 'act' if nev % 2 == 0 else 'dve'
                            nev += 1
                            if c0 >= C_GATE:
                                P.op('act', [pk], [f'gst{ei}'], lambda e, m=m, n=n, ei=ei, pst=pst: e.activation(
                                    out=gst[ei][:m, :n], in_=pst[:m, :n], func=AF.Sigmoid))
                                P.dma(self.gT[c0 - C_GATE:c0 - C_GATE + m, t0:t0 + n], gst[ei][:m, :n], [f'gst{ei}'], ['gT'])
                            else:
                                if eng == 'act':
                                    P.op('act', [pk], [f'zst{ei}'], lambda e, m=m, n=n, ei=ei, pst=pst: e.activation(
                                        out=zst[ei][:m, :n], in_=pst[:m, :n], func=AF.Copy))
                                else:
                                    P.op('dve', [pk], [f'zst{ei}'], lambda e, m=m, n=n, ei=ei, pst=pst: e.tensor_copy(
                                        out=zst[ei][:m, :n], in_=pst[:m, :n]))
                                P.dma(self.zT[c0:c0 + m, t0:t0 + n], zst[ei][:m, :n], [f'zst{ei}'], ['zT'])
            for (s0, vo) in ((C_NAV, 0), (C_GV, 1024)):
                for b0 in range(0, 1024, 512):
                    si = nload % 3
                    nload += 1
                    wk = f'wsl{si}'
                    P.dma(wsl[si][:, :, :], w[:, :, s0 + b0:s0 + b0 + 512], [], [wk], q='pool')
                    for tt in range(TT // 128):
                        pi = npsum % 4
                        npsum += 1
                        pst = self.ps[pi]
                        pk = f'ps{pi}'
                        for kc in range(KC):
                            P.op('pe', [wk, 'hT'], [pk], lambda e, kc=kc, tt=tt, si=si, pst=pst: e.matmul(
                                pst[:, :], lhsT=hT[:, kc, tt * 128:(tt + 1) * 128], rhs=wsl[si][:, kc, :],
                                start=(kc == 0), stop=(kc == KC - 1)))
                        ei = nev % 4
                        eng = 'act' if nev % 2 == 0 else 'dve'
                        nev += 1
                        if eng == 'act':
                            P.op('act', [pk], [f'gst{ei}'], lambda e, ei=ei, pst=pst: e.activation(
                                out=gst[ei][:, :], in_=pst[:, :], func=AF.Copy))
                        else:
                            P.op('dve', [pk], [f'gst{ei}'], lambda e, ei=ei, pst=pst: e.tensor_copy(
                                out=gst[ei][:, :], in_=pst[:, :]))
                        P.dma(self.vtm[tt * 128:(tt + 1) * 128, vo + b0:vo + b0 + 512], gst[ei][:, :], [f'gst{ei}'], ['vtm'])
            if 'z' in self.debug:
                self.dbg(f'z{l}', lambda o: P.dma(o, self.zT, ['zT'], []), [C_GATE, TT])
                self.dbg(f'g{l}', lambda o: P.dma(o, self.gT, ['gT'], []), [3 * D, TT], BF16)
                self.dbg(f'vtm{l}', lambda o: P.dma(o, self.vtm, ['vtm'], []), [TT, 2048], BF16)


    def na_mixer(self, l):
        nc, P = self.nc, self.P
        with_ctx = l < DEPTH - 1
        vt_all = self.vtm.rearrange("(tt p) c -> p tt c", p=128)
        vt_odd = self.vtm[64:64 + 17 * 128, :].rearrange("(tt p) c -> p tt c", p=128)
        with contextlib.ExitStack() as st:
            zq = [self.sb(st, f"naz{i}", [128, TT]) for i in range(2)]
            sqb = self.sb(st, "nasq", [128, 512], BF16)
            rtb = self.sb(st, "nart", [128, 512])
            qk = [self.sb(st, "qn", [128, TT], BF16), self.sb(st, "kn", [128, TT], BF16)]
            vte = self.sb(st, "vte", [128, 18, 128], BF16)
            vto = self.sb(st, "vto", [128, 17, 128], BF16)
            tbl = [self.sb(st, f"natbl{i}", [128, 14, 64]) for i in range(2)]
            sT = [self.sb(st, f"sT{i}", [128, 4, 64]) for i in range(2)]
            pT = [self.sb(st, f"pT{i}", [128, 6, 64], BF16) for i in range(2)]
            pTc = self.sb(st, "pTc", [128, 2, 256], BF16)
            rsb = [self.sb(st, f"nars{i}", [128, 256]) for i in range(2)]
            yna = [self.sb(st, f"yna{i}", [128, TT], BF16) for i in range(2)]
            g8 = self.sb(st, "g8", [128, 2])
            P.op('dve', [f'vecs{l}'], ['g8'], lambda e: e.tensor_scalar(
                out=g8[:, 0:1], in0=self.vec(l, 'naq'), scalar1=0.125, scalar2=None, op0=ALU.mult))
            P.op('dve', [f'vecs{l}'], ['g8'], lambda e: e.tensor_copy(out=g8[:, 1:2], in_=self.vec(l, 'nak')))
            it = 0
            for hp in range(8):
                for w_, c0 in ((0, C_NAQ), (1, C_NAK)):
                    z = zq[w_]
                    zk = f'naz{w_}'
                    P.dma(z[:], self.zT[c0 + hp * 128:c0 + (hp + 1) * 128, :], ['zT'], [zk])
                    dst = qk[w_]
                    dk = 'qn' if w_ == 0 else 'kn'
                    for (t0, n) in TILES:
                        P.op('act', [zk], ['nasq'], lambda e, z=z, t0=t0, n=n: e.activation(
                            out=sqb[:, :n], in_=z[:, t0:t0 + n], func=AF.Square))
                        P.op('pe', ['nasq', 'bd_bf'], ['ps4'], lambda e, n=n: e.matmul(
                            self.ps[4][:, :n], lhsT=self.bd_bf[:], rhs=sqb[:, :n], start=True, stop=True))
                        P.op('act', ['ps4'], ['nart'], lambda e, n=n: e.activation(
                            out=rtb[:, :n], in_=self.ps[4][:, :n], func=AF.Sqrt, scale=1.0 / 64, bias=self.eps_t[:, 0:1]))
                        P.op('dve', ['nart'], ['nart'], lambda e, n=n: e.reciprocal(out=rtb[:, :n], in_=rtb[:, :n]))
                        P.op('dve', [zk, 'nart', 'g8'], [dk], lambda e, z=z, t0=t0, n=n, dst=dst, w_=w_: e.scalar_tensor_tensor(
                            out=dst[:, t0:t0 + n], in0=z[:, t0:t0 + n], scalar=g8[:, w_:w_ + 1], in1=rtb[:, :n],
                            op0=ALU.mult, op1=ALU.mult))
                P.dma(vte[:], vt_all[:, :, hp * 128:(hp + 1) * 128], ['vtm'], ['vte'])
                P.dma(vto[:], vt_odd[:, :, hp * 128:(hp + 1) * 128], ['vtm'], ['vto'])
                qn, kn = qk
                y = yna[hp % 2]
                yk = f'yna{hp % 2}'
                for e_ in range(2):
                    h = 2 * hp + e_
                    tb = tbl[h % 2]
                    tk = f'natbl{h % 2}'
                    P.dma(tb[:].rearrange("p a b -> p (a b)"), self.natab[l, h], [], [tk])
                    pr = slice(e_ * 64, (e_ + 1) * 64)
                    for r in range(32):
                        rs = min(max(r - 4, 0), 24)
                        dlt = rs - r
                        tq = NCTX + r * 64
                        b = it % 2
                        it += 1
                        psS, psSk = self.ps[b], f'ps{b}'
                        psO, psOk = self.ps[2 + b], f'ps{2 + b}'
                        for j in range(6):
                            kt = NCTX + (rs + 2 * j) * 64 if j < 4 else (j - 4) * 128
                            P.op('pe', ['qn', 'kn'], [psSk], lambda e, j=j, kt=kt, tq=tq, psS=psS: e.matmul(
                                psS[:, j * 64:(j + 1) * 64], lhsT=kn[pr, kt:kt + 128], rhs=qn[pr, tq:tq + 64],
                                start=True, stop=True), rg=e_ * 64)
                        d0 = dlt + 7
                        tv = tb[:].rearrange("p (u two) c -> p u two c", two=2)[:, d0 // 2:d0 // 2 + 4, d0 % 2, :]
                        P.op('dve', [psSk, tk], [f'sT{b}'], lambda e, psS=psS, tv=tv, b=b: e.tensor_tensor(
                            out=sT[b][:], in0=psS[:, 0:256].rearrange("p (j c) -> p j c", c=64), in1=tv, op=ALU.add))
                        P.op('act', [f'sT{b}'], [f'pT{b}'], lambda e, b=b: e.activation(
                            out=pT[b][:, 0:4, :], in_=sT[b][:], func=AF.Exp))
                        P.op('act', [psSk], [f'pT{b}'], lambda e, psS=psS, b=b: e.activation(
                            out=pT[b][:, 4:6, :], in_=psS[:, 256:384].rearrange("p (j c) -> p j c", c=64), func=AF.Exp))
                        for part in range(2):
                            for j in range(6):
                                if part == 1:
                                    lhs = self.ones_bf[:, 0:64]
                                    rk_ = 'ones_bf'
                                elif j >= 4:
                                    lhs = vte[:, j - 4, pr]
                                    rk_ = 'vte'
                                elif rs % 2 == 0:
                                    lhs = vte[:, 2 + rs // 2 + j, pr]
                                    rk_ = 'vte'
                                else:
                                    lhs = vto[:, (rs + 1) // 2 + 1 + j, pr]
                                    rk_ = 'vto'
                                P.op('pe', [rk_, f'pT{b}'], [psOk], lambda e, lhs=lhs, j=j, part=part, psO=psO, b=b: e.matmul(
                                    psO[pr, part * 64:(part + 1) * 64], lhsT=lhs, rhs=pT[b][:, j, :],
                                    start=(j == 0), stop=(j == 5)))
                        P.op('dve', [psOk], [f'nars{b}'], lambda e, psO=psO, b=b: e.reciprocal(
                            out=rsb[b][pr, 0:64], in_=psO[pr, 64:128]))
                        P.op('dve', [psOk, f'nars{b}'], [yk], lambda e, psO=psO, b=b, tq=tq, y=y: e.tensor_tensor(
                            out=y[pr, tq:tq + 64], in0=psO[pr, 0:64], in1=rsb[b][pr, 0:64], op=ALU.mult))
                    if with_ctx:
                        b = it % 2
                        it += 1
                        psS, psSk = self.ps[b], f'ps{b}'
                        psO, psOk = self.ps[2 + b], f'ps{2 + b}'
                        for j in range(2):
                            P.op('pe', ['qn', 'kn'], [psSk], lambda e, j=j, psS=psS: e.matmul(
                                psS[:, j * 256:(j + 1) * 256], lhsT=kn[pr, j * 128:(j + 1) * 128], rhs=qn[pr, 0:256],
                                start=True, stop=True), rg=e_ * 64)
                        P.op('act', [psSk], ['pTc'], lambda e, psS=psS: e.activation(
                            out=pTc[:].rearrange("p j c -> p (j c)"), in_=psS[:, :], func=AF.Exp))
                        for part in range(2):
                            for j in range(2):
                                lhs = self.ones_bf[:, 0:64] if part == 1 else vte[:, j, pr]
                                P.op('pe', ['vte', 'ones_bf', 'pTc'], [psOk], lambda e, lhs=lhs, j=j, part=part, psO=psO: e.matmul(
                                    psO[pr, part * 256:(part + 1) * 256], lhsT=lhs, rhs=pTc[:, j, :],
                                    start=(j == 0), stop=(j == 1)))
                        P.op('dve', [psOk], [f'nars{b}'], lambda e, psO=psO, b=b: e.reciprocal(
                            out=rsb[b][pr, :], in_=psO[pr, 256:512]))
                        P.op('dve', [psOk, f'nars{b}'], [yk], lambda e, psO=psO, b=b, y=y: e.tensor_tensor(
                            out=y[pr, 0:256], in0=psO[pr, 0:256], in1=rsb[b][pr, :], op=ALU.mult))
                t_lo = 0 if with_ctx else NCTX
                P.dma(self.yT[0, hp * 128:(hp + 1) * 128, t_lo:], y[:, t_lo:], [yk], ['yT'])
            if 'na' in self.debug:
                self.dbg(f'yna{l}', lambda o: P.dma(o, self.yT[0], ['yT'], []), [1024, TT], BF16)


    def gla_mixer(self, l):
        nc, P = self.nc, self.P
        with_ctx = l < DEPTH - 1
        NCH = TT // 64
        NT = TT // 128
        qscale = 128 ** -0.5
        vt_all = self.vtm.rearrange("(tt p) c -> p tt c", p=128)
        with contextlib.ExitStack() as st:
            cos = self.sb(st, "cos", [128, SEQ])
            sin = self.sb(st, "sin", [128, SEQ])
            msk = self.sb(st, "cmsk", [128, TT])
            qk32 = [self.sb(st, "gq32", [128, TT]), self.sb(st, "gk32", [128, TT])]
            zb = self.sb(st, "gzb", [128, 512], BF16)
            rz = self.sb(st, "grz", [128, 2, TT])
            gd = [self.sb(st, f"ggd{d}", [16, TT]) for d in range(2)]
            gw2 = self.sb(st, "gw2", [16, 2, 512])
            nb = self.sb(st, "gnb", [128, 8])
            T1 = self.sb(st, "gT1", [128, TT]); T2 = self.sb(st, "gT2", [128, TT]); T3 = self.sb(st, "gT3", [128, TT])
            qi = self.sb(st, "gqi", [128, TT], BF16); kj = self.sb(st, "gkj", [128, TT], BF16)
            kd = self.sb(st, "gkd", [128, TT], BF16); qb = self.sb(st, "gqb", [128, TT], BF16)
            dec = self.sb(st, "gdec", [128, NCH])
            Vt = self.sb(st, "gVt", [128, NT, 256], BF16)
            kdT = self.sb(st, "gkdT", [128, NT, 128], BF16)
            Abf = [self.sb(st, f"gA{i}", [128, 128], BF16) for i in range(2)]
            S32 = self.sb(st, "gS32", [128, 256])
            Sbf = [self.sb(st, f"gSbf{i}", [128, 256], BF16) for i in range(2)]
            yf = self.sb(st, "gyf", [128, 2, TT], BF16)
            yo = self.sb(st, "gyo", [128, 2, TT], BF16)
            yt = [self.sb(st, f"gyt{i}", [128, 2, 128]) for i in range(2)]
            ysq = self.sb(st, "gysq", [128, 2, 128], BF16)
            yrt = self.sb(st, "gyrt", [128, 128])
            P.dma(cos[:], self.consts[:, 512:512 + SEQ], [], ['cos'])
            P.dma(sin[:], self.consts[:, 512 + SEQ:512 + 2 * SEQ], [], ['sin'])
            P.op('pool', [], ['cmsk'], lambda e: e.memset(msk[:], 1.0))
            P.op('pool', ['cmsk'], ['cmsk'], lambda e: e.memset(msk[:].rearrange("p (c j) -> p c j", j=64)[:, :, 0:1], 0.0))
            for d in range(2):
                P.dma(gd[d][:], self.zT[C_GGD + 16 * d:C_GGD + 16 * (d + 1), :], ['zT'], [f'ggd{d}'])
            P.dma(gw2[:], self.gla_w2[l].rearrange("d k c -> k d c"), [], ['gw2'])
            P.op('dve', [f'vecs{l}'], ['gnb'], lambda e: e.tensor_scalar(
                out=nb[:], in0=self.vec(l, 'ggb'), scalar1=-1.0, scalar2=None, op0=ALU.mult))
            gng = self.vec(l, 'gng')
            c3 = lambda t: t[:].rearrange("p (c j) -> p c j", j=64)
            sbi = 0
            for h in range(4):
                for w_, c0 in ((0, C_GQ), (1, C_GK)):
                    z = qk32[w_]
                    zk = 'gq32' if w_ == 0 else 'gk32'
                    P.dma(z[:], self.zT[c0 + h * 128:c0 + (h + 1) * 128, :], ['zT'], [zk])
                    for ti in range(4):
                        t0 = NCTX + ti * 512
                        P.op('act', [zk], ['gzb'], lambda e, z=z, t0=t0: e.activation(out=zb[:], in_=z[:, t0:t0 + 512], func=AF.Copy))
                        P.op('pe', ['gzb', 'cst_bf'], ['ps4'], lambda e: e.matmul(
                            self.ps[4][:, :], lhsT=self.perm_bf, rhs=zb[:], start=True, stop=True))
                        P.op('dve', ['ps4', 'sin'], ['gT3'], lambda e, ti=ti: e.tensor_tensor(
                            out=T3[:, 0:512], in0=self.ps[4][:, :], in1=sin[:, ti * 512:(ti + 1) * 512], op=ALU.mult))
                        P.op('dve', [zk, 'cos'], [zk], lambda e, z=z, t0=t0, ti=ti: e.tensor_tensor(
                            out=z[:, t0:t0 + 512], in0=z[:, t0:t0 + 512], in1=cos[:, ti * 512:(ti + 1) * 512], op=ALU.mult))
                        P.op('dve', [zk, 'gT3'], [zk], lambda e, z=z, t0=t0: e.tensor_tensor(
                            out=z[:, t0:t0 + 512], in0=z[:, t0:t0 + 512], in1=T3[:, 0:512], op=ALU.add))
                q32, k32 = qk32
                P.dma(rz[:], self.zT[C_GR + h * 256:C_GR + (h + 1) * 256, :].rearrange("(c p) t -> p c t", p=128), ['zT'], ['grz'])
                P.op('act', ['grz'], ['grz'], lambda e: e.activation(out=rz[:], in_=rz[:], func=AF.Silu))
                P.dma(Vt[:], vt_all[:, :, 1024 + h * 256:1024 + (h + 1) * 256], ['vtm'], ['gVt'])
                for d in range(2):
                    for (t0, n) in TILES:
                        P.op('pe', ['gw2', f'ggd{d}'], ['ps4'], lambda e, t0=t0, n=n, d=d, h=h: e.matmul(
                            self.ps[4][:, :n], lhsT=gw2[:, d, h * 128:(h + 1) * 128], rhs=gd[d][:, t0:t0 + n], start=True, stop=True), rg=0)
                        P.op('act', ['ps4', 'gnb'], ['gT1'], lambda e, t0=t0, n=n, d=d, h=h: e.activation(
                            out=T1[:, t0:t0 + n], in_=self.ps[4][:, :n], func=AF.Exp, scale=-1.0, bias=nb[:, d * 4 + h:d * 4 + h + 1]))
                    P.op('act', ['gT1'], ['gT1'], lambda e: e.activation(out=T1[:], in_=T1[:], func=AF.Ln, bias=1.0, scale=1.0))
                    P.op('dve', ['cmsk', 'gT1'], ['gT2'], lambda e: e.tensor_tensor_scan(
                        out=T2[:], data0=msk[:], data1=T1[:], initial=0.0, op0=ALU.mult, op1=ALU.add))
                    if d == 1:
                        P.op('dve', ['gT2'], ['gT3'], lambda e: e.tensor_tensor(
                            out=c3(T3), in0=c3(T2)[:, :, 63:64].to_broadcast([128, NCH, 64]), in1=c3(T2), op=ALU.subtract))
                        P.op('dve', ['gT3', 'gT1'], ['gT2'], lambda e: e.tensor_tensor(out=T2[:], in0=T3[:], in1=T1[:], op=ALU.add))
                    tot = c3(T2)[:, :, 63:64] if d == 0 else c3(T2)[:, :, 0:1]
                    cref = c3(T2)[:, :, 32:33] if d == 0 else c3(T2)[:, :, 31:32]
                    P.op('act', ['gT2'], ['gdec'], lambda e, tot=tot: e.activation(
                        out=dec[:].unsqueeze(2), in_=tot, func=AF.Exp, scale=-1.0 / 16))
                    P.op('dve', ['gT2'], ['gT3'], lambda e, cref=cref: e.tensor_tensor(
                        out=c3(T3), in0=c3(T2), in1=cref.to_broadcast([128, NCH, 64]), op=ALU.subtract))
                    P.op('act', ['gT3'], ['gT1'], lambda e: e.activation(out=T1[:], in_=T3[:], func=AF.Exp, scale=-1.0 / 16))
                    P.op('dve', ['gq32', 'gT1'], ['gqi'], lambda e: e.scalar_tensor_tensor(
                        out=qi[:], in0=q32[:], scalar=qscale, in1=T1[:], op0=ALU.mult, op1=ALU.mult))
                    P.op('act', ['gT3'], ['gT1'], lambda e: e.activation(out=T1[:], in_=T3[:], func=AF.Exp, scale=1.0 / 16))
                    P.op('dve', ['gk32', 'gT1'], ['gkj'], lambda e: e.tensor_tensor(out=kj[:], in0=k32[:], in1=T1[:], op=ALU.mult))
                    P.op('dve', ['gT2'], ['gT3'], lambda e, tot=tot: e.tensor_tensor(
                        out=c3(T3), in0=tot.to_broadcast([128, NCH, 64]), in1=c3(T2), op=ALU.subtract))
                    P.op('act', ['gT3'], ['gT1'], lambda e: e.activation(out=T1[:], in_=T3[:], func=AF.Exp, scale=-1.0 / 16))
                    P.op('dve', ['gk32', 'gT1'], ['gkd'], lambda e: e.tensor_tensor(out=kd[:], in0=k32[:], in1=T1[:], op=ALU.mult))
                    P.op('act', ['gT2'], ['gT1'], lambda e: e.activation(out=T1[:], in_=T2[:], func=AF.Exp, scale=-1.0 / 16))
                    P.op('dve', ['gq32', 'gT1'], ['gqb'], lambda e: e.scalar_tensor_tensor(
                        out=qb[:], in0=q32[:], scalar=qscale, in1=T1[:], op0=ALU.mult, op1=ALU.mult))
                    for g0 in range(0, NT, 4):
                        g1 = min(NT, g0 + 4)
                        for tt in range(g0, g1):
                            P.op('pe', ['gkd', 'cst_bf'], ['psb'], lambda e, tt=tt, g0=g0: e.transpose(
                                self.psb[:, (tt - g0) * 128:(tt - g0 + 1) * 128], kd[:, tt * 128:(tt + 1) * 128], self.ident_bf))
                        P.op('act', ['psb'], ['gkdT'], lambda e, g0=g0, g1=g1: e.activation(
                            out=kdT[:, g0:g1, :].rearrange("p a b -> p (a b)"), in_=self.psb[:, 0:(g1 - g0) * 128], func=AF.Copy))
                    P.op('dve', [], ['gS32'], lambda e: e.memset(S32[:], 0.0))
                    P.op('dve', [], [f'gSbf{sbi % 2}'], lambda e, sbi=sbi: e.memset(Sbf[sbi % 2][:], 0.0))
                    order = list(range(NT)) if d == 0 else [1, 0] + list(range(NT - 1, 1, -1))
                    maskd = self.mask_f if d == 0 else self.mask_b
                    for it_, tt in enumerate(order):
                        tsl = slice(tt * 128, (tt + 1) * 128)
                        ab = it_ % 2
                        P.op('pe', ['gkj', 'gqi'], ['ps5'], lambda e, tsl=tsl: e.matmul(
                            self.ps[5][:, 0:128], lhsT=kj[:, tsl], rhs=qi[:, tsl], start=True, stop=True))
                        P.op('dve', ['ps5', 'cst_f'], [f'gA{ab}'], lambda e, ab=ab, maskd=maskd: e.tensor_tensor(
                            out=Abf[ab][:], in0=self.ps[5][:, 0:128], in1=maskd, op=ALU.mult))
                        halves = (0, 1) if d == 0 else (1, 0)
                        psy = [self.ps[0 + 2 * (it_ % 2)], self.ps[1 + 2 * (it_ % 2)]]
                        psyk = [f'ps{0 + 2 * (it_ % 2)}', f'ps{1 + 2 * (it_ % 2)}']
                        for hi, hf in enumerate(halves):
                            csl = slice(hf * 64, (hf + 1) * 64)
                            tok = slice(tt * 128 + hf * 64, tt * 128 + (hf + 1) * 64)
                            sk = f'gSbf{sbi % 2}'
                            Sb = Sbf[sbi % 2]
                            for vc in range(2):
                                if hi == 0:
                                    P.op('pe', ['gVt', f'gA{ab}'], [psyk[vc]], lambda e, vc=vc, tt=tt, ab=ab, psy=psy: e.matmul(
                                        psy[vc][:, 0:128], lhsT=Vt[:, tt, vc * 128:(vc + 1) * 128], rhs=Abf[ab][:], start=True, stop=False))
                                P.op('pe', [sk, 'gqb'], [psyk[vc]], lambda e, vc=vc, Sb=Sb, csl=csl, tok=tok, hi=hi, psy=psy: e.matmul(
                                    psy[vc][:, csl], lhsT=Sb[:, vc * 128:(vc + 1) * 128], rhs=qb[:, tok], start=False, stop=(hi == 1)))
                            ch = tt * 2 + hf
                            P.op('pe', ['gkdT', 'gVt'], ['ps6'], lambda e, csl=csl, tt=tt: e.matmul(
                                self.ps[6][:, 0:256], lhsT=kdT[csl, tt, :], rhs=Vt[csl, tt, :], start=True, stop=True), rg=csl.start)
                            P.op('dve', ['ps6', 'gS32', 'gdec'], ['gS32'], lambda e, ch=ch: e.scalar_tensor_tensor(
                                out=S32[:], in0=S32[:], scalar=dec[:, ch:ch + 1], in1=self.ps[6][:, 0:256], op0=ALU.mult, op1=ALU.add))
                            sbi += 1
                            P.op('act', ['gS32'], [f'gSbf{sbi % 2}'], lambda e, sbi=sbi: e.activation(
                                out=Sbf[sbi % 2][:], in_=S32[:], func=AF.Copy))
                        if d == 0:
                            for vc in range(2):
                                P.op('act', [psyk[vc]], ['gyf'], lambda e, vc=vc, tsl=tsl, psy=psy: e.activation(
                                    out=yf[:, vc, tsl], in_=psy[vc][:, 0:128], func=AF.Copy))
                        elif with_ctx or tt >= 2:
                            ytb = yt[it_ % 2]
                            ytk = f'gyt{it_ % 2}'
                            for vc in range(2):
                                P.op('dve', [psyk[vc], 'gyf'], [ytk], lambda e, vc=vc, tsl=tsl, psy=psy, ytb=ytb: e.tensor_tensor(
                                    out=ytb[:, vc, :], in0=psy[vc][:, 0:128], in1=yf[:, vc, tsl], op=ALU.add))
                            P.op('act', [ytk], ['gysq'], lambda e, ytb=ytb: e.activation(out=ysq[:], in_=ytb[:], func=AF.Square))
                            for vc in range(2):
                                P.op('pe', ['gysq', 'ones_bf'], ['ps4'], lambda e, vc=vc: e.matmul(
                                    self.ps[4][:, 0:128], lhsT=self.ones_bf[:], rhs=ysq[:, vc, :], start=(vc == 0), stop=(vc == 1)))
                            P.op('act', ['ps4'], ['gyrt'], lambda e: e.activation(
                                out=yrt[:], in_=self.ps[4][:, 0:128], func=AF.Sqrt, scale=1.0 / 256, bias=self.eps_t[:, 0:1]))
                            P.op('dve', ['gyrt'], ['gyrt'], lambda e: e.reciprocal(out=yrt[:], in_=yrt[:]))
                            for vc in range(2):
                                P.op('dve', [ytk, 'gyrt', f'vecs{l}'], [ytk], lambda e, vc=vc, ytb=ytb: e.scalar_tensor_tensor(
                                    out=ytb[:, vc, :], in0=ytb[:, vc, :], scalar=gng[:, vc:vc + 1], in1=yrt[:], op0=ALU.mult, op1=ALU.mult))
                                P.op('dve', [ytk, 'grz'], ['gyo'], lambda e, vc=vc, ytb=ytb, tsl=tsl: e.tensor_tensor(
                                    out=yo[:, vc, tsl], in0=ytb[:, vc, :], in1=rz[:, vc, tsl], op=ALU.mult))
                t_lo = 0 if with_ctx else NCTX
                P.dma(self.yT[1, h * 256:(h + 1) * 256, t_lo:].rearrange("(c p) t -> p c t", p=128), yo[:, :, t_lo:], ['gyo'], ['yT'])
            if 'gla' in self.debug:
                self.dbg(f'ygla{l}', lambda o: P.dma(o, self.yT[1], ['yT'], []), [1024, TT], BF16)


    def _shift(self, z, zk, tmp, tmpk, om, hm, m):
        P = self.P
        P.op('dve', [zk], [tmpk], lambda e: e.tensor_tensor(out=tmp[:m, 1:TT - 1], in0=z[:m, 0:TT - 2], in1=z[:m, 2:TT], op=ALU.add))
        for (dst, src_) in ((0, 1), (NCTX - 1, NCTX - 2), (NCTX, NCTX + 1), (TT - 1, TT - 2)):
            P.op('dve', [zk, tmpk], [tmpk], lambda e, dst=dst, src_=src_: e.tensor_copy(out=tmp[:m, dst:dst + 1], in_=z[:m, src_:src_ + 1]))
        P.op('dve', [tmpk], [tmpk], lambda e: e.tensor_scalar(out=tmp[:m, :], in0=tmp[:m, :], scalar1=hm, scalar2=None, op0=ALU.mult))
        P.op('dve', [zk, tmpk], [zk], lambda e: e.scalar_tensor_tensor(out=z[:m, :], in0=z[:m, :], scalar=om, in1=tmp[:m, :], op0=ALU.mult, op1=ALU.add))

    def _mu_prep(self, st, l):
        P = self.P
        o0, _ = VEC_SLOTS['mu_r']
        mu = self.vecs[l][:, o0:o0 + 30]
        om = self.sb(st, "rw_om", [128, 30]); hm = self.sb(st, "rw_hm", [128, 30])
        P.op('dve', [f'vecs{l}'], ['rw_om'], lambda e: e.tensor_scalar(out=om[:], in0=mu, scalar1=-1.0, scalar2=1.0, op0=ALU.mult, op1=ALU.add))
        P.op('dve', [f'vecs{l}'], ['rw_hm'], lambda e: e.tensor_scalar(out=hm[:], in0=mu, scalar1=0.5, scalar2=None, op0=ALU.mult))
        return om, hm

    def rwkv_pre(self, l):
        nc, P = self.nc, self.P
        with contextlib.ExitStack() as st:
            om, hm = self._mu_prep(st, l)
            zt = self.sb(st, "rp_z", [128, TT]); tmp = self.sb(st, "rp_tmp", [128, TT])
            twd = [self.sb(st, f"rp_twd{d}", [96, TT], BF16) for d in range(2)]
            adb = [self.sb(st, f"rp_adb{d}", [96, TT], BF16) for d in range(2)]
            sgd = self.sb(st, "rp_sgd", [128, 2, TT], BF16)
            w2 = self.sb(st, "rp_w2", [96, 2, 1024], BF16); a2 = self.sb(st, "rp_a2", [96, 2, 1024], BF16)
            g2 = self.sb(st, "rp_g2", [128, 2, 1024], BF16)
            stg = [self.sb(st, f"rp_st{i}", [128, 512]) for i in range(4)]
            stb = [self.sb(st, f"rp_sb{i}", [128, 512], BF16) for i in range(2)]
            P.dma(w2[:], self.rw_w2[l].rearrange("d k c -> k d c"), [], ['rp_w2'], q='pool')
            P.dma(a2[:], self.rw_a2[l].rearrange("d k c -> k d c"), [], ['rp_a2'], q='pool')
            P.dma(g2[:], self.rw_g2[l].rearrange("(c p) n -> p c n", p=128), [], ['rp_g2'], q='pool')
            for d in range(2):
                P.dma(zt[:96, :], self.zT[C_RWD + 96 * d:C_RWD + 96 * (d + 1), :], ['zT'], ['rp_z'])
                self._shift(zt, 'rp_z', tmp, 'rp_tmp', om[:96, 24 + d:25 + d], hm[:96, 24 + d:25 + d], 96)
                P.op('act', ['rp_z'], [f'rp_twd{d}'], lambda e, d=d: e.activation(out=twd[d][:], in_=zt[:96, :], func=AF.Tanh))
                P.dma(zt[:96, :], self.zT[C_RAD + 96 * d:C_RAD + 96 * (d + 1), :], ['zT'], ['rp_z'])
                self._shift(zt, 'rp_z', tmp, 'rp_tmp', om[:96, 26 + d:27 + d], hm[:96, 26 + d:27 + d], 96)
                P.op('act', ['rp_z'], [f'rp_adb{d}'], lambda e, d=d: e.activation(out=adb[d][:], in_=zt[:96, :], func=AF.Copy))
            for c in range(2):
                P.dma(zt[:, :], self.zT[C_RGD + 128 * c:C_RGD + 128 * (c + 1), :], ['zT'], ['rp_z'])
                self._shift(zt, 'rp_z', tmp, 'rp_tmp', om[:, 28 + c:29 + c], hm[:, 28 + c:29 + c], 128)
                P.op('act', ['rp_z'], ['rp_sgd'], lambda e, c=c: e.activation(out=sgd[:, c, :], in_=zt[:, :], func=AF.Sigmoid))
            w0 = self.vec(l, 'w0'); a0 = self.vec(l, 'a0')
            n_ = 0
            for hp in range(8):
                cs = slice(hp * 128, (hp + 1) * 128)
                for (t0, n) in TILES:
                    for d in range(2):
                        for which in range(2):
                            pi = n_ % 4; n_ += 1
                            pst, pk = self.ps[pi], f'ps{pi}'
                            wm, src_, bias_ = ((w2, twd[d], w0), (a2, adb[d], a0))[which]
                            rk_ = [('rp_w2', f'rp_twd{d}'), ('rp_a2', f'rp_adb{d}')][which]
                            P.op('pe', list(rk_), [pk], lambda e, wm=wm, src_=src_, d=d, t0=t0, n=n, pst=pst, cs=cs: e.matmul(
                                pst[:, :n], lhsT=wm[:, d, cs], rhs=src_[:, t0:t0 + n], start=True, stop=True), rg=0)
                            sg = stg[pi]
                            P.op('act', [pk, f'vecs{l}'], [f'rp_st{pi}'], lambda e, pst=pst, sg=sg, n=n, bias_=bias_, d=d, hp=hp: e.activation(
                                out=sg[:, :n], in_=pst[:, :n], func=AF.Sigmoid, bias=bias_[:, d * 8 + hp:d * 8 + hp + 1], scale=1.0))
                            if which == 0:
                                P.op('dve', [f'rp_st{pi}'], [f'rp_st{pi}'], lambda e, sg=sg, n=n: e.tensor_scalar(
                                    out=sg[:, :n], in0=sg[:, :n], scalar1=-0.6065306597126334, scalar2=None, op0=ALU.mult))
                                P.dma(self.ldT[d, cs, t0:t0 + n], sg[:, :n], [f'rp_st{pi}'], ['ldT'])
                            else:
                                P.dma(self.aT[d, cs, t0:t0 + n], sg[:, :n], [f'rp_st{pi}'], ['aT'])
                    pi = n_ % 4; n_ += 1
                    pst, pk = self.ps[pi], f'ps{pi}'
                    for c in range(2):
                        P.op('pe', ['rp_g2', 'rp_sgd'], [pk], lambda e, c=c, t0=t0, n=n, pst=pst, cs=cs: e.matmul(
                            pst[:, :n], lhsT=g2[:, c, cs], rhs=sgd[:, c, t0:t0 + n], start=(c == 0), stop=(c == 1)))
                    bi = n_ % 2
                    P.op('act', [pk], [f'rp_sb{bi}'], lambda e, pst=pst, n=n, bi=bi: e.activation(out=stb[bi][:, :n], in_=pst[:, :n], func=AF.Copy))
                    P.dma(self.g2T[cs, t0:t0 + n], stb[bi][:, :n], [f'rp_sb{bi}'], ['g2T'])

    def rwkv_mixer(self, l):
        nc, P = self.nc, self.P
        with_ctx = l < DEPTH - 1
        NCH = TT // 64
        NT = TT // 128
        with contextlib.ExitStack() as st:
            om, hm = self._mu_prep(st, l)
            A = [self.sb(st, f"rwA{i}", [128, TT]) for i in range(7)]
            Ak = [f'rwA{i}' for i in range(7)]
            msk = self.sb(st, "rmsk", [128, TT])
            vb = self.sb(st, "rw_vb", [128, TT], BF16)
            bon = self.sb(st, "rw_bon", [128, TT], BF16)
            gsb = self.sb(st, "rw_g", [128, 512], BF16)
            al = self.sb(st, "rw_al", [128, TT], BF16); rho = self.sb(st, "rw_rho", [128, TT], BF16)
            be = self.sb(st, "rw_be", [128, TT], BF16); ka = self.sb(st, "rw_ka", [128, TT], BF16)
            Bp = self.sb(st, "rw_Bp", [128, TT], BF16); Kp = self.sb(st, "rw_Kp", [128, TT], BF16)
            al_tm = self.sb(st, "rw_altm", [128, NT, 128], BF16); Bp_tm = self.sb(st, "rw_Bptm", [128, NT, 128], BF16)
            Kp_tm = self.sb(st, "rw_Kptm", [128, NT, 128], BF16); V_tm = self.sb(st, "rw_Vtm", [128, NT, 128], BF16)
            etot = self.sb(st, "rw_etot", [128, NCH])
            XN = [[self.sb(st, f"rw_X{i}", [128, 512], BF16), self.sb(st, f"rw_N{i}", [128, 512], BF16)] for i in range(2)]
            Q = [self.sb(st, f"rw_Q{i}", [128, 512], BF16) for i in range(2)]
            LkT = self.sb(st, "rw_LkT", [128, 512], BF16); MbT = self.sb(st, "rw_MbT", [128, 512], BF16); MkT = self.sb(st, "rw_MkT", [128, 512], BF16)
            Hh = self.sb(st, "rw_H", [128, 4, 64], BF16); P1n = self.sb(st, "rw_P1n", [128, 4, 64], BF16); Gt = self.sb(st, "rw_G", [128, 4, 64], BF16)
            R32 = self.sb(st, "rw_R32", [128, TT]); yloc = self.sb(st, "rw_yloc", [128, TT]); yacc = self.sb(st, "rw_yacc", [128, TT])
            Phi = self.sb(st, "rw_Phi", [128, NCH, 128]); Dd = self.sb(st, "rw_D", [128, NCH, 128], BF16)
            ptmp = self.sb(st, "rw_ptmp", [128, 128])
            Sbd = [self.sb(st, f"rw_S{i}", [128, 128]) for i in range(2)]
            ka1 = self.sb(st, "rw_ka1", [128, 8])
            rt = self.sb(st, "rw_rt", [128, 512]); t5 = self.sb(st, "rw_t5", [128, 512]); sq5 = self.sb(st, "rw_sq5", [128, 512], BF16)
            yo = vb
            geps = self.sb(st, "rw_geps", [128, 1])
            P.op('pool', [], ['rw_geps'], lambda e: e.memset(geps[:], 64e-5))
            P.op('pool', [], ['rmsk'], lambda e: e.memset(msk[:], 1.0))
            P.op('pool', ['rmsk'], ['rmsk'], lambda e: e.memset(msk[:].rearrange("p (c j) -> p c j", j=64)[:, :, 0:1], 0.0))
            P.op('dve', [f'vecs{l}'], ['rw_ka1'], lambda e: e.tensor_scalar(
                out=ka1[:], in0=self.vec(l, 'ka'), scalar1=-1.0, scalar2=1.0, op0=ALU.mult, op1=ALU.add))
            c3 = lambda t: t[:].rearrange("p (c j) -> p c j", j=64)
            kkv = self.vec(l, 'kk'); kav = self.vec(l, 'ka'); rkv = self.vec(l, 'rk')
            gng = self.vec(l, 'gng_rw'); gnb = self.vec(l, 'gnb_rw')
            si = 0
            for hp in range(8):
                cs = slice(hp * 128, (hp + 1) * 128)
                r32, k32, v32, kk32 = A[0], A[1], A[2], A[3]
                for i_, c0 in enumerate((C_RR, C_RK, C_RV)):
                    P.dma(A[i_][:], self.zT[c0 + hp * 128:c0 + (hp + 1) * 128, :], ['zT'], [Ak[i_]])
                    ci = i_ * 8 + hp
                    self._shift(A[i_], Ak[i_], A[6], Ak[6], om[:, ci:ci + 1], hm[:, ci:ci + 1], 128)
                P.op('act', [Ak[2]], ['rw_vb'], lambda e: e.activation(out=vb[:], in_=v32[:], func=AF.Copy))
                for (srcb, srck, dst, dstk) in ((vb, 'rw_vb', V_tm, 'rw_Vtm'),):
                    for g0 in range(0, NT, 4):
                        g1 = min(NT, g0 + 4)
                        for tt in range(g0, g1):
                            P.op('pe', [srck, 'cst_bf'], ['psb'], lambda e, tt=tt, g0=g0, srcb=srcb: e.transpose(
                                self.psb[:, (tt - g0) * 128:(tt - g0 + 1) * 128], srcb[:, tt * 128:(tt + 1) * 128], self.ident_bf))
                        P.op('act', ['psb'], [dstk], lambda e, g0=g0, g1=g1, dst=dst: e.activation(
                            out=dst[:, g0:g1, :].rearrange("p a b -> p (a b)"), in_=self.psb[:, 0:(g1 - g0) * 128], func=AF.Copy))
                P.op('dve', [Ak[1], f'vecs{l}'], [Ak[3]], lambda e: e.tensor_scalar(
                    out=kk32[:], in0=k32[:], scalar1=kkv[:, hp:hp + 1], scalar2=None, op0=ALU.mult))
                P.op('dve', [Ak[0], Ak[1], f'vecs{l}'], [Ak[6]], lambda e: e.scalar_tensor_tensor(
                    out=A[6][:], in0=r32[:], scalar=rkv[:, hp:hp + 1], in1=k32[:], op0=ALU.mult, op1=ALU.mult))
                for (t0, n) in TILES:
                    P.op('act', [Ak[3]], ['rw_sq5'], lambda e, t0=t0, n=n: e.activation(out=sq5[:, :n], in_=kk32[:, t0:t0 + n], func=AF.Square))
                    P.op('pe', ['rw_sq5', 'bd_bf'], ['ps4'], lambda e, n=n: e.matmul(self.ps[4][:, :n], lhsT=self.bd_bf[:], rhs=sq5[:, :n], start=True, stop=True))
                    P.op('act', ['ps4'], ['rw_rt'], lambda e, n=n: e.activation(out=rt[:, :n], in_=self.ps[4][:, :n], func=AF.Sqrt, scale=1.0, bias=self.eps_t[:, 0:1]))
                    P.op('dve', ['rw_rt'], ['rw_rt'], lambda e, n=n: e.reciprocal(out=rt[:, :n], in_=rt[:, :n]))
                    P.op('dve', [Ak[3], 'rw_rt'], [Ak[3]], lambda e, t0=t0, n=n: e.tensor_tensor(out=kk32[:, t0:t0 + n], in0=kk32[:, t0:t0 + n], in1=rt[:, :n], op=ALU.mult))
                    P.op('act', [Ak[6]], ['rw_sq5'], lambda e, t0=t0, n=n: e.activation(out=sq5[:, :n], in_=A[6][:, t0:t0 + n], func=AF.Copy))
                    P.op('pe', ['rw_sq5', 'bd_bf'], ['ps5'], lambda e, n=n: e.matmul(self.ps[5][:, :n], lhsT=self.bd_bf[:], rhs=sq5[:, :n], start=True, stop=True))
                    P.op('dve', ['ps5', Ak[2]], ['rw_bon'], lambda e, t0=t0, n=n: e.tensor_tensor(out=bon[:, t0:t0 + n], in0=self.ps[5][:, :n], in1=v32[:, t0:t0 + n], op=ALU.mult))
                for d in range(2):
                    ld, a_, cin, E = A[4], A[5], A[2], A[6]
                    ldk, ak_, cink, Ek = Ak[4], Ak[5], Ak[2], Ak[6]
                    P.dma(ld[:], self.ldT[d, cs, :], ['ldT'], [ldk])
                    P.dma(a_[:], self.aT[d, cs, :], ['aT'], [ak_])
                    P.op('dve', ['rmsk', ldk], [cink], lambda e: e.tensor_tensor_scan(
                        out=cin[:], data0=msk[:], data1=ld[:], initial=0.0, op0=ALU.mult, op1=ALU.add))
                    if d == 1:
                        P.op('dve', [cink], [Ek], lambda e: e.tensor_tensor(
                            out=c3(E), in0=c3(cin)[:, :, 63:64].to_broadcast([128, NCH, 64]), in1=c3(cin), op=ALU.subtract))
                        P.op('dve', [Ek, ldk], [cink], lambda e: e.tensor_tensor(out=cin[:], in0=E[:], in1=ld[:], op=ALU.add))
                    tot = c3(cin)[:, :, 63:64] if d == 0 else c3(cin)[:, :, 0:1]
                    P.op('act', [cink], ['rw_etot'], lambda e, tot=tot: e.activation(out=etot[:].unsqueeze(2), in_=tot, func=AF.Exp))
                    P.op('dve', [cink, ldk], [ldk], lambda e: e.tensor_tensor(out=ld[:], in0=cin[:], in1=ld[:], op=ALU.subtract))
                    P.op('act', [ldk], [Ek], lambda e: e.activation(out=E[:], in_=ld[:], func=AF.Exp))
                    P.op('dve', [Ak[3], Ek], ['rw_al'], lambda e: e.tensor_tensor(out=al[:], in0=kk32[:], in1=E[:], op=ALU.mult))
                    P.op('act', [cink], [Ek], lambda e: e.activation(out=E[:], in_=cin[:], func=AF.Exp))
                    rho32 = ld
                    P.op('dve', [Ak[0], Ek], [ldk], lambda e: e.tensor_tensor(out=rho32[:], in0=r32[:], in1=E[:], op=ALU.mult))
                    P.op('act', [ldk], ['rw_rho'], lambda e: e.activation(out=rho[:], in_=rho32[:], func=AF.Copy))
                    P.op('act', [cink], [Ek], lambda e: e.activation(out=E[:], in_=cin[:], func=AF.Exp, scale=-1.0))
                    P.op('dve', [Ek, ak_], [Ek], lambda e: e.tensor_tensor(out=E[:], in0=E[:], in1=a_[:], op=ALU.mult))
                    P.op('dve', [Ak[3], Ek], ['rw_be'], lambda e: e.tensor_tensor(out=be[:], in0=kk32[:], in1=E[:], op=ALU.mult))
                    P.op('act', [cink], [Ek], lambda e: e.activation(out=E[:], in_=cin[:], func=AF.Exp, scale=-1.0))
                    P.op('dve', [ak_, f'vecs{l}', 'rw_ka1'], [ak_], lambda e: e.tensor_scalar(
                        out=a_[:], in0=a_[:], scalar1=kav[:, hp:hp + 1], scalar2=ka1[:, hp:hp + 1], op0=ALU.mult, op1=ALU.add))
                    P.op('dve', [ak_, Ak[1]], [ak_], lambda e: e.tensor_tensor(out=a_[:], in0=a_[:], in1=k32[:], op=ALU.mult))
                    P.op('dve', [ak_, Ek], ['rw_ka'], lambda e: e.tensor_tensor(out=ka[:], in0=a_[:], in1=E[:], op=ALU.mult))
                    eb = etot[:].unsqueeze(2).to_broadcast([128, NCH, 64])
                    P.op('dve', ['rw_be', 'rw_etot'], ['rw_Bp'], lambda e: e.tensor_tensor(out=c3(Bp), in0=c3(be), in1=eb, op=ALU.mult))
                    P.op('dve', ['rw_ka', 'rw_etot'], ['rw_Kp'], lambda e: e.tensor_tensor(out=c3(Kp), in0=c3(ka), in1=eb, op=ALU.mult))
                    self.stopat(1)
                    for (srcb, srck, dst, dstk) in ((al, 'rw_al', al_tm, 'rw_altm'), (Bp, 'rw_Bp', Bp_tm, 'rw_Bptm'), (Kp, 'rw_Kp', Kp_tm, 'rw_Kptm')):
                        for g0 in range(0, NT, 4):
                            g1 = min(NT, g0 + 4)
                            for tt in range(g0, g1):
                                P.op('pe', [srck, 'cst_bf'], ['psb'], lambda e, tt=tt, g0=g0, srcb=srcb: e.transpose(
                                    self.psb[:, (tt - g0) * 128:(tt - g0 + 1) * 128], srcb[:, tt * 128:(tt + 1) * 128], self.ident_bf))
                            P.op('act', ['psb'], [dstk], lambda e, g0=g0, g1=g1, dst=dst: e.activation(
                                out=dst[:, g0:g1, :].rearrange("p a b -> p (a b)"), in_=self.psb[:, 0:(g1 - g0) * 128], func=AF.Copy))
                    self.stopat(2)
                    m_st = self.mask_fs if d == 0 else self.mask_bs
                    m_in = self.mask_f if d == 0 else self.mask_b
                    m_ts = self.mask_bs if d == 0 else self.mask_fs
                    bc4 = lambda m: m.unsqueeze(1).to_broadcast([128, 4, 128])
                    v4 = lambda t: t[:].rearrange("p (a b) -> p a b", b=128)
                    for g0 in range(0, NT, 2):
                        probs = [(tt, e_) for tt in (g0, g0 + 1) for e_ in range(2)]
                        X, N_ = XN[0]
                        for pi_, (tt, e_) in enumerate(probs):
                            pr = slice(e_ * 64, (e_ + 1) * 64); tsl = slice(tt * 128, (tt + 1) * 128); osl = slice(pi_ * 128, (pi_ + 1) * 128)
                            for (pidx, lh, rh, rks) in ((0, be, al, ['rw_be', 'rw_al']), (1, al, be, ['rw_al', 'rw_be']), (2, ka, al, ['rw_ka', 'rw_al']),
                                                        (3, be, rho, ['rw_be', 'rw_rho']), (5, ka, rho, ['rw_ka', 'rw_rho'])):
                                P.op('pe', rks, [f'ps{pidx}'], lambda e, pidx=pidx, lh=lh, rh=rh, pr=pr, tsl=tsl, osl=osl: e.matmul(
                                    self.ps[pidx][:, osl], lhsT=lh[pr, tsl], rhs=rh[pr, tsl], start=True, stop=True), rg=e_ * 64)
                        self.stopat(25)
                        P.op('dve', ['ps0', 'cst_f'], ['rw_X0'], lambda e: e.scalar_tensor_tensor(
                            out=v4(X), in0=v4(self.ps[0]), scalar=-1.0, in1=bc4(m_st), op0=ALU.mult, op1=ALU.mult))
                        self.stopat(26)
                        P.op('dve', ['ps1', 'cst_f'], ['rw_N0'], lambda e: e.scalar_tensor_tensor(
                            out=v4(N_), in0=v4(self.ps[1]), scalar=-1.0, in1=bc4(m_ts), op0=ALU.mult, op1=ALU.mult))
                        self.stopat(27)
                        P.op('dve', ['ps2', 'cst_f'], ['rw_LkT'], lambda e: e.tensor_tensor(out=v4(LkT), in0=v4(self.ps[2]), in1=bc4(m_st), op=ALU.mult))
                        P.op('dve', ['ps3', 'cst_f'], ['rw_MbT'], lambda e: e.tensor_tensor(out=v4(MbT), in0=v4(self.ps[3]), in1=bc4(m_in), op=ALU.mult))
                        P.op('dve', ['ps5', 'cst_f'], ['rw_MkT'], lambda e: e.tensor_tensor(out=v4(MkT), in0=v4(self.ps[5]), in1=bc4(m_in), op=ALU.mult))
                        self.stopat(3)
                        P.op('dve', ['rw_X0', 'cst_bf'], ['rw_Q0'], lambda e: e.tensor_tensor(
                            out=v4(Q[0]), in0=v4(X), in1=self.ident_bf.unsqueeze(1).to_broadcast([128, 4, 128]), op=ALU.add))
                        qi_ = 0
                        for j in range(1, 6):
                            Xo, No = XN[(j - 1) % 2]
                            Xn, Nn = XN[j % 2]
                            xo_k, no_k = f'rw_X{(j - 1) % 2}', f'rw_N{(j - 1) % 2}'
                            xn_k, nn_k = f'rw_X{j % 2}', f'rw_N{j % 2}'
                            for pi_ in range(4):
                                osl = slice(pi_ * 128, (pi_ + 1) * 128)
                                P.op('pe', [xo_k, no_k], ['ps0'], lambda e, osl=osl, Xo=Xo, No=No: e.matmul(
                                    self.ps[0][:, osl], lhsT=No[:, osl], rhs=Xo[:, osl], start=True, stop=True))
                                P.op('pe', [xo_k, no_k], ['ps1'], lambda e, osl=osl, Xo=Xo, No=No: e.matmul(
                                    self.ps[1][:, osl], lhsT=Xo[:, osl], rhs=No[:, osl], start=True, stop=True))
                            P.op('act', ['ps0'], [xn_k], lambda e, Xn=Xn: e.activation(out=Xn[:], in_=self.ps[0][:], func=AF.Copy))
                            P.op('act', ['ps1'], [nn_k], lambda e, Nn=Nn: e.activation(out=Nn[:], in_=self.ps[1][:], func=AF.Copy))
                            Qo, Qn = Q[qi_ % 2], Q[(qi_ + 1) % 2]
                            qo_k, qn_k = f'rw_Q{qi_ % 2}', f'rw_Q{(qi_ + 1) % 2}'
                            for pi_ in range(4):
                                osl = slice(pi_ * 128, (pi_ + 1) * 128)
                                P.op('pe', [nn_k, qo_k], ['ps2'], lambda e, osl=osl, Nn=Nn, Qo=Qo: e.matmul(
                                    self.ps[2][:, osl], lhsT=Nn[:, osl], rhs=Qo[:, osl], start=True, stop=True))
                            P.op('dve', ['ps2', qo_k], [qn_k], lambda e, Qo=Qo, Qn=Qn: e.tensor_tensor(out=Qn[:], in0=self.ps[2][:], in1=Qo[:], op=ALU.add))
                            qi_ += 1
                        self.stopat(4)
                        Qf = Q[qi_ % 2]; qf_k = f'rw_Q{qi_ % 2}'
                        for pi_, (tt, e_) in enumerate(probs):
                            pr = slice(e_ * 64, (e_ + 1) * 64); osl = slice(pi_ * 128, (pi_ + 1) * 128)
                            P.op('pe', ['rw_LkT', 'rw_Vtm'], ['ps3'], lambda e, osl=osl, tt=tt, pr=pr, pi_=pi_: e.matmul(
                                self.ps[3][:, pi_ * 64:(pi_ + 1) * 64], lhsT=LkT[:, osl], rhs=V_tm[:, tt, pr], start=True, stop=True))
                            P.op('pe', [qf_k, 'rw_altm'], ['ps3'], lambda e, osl=osl, tt=tt, pr=pr, pi_=pi_, Qf=Qf: e.matmul(
                                self.ps[3][:, 256 + pi_ * 64:256 + (pi_ + 1) * 64], lhsT=Qf[:, osl], rhs=al_tm[:, tt, pr], start=True, stop=True))
                        P.op('act', ['ps3'], ['rw_H'], lambda e: e.activation(out=Hh[:].rearrange("p a b -> p (a b)"), in_=self.ps[3][:, 0:256], func=AF.Copy))
                        P.op('act', ['ps3'], ['rw_G'], lambda e: e.activation(out=Gt[:].rearrange("p a b -> p (a b)"), in_=self.ps[3][:, 256:512], func=AF.Copy))
                        for pi_, (tt, e_) in enumerate(probs):
                            osl = slice(pi_ * 128, (pi_ + 1) * 128)
                            P.op('pe', [qf_k, 'rw_H'], ['ps5'], lambda e, osl=osl, pi_=pi_, Qf=Qf: e.matmul(
                                self.ps[5][:, pi_ * 64:(pi_ + 1) * 64], lhsT=Qf[:, osl], rhs=Hh[:, pi_, :], start=True, stop=True))
                        P.op('act', ['ps5'], ['rw_P1n'], lambda e: e.activation(out=P1n[:].rearrange("p a b -> p (a b)"), in_=self.ps[5][:, 0:256], func=AF.Identity, scale=-1.0))
                        self.stopat(5)
                        for ti_, tt in enumerate((g0, g0 + 1)):
                            tsl = slice(tt * 128, (tt + 1) * 128)
                            for e_ in range(2):
                                pi_ = ti_ * 2 + e_
                                pr = slice(e_ * 64, (e_ + 1) * 64); osl = slice(pi_ * 128, (pi_ + 1) * 128)
                                P.op('pe', ['rw_G', 'rw_MbT'], ['ps0'], lambda e, pr=pr, osl=osl, pi_=pi_, ti_=ti_: e.matmul(
                                    self.ps[0][pr, ti_ * 128:(ti_ + 1) * 128], lhsT=Gt[:, pi_, :], rhs=MbT[:, osl], start=True, stop=True))
                                P.op('pe', ['rw_Vtm', 'rw_MkT'], ['ps1'], lambda e, pr=pr, osl=osl, tt=tt, ti_=ti_: e.matmul(
                                    self.ps[1][pr, ti_ * 128:(ti_ + 1) * 128], lhsT=V_tm[:, tt, pr], rhs=MkT[:, osl], start=True, stop=False))
                                P.op('pe', ['rw_P1n', 'rw_MbT'], ['ps1'], lambda e, pr=pr, osl=osl, pi_=pi_, ti_=ti_: e.matmul(
                                    self.ps[1][pr, ti_ * 128:(ti_ + 1) * 128], lhsT=P1n[:, pi_, :], rhs=MbT[:, osl], start=False, stop=True))
                            for hf in range(2):
                                ch = tt * 2 + hf
                                csl = slice(hf * 64, (hf + 1) * 64)
                                gsl = Gt[csl, ti_ * 2:ti_ * 2 + 2, :].rearrange("p a b -> p (a b)")
                                p1sl = P1n[csl, ti_ * 2:ti_ * 2 + 2, :].rearrange("p a b -> p (a b)")
                                col = slice((ti_ * 2 + hf) * 128, (ti_ * 2 + hf + 1) * 128)
                                P.op('pe', ['rw_G', 'rw_Bptm'], ['ps2'], lambda e, gsl=gsl, csl=csl, tt=tt, col=col: e.matmul(
                                    self.ps[2][:, col], lhsT=gsl, rhs=Bp_tm[csl, tt, :], start=True, stop=True), rg=hf * 64)
                                P.op('pe', ['rw_Kptm', 'rw_Vtm'], ['ps6'], lambda e, csl=csl, tt=tt, col=col: e.matmul(
                                    self.ps[6][:, col], lhsT=Kp_tm[csl, tt, :], rhs=V_tm[csl, tt, :], start=True, stop=False), rg=hf * 64)
                                P.op('pe', ['rw_Bptm', 'rw_P1n'], ['ps6'], lambda e, csl=csl, tt=tt, col=col, p1sl=p1sl: e.matmul(
                                    self.ps[6][:, col], lhsT=Bp_tm[csl, tt, :], rhs=p1sl, start=False, stop=True), rg=hf * 64)
                        t2 = slice(g0 * 128, (g0 + 2) * 128)
                        P.op('dve', ['ps0', ldk], ['rw_R32'], lambda e, t2=t2: e.tensor_tensor(out=R32[:, t2], in0=rho32[:, t2], in1=self.ps[0][:, 0:256], op=ALU.subtract))
                        P.op('act', ['ps1'], ['rw_yloc'], lambda e, t2=t2: e.activation(out=yloc[:, t2], in_=self.ps[1][:, 0:256], func=AF.Copy))
                        for q_ in range(4):
                            ch = g0 * 2 + q_
                            col = slice(q_ * 128, (q_ + 1) * 128)
                            P.op('dve', ['ps2', 'cst_f'], ['rw_ptmp'], lambda e, col=col: e.tensor_tensor(out=ptmp[:], in0=self.ps[2][:, col], in1=self.mask_bd, op=ALU.mult))
                            P.op('dve', ['rw_ptmp', 'rw_etot', 'cst_f'], ['rw_Phi'], lambda e, ch=ch: e.scalar_tensor_tensor(
                                out=Phi[:, ch, :], in0=self.ident32, scalar=etot[:, ch:ch + 1], in1=ptmp[:], op0=ALU.mult, op1=ALU.subtract))
                        P.op('dve', ['ps6', 'cst_f'], ['rw_D'], lambda e, g0=g0: e.tensor_tensor(
                            out=Dd[:, g0 * 2:g0 * 2 + 4, :], in0=v4(self.ps[6]), in1=bc4(self.mask_bd), op=ALU.mult))
                        self.stopat(6)
                    self.stopat(7)
                    P.op('dve', [], [f'rw_S{si % 2}'], lambda e, si=si: e.memset(Sbd[si % 2][:], 0.0))
                    order = list(range(NCH)) if d == 0 else [3, 2, 1, 0] + list(range(NCH - 1, 3, -1))
                    for n_i, ch in enumerate(order):
                        S_ = Sbd[si % 2]; sk = f'rw_S{si % 2}'
                        tok = slice(ch * 64, (ch + 1) * 64)
                        yb = n_i % 2
                        need_y = with_ctx or ch >= 4
                        if need_y:
                            P.op('pe', [sk, 'rw_R32'], [f'ps{3 + yb}'], lambda e, S_=S_, tok=tok, yb=yb: e.matmul(
                                self.ps[3 + yb][:, 0:64], lhsT=S_[:], rhs=R32[:, tok], start=True, stop=True))
                            if d == 0:
                                P.op('dve', [f'ps{3 + yb}', 'rw_yloc'], ['rw_yacc'], lambda e, tok=tok, yb=yb: e.tensor_tensor(
                                    out=yacc[:, tok], in0=self.ps[3 + yb][:, 0:64], in1=yloc[:, tok], op=ALU.add))
                            else:
                                P.op('dve', [f'ps{3 + yb}', 'rw_yloc'], ['rw_yloc'], lambda e, tok=tok, yb=yb: e.tensor_tensor(
                                    out=yloc[:, tok], in0=self.ps[3 + yb][:, 0:64], in1=yloc[:, tok], op=ALU.add))
                                P.op('pool', ['rw_yloc', 'rw_yacc'], ['rw_yacc'], lambda e, tok=tok: e.tensor_tensor(
                                    out=yacc[:, tok], in0=yacc[:, tok], in1=yloc[:, tok], op=ALU.add))
                        P.op('pe', [sk, 'rw_Phi'], [f'ps{5 + yb}'], lambda e, S_=S_, ch=ch, yb=yb: e.matmul(
                            self.ps[5 + yb][:, 0:128], lhsT=Phi[:, ch, :], rhs=S_[:], start=True, stop=True))
                        si += 1
                        P.op('dve', [f'ps{5 + yb}', 'rw_D'], [f'rw_S{si % 2}'], lambda e, ch=ch, yb=yb, si=si: e.tensor_tensor(
                            out=Sbd[si % 2][:], in0=self.ps[5 + yb][:, 0:128], in1=Dd[:, ch, :], op=ALU.add))
                    self.stopat(8)
                for (t0, n) in TILES:
                    if not with_ctx and t0 < NCTX:
                        continue
                    ya = yacc[:, t0:t0 + n]
                    P.op('act', ['rw_yacc'], ['rw_sq5'], lambda e, ya=ya, n=n: e.activation(out=sq5[:, :n], in_=ya, func=AF.Copy))
                    P.op('pe', ['rw_sq5', 'bd_bf'], ['ps4'], lambda e, n=n: e.matmul(self.ps[4][:, :n], lhsT=self.bd_bf[:], rhs=sq5[:, :n], start=True, stop=True))
                    P.op('dve', ['ps4', 'rw_yacc'], ['rw_t5'], lambda e, ya=ya, n=n: e.scalar_tensor_tensor(
                        out=t5[:, :n], in0=self.ps[4][:, :n], scalar=-1.0 / 64, in1=ya, op0=ALU.mult, op1=ALU.add))
                    P.op('act', ['rw_t5'], ['rw_sq5'], lambda e, n=n: e.activation(out=sq5[:, :n], in_=t5[:, :n], func=AF.Square))
                    P.op('pe', ['rw_sq5', 'bd_bf'], ['ps4'], lambda e, n=n: e.matmul(self.ps[4][:, :n], lhsT=self.bd_bf[:], rhs=sq5[:, :n], start=True, stop=True))
                    P.op('act', ['ps4'], ['rw_rt'], lambda e, n=n: e.activation(out=rt[:, :n], in_=self.ps[4][:, :n], func=AF.Sqrt, scale=1.0 / 64, bias=geps[:, 0:1]))
                    P.op('dve', ['rw_rt'], ['rw_rt'], lambda e, n=n: e.reciprocal(out=rt[:, :n], in_=rt[:, :n]))
                    P.op('dve', ['rw_t5', 'rw_rt'], ['rw_t5'], lambda e, n=n: e.tensor_tensor(out=t5[:, :n], in0=t5[:, :n], in1=rt[:, :n], op=ALU.mult))
                    P.op('dve', ['rw_t5', f'vecs{l}'], ['rw_t5'], lambda e, n=n: e.tensor_scalar(
                        out=t5[:, :n], in0=t5[:, :n], scalar1=gng[:, hp:hp + 1], scalar2=gnb[:, hp:hp + 1], op0=ALU.mult, op1=ALU.add))
                    P.op('dve', ['rw_t5', 'rw_bon'], ['rw_t5'], lambda e, t0=t0, n=n: e.tensor_tensor(out=t5[:, :n], in0=t5[:, :n], in1=bon[:, t0:t0 + n], op=ALU.add))
                    P.dma(gsb[:, :n], self.g2T[cs, t0:t0 + n], ['g2T'], ['rw_g'])
                    P.op('dve', ['rw_t5', 'rw_g'], ['rw_vb'], lambda e, t0=t0, n=n: e.tensor_tensor(out=yo[:, t0:t0 + n], in0=t5[:, :n], in1=gsb[:, :n], op=ALU.mult))
                t_lo = 0 if with_ctx else NCTX
                P.dma(self.yT[2, cs, t_lo:], yo[:, t_lo:], ['rw_vb'], ['yT'])
            if 'rwkv' in self.debug:
                self.dbg(f'yrw{l}', lambda o: P.dma(o, self.yT[2], ['yT'], []), [1024, TT], BF16)


    def merge_moe(self, l):
        nc, P = self.nc, self.P
        with_ctx = l < DEPTH - 1
        last = l == DEPTH - 1
        src = (self.xT0 if l == 0 else self.xT).rearrange("(kc p) t -> p kc t", p=128)
        dstx = self.xT.rearrange("(kc p) t -> p kc t", p=128)
        dsto = self.outT.rearrange("(kc p) t -> p kc t", p=128)
        gTv = self.gT.rearrange("(i c p) t -> p i c t", i=3, p=128)
        yTv = self.yT.rearrange("i (c p) t -> p i c t", p=128)
        wbv = self.w_branch[l].rearrange("i (kc p) c -> p i kc c", p=128)
        wov = self.w_out[l].rearrange("(kc p) c -> p kc c", p=128)
        BIG = 1.0e4
        with contextlib.ExitStack() as st:
            xg = self.sb(st, "mm_xg", [128, KC, 512])
            rw32 = self.sb(st, "mm_rw32", [128, KC, 16])
            rbias = self.sb(st, "mm_rbias", [128, 16])
            sel = self.sb(st, "mm_sel", [16, 16, 128])
            P.dma(rw32[:], self.router_w.rearrange("(kc p) e -> p kc e", p=128), [], ['mm_rw32'])
            P.dma(rbias[:], self.router_b[0:1, :].partition_broadcast(128), [], ['mm_rbias'])
            P.op('dve', ['cst_f'], ['mm_sel'], lambda e: e.tensor_copy(
                out=sel[:], in_=self.ident32[0:16, 0:16].unsqueeze(2).to_broadcast([16, 16, 128])))
            wld = 0
            for (t0, n) in TILES:
                if t0 < NCTX and not with_ctx:
                    continue
                j = 1 if t0 < NCTX else 0
                nb = n // 128
                P.dma(xg[:, :, :n], src[:, :, t0:t0 + n], ['xT'], ['mm_xg'])
                with contextlib.ExitStack() as s2:
                    yb = self.sb(s2, "mm_y", [128, 3, 8, 512], BF16)
                    mg = self.sb(s2, "mm_mg", [128, KC, 512], BF16)
                    gb = [self.sb(s2, f"mm_g{i}", [128, 3, 512], BF16) for i in range(2)]
                    wb = [self.sb(s2, f"mm_wb{i}", [128, 3, 8, 512], BF16) for i in range(2)]
                    wo = [self.sb(s2, f"mm_wo{i}", [128, KC, 512], BF16) for i in range(2)]
                    m32 = self.sb(s2, "mm_m32", [128, 512])
                    tm = self.sb(s2, "mm_tm", [128, 512])
                    for i in range(3):
                        P.dma(yb[:, i, :, :n], yTv[:, i, :, t0:t0 + n], ['yT'], ['mm_y'])
                    pi = 0
                    for cb in range(4):
                        wk = f'mm_wb{cb % 2}'
                        for i in range(3):
                            P.dma(wb[cb % 2][:, i], wbv[:, i, :, cb * 512:(cb + 1) * 512], [], [wk], q='pool')
                        for dcl in range(4):
                            dc = cb * 4 + dcl
                            gk = f'mm_g{dc % 2}'
                            P.dma(gb[dc % 2][:, :, :n], gTv[:, :, dc, t0:t0 + n], ['gT'], [gk])
                            for i in range(3):
                                pst, pk = self.ps[pi % 4], f'ps{pi % 4}'
                                pi += 1
                                for kc in range(8):
                                    P.op('pe', [wk, 'mm_y'], [pk], lambda e, i=i, kc=kc, cb=cb, dcl=dcl, pst=pst: e.matmul(
                                        pst[:, :n], lhsT=wb[cb % 2][:, i, kc, dcl * 128:(dcl + 1) * 128], rhs=yb[:, i, kc, :n],
                                        start=(kc == 0), stop=(kc == 7)))
                                if i == 0:
                                    P.op('dve', [pk, gk], ['mm_m32'], lambda e, pst=pst, dc=dc: e.tensor_tensor(
                                        out=m32[:, :n], in0=pst[:, :n], in1=gb[dc % 2][:, 0, :n], op=ALU.mult))
                                else:
                                    P.op('dve', [pk, gk], ['mm_tm'], lambda e, pst=pst, dc=dc, i=i: e.tensor_tensor(
                                        out=tm[:, :n], in0=pst[:, :n], in1=gb[dc % 2][:, i, :n], op=ALU.mult))
                                    if i == 1:
                                        P.op('dve', ['mm_tm', 'mm_m32'], ['mm_m32'], lambda e: e.tensor_tensor(
                                            out=m32[:, :n], in0=m32[:, :n], in1=tm[:, :n], op=ALU.add))
                                    else:
                                        P.op('dve', ['mm_tm', 'mm_m32'], ['mm_mg'], lambda e, dc=dc: e.tensor_tensor(
                                            out=mg[:, dc, :n], in0=m32[:, :n], in1=tm[:, :n], op=ALU.add))
                    for cb in range(4):
                        wk = f'mm_wo{cb % 2}'
                        P.dma(wo[cb % 2][:], wov[:, :, cb * 512:(cb + 1) * 512], [], [wk], q='pool')
                        for dcl in range(4):
                            dc = cb * 4 + dcl
                            pst, pk = self.ps[pi % 4], f'ps{pi % 4}'
                            pi += 1
                            for kc in range(KC):
                                P.op('pe', [wk, 'mm_mg'], [pk], lambda e, kc=kc, cb=cb, dcl=dcl, pst=pst: e.matmul(
                                    pst[:, :n], lhsT=wo[cb % 2][:, kc, dcl * 128:(dcl + 1) * 128], rhs=mg[:, kc, :n],
                                    start=(kc == 0), stop=(kc == KC - 1)))
                            P.op('dve', [pk, 'mm_xg', f'mod{l}'], ['mm_xg'], lambda e, pst=pst, dc=dc: e.scalar_tensor_tensor(
                                out=xg[:, dc, :n], in0=pst[:, :n], scalar=self.mod[l][:, 32 + dc, j:j + 1], in1=xg[:, dc, :n],
                                op0=ALU.mult, op1=ALU.add))
                    if 'x1' in self.debug:
                        self.dbg(f'x1_{l}_{t0}', lambda o: P.dma(o.rearrange("(kc p) t -> p kc t", p=128), xg[:, :, :n], ['mm_xg'], []), [D, n])
                    P.barrier()
                with contextlib.ExitStack() as s2:
                    h2 = self.sb(s2, "mo_h2", [128, KC, 512], BF16)
                    sq = h2
                    rt = self.sb(s2, "mo_rt", [128, 512])
                    lg = self.sb(s2, "mo_lg", [16, 512])
                    R = {nm: self.sb(s2, "mo_" + nm, [128, 4, 16]) for nm in ('s', 'bz', 'eq', 'b2', 'mb', 'e1', 'w')}
                    r4 = {nm: self.sb(s2, "mo_" + nm, [128, 4, 4]) for nm in ('m1', 'm2', 'gsel')}
                    r1 = {nm: self.sb(s2, "mo_" + nm, [128, 4]) for nm in ('gmax', 't1', 't2', 'ws')}
                    cT = self.sb(s2, "mo_cT", [16, 512])
                    bce = [self.sb(s2, f"mo_bce{i}", [128, 512]) for i in range(2)]
                    sg = [self.sb(s2, f"mo_sg{i}", [128, 512]) for i in range(2)]
                    act = [self.sb(s2, f"mo_act{i}", [128, 4, 512], BF16) for i in range(2)]
                    wg = [self.sb(s2, f"mo_wg{i}", [128, KC, 512], BF16) for i in range(2)]
                    wu = [self.sb(s2, f"mo_wu{i}", [128, KC, 512], BF16) for i in range(2)]
                    s3 = contextlib.ExitStack()
                    xn = self.sb(s3, "mo_xn", [128, KC, 512])
                    P.op('act', ['mm_xg'], ['mo_h2'], lambda e: e.activation(out=sq[:, :, :n], in_=xg[:, :, :n], func=AF.Square))
                    for kc in range(KC):
                        P.op('pe', ['mo_h2', 'ones_bf'], ['ps6'], lambda e, kc=kc: e.matmul(
                            self.ps[6][:, :n], lhsT=self.ones_bf[:], rhs=sq[:, kc, :n], start=(kc == 0), stop=(kc == KC - 1)))
                    P.op('act', ['ps6'], ['mo_rt'], lambda e: e.activation(out=rt[:, :n], in_=self.ps[6][:, :n], func=AF.Sqrt,
                                                                      scale=1.0 / D, bias=self.eps_t[:, 0:1]))
                    P.op('dve', ['mo_rt'], ['mo_rt'], lambda e: e.reciprocal(out=rt[:, :n], in_=rt[:, :n]))
                    P.op('dve', ['mm_xg', 'mo_rt'], ['mo_xn'], lambda e: e.tensor_tensor(
                        out=xn[:, :, :n], in0=xg[:, :, :n], in1=rt[:, :n].unsqueeze(1).to_broadcast([128, KC, n]), op=ALU.mult))
                    for kc in range(KC):
                        P.op('act', ['mo_xn', f'gm2_{l}', f'mod{l}'], ['mo_xn'], lambda e, kc=kc: e.activation(
                            out=xn[:, kc, :n], in_=xn[:, kc, :n], func=AF.Identity,
                            scale=self.gm2[l][:, kc, j:j + 1], bias=self.mod[l][:, 48 + kc, j:j + 1]))
                    P.op('dve', ['mo_xn'], ['mo_h2'], lambda e: e.tensor_copy(out=h2[:, :, :n], in_=xn[:, :, :n]))
                    for kc in range(KC):
                        P.op('pe', ['mo_xn', 'mm_rw32'], ['ps5'], lambda e, kc=kc: e.matmul(
                            self.ps[5][0:16, :n], lhsT=rw32[:, kc, :], rhs=xn[:, kc, :n], start=(kc == 0), stop=(kc == KC - 1)))
                    P.op('act', ['ps5'], ['mo_lg'], lambda e: e.activation(out=lg[:, :n], in_=self.ps[5][0:16, :n], func=AF.Copy))
                    for b_ in range(nb):
                        P.op('pe', ['mo_lg', 'cst_f'], ['ps4'], lambda e, b_=b_: e.transpose(
                            self.ps[4][:, b_ * 16:(b_ + 1) * 16], lg[0:16, b_ * 128:(b_ + 1) * 128], self.ident32[0:16, 0:16]))
                    s_, bz, eq, b2, mb, e1, w_ = (R[k][:, :nb, :] for k in ('s', 'bz', 'eq', 'b2', 'mb', 'e1', 'w'))
                    m1, m2, gsel = (r4[k][:, :nb, :] for k in ('m1', 'm2', 'gsel'))
                    gmax, t1, t2, ws = (r1[k][:, :nb] for k in ('gmax', 't1', 't2', 'ws'))
                    g4 = lambda a: a.rearrange("p b (g k) -> p b g k", k=4)
                    V = lambda reads, writes, fn: P.op('dve', reads, writes, fn)
                    P.op('act', ['ps4'], ['mo_s'], lambda e: e.activation(
                        out=s_, in_=self.ps[4][:, 0:nb * 16].rearrange("p (b k) -> p b k", k=16), func=AF.Sigmoid))
                    V(['mo_s', 'mm_rbias'], ['mo_bz'], lambda e: e.tensor_tensor(out=bz, in0=s_, in1=rbias[:].unsqueeze(1).to_broadcast([128, nb, 16]), op=ALU.add))
                    V(['mo_bz'], ['mo_m1'], lambda e: e.tensor_reduce(out=m1, in_=g4(bz), axis=AX.X, op=ALU.max))
                    V(['mo_bz', 'mo_m1'], ['mo_eq'], lambda e: e.tensor_tensor(out=g4(eq), in0=g4(bz), in1=m1.unsqueeze(3).to_broadcast([128, nb, 4, 4]), op=ALU.is_equal))
                    V(['mo_eq', 'mo_bz'], ['mo_b2'], lambda e: e.scalar_tensor_tensor(out=b2, in0=eq, scalar=-BIG, in1=bz, op0=ALU.mult, op1=ALU.add))
                    V(['mo_b2'], ['mo_m2'], lambda e: e.tensor_reduce(out=m2, in_=g4(b2), axis=AX.X, op=ALU.max))
                    V(['mo_m1', 'mo_m2'], ['mo_m1'], lambda e: e.tensor_tensor(out=m1, in0=m1, in1=m2, op=ALU.add))
                    V(['mo_m1'], ['mo_gmax'], lambda e: e.tensor_reduce(out=gmax, in_=m1, axis=AX.X, op=ALU.max))
                    V(['mo_m1', 'mo_gmax'], ['mo_gsel'], lambda e: e.tensor_tensor(out=gsel, in0=m1, in1=gmax.unsqueeze(2).to_broadcast([128, nb, 4]), op=ALU.is_equal))
                    V(['mo_gsel'], ['mo_gsel'], lambda e: e.tensor_scalar(out=gsel, in0=gsel, scalar1=BIG, scalar2=-BIG, op0=ALU.mult, op1=ALU.add))
                    V(['mo_bz', 'mo_gsel'], ['mo_mb'], lambda e: e.tensor_tensor(out=g4(mb), in0=g4(bz), in1=gsel.unsqueeze(3).to_broadcast([128, nb, 4, 4]), op=ALU.add))
                    V(['mo_mb'], ['mo_t1'], lambda e: e.tensor_reduce(out=t1, in_=mb, axis=AX.X, op=ALU.max))
                    V(['mo_mb', 'mo_t1'], ['mo_e1'], lambda e: e.tensor_tensor(out=e1, in0=mb, in1=t1.unsqueeze(2).to_broadcast([128, nb, 16]), op=ALU.is_equal))
                    V(['mo_e1', 'mo_mb'], ['mo_b2'], lambda e: e.scalar_tensor_tensor(out=b2, in0=e1, scalar=-BIG, in1=mb, op0=ALU.mult, op1=ALU.add))
                    V(['mo_b2'], ['mo_t2'], lambda e: e.tensor_reduce(out=t2, in_=b2, axis=AX.X, op=ALU.max))
                    V(['mo_b2', 'mo_t2'], ['mo_eq'], lambda e: e.tensor_tensor(out=eq, in0=b2, in1=t2.unsqueeze(2).to_broadcast([128, nb, 16]), op=ALU.is_equal))
                    V(['mo_eq', 'mo_e1'], ['mo_e1'], lambda e: e.tensor_tensor(out=e1, in0=e1, in1=eq, op=ALU.add))
                    V(['mo_e1', 'mo_s'], ['mo_w'], lambda e: e.tensor_tensor(out=w_, in0=e1, in1=s_, op=ALU.mult))
                    V(['mo_w'], ['mo_ws'], lambda e: e.tensor_reduce(out=ws, in_=w_, axis=AX.X, op=ALU.add))
                    V(['mo_ws'], ['mo_ws'], lambda e: e.reciprocal(out=ws, in_=ws))
                    V(['mo_w', 'mo_ws'], ['mo_w'], lambda e: e.tensor_tensor(out=w_, in0=w_, in1=ws.unsqueeze(2).to_broadcast([128, nb, 16]), op=ALU.mult))
                    for b_ in range(nb):
                        P.op('pe', ['mo_w', 'cst_f'], ['ps5'], lambda e, b_=b_: e.transpose(
                            self.ps[5][0:16, b_ * 128:(b_ + 1) * 128], R['w'][:, b_, :], self.ident32))
                    P.op('act', ['ps5'], ['mo_cT'], lambda e: e.activation(out=cT[:, :n], in_=self.ps[5][0:16, :n], func=AF.Copy))
                    if 'comb' in self.debug:
                        self.dbg(f'comb_{l}_{t0}', lambda o: P.dma(o, cT[:, :n], ['mo_cT'], []), [16, n])
                    P.barrier()
                    s3.close()
                    s3 = contextlib.ExitStack()
                    wd = [self.sb(s3, f"mo_wd{i}", [128, 4, D], BF16) for i in range(2)]
                    pi = 0
                    for ex in range(16):
                        b = ex % 2
                        P.dma(wg[b][:], self.moe_g[l, ex].rearrange("(kc p) f -> p kc f", p=128), [], [f'mo_wg{b}'], q='pool')
                        P.dma(wu[b][:], self.moe_u[l, ex].rearrange("(kc p) f -> p kc f", p=128), [], [f'mo_wu{b}'], q='pool')
                        P.dma(wd[b][:], self.moe_d[l, ex].rearrange("(fc p) d -> p fc d", p=128), [], [f'mo_wd{b}'], q='pool')
                        P.op('pe', ['mm_sel', 'mo_cT'], ['ps6'], lambda e, ex=ex: e.matmul(
                            self.ps[6][:, :n], lhsT=sel[:, ex, :], rhs=cT[:, :n], start=True, stop=True), rg=0)
                        P.op('act', ['ps6'], [f'mo_bce{b}'], lambda e, b=b: e.activation(out=bce[b][:, :n], in_=self.ps[6][:, :n], func=AF.Copy))
                        for fc in range(4):
                            pg, pgk = self.ps[pi % 4], f'ps{pi % 4}'
                            pu, puk = self.ps[(pi + 1) % 4], f'ps{(pi + 1) % 4}'
                            pi += 2
                            for kc in range(KC):
                                P.op('pe', [f'mo_wg{b}', 'mo_h2'], [pgk], lambda e, kc=kc, fc=fc, b=b, pg=pg: e.matmul(
                                    pg[:, :n], lhsT=wg[b][:, kc, fc * 128:(fc + 1) * 128], rhs=h2[:, kc, :n], start=(kc == 0), stop=(kc == KC - 1)))
                            for kc in range(KC):
                                P.op('pe', [f'mo_wu{b}', 'mo_h2'], [puk], lambda e, kc=kc, fc=fc, b=b, pu=pu: e.matmul(
                                    pu[:, :n], lhsT=wu[b][:, kc, fc * 128:(fc + 1) * 128], rhs=h2[:, kc, :n], start=(kc == 0), stop=(kc == KC - 1)))
                            sb_ = fc % 2
                            P.op('act', [pgk], [f'mo_sg{sb_}'], lambda e, pg=pg, sb_=sb_: e.activation(out=sg[sb_][:, :n], in_=pg[:, :n], func=AF.Silu))
                            P.op('dve', [puk, f'mo_sg{sb_}'], [f'mo_sg{sb_}'], lambda e, pu=pu, sb_=sb_: e.tensor_tensor(
                                out=sg[sb_][:, :n], in0=pu[:, :n], in1=sg[sb_][:, :n], op=ALU.mult))
                            P.op('dve', [f'mo_sg{sb_}', f'mo_bce{b}'], [f'mo_act{b}'], lambda e, sb_=sb_, b=b, fc=fc: e.tensor_tensor(
                                out=act[b][:, fc, :n], in0=sg[sb_][:, :n], in1=bce[b][:, :n], op=ALU.mult))
                        for dc in range(KC):
                            pd, pdk = self.ps[pi % 4], f'ps{pi % 4}'
                            pi += 1
                            for fc in range(4):
                                P.op('pe', [f'mo_wd{b}', f'mo_act{b}'], [pdk], lambda e, fc=fc, dc=dc, b=b, pd=pd: e.matmul(
                                    pd[:, :n], lhsT=wd[b][:, fc, dc * 128:(dc + 1) * 128], rhs=act[b][:, fc, :n], start=(fc == 0), stop=(fc == 3)))
                            P.op('dve', [pdk, 'mm_xg', f'mod{l}'], ['mm_xg'], lambda e, pd=pd, dc=dc: e.scalar_tensor_tensor(
                                out=xg[:, dc, :n], in0=pd[:, :n], scalar=self.mod[l][:, 80 + dc, j:j + 1], in1=xg[:, dc, :n],
                                op0=ALU.mult, op1=ALU.add))
                    if last:
                        P.dma(dsto[:, :, t0 - NCTX:t0 - NCTX + n], xg[:, :, :n], ['mm_xg'], ['outT'])
                    else:
                        P.dma(dstx[:, :, t0:t0 + n], xg[:, :, :n], ['mm_xg'], ['xT'])
                    if 'x2' in self.debug:
                        self.dbg(f'x2_{l}_{t0}', lambda o: P.dma(o.rearrange("(kc p) t -> p kc t", p=128), xg[:, :, :n], ['mm_xg'], []), [D, n])
                    P.barrier()
                    s3.close()

    def final_out(self):
        P = self.P
        P.dma(self.outT, self.xT[:, NCTX:], ['xT'], [])


def na_tables(rpb):
    c = np.arange(64)
    cs = np.clip(c - 8, 0, 48)
    kc = np.arange(64)
    inwin = (kc[:, None] >= cs[None, :]) & (kc[:, None] < cs[None, :] + 16)
    dc = np.clip(kc[:, None] - c[None, :] + 15, 0, 30)
    out = np.full((16, 2, 64, 14, 64), NEG, np.float32)
    for jj in range(2):
        for dr in range(14):
            g = rpb[:, dr + jj][:, dc]
            out[:, jj, :, dr, :] = np.where(inwin[None], g, np.float32(NEG))
    return out.reshape(16, 128, 14 * 64)


NCONST = 512 + 2 * SEQ + 384


def make_consts():
    c = np.zeros((128, NCONST), np.float32)
    p = np.arange(128)
    c[p, p] = 1.0
    c[p, 128 + (p ^ 32)] = 1.0
    j = p[:, None]; i = p[None, :]
    same = (j // 64) == (i // 64)
    c[:, 256:384] = (same & (j <= i)).astype(np.float32)
    c[:, 384:512] = (same & (j >= i)).astype(np.float32)
    t = np.arange(SEQ)
    pos = np.where(p[:, None] < 64, (t // 64)[None, :], (t % 64)[None, :]).astype(np.float32)
    inv = (10000.0 ** (-np.arange(0, 64, 2, dtype=np.float32) / 64)).astype(np.float32)
    ang = pos * inv[(p % 32)][:, None]
    c[:, 512:512 + SEQ] = np.cos(ang)
    sgn = np.where((p % 64) < 32, -1.0, 1.0).astype(np.float32)
    c[:, 512 + SEQ:512 + 2 * SEQ] = np.sin(ang) * sgn[:, None]
    o = 512 + 2 * SEQ
    c[:, o:o + 128] = (same & (j < i)).astype(np.float32)
    c[:, o + 128:o + 256] = (same & (j > i)).astype(np.float32)
    c[:, o + 256:o + 384] = same.astype(np.float32)
    return c


def host_inputs(inp, b):
    xT0 = np.ascontiguousarray(np.concatenate([inp['ctx'][b], inp['x'][b]], axis=0).T)
    cT = np.stack([fm(inp['c'][b]), fm(inp['c_ctx'])], axis=-1).reshape(128, 32)
    return {
        'xT0': xT0, 'cT': np.ascontiguousarray(cT),
        'ada_w': inp['ada_w'], 'w_in': inp['w_in'],
        'vecs': np.stack([pack_vecs(inp, l) for l in range(DEPTH)]),
        'natab': np.stack([na_tables(inp['na_rpb'][l]) for l in range(DEPTH)]),
        'consts': make_consts(), 'gla_gate_w2': inp['gla_gate_w2'],
        'rw_w2': inp['rw_w2'], 'rw_a2': inp['rw_a2'], 'rw_g2': inp['rw_g2'],
        'w_branch': inp['w_branch'], 'w_out': inp['w_out'], 'router_w': inp['router_w'],
        'router_bias': inp['router_bias'].reshape(1, 16),
        'moe_w_gate': inp['moe_w_gate'], 'moe_w_up': inp['moe_w_up'], 'moe_w_down': inp['moe_w_down'],
    }


def kernel(**inputs):
    inp = {k: np.asarray(v) for k, v in inputs.items()}
    bld = Builder()
    nc = bld.build()
    in_maps = [host_inputs(inp, c % 4) for c in range(8)]
    res = run_bass_kernel_spmd(nc, in_maps, core_ids=list(range(8)))
    out = np.stack([np.ascontiguousarray(res.results[b]["outT"].T) for b in range(4)], axis=0)
    return out.astype(np.float32)
```

```python
import contextlib
import os
import numpy as np
import concourse.bass as bass
import concourse.mybir as mybir
from concourse.bass_utils import run_bass_kernel_spmd

F32 = mybir.dt.float32
BF16 = mybir.dt.bfloat16
AF = mybir.ActivationFunctionType
ALU = mybir.AluOpType
AX = mybir.AxisListType

D = 2048
KC = 16
NCTX = 256
SEQ = 2048
TT = NCTX + SEQ
D_IN = 16032
EPS = 1e-6
NEG = -30000.0
DEPTH = 2

C_NAQ, C_NAK, C_NAV = 0, 1024, 2048
C_GQ, C_GK, C_GV, C_GR, C_GGD = 3072, 3584, 4096, 5120, 6144
C_RW = 6176
C_RR, C_RK, C_RV, C_RWD, C_RAD, C_RGD = C_RW, C_RW + 1024, C_RW + 2048, C_RW + 3072, C_RW + 3264, C_RW + 3456
C_GATE = 9888

TILES = [(0, 256), (256, 512), (768, 512), (1280, 512), (1792, 512)]

NDS = 12


class PEProxy:
    def __init__(self, real):
        self._real = real
        self._last = None
        self._dummy = None

    def _sep(self, out, w):
        K = w.shape[0]
        M = 1
        for d_ in w.shape[1:]:
            M *= d_
        t = None if K == 128 else (w.base_partition(), K)
        if t is not None and self._last is not None and t != self._last and self._dummy is not None:
            self._dummy(self._real)
        self._last = t

    def matmul(self, out, lhsT=None, rhs=None, **kw):
        self._sep(out, lhsT)
        return self._real.matmul(out, lhsT=lhsT, rhs=rhs, **kw)

    def transpose(self, out, in_, identity, **kw):
        self._sep(out, in_)
        return self._real.transpose(out, in_, identity, **kw)

    def __getattr__(self, name):
        return getattr(self._real, name)


class Prog:
    def __init__(self, nc, es):
        self.nc = nc
        self.e = dict(pe=PEProxy(nc.tensor), act=nc.scalar, dve=nc.vector, pool=nc.gpsimd, sp=nc.sync)
        self.sem = {k: es.enter_context(nc.semaphore("s_" + k)) for k in self.e}
        self.cnt = {k: 0 for k in self.e}
        self.seen = {k: {} for k in self.e}
        self.lw = {}
        self.rd = {}
        self.dsem = [es.enter_context(nc.semaphore(f"dq{i}")) for i in range(NDS)]
        self.dcnt = [0] * NDS
        self.dnext = 0

    def semh(self, k):
        return self.dsem[k[1]] if isinstance(k, tuple) else self.sem[k]

    def _wait(self, eng, k, v):
        if self.seen[eng].get(k, 0) < v:
            self.e[eng].wait_ge(self.semh(k), v)
            self.seen[eng][k] = v

    def set_parent(self, child, parent):
        self.parent = getattr(self, 'parent', {})
        self.children = getattr(self, 'children', {})
        self.parent[child] = parent
        self.children.setdefault(parent, []).append(child)

    def _expand(self, bs):
        par = getattr(self, 'parent', {})
        chl = getattr(self, 'children', {})
        out = []
        for b in bs:
            out.append(b)
            if b in par:
                out.append(par[b])
            out.extend(chl.get(b, ()))
        return out

    def _deps(self, eng, reads, writes):
        reads = self._expand(reads)
        writes = self._expand(writes)
        deps = {}
        for b in reads:
            lw = self.lw.get(b)
            if lw:
                deps[lw[0]] = max(deps.get(lw[0], 0), lw[1])
        for b in writes:
            lw = self.lw.get(b)
            if lw:
                deps[lw[0]] = max(deps.get(lw[0], 0), lw[1])
            for k, v in self.rd.get(b, {}).items():
                deps[k] = max(deps.get(k, 0), v)
        for k, v in deps.items():
            if k == 'pe' and eng == 'pe':
                continue
            self._wait(eng, k, v)

    def _mark(self, pt, reads, writes):
        for b in writes:
            self.lw[b] = pt
            self.rd[b] = {}
        for b in reads:
            d = self.rd.setdefault(b, {})
            d[pt[0]] = max(d.get(pt[0], 0), pt[1])

    halt = False

    pe_rg = None
    dummy = None

    def op(self, eng, reads, writes, fn, rg=None):
        if self.halt:
            return None
        if eng == 'pe':
            pass
        self._deps(eng, reads, writes)
        ins = fn(self.e[eng])
        self.cnt[eng] += 1
        ins.then_inc(self.sem[eng], 1)
        self._mark((eng, self.cnt[eng]), reads, writes)
        return ins

    def dma(self, out, in_, reads, writes, q='sp', **kw):
        if self.halt:
            return None
        i = self.dnext % NDS
        self.dnext += 1
        k = ('d', i)
        if self.dcnt[i]:
            self._wait(q, k, self.dcnt[i])
        self._deps(q, reads, writes)
        ins = self.e[q].dma_start(out=out, in_=in_, **kw)
        self.dcnt[i] += 16
        ins.then_inc(self.dsem[i], 16)
        self._mark((k, self.dcnt[i]), reads, writes)
        return ins

    def barrier(self):
        for q in self.e:
            for i in range(NDS):
                if self.dcnt[i]:
                    self._wait(q, ('d', i), self.dcnt[i])
            for k in self.e:
                if k != q and self.cnt[k]:
                    self._wait(q, k, self.cnt[k])
        self.lw = {}
        self.rd = {}

    def finish(self, q='sp'):
        for i in range(NDS):
            if self.dcnt[i]:
                self.e[q].wait_ge(self.dsem[i], self.dcnt[i])
        for k in self.e:
            if k != q and self.cnt[k]:
                self.e[q].wait_ge(self.sem[k], self.cnt[k])


def fm(v):
    v = np.asarray(v, np.float32)
    return np.ascontiguousarray(v.reshape(-1, 128).T)


VEC_SLOTS = {}


def _vec_layout():
    off = 0
    def add(name, n):
        nonlocal off
        VEC_SLOTS[name] = (off, n)
        off += n
    add('n1g', 16); add('n2g', 16); add('adab', 96)
    add('naq', 1); add('nak', 1)
    add('ggb', 8)
    add('gng', 2)
    add('mu_r', 8); add('mu_k', 8); add('mu_v', 8); add('mu_wd', 2); add('mu_ad', 2); add('mu_gd', 2)
    add('w0', 16); add('a0', 16); add('kk', 8); add('ka', 8); add('rk', 8); add('gng_rw', 8); add('gnb_rw', 8)
    return off


NV = _vec_layout()


def pack_vecs(inp, l):
    v = np.zeros((128, NV), np.float32)
    def put(name, arr):
        o, n = VEC_SLOTS[name]
        assert arr.shape == (128, n), (name, arr.shape)
        v[:, o:o + n] = arr
    put('n1g', fm(inp['norm1_g'][l])); put('n2g', fm(inp['norm2_g'][l])); put('adab', fm(inp['ada_b'][l]))
    put('naq', np.tile(inp['na_q_norm'][l], 2)[:, None]); put('nak', np.tile(inp['na_k_norm'][l], 2)[:, None])
    put('ggb', fm(inp['gla_gate_b'][l].reshape(-1)))
    put('gng', fm(inp['gla_norm_g'][l]))
    mu = inp['rw_mu'][l]
    put('mu_r', fm(mu[0:1024])); put('mu_k', fm(mu[1024:2048])); put('mu_v', fm(mu[2048:3072]))
    def pad96(a):
        o = np.zeros((128, 2), np.float32); o[:96, 0] = a[:96]; o[:96, 1] = a[96:192]; return o
    put('mu_wd', pad96(mu[3072:3264])); put('mu_ad', pad96(mu[3264:3456])); put('mu_gd', fm(mu[3456:3712]))
    put('w0', fm(inp['rw_w0'][l].reshape(-1))); put('a0', fm(inp['rw_a0'][l].reshape(-1)))
    put('kk', fm(inp['rw_k_k'][l])); put('ka', fm(inp['rw_k_a'][l])); put('rk', fm(inp['rw_r_k'][l].reshape(-1)))
    put('gng_rw', fm(inp['rw_gn_g'][l])); put('gnb_rw', fm(inp['rw_gn_b'][l]))
    return v


class _Stop(Exception):
    pass


class Builder:
    rw_stop = 0
    rw_slots = 2

    def stopat(self, k):
        if self.rw_stop == k:
            self.P.halt = True

    def __init__(self, layers=(0, 1), upto='all', debug=()):
        self.layers = layers
        self.upto = upto
        self.debug = set(debug)
        self.nc = bass.Bass("TRN2", target_bir_lowering=False)
        self.es = contextlib.ExitStack()
        self.dbg_outs = {}

    def din(self, name, shape, dt=F32):
        return self.nc.dram_tensor(name, list(shape), dt, kind="ExternalInput").ap()

    def dout(self, name, shape, dt=F32):
        return self.nc.dram_tensor(name, list(shape), dt, kind="ExternalOutput").ap()

    def dscr(self, name, shape, dt=F32):
        return self.nc.dram_tensor(name, list(shape), dt, kind="Internal").ap()

    def sb(self, st, name, shape, dt=F32):
        self._uid = getattr(self, '_uid', 0) + 1
        return st.enter_context(self.nc.sbuf_tensor(f"{name}_u{self._uid}", list(shape), dt))

    def vec(self, l, name):
        o, n = VEC_SLOTS[name]
        return self.vecs[l][:, o:o + n]

    def build(self):
        nc = self.nc
        es = self.es
        with es:
            self.P = P = Prog(nc, es)
            self.xT0 = self.din("xT0", [D, TT])
            self.cT = self.din("cT", [128, 32])
            self.ada_w = self.din("ada_w", [DEPTH, D, 6 * D])
            self.w_in = self.din("w_in", [DEPTH, D, D_IN])
            self.vecs_d = self.din("vecs", [DEPTH, 128, NV])
            self.outT = self.dout("outT", [D, SEQ])
            self.xT = self.dscr("xT_s", [D, TT])
            self.zT = self.dscr("zT_s", [C_GATE, TT])
            self.gT = self.dscr("gT_s", [3 * D, TT], BF16)
            self.vtm = self.dscr("vtm_s", [TT, 2048], BF16)
            self.yT = self.dscr("yT_s", [3, 1024, TT], BF16)
            self.natab = self.din("natab", [DEPTH, 16, 128, 14 * 64])
            self.consts = self.din("consts", [128, NCONST])
            self.gla_w2 = self.din("gla_gate_w2", [DEPTH, 2, 16, 512])
            self.rw_w2 = self.din("rw_w2", [DEPTH, 2, 96, 1024])
            self.rw_a2 = self.din("rw_a2", [DEPTH, 2, 96, 1024])
            self.rw_g2 = self.din("rw_g2", [DEPTH, 256, 1024])
            self.w_branch = self.din("w_branch", [DEPTH, 3, 1024, D])
            self.w_out = self.din("w_out", [DEPTH, D, D])
            self.router_w = self.din("router_w", [D, 16])
            self.router_b = self.din("router_bias", [1, 16])
            self.moe_g = self.din("moe_w_gate", [DEPTH, 16, D, 512])
            self.moe_u = self.din("moe_w_up", [DEPTH, 16, D, 512])
            self.moe_d = self.din("moe_w_down", [DEPTH, 16, 512, D])
            self.ldT = self.dscr("ldT_s", [2, 1024, TT])
            self.aT = self.dscr("aT_s", [2, 1024, TT])
            self.g2T = self.dscr("g2T_s", [1024, TT], BF16)
            self.ones_bf = self.sb(es, "ones_bf", [128, 128], BF16)
            self.vecs = [self.sb(es, f"vecs{l}", [128, NV]) for l in range(DEPTH)]
            self.mod = [self.sb(es, f"mod{l}", [128, 96, 2]) for l in range(DEPTH)]
            self.gm1 = [self.sb(es, f"gm1_{l}", [128, 16, 2]) for l in range(DEPTH)]
            self.gm2 = [self.sb(es, f"gm2_{l}", [128, 16, 2]) for l in range(DEPTH)]
            self.ps = [es.enter_context(nc.psum_tensor(f"ps{i}", [128, 512], F32)) for i in range(7)]
            self.psb = es.enter_context(nc.psum_tensor("psb", [128, 1024], BF16))
            psd = self.psb[:, 512:1024].bitcast(F32)
            P.e['pe']._dummy = lambda e: e.matmul(psd[:, 0:1], lhsT=self.ones_bf[:], rhs=self.ones_bf[:, 0:1], start=True, stop=True)
            cst = self.sb(es, "cst_f", [128, 4 * 128])
            self.cst_bf = self.sb(es, "cst_bf", [128, 4 * 128], BF16)
            P.dma(cst[:], self.consts[:, 0:512], [], ['cst_f'])
            P.op('dve', ['cst_f'], ['cst_bf'], lambda e: e.tensor_copy(out=self.cst_bf[:], in_=cst[:]))
            self.ident_bf = self.cst_bf[:, 0:128]
            self.perm_bf = self.cst_bf[:, 128:256]
            self.mask_f = cst[:, 256:384]
            self.mask_b = cst[:, 384:512]
            self.ident32 = cst[:, 0:128]
            cst2 = self.sb(es, "cst2", [128, 384])
            P.dma(cst2[:], self.consts[:, 512 + 2 * SEQ:512 + 2 * SEQ + 384], [], ['cst_f'])
            self.mask_fs = cst2[:, 0:128]
            self.mask_bs = cst2[:, 128:256]
            self.mask_bd = cst2[:, 256:384]
            P.op('pool', [], ['ones_bf'], lambda e: e.memset(self.ones_bf[:], 1.0))
            self.bd_bf = self.sb(es, "bd_bf", [128, 128], BF16)
            P.op('pool', [], ['bd_bf'], lambda e: e.memset(self.bd_bf[:], 0.0))
            P.op('pool', ['bd_bf'], ['bd_bf'], lambda e: e.memset(self.bd_bf[0:64, 0:64], 1.0))
            P.op('pool', ['bd_bf'], ['bd_bf'], lambda e: e.memset(self.bd_bf[64:128, 64:128], 1.0))
            self.eps_t = self.sb(es, "eps_t", [128, 1])
            P.op('pool', [], ['eps_t'], lambda e: e.memset(self.eps_t[:], EPS))
            for l in range(DEPTH):
                P.dma(self.vecs[l][:], self.vecs_d[l], [], [f'vecs{l}'])

            with nc.named_scope('prologue'):
                self.prologue()
            for l in self.layers:
                self.layer(l)
                if self.upto != 'all':
                    break
            P.finish()
        return nc

    def dbg(self, name, src_ap_fn, shape, dt=F32, reads=()):
        o = self.dout("dbg_" + name, shape, dt)
        self.dbg_outs[name] = o
        src_ap_fn(o)

    def prologue(self):
        nc, P = self.nc, self.P
        with contextlib.ExitStack() as st:
            sc = self.sb(st, "sc", [128, 32])
            sc2 = self.sb(st, "sc2", [128, 32])
            awb = [self.sb(st, f"awb{i}", [128, 16, 512]) for i in range(2)]
            P.dma(sc[:], self.cT, [], ['sc'])
            P.op('act', ['sc'], ['sc2'], lambda e: e.activation(out=sc2[:], in_=sc[:], func=AF.Silu))
            for l in range(DEPTH):
                aw = self.ada_w[l].rearrange("(kc p) c -> p kc c", p=128)
                adab = self.vec(l, 'adab')
                for g in range(24):
                    wt = awb[g % 2]
                    wk = f'awb{g % 2}'
                    P.dma(wt[:], aw[:, :, g * 512:(g + 1) * 512], [], [wk], q=('sp' if g % 2 == 0 else 'act'))
                    pst = self.ps[g % 2]
                    pk = f'ps{g % 2}'
                    for f in range(4):
                        for kc in range(KC):
                            P.op('pe', [wk, 'sc2'], [pk], lambda e, f=f, kc=kc: e.matmul(
                                pst[:, f * 2:(f + 1) * 2], lhsT=wt[:, kc, f * 128:(f + 1) * 128],
                                rhs=sc2[:, kc * 2:(kc + 1) * 2], start=(kc == 0), stop=(kc == KC - 1)))
                    P.op('dve', [pk, f'vecs{l}'], [f'mod{l}'], lambda e, g=g: e.tensor_tensor(
                        out=self.mod[l][:, g * 4:(g + 1) * 4, :],
                        in0=pst[:, 0:8].rearrange("p (f j) -> p f j", j=2),
                        in1=adab[:, g * 4:(g + 1) * 4].unsqueeze(2).to_broadcast([128, 4, 2]), op=ALU.add))
                for (gm, nm, sco, key) in ((self.gm1[l], 'n1g', 16, f'gm1_{l}'), (self.gm2[l], 'n2g', 64, f'gm2_{l}')):
                    P.op('dve', [f'mod{l}'], [key], lambda e, gm=gm, sco=sco: e.tensor_scalar(
                        out=gm[:], in0=self.mod[l][:, sco:sco + 16, :], scalar1=1.0, scalar2=None, op0=ALU.add))
                    P.op('dve', [key, f'vecs{l}'], [key], lambda e, gm=gm, nm=nm: e.tensor_tensor(
                        out=gm[:], in0=gm[:], in1=self.vec(l, nm).unsqueeze(2).to_broadcast([128, 16, 2]), op=ALU.mult))
            if 'mod' in self.debug:
                for l in range(DEPTH):
                    self.dbg(f'mod{l}', lambda o, l=l: P.dma(o, self.mod[l][:].rearrange("p a b -> p (a b)"), [f'mod{l}'], []), [128, 192])
            P.barrier()

    def norm_tile(self, st_bufs, x, xk, n, j, gm, gmk, shmod, shoff, modk, out_fn, outk, ps_i=6):
        P = self.P
        sq, rt = st_bufs
        pst = self.ps[ps_i]
        pk = f'ps{ps_i}'
        P.op('act', [xk], ['nsq'], lambda e: e.activation(out=sq[:, :, :n], in_=x, func=AF.Square))
        for kc in range(KC):
            P.op('pe', ['nsq', 'ones_bf'], [pk], lambda e, kc=kc: e.matmul(
                pst[:, :n], lhsT=self.ones_bf[:], rhs=sq[:, kc, :n], start=(kc == 0), stop=(kc == KC - 1)))
        P.op('act', [pk], ['nrt'], lambda e: e.activation(out=rt[:, :n], in_=pst[:, :n], func=AF.Sqrt,
                                                         scale=1.0 / D, bias=self.eps_t[:, 0:1]))
        P.op('dve', ['nrt'], ['nrt'], lambda e: e.reciprocal(out=rt[:, :n], in_=rt[:, :n]))
        P.op('dve', [xk, 'nrt'], [xk], lambda e: e.tensor_tensor(
            out=x, in0=x, in1=rt[:, :n].unsqueeze(1).to_broadcast([128, KC, n]), op=ALU.mult))
        for kc in range(KC):
            P.op('act', [xk, gmk, modk], [outk], lambda e, kc=kc: e.activation(
                out=out_fn(kc), in_=x[:, kc, :], func=AF.Identity,
                scale=gm[:, kc, j:j + 1], bias=shmod[:, shoff + kc, j:j + 1]))

    def layer(self, l):
        nc, P = self.nc, self.P
        src = self.xT0 if l == 0 else self.xT
        with contextlib.ExitStack() as st:
            hT = self.sb(st, "hT", [128, KC, TT], BF16)
            with contextlib.ExitStack() as st2:
                xb = [self.sb(st2, f"xb{i}", [128, KC, 512]) for i in range(2)]
                sq = self.sb(st2, "nsq", [128, KC, 512], BF16)
                rt = self.sb(st2, "nrt", [128, 512])
                for ti, (t0, n) in enumerate(TILES):
                    x = xb[ti % 2]
                    xk = f'xb{ti % 2}'
                    j = 1 if t0 < NCTX else 0
                    P.dma(x[:, :, :n], src.rearrange("(kc p) t -> p kc t", p=128)[:, :, t0:t0 + n], ['xT'], [xk])
                    self.norm_tile((sq, rt), x[:, :, :n], xk, n, j, self.gm1[l], f'gm1_{l}', self.mod[l], 0, f'mod{l}',
                                   lambda kc, t0=t0, n=n: hT[:, kc, t0:t0 + n], 'hT')
                if 'hT' in self.debug:
                    self.dbg(f'hT{l}', lambda o: P.dma(o.rearrange("(kc p) t -> p kc t", p=128), hT[:], ['hT'], []), [D, TT], BF16)
                P.barrier()
            if self.upto == 'norm':
                return
            with nc.named_scope(f'L{l}_inproj'):
                self.inproj(l, hT)
            P.barrier()
        if self.upto == 'inproj':
            return
        with nc.named_scope(f'L{l}_na'):
            self.na_mixer(l)
        P.barrier()
        if self.upto == 'na':
            return
        with nc.named_scope(f'L{l}_gla'):
            self.gla_mixer(l)
        P.barrier()
        if self.upto == 'gla':
            return
        with nc.named_scope(f'L{l}_rwpre'):
            self.rwkv_pre(l)
        P.barrier()
        if self.upto == 'rwpre':
            self.dbg(f'ld{l}', lambda o: P.dma(o, self.ldT, ['ldT'], []), [2, 1024, TT])
            self.dbg(f'a{l}', lambda o: P.dma(o, self.aT, ['aT'], []), [2, 1024, TT])
            self.dbg(f'g2{l}', lambda o: P.dma(o, self.g2T, ['g2T'], []), [1024, TT], BF16)
            return
        with nc.named_scope(f'L{l}_rwkv'):
            self.rwkv_mixer(l)
        P.halt = False
        P.barrier()
        if self.upto == 'rwkv':
            return
        with nc.named_scope(f'L{l}_mergemoe'):
            self.merge_moe(l)
        P.barrier()

    def inproj(self, l, hT):
        nc, P = self.nc, self.P
        w = self.w_in[l].rearrange("(kc p) c -> p kc c", p=128)
        with contextlib.ExitStack() as st:
            wsl = [self.sb(st, f"wsl{i}", [128, KC, 512], BF16) for i in range(3)]
            zst = [self.sb(st, f"zst{i}", [128, 512]) for i in range(4)]
            gst = [self.sb(st, f"gst{i}", [128, 512], BF16) for i in range(4)]
            segs = [(0, 2048), (C_GQ, C_GV), (C_GR, C_GGD), (C_GGD, C_GGD + 16), (C_GGD + 16, C_RW),
                    (C_RR, C_RWD), (C_RWD, C_RWD + 96), (C_RWD + 96, C_RAD), (C_RAD, C_RAD + 96), (C_RAD + 96, C_RGD),
                    (C_RGD, C_GATE), (C_GATE, D_IN)]
            nload = 0
            nev = 0
            npsum = 0
            for (s0, s1) in segs:
                for b0 in range(s0, s1, 512):
                    bw = min(512, s1 - b0)
                    si = nload % 3
                    nload += 1
                    wk = f'wsl{si}'
                    P.dma(wsl[si][:, :, :bw], w[:, :, b0:b0 + bw], [], [wk], q='pool')
                    for c0 in range(b0, b0 + bw, 128):
                        m = min(128, b0 + bw - c0)
                        for (t0, n) in TILES:
                            pi = npsum % 4
                            npsum += 1
                            pst = self.ps[pi]
                            pk = f'ps{pi}'
                            for kc in range(KC):
                                P.op('pe', [wk, 'hT'], [pk], lambda e, kc=kc, c0=c0, m=m, t0=t0, n=n, si=si, pst=pst: e.matmul(
                                    pst[:m, :n], lhsT=wsl[si][:, kc, c0 - b0:c0 - b0 + m], rhs=hT[:, kc, t0:t0 + n],
                                    start=(kc == 0), stop=(kc == KC - 1)))
                            ei = nev % 4
                            eng = 'act' if nev % 2 == 0 else 'dve'
                            nev += 1
                            if c0 >= C_GATE:
                                P.op('act', [pk], [f'gst{ei}'], lambda e, m=m, n=n, ei=ei, pst=pst: e.activation(
                                    out=gst[ei][:m, :n], in_=pst[:m, :n], func=AF.Sigmoid))
                                P.dma(self.gT[c0 - C_GATE:c0 - C_GATE + m, t0:t0 + n], gst[ei][:m, :n], [f'gst{ei}'], ['gT'])
                            else:
                                if eng == 'act':
                                    P.op('act', [pk], [f'zst{ei}'], lambda e, m=m, n=n, ei=ei, pst=pst: e.activation(
                                        out=zst[ei][:m, :n], in_=pst[:m, :n], func=AF.Copy))
                                else:
                                    P.op('dve', [pk], [f'zst{ei}'], lambda e, m=m, n=n, ei=ei, pst=pst: e.tensor_copy(
                                        out=zst[ei][:m, :n], in_=pst[:m, :n]))
                                P.dma(self.zT[c0:c0 + m, t0:t0 + n], zst[ei][:m, :n], [f'zst{ei}'], ['zT'])
            for (s0, vo) in ((C_NAV, 0), (C_GV, 1024)):
                for b0 in range(0, 1024, 512):
                    si = nload % 3
                    nload += 1
                    wk = f'wsl{si}'
                    P.dma(wsl[si][:, :, :], w[:, :, s0 + b0:s0 + b0 + 512], [], [wk], q='pool')
                    for tt in range(TT // 128):
                        pi = npsum % 4
                        npsum += 1
                        pst = self.ps[pi]
                        pk = f'ps{pi}'
                        for kc in range(KC):
                            P.op('pe', [wk, 'hT'], [pk], lambda e, kc=kc, tt=tt, si=si, pst=pst: e.matmul(
                                pst[:, :], lhsT=hT[:, kc, tt * 128:(tt + 1) * 128], rhs=wsl[si][:, kc, :],
                                start=(kc == 0), stop=(kc == KC - 1)))
                        ei = nev % 4
                        eng = 'act' if nev % 2 == 0 else 'dve'
                        nev += 1
                        if eng == 'act':
                            P.op('act', [pk], [f'gst{ei}'], lambda e, ei=ei, pst=pst: e.activation(
                                out=gst[ei][:, :], in_=pst[:, :], func=AF.Copy))
                        else:
                            P.op('dve', [pk], [f'gst{ei}'], lambda e, ei=ei, pst=pst: e.tensor_copy(
                                out=gst[ei][:, :], in_=pst[:, :]))
                        P.dma(self.vtm[tt * 128:(tt + 1) * 128, vo + b0:vo + b0 + 512], gst[ei][:, :], [f'gst{ei}'], ['vtm'])
            if 'z' in self.debug:
                self.dbg(f'z{l}', lambda o: P.dma(o, self.zT, ['zT'], []), [C_GATE, TT])
                self.dbg(f'g{l}', lambda o: P.dma(o, self.gT, ['gT'], []), [3 * D, TT], BF16)
                self.dbg(f'vtm{l}', lambda o: P.dma(o, self.vtm, ['vtm'], []), [TT, 2048], BF16)


    def na_mixer(self, l):
        nc, P = self.nc, self.P
        with_ctx = l < DEPTH - 1
        vt_all = self.vtm.rearrange("(tt p) c -> p tt c", p=128)
        vt_odd = self.vtm[64:64 + 17 * 128, :].rearrange("(tt p) c -> p tt c", p=128)
        with contextlib.ExitStack() as st:
            zq = [self.sb(st, f"naz{i}", [128, TT]) for i in range(2)]
            sqb = self.sb(st, "nasq", [128, 512], BF16)
            rtb = self.sb(st, "nart", [128, 512])
            qk = [self.sb(st, "qn", [128, TT], BF16), self.sb(st, "kn", [128, TT], BF16)]
            vte = self.sb(st, "vte", [128, 18, 128], BF16)
            vto = self.sb(st, "vto", [128, 17, 128], BF16)
            tbl = [self.sb(st, f"natbl{i}", [128, 14, 64]) for i in range(2)]
            sT = [self.sb(st, f"sT{i}", [128, 4, 64]) for i in range(2)]
            pT = [self.sb(st, f"pT{i}", [128, 6, 64], BF16) for i in range(2)]
            pTc = self.sb(st, "pTc", [128, 2, 256], BF16)
            rsb = [self.sb(st, f"nars{i}", [128, 256]) for i in range(2)]
            yna = [self.sb(st, f"yna{i}", [128, TT], BF16) for i in range(2)]
            g8 = self.sb(st, "g8", [128, 2])
            qm = [self.sb(st, f"qm{i}", [128, TT], BF16) for i in range(2)]
            P.op('dve', [f'vecs{l}'], ['g8'], lambda e: e.tensor_scalar(
                out=g8[:, 0:1], in0=self.vec(l, 'naq'), scalar1=0.125, scalar2=None, op0=ALU.mult))
            P.op('dve', [f'vecs{l}'], ['g8'], lambda e: e.tensor_copy(out=g8[:, 1:2], in_=self.vec(l, 'nak')))
            it = 0
            for hp in range(8):
                for w_, c0 in ((0, C_NAQ), (1, C_NAK)):
                    z = zq[w_]
                    zk = f'naz{w_}'
                    P.dma(z[:], self.zT[c0 + hp * 128:c0 + (hp + 1) * 128, :], ['zT'], [zk])
                    dst = qk[w_]
                    dk = 'qn' if w_ == 0 else 'kn'
                    for (t0, n) in TILES:
                        P.op('act', [zk], ['nasq'], lambda e, z=z, t0=t0, n=n: e.activation(
                            out=sqb[:, :n], in_=z[:, t0:t0 + n], func=AF.Square))
                        P.op('pe', ['nasq', 'bd_bf'], ['ps4'], lambda e, n=n: e.matmul(
                            self.ps[4][:, :n], lhsT=self.bd_bf[:], rhs=sqb[:, :n], start=True, stop=True))
                        P.op('act', ['ps4'], ['nart'], lambda e, n=n: e.activation(
                            out=rtb[:, :n], in_=self.ps[4][:, :n], func=AF.Sqrt, scale=1.0 / 64, bias=self.eps_t[:, 0:1]))
                        P.op('dve', ['nart'], ['nart'], lambda e, n=n: e.reciprocal(out=rtb[:, :n], in_=rtb[:, :n]))
                        P.op('dve', [zk, 'nart', 'g8'], [dk], lambda e, z=z, t0=t0, n=n, dst=dst, w_=w_: e.scalar_tensor_tensor(
                            out=dst[:, t0:t0 + n], in0=z[:, t0:t0 + n], scalar=g8[:, w_:w_ + 1], in1=rtb[:, :n],
                            op0=ALU.mult, op1=ALU.mult))
                for e_ in range(2):
                    P.op('pool', [], [f'qm{e_}'], lambda e, e_=e_: e.memset(qm[e_][:], 0.0))
                    pr = slice(e_ * 64, (e_ + 1) * 64)
                    P.op('act', ['qn', f'qm{e_}'], [f'qm{e_}'], lambda e, e_=e_, pr=pr: e.activation(out=qm[e_][pr, :], in_=qk[0][pr, :], func=AF.Copy))
                P.dma(vte[:], vt_all[:, :, hp * 128:(hp + 1) * 128], ['vtm'], ['vte'])
                P.dma(vto[:], vt_odd[:, :, hp * 128:(hp + 1) * 128], ['vtm'], ['vto'])
                qn, kn = qk
                y = yna[hp % 2]
                yk = f'yna{hp % 2}'
                stages = []
                for e_ in range(2):
                    h = 2 * hp + e_
                    tb = tbl[h % 2]
                    tk = f'natbl{h % 2}'
                    pr = slice(e_ * 64, (e_ + 1) * 64)
                    for r in range(32):
                        rs = min(max(r - 4, 0), 24)
                        dlt = rs - r
                        tq = NCTX + r * 64
                        b = it % 2
                        it += 1
                        psS, psSk = self.ps[b], f'ps{b}'
                        psO, psOk = self.ps[2 + b], f'ps{2 + b}'

                        def s1(e_=e_, h=h, tb=tb, tk=tk, pr=pr, r=r, rs=rs, dlt=dlt, tq=tq, b=b, psS=psS, psSk=psSk):
                            if r == 0:
                                P.dma(tb[:].rearrange("p a b -> p (a b)"), self.natab[l, h], [], [tk])
                            for j in range(6):
                                kt = NCTX + (rs + 2 * j) * 64 if j < 4 else (j - 4) * 128
                                P.op('pe', [f'qm{e_}', 'kn'], [psSk], lambda e, j=j, kt=kt: e.matmul(
                                    psS[:, j * 64:(j + 1) * 64], lhsT=kn[:, kt:kt + 128], rhs=qm[e_][:, tq:tq + 64],
                                    start=True, stop=True))
                            d0 = dlt + 7
                            tv = tb[:].rearrange("p (u two) c -> p u two c", two=2)[:, d0 // 2:d0 // 2 + 4, d0 % 2, :]
                            P.op('dve', [psSk, tk], [f'sT{b}'], lambda e: e.tensor_tensor(
                                out=sT[b][:], in0=psS[:, 0:256].rearrange("p (j c) -> p j c", c=64), in1=tv, op=ALU.add))
                            P.op('act', [f'sT{b}'], [f'pT{b}'], lambda e: e.activation(
                                out=pT[b][:, 0:4, :], in_=sT[b][:], func=AF.Exp))
                            P.op('act', [psSk], [f'pT{b}'], lambda e: e.activation(
                                out=pT[b][:, 4:6, :], in_=psS[:, 256:384].rearrange("p (j c) -> p j c", c=64), func=AF.Exp))

                        def s2(pr=pr, rs=rs, tq=tq, b=b, psO=psO, psOk=psOk):
                            for part in range(2):
                                for j in range(6):
                                    if part == 1:
                                        lhs = self.ones_bf[:, :]
                                        rk_ = 'ones_bf'
                                    elif j >= 4:
                                        lhs = vte[:, j - 4, :]
                                        rk_ = 'vte'
                                    elif rs % 2 == 0:
                                        lhs = vte[:, 2 + rs // 2 + j, :]
                                        rk_ = 'vte'
                                    else:
                                        lhs = vto[:, (rs + 1) // 2 + 1 + j, :]
                                        rk_ = 'vto'
                                    P.op('pe', [rk_, f'pT{b}'], [psOk], lambda e, lhs=lhs, j=j, part=part: e.matmul(
                                        psO[:, part * 64:(part + 1) * 64], lhsT=lhs, rhs=pT[b][:, j, :],
                                        start=(j == 0), stop=(j == 5)))
                            P.op('dve', [psOk], [f'nars{b}'], lambda e: e.reciprocal(
                                out=rsb[b][pr, 0:64], in_=psO[pr, 64:128]))
                            P.op('dve', [psOk, f'nars{b}'], [yk], lambda e: e.tensor_tensor(
                                out=y[pr, tq:tq + 64], in0=psO[pr, 0:64], in1=rsb[b][pr, 0:64], op=ALU.mult))
                        stages.append((s1, s2))
                    if with_ctx:
                        b = it % 2
                        it += 1
                        psS, psSk = self.ps[b], f'ps{b}'
                        psO, psOk = self.ps[2 + b], f'ps{2 + b}'

                        def s1(e_=e_, pr=pr, psS=psS, psSk=psSk):
                            for j in range(2):
                                P.op('pe', [f'qm{e_}', 'kn'], [psSk], lambda e, j=j: e.matmul(
                                    psS[:, j * 256:(j + 1) * 256], lhsT=kn[:, j * 128:(j + 1) * 128], rhs=qm[e_][:, 0:256],
                                    start=True, stop=True))
                            P.op('act', [psSk], ['pTc'], lambda e: e.activation(
                                out=pTc[:].rearrange("p j c -> p (j c)"), in_=psS[:, :], func=AF.Exp))

                        def s2(pr=pr, b=b, psO=psO, psOk=psOk):
                            for part in range(2):
                                for j in range(2):
                                    lhs = self.ones_bf[:, :] if part == 1 else vte[:, j, :]
                                    P.op('pe', ['vte', 'ones_bf', 'pTc'], [psOk], lambda e, lhs=lhs, j=j, part=part: e.matmul(
                                        psO[:, part * 256:(part + 1) * 256], lhsT=lhs, rhs=pTc[:, j, :],
                                        start=(j == 0), stop=(j == 1)))
                            P.op('dve', [psOk], [f'nars{b}'], lambda e: e.reciprocal(
                                out=rsb[b][pr, :], in_=psO[pr, 256:512]))
                            P.op('dve', [psOk, f'nars{b}'], [yk], lambda e: e.tensor_tensor(
                                out=y[pr, 0:256], in0=psO[pr, 0:256], in1=rsb[b][pr, :], op=ALU.mult))
                        stages.append((s1, s2))
                for k_ in range(len(stages) + 1):
                    if k_ < len(stages):
                        stages[k_][0]()
                    if k_ >= 1:
                        stages[k_ - 1][1]()
                t_lo = 0 if with_ctx else NCTX
                P.dma(self.yT[0, hp * 128:(hp + 1) * 128, t_lo:], y[:, t_lo:], [yk], ['yT'])
            if 'na' in self.debug:
                self.dbg(f'yna{l}', lambda o: P.dma(o, self.yT[0], ['yT'], []), [1024, TT], BF16)


    def gla_mixer(self, l):
        nc, P = self.nc, self.P
        with_ctx = l < DEPTH - 1
        NCH = TT // 64
        NT = TT // 128
        qscale = 128 ** -0.5
        vt_all = self.vtm.rearrange("(tt p) c -> p tt c", p=128)
        with contextlib.ExitStack() as st:
            cos = self.sb(st, "cos", [128, SEQ])
            sin = self.sb(st, "sin", [128, SEQ])
            msk = self.sb(st, "cmsk", [128, TT])
            qk32 = [self.sb(st, "gq32", [128, TT]), self.sb(st, "gk32", [128, TT])]
            zb = self.sb(st, "gzb", [128, 512], BF16)
            rz = self.sb(st, "grz", [128, 2, TT])
            gd = [self.sb(st, f"ggd{d}", [16, TT]) for d in range(2)]
            gw2 = self.sb(st, "gw2", [16, 2, 512])
            nb = self.sb(st, "gnb", [128, 8])
            T1 = self.sb(st, "gT1", [128, TT]); T2 = self.sb(st, "gT2", [128, TT]); T3 = self.sb(st, "gT3", [128, TT])
            qi = self.sb(st, "gqi", [128, TT], BF16); kj = self.sb(st, "gkj", [128, TT], BF16)
            kd = self.sb(st, "gkd", [128, TT], BF16); qb = self.sb(st, "gqb", [128, TT], BF16)
            dec = self.sb(st, "gdec", [128, NCH])
            Vt = self.sb(st, "gVt", [128, NT, 256], BF16)
            kdT = self.sb(st, "gkdT", [128, NT, 128], BF16)
            Abf = [self.sb(st, f"gA{i}", [128, 128], BF16) for i in range(2)]
            S32 = self.sb(st, "gS32", [128, 256])
            Sbf = [self.sb(st, f"gSbf{i}", [128, 256], BF16) for i in range(2)]
            yf = self.sb(st, "gyf", [128, 2, TT], BF16)
            yo = self.sb(st, "gyo", [128, 2, TT], BF16)
            yt = [self.sb(st, f"gyt{i}", [128, 2, 128]) for i in range(2)]
            ysq = self.sb(st, "gysq", [128, 2, 128], BF16)
            yrt = self.sb(st, "gyrt", [128, 128])
            P.dma(cos[:], self.consts[:, 512:512 + SEQ], [], ['cos'])
            P.dma(sin[:], self.consts[:, 512 + SEQ:512 + 2 * SEQ], [], ['sin'])
            P.op('pool', [], ['cmsk'], lambda e: e.memset(msk[:], 1.0))
            P.op('pool', ['cmsk'], ['cmsk'], lambda e: e.memset(msk[:].rearrange("p (c j) -> p c j", j=64)[:, :, 0:1], 0.0))
            for d in range(2):
                P.dma(gd[d][:], self.zT[C_GGD + 16 * d:C_GGD + 16 * (d + 1), :], ['zT'], [f'ggd{d}'])
            P.dma(gw2[:], self.gla_w2[l].rearrange("d k c -> k d c"), [], ['gw2'])
            P.op('dve', [f'vecs{l}'], ['gnb'], lambda e: e.tensor_scalar(
                out=nb[:], in0=self.vec(l, 'ggb'), scalar1=-1.0, scalar2=None, op0=ALU.mult))
            gng = self.vec(l, 'gng')
            c3 = lambda t: t[:].rearrange("p (c j) -> p c j", j=64)
            sbi = 0
            for h in range(4):
                for w_, c0 in ((0, C_GQ), (1, C_GK)):
                    z = qk32[w_]
                    zk = 'gq32' if w_ == 0 else 'gk32'
                    P.dma(z[:], self.zT[c0 + h * 128:c0 + (h + 1) * 128, :], ['zT'], [zk])
                    for ti in range(4):
                        t0 = NCTX + ti * 512
                        P.op('act', [zk], ['gzb'], lambda e, z=z, t0=t0: e.activation(out=zb[:], in_=z[:, t0:t0 + 512], func=AF.Copy))
                        P.op('pe', ['gzb', 'cst_bf'], ['ps4'], lambda e: e.matmul(
                            self.ps[4][:, :], lhsT=self.perm_bf, rhs=zb[:], start=True, stop=True))
                        P.op('dve', ['ps4', 'sin'], ['gT3'], lambda e, ti=ti: e.tensor_tensor(
                            out=T3[:, 0:512], in0=self.ps[4][:, :], in1=sin[:, ti * 512:(ti + 1) * 512], op=ALU.mult))
                        P.op('dve', [zk, 'cos'], [zk], lambda e, z=z, t0=t0, ti=ti: e.tensor_tensor(
                            out=z[:, t0:t0 + 512], in0=z[:, t0:t0 + 512], in1=cos[:, ti * 512:(ti + 1) * 512], op=ALU.mult))
                        P.op('dve', [zk, 'gT3'], [zk], lambda e, z=z, t0=t0: e.tensor_tensor(
                            out=z[:, t0:t0 + 512], in0=z[:, t0:t0 + 512], in1=T3[:, 0:512], op=ALU.add))
                q32, k32 = qk32
                P.dma(rz[:], self.zT[C_GR + h * 256:C_GR + (h + 1) * 256, :].rearrange("(c p) t -> p c t", p=128), ['zT'], ['grz'])
                P.op('act', ['grz'], ['grz'], lambda e: e.activation(out=rz[:], in_=rz[:], func=AF.Silu))
                P.dma(Vt[:], vt_all[:, :, 1024 + h * 256:1024 + (h + 1) * 256], ['vtm'], ['gVt'])
                for d in range(2):
                    for (t0, n) in TILES:
                        P.op('pe', ['gw2', f'ggd{d}'], ['ps4'], lambda e, t0=t0, n=n, d=d, h=h: e.matmul(
                            self.ps[4][:, :n], lhsT=gw2[:, d, h * 128:(h + 1) * 128], rhs=gd[d][:, t0:t0 + n], start=True, stop=True), rg=0)
                        P.op('act', ['ps4', 'gnb'], ['gT1'], lambda e, t0=t0, n=n, d=d, h=h: e.activation(
                            out=T1[:, t0:t0 + n], in_=self.ps[4][:, :n], func=AF.Exp, scale=-1.0, bias=nb[:, d * 4 + h:d * 4 + h + 1]))
                    P.op('act', ['gT1'], ['gT1'], lambda e: e.activation(out=T1[:], in_=T1[:], func=AF.Ln, bias=1.0, scale=1.0))
                    P.op('dve', ['cmsk', 'gT1'], ['gT2'], lambda e: e.tensor_tensor_scan(
                        out=T2[:], data0=msk[:], data1=T1[:], initial=0.0, op0=ALU.mult, op1=ALU.add))
                    if d == 1:
                        P.op('dve', ['gT2'], ['gT3'], lambda e: e.tensor_tensor(
                            out=c3(T3), in0=c3(T2)[:, :, 63:64].to_broadcast([128, NCH, 64]), in1=c3(T2), op=ALU.subtract))
                        P.op('dve', ['gT3', 'gT1'], ['gT2'], lambda e: e.tensor_tensor(out=T2[:], in0=T3[:], in1=T1[:], op=ALU.add))
                    tot = c3(T2)[:, :, 63:64] if d == 0 else c3(T2)[:, :, 0:1]
                    cref = c3(T2)[:, :, 32:33] if d == 0 else c3(T2)[:, :, 31:32]
                    P.op('act', ['gT2'], ['gdec'], lambda e, tot=tot: e.activation(
                        out=dec[:].unsqueeze(2), in_=tot, func=AF.Exp, scale=-1.0 / 16))
                    P.op('dve', ['gT2'], ['gT3'], lambda e, cref=cref: e.tensor_tensor(
                        out=c3(T3), in0=c3(T2), in1=cref.to_broadcast([128, NCH, 64]), op=ALU.subtract))
                    P.op('act', ['gT3'], ['gT1'], lambda e: e.activation(out=T1[:], in_=T3[:], func=AF.Exp, scale=-1.0 / 16))
                    P.op('dve', ['gq32', 'gT1'], ['gqi'], lambda e: e.scalar_tensor_tensor(
                        out=qi[:], in0=q32[:], scalar=qscale, in1=T1[:], op0=ALU.mult, op1=ALU.mult))
                    P.op('act', ['gT3'], ['gT1'], lambda e: e.activation(out=T1[:], in_=T3[:], func=AF.Exp, scale=1.0 / 16))
                    P.op('dve', ['gk32', 'gT1'], ['gkj'], lambda e: e.tensor_tensor(out=kj[:], in0=k32[:], in1=T1[:], op=ALU.mult))
                    P.op('dve', ['gT2'], ['gT3'], lambda e, tot=tot: e.tensor_tensor(
                        out=c3(T3), in0=tot.to_broadcast([128, NCH, 64]), in1=c3(T2), op=ALU.subtract))
                    P.op('act', ['gT3'], ['gT1'], lambda e: e.activation(out=T1[:], in_=T3[:], func=AF.Exp, scale=-1.0 / 16))
                    P.op('dve', ['gk32', 'gT1'], ['gkd'], lambda e: e.tensor_tensor(out=kd[:], in0=k32[:], in1=T1[:], op=ALU.mult))
                    P.op('act', ['gT2'], ['gT1'], lambda e: e.activation(out=T1[:], in_=T2[:], func=AF.Exp, scale=-1.0 / 16))
                    P.op('dve', ['gq32', 'gT1'], ['gqb'], lambda e: e.scalar_tensor_tensor(
                        out=qb[:], in0=q32[:], scalar=qscale, in1=T1[:], op0=ALU.mult, op1=ALU.mult))
                    for g0 in range(0, NT, 4):
                        g1 = min(NT, g0 + 4)
                        for tt in range(g0, g1):
                            P.op('pe', ['gkd', 'cst_bf'], ['psb'], lambda e, tt=tt, g0=g0: e.transpose(
                                self.psb[:, (tt - g0) * 128:(tt - g0 + 1) * 128], kd[:, tt * 128:(tt + 1) * 128], self.ident_bf))
                        P.op('act', ['psb'], ['gkdT'], lambda e, g0=g0, g1=g1: e.activation(
                            out=kdT[:, g0:g1, :].rearrange("p a b -> p (a b)"), in_=self.psb[:, 0:(g1 - g0) * 128], func=AF.Copy))
                    P.op('dve', [], ['gS32'], lambda e: e.memset(S32[:], 0.0))
                    P.op('dve', [], [f'gSbf{sbi % 2}'], lambda e, sbi=sbi: e.memset(Sbf[sbi % 2][:], 0.0))
                    order = list(range(NT)) if d == 0 else [1, 0] + list(range(NT - 1, 1, -1))
                    maskd = self.mask_f if d == 0 else self.mask_b
                    for it_, tt in enumerate(order):
                        tsl = slice(tt * 128, (tt + 1) * 128)
                        ab = it_ % 2
                        P.op('pe', ['gkj', 'gqi'], ['ps5'], lambda e, tsl=tsl: e.matmul(
                            self.ps[5][:, 0:128], lhsT=kj[:, tsl], rhs=qi[:, tsl], start=True, stop=True))
                        P.op('dve', ['ps5', 'cst_f'], [f'gA{ab}'], lambda e, ab=ab, maskd=maskd: e.tensor_tensor(
                            out=Abf[ab][:], in0=self.ps[5][:, 0:128], in1=maskd, op=ALU.mult))
                        halves = (0, 1) if d == 0 else (1, 0)
                        psy = [self.ps[0 + 2 * (it_ % 2)], self.ps[1 + 2 * (it_ % 2)]]
                        psyk = [f'ps{0 + 2 * (it_ % 2)}', f'ps{1 + 2 * (it_ % 2)}']
                        for hi, hf in enumerate(halves):
                            csl = slice(hf * 64, (hf + 1) * 64)
                            tok = slice(tt * 128 + hf * 64, tt * 128 + (hf + 1) * 64)
                            sk = f'gSbf{sbi % 2}'
                            Sb = Sbf[sbi % 2]
                            for vc in range(2):
                                if hi == 0:
                                    P.op('pe', ['gVt', f'gA{ab}'], [psyk[vc]], lambda e, vc=vc, tt=tt, ab=ab, psy=psy: e.matmul(
                                        psy[vc][:, 0:128], lhsT=Vt[:, tt, vc * 128:(vc + 1) * 128], rhs=Abf[ab][:], start=True, stop=False))
                                P.op('pe', [sk, 'gqb'], [psyk[vc]], lambda e, vc=vc, Sb=Sb, csl=csl, tok=tok, hi=hi, psy=psy: e.matmul(
                                    psy[vc][:, csl], lhsT=Sb[:, vc * 128:(vc + 1) * 128], rhs=qb[:, tok], start=False, stop=(hi == 1)))
                            ch = tt * 2 + hf
                            P.op('pe', ['gkdT', 'gVt'], ['ps6'], lambda e, csl=csl, tt=tt: e.matmul(
                                self.ps[6][:, 0:256], lhsT=kdT[csl, tt, :], rhs=Vt[csl, tt, :], start=True, stop=True), rg=csl.start)
                            P.op('dve', ['ps6', 'gS32', 'gdec'], ['gS32'], lambda e, ch=ch: e.scalar_tensor_tensor(
                                out=S32[:], in0=S32[:], scalar=dec[:, ch:ch + 1], in1=self.ps[6][:, 0:256], op0=ALU.mult, op1=ALU.add))
                            sbi += 1
                            P.op('act', ['gS32'], [f'gSbf{sbi % 2}'], lambda e, sbi=sbi: e.activation(
                                out=Sbf[sbi % 2][:], in_=S32[:], func=AF.Copy))
                        if d == 0:
                            for vc in range(2):
                                P.op('act', [psyk[vc]], ['gyf'], lambda e, vc=vc, tsl=tsl, psy=psy: e.activation(
                                    out=yf[:, vc, tsl], in_=psy[vc][:, 0:128], func=AF.Copy))
                        elif with_ctx or tt >= 2:
                            ytb = yt[it_ % 2]
                            ytk = f'gyt{it_ % 2}'
                            for vc in range(2):
                                P.op('dve', [psyk[vc], 'gyf'], [ytk], lambda e, vc=vc, tsl=tsl, psy=psy, ytb=ytb: e.tensor_tensor(
                                    out=ytb[:, vc, :], in0=psy[vc][:, 0:128], in1=yf[:, vc, tsl], op=ALU.add))
                            P.op('act', [ytk], ['gysq'], lambda e, ytb=ytb: e.activation(out=ysq[:], in_=ytb[:], func=AF.Square))
                            for vc in range(2):
                                P.op('pe', ['gysq', 'ones_bf'], ['ps4'], lambda e, vc=vc: e.matmul(
                                    self.ps[4][:, 0:128], lhsT=self.ones_bf[:], rhs=ysq[:, vc, :], start=(vc == 0), stop=(vc == 1)))
                            P.op('act', ['ps4'], ['gyrt'], lambda e: e.activation(
                                out=yrt[:], in_=self.ps[4][:, 0:128], func=AF.Sqrt, scale=1.0 / 256, bias=self.eps_t[:, 0:1]))
                            P.op('dve', ['gyrt'], ['gyrt'], lambda e: e.reciprocal(out=yrt[:], in_=yrt[:]))
                            for vc in range(2):
                                P.op('dve', [ytk, 'gyrt', f'vecs{l}'], [ytk], lambda e, vc=vc, ytb=ytb: e.scalar_tensor_tensor(
                                    out=ytb[:, vc, :], in0=ytb[:, vc, :], scalar=gng[:, vc:vc + 1], in1=yrt[:], op0=ALU.mult, op1=ALU.mult))
                                P.op('dve', [ytk, 'grz'], ['gyo'], lambda e, vc=vc, ytb=ytb, tsl=tsl: e.tensor_tensor(
                                    out=yo[:, vc, tsl], in0=ytb[:, vc, :], in1=rz[:, vc, tsl], op=ALU.mult))
                t_lo = 0 if with_ctx else NCTX
                P.dma(self.yT[1, h * 256:(h + 1) * 256, t_lo:].rearrange("(c p) t -> p c t", p=128), yo[:, :, t_lo:], ['gyo'], ['yT'])
            if 'gla' in self.debug:
                self.dbg(f'ygla{l}', lambda o: P.dma(o, self.yT[1], ['yT'], []), [1024, TT], BF16)


    def _shift(self, z, zk, tmp, tmpk, om, hm, m):
        P = self.P
        P.op('dve', [zk], [tmpk], lambda e: e.tensor_tensor(out=tmp[:m, 1:TT - 1], in0=z[:m, 0:TT - 2], in1=z[:m, 2:TT], op=ALU.add))
        for (dst, src_) in ((0, 1), (NCTX - 1, NCTX - 2), (NCTX, NCTX + 1), (TT - 1, TT - 2)):
            P.op('dve', [zk, tmpk], [tmpk], lambda e, dst=dst, src_=src_: e.tensor_copy(out=tmp[:m, dst:dst + 1], in_=z[:m, src_:src_ + 1]))
        P.op('dve', [tmpk], [tmpk], lambda e: e.tensor_scalar(out=tmp[:m, :], in0=tmp[:m, :], scalar1=hm, scalar2=None, op0=ALU.mult))
        P.op('dve', [zk, tmpk], [zk], lambda e: e.scalar_tensor_tensor(out=z[:m, :], in0=z[:m, :], scalar=om, in1=tmp[:m, :], op0=ALU.mult, op1=ALU.add))

    def _mu_prep(self, st, l):
        P = self.P
        o0, _ = VEC_SLOTS['mu_r']
        mu = self.vecs[l][:, o0:o0 + 30]
        om = self.sb(st, "rw_om", [128, 30]); hm = self.sb(st, "rw_hm", [128, 30])
        P.op('dve', [f'vecs{l}'], ['rw_om'], lambda e: e.tensor_scalar(out=om[:], in0=mu, scalar1=-1.0, scalar2=1.0, op0=ALU.mult, op1=ALU.add))
        P.op('dve', [f'vecs{l}'], ['rw_hm'], lambda e: e.tensor_scalar(out=hm[:], in0=mu, scalar1=0.5, scalar2=None, op0=ALU.mult))
        return om, hm

    def rwkv_pre(self, l):
        nc, P = self.nc, self.P
        with contextlib.ExitStack() as st:
            om, hm = self._mu_prep(st, l)
            zt = self.sb(st, "rp_z", [128, TT]); tmp = self.sb(st, "rp_tmp", [128, TT])
            twd = [self.sb(st, f"rp_twd{d}", [96, TT], BF16) for d in range(2)]
            adb = [self.sb(st, f"rp_adb{d}", [96, TT], BF16) for d in range(2)]
            sgd = self.sb(st, "rp_sgd", [128, 2, TT], BF16)
            w2 = self.sb(st, "rp_w2", [96, 2, 1024], BF16); a2 = self.sb(st, "rp_a2", [96, 2, 1024], BF16)
            g2 = self.sb(st, "rp_g2", [128, 2, 1024], BF16)
            stg = [self.sb(st, f"rp_st{i}", [128, 512]) for i in range(4)]
            stb = [self.sb(st, f"rp_sb{i}", [128, 512], BF16) for i in range(2)]
            P.dma(w2[:], self.rw_w2[l].rearrange("d k c -> k d c"), [], ['rp_w2'], q='pool')
            P.dma(a2[:], self.rw_a2[l].rearrange("d k c -> k d c"), [], ['rp_a2'], q='pool')
            P.dma(g2[:], self.rw_g2[l].rearrange("(c p) n -> p c n", p=128), [], ['rp_g2'], q='pool')
            for d in range(2):
                P.dma(zt[:96, :], self.zT[C_RWD + 96 * d:C_RWD + 96 * (d + 1), :], ['zT'], ['rp_z'])
                self._shift(zt, 'rp_z', tmp, 'rp_tmp', om[:96, 24 + d:25 + d], hm[:96, 24 + d:25 + d], 96)
                P.op('act', ['rp_z'], [f'rp_twd{d}'], lambda e, d=d: e.activation(out=twd[d][:], in_=zt[:96, :], func=AF.Tanh))
                P.dma(zt[:96, :], self.zT[C_RAD + 96 * d:C_RAD + 96 * (d + 1), :], ['zT'], ['rp_z'])
                self._shift(zt, 'rp_z', tmp, 'rp_tmp', om[:96, 26 + d:27 + d], hm[:96, 26 + d:27 + d], 96)
                P.op('act', ['rp_z'], [f'rp_adb{d}'], lambda e, d=d: e.activation(out=adb[d][:], in_=zt[:96, :], func=AF.Copy))
            for c in range(2):
                P.dma(zt[:, :], self.zT[C_RGD + 128 * c:C_RGD + 128 * (c + 1), :], ['zT'], ['rp_z'])
                self._shift(zt, 'rp_z', tmp, 'rp_tmp', om[:, 28 + c:29 + c], hm[:, 28 + c:29 + c], 128)
                P.op('act', ['rp_z'], ['rp_sgd'], lambda e, c=c: e.activation(out=sgd[:, c, :], in_=zt[:, :], func=AF.Sigmoid))
            w0 = self.vec(l, 'w0'); a0 = self.vec(l, 'a0')
            n_ = 0
            for hp in range(8):
                cs = slice(hp * 128, (hp + 1) * 128)
                for (t0, n) in TILES:
                    for d in range(2):
                        for which in range(2):
                            pi = n_ % 4; n_ += 1
                            pst, pk = self.ps[pi], f'ps{pi}'
                            wm, src_, bias_ = ((w2, twd[d], w0), (a2, adb[d], a0))[which]
                            rk_ = [('rp_w2', f'rp_twd{d}'), ('rp_a2', f'rp_adb{d}')][which]
                            P.op('pe', list(rk_), [pk], lambda e, wm=wm, src_=src_, d=d, t0=t0, n=n, pst=pst, cs=cs: e.matmul(
                                pst[:, :n], lhsT=wm[:, d, cs], rhs=src_[:, t0:t0 + n], start=True, stop=True), rg=0)
                            sg = stg[pi]
                            P.op('act', [pk, f'vecs{l}'], [f'rp_st{pi}'], lambda e, pst=pst, sg=sg, n=n, bias_=bias_, d=d, hp=hp: e.activation(
                                out=sg[:, :n], in_=pst[:, :n], func=AF.Sigmoid, bias=bias_[:, d * 8 + hp:d * 8 + hp + 1], scale=1.0))
                            if which == 0:
                                P.op('dve', [f'rp_st{pi}'], [f'rp_st{pi}'], lambda e, sg=sg, n=n: e.tensor_scalar(
                                    out=sg[:, :n], in0=sg[:, :n], scalar1=-0.6065306597126334, scalar2=None, op0=ALU.mult))
                                P.dma(self.ldT[d, cs, t0:t0 + n], sg[:, :n], [f'rp_st{pi}'], ['ldT'])
                            else:
                                P.dma(self.aT[d, cs, t0:t0 + n], sg[:, :n], [f'rp_st{pi}'], ['aT'])
                    pi = n_ % 4; n_ += 1
                    pst, pk = self.ps[pi], f'ps{pi}'
                    for c in range(2):
                        P.op('pe', ['rp_g2', 'rp_sgd'], [pk], lambda e, c=c, t0=t0, n=n, pst=pst, cs=cs: e.matmul(
                            pst[:, :n], lhsT=g2[:, c, cs], rhs=sgd[:, c, t0:t0 + n], start=(c == 0), stop=(c == 1)))
                    bi = n_ % 2
                    P.op('act', [pk], [f'rp_sb{bi}'], lambda e, pst=pst, n=n, bi=bi: e.activation(out=stb[bi][:, :n], in_=pst[:, :n], func=AF.Copy))
                    P.dma(self.g2T[cs, t0:t0 + n], stb[bi][:, :n], [f'rp_sb{bi}'], ['g2T'])

    def rwkv_mixer(self, l):
        nc, P = self.nc, self.P
        with_ctx = l < DEPTH - 1
        NCH = TT // 64
        NT = TT // 128
        with contextlib.ExitStack() as st:
            om, hm = self._mu_prep(st, l)
            A = [self.sb(st, f"rwA{i}", [128, TT]) for i in range(7)]
            Ak = [f'rwA{i}' for i in range(7)]
            msk = self.sb(st, "rmsk", [128, TT])
            vb = self.sb(st, "rw_vb", [128, TT], BF16)
            bon = self.sb(st, "rw_bon", [128, TT], BF16)
            gsb = self.sb(st, "rw_g", [128, 512], BF16)
            al = self.sb(st, "rw_al", [128, TT], BF16); rho = self.sb(st, "rw_rho", [128, TT], BF16)
            be = self.sb(st, "rw_be", [128, TT], BF16); ka = self.sb(st, "rw_ka", [128, TT], BF16)
            Bp = self.sb(st, "rw_Bp", [128, TT], BF16); Kp = self.sb(st, "rw_Kp", [128, TT], BF16)
            al_tm = self.sb(st, "rw_altm", [128, NT, 128], BF16); Bp_tm = self.sb(st, "rw_Bptm", [128, NT, 128], BF16)
            Kp_tm = self.sb(st, "rw_Kptm", [128, NT, 128], BF16); V_tm = self.sb(st, "rw_Vtm", [128, NT, 128], BF16)
            etot = self.sb(st, "rw_etot", [128, NCH])
            slots = []
            s0 = dict(XN=[[self.sb(st, f"rw_X{i}", [128, 512], BF16), self.sb(st, f"rw_N{i}", [128, 512], BF16)] for i in range(2)],
                      Q=[self.sb(st, f"rw_Q{i}", [128, 512], BF16) for i in range(2)],
                      LkT=self.sb(st, "rw_LkT", [128, 512], BF16), MbT=self.sb(st, "rw_MbT", [128, 512], BF16), MkT=self.sb(st, "rw_MkT", [128, 512], BF16),
                      H=self.sb(st, "rw_H", [128, 256], BF16)[:], P1n=self.sb(st, "rw_P1n", [128, 256], BF16)[:], G=self.sb(st, "rw_G", [128, 256], BF16)[:],
                      ptmp=self.sb(st, "rw_ptmp", [128, 128])[:], banks=(self.ps[0], self.ps[1], self.ps[2]), bank_ids=(0, 1, 2))
            slots.append(s0)
            a2b = A[2][:].bitcast(BF16)
            cut = lambda i: a2b[:, i * 512:(i + 1) * 512]
            rt = self.sb(st, "rw_rt", [128, 512]); t5 = self.sb(st, "rw_t5", [128, 512])
            t5b = t5[:].bitcast(BF16)
            s1_ = dict(XN=[[cut(0), cut(1)], [cut(2), cut(3)]], Q=[cut(4), cut(5)], LkT=cut(6), MbT=cut(7), MkT=cut(8),
                       H=t5b[:, 0:256], P1n=t5b[:, 256:512], G=t5b[:, 512:768], ptmp=rt[:, 0:128],
                       banks=(self.ps[3], self.ps[5], self.ps[6]), bank_ids=(3, 5, 6))
            slots.append(s1_)
            for nm in ('rw_X0', 'rw_N0', 'rw_X1', 'rw_N1', 'rw_Q0', 'rw_Q1', 'rw_LkT', 'rw_MbT', 'rw_MkT'):
                P.set_parent(nm + '_s1', Ak[2])
            for nm in ('rw_H', 'rw_P1n', 'rw_G'):
                P.set_parent(nm + '_s1', 'rw_t5')
            P.set_parent('rw_ptmp_s1', 'rw_rt')
            a5b = A[5][:].bitcast(BF16); a6b = A[6][:].bitcast(BF16)
            al_m = [a5b[:, 0:TT], a5b[:, TT:2 * TT]]
            rho_m = [a6b[:, 0:TT], a6b[:, TT:2 * TT]]
            P.set_parent('rw_alm', Ak[5]); P.set_parent('rw_rhom', Ak[6])
            R32 = self.sb(st, "rw_R32", [128, TT]); yloc = self.sb(st, "rw_yloc", [128, TT]); yacc = self.sb(st, "rw_yacc", [128, TT])
            Phi = self.sb(st, "rw_Phi", [128, NCH, 128]); Dd = self.sb(st, "rw_D", [128, NCH, 128], BF16)
            Sbd = [self.sb(st, f"rw_S{i}", [128, 128]) for i in range(2)]
            ka1 = self.sb(st, "rw_ka1", [128, 8])
            sq5 = self.sb(st, "rw_sq5", [128, 512], BF16)
            yo = vb
            geps = self.sb(st, "rw_geps", [128, 1])
            P.op('pool', [], ['rw_geps'], lambda e: e.memset(geps[:], 64e-5))
            P.op('pool', [], ['rmsk'], lambda e: e.memset(msk[:], 1.0))
            P.op('pool', ['rmsk'], ['rmsk'], lambda e: e.memset(msk[:].rearrange("p (c j) -> p c j", j=64)[:, :, 0:1], 0.0))
            P.op('dve', [f'vecs{l}'], ['rw_ka1'], lambda e: e.tensor_scalar(
                out=ka1[:], in0=self.vec(l, 'ka'), scalar1=-1.0, scalar2=1.0, op0=ALU.mult, op1=ALU.add))
            c3 = lambda t: t[:].rearrange("p (c j) -> p c j", j=64)
            kkv = self.vec(l, 'kk'); kav = self.vec(l, 'ka'); rkv = self.vec(l, 'rk')
            gng = self.vec(l, 'gng_rw'); gnb = self.vec(l, 'gnb_rw')
            si = 0
            for hp in range(8):
                cs = slice(hp * 128, (hp + 1) * 128)
                r32, k32, v32, kk32 = A[0], A[1], A[2], A[3]
                for i_, c0 in enumerate((C_RR, C_RK, C_RV)):
                    P.dma(A[i_][:], self.zT[c0 + hp * 128:c0 + (hp + 1) * 128, :], ['zT'], [Ak[i_]])
                    ci = i_ * 8 + hp
                    self._shift(A[i_], Ak[i_], A[6], Ak[6], om[:, ci:ci + 1], hm[:, ci:ci + 1], 128)
                P.op('act', [Ak[2]], ['rw_vb'], lambda e: e.activation(out=vb[:], in_=v32[:], func=AF.Copy))
                for (srcb, srck, dst, dstk) in ((vb, 'rw_vb', V_tm, 'rw_Vtm'),):
                    for g0 in range(0, NT, 4):
                        g1 = min(NT, g0 + 4)
                        for tt in range(g0, g1):
                            P.op('pe', [srck, 'cst_bf'], ['psb'], lambda e, tt=tt, g0=g0, srcb=srcb: e.transpose(
                                self.psb[:, (tt - g0) * 128:(tt - g0 + 1) * 128], srcb[:, tt * 128:(tt + 1) * 128], self.ident_bf))
                        P.op('act', ['psb'], [dstk], lambda e, g0=g0, g1=g1, dst=dst: e.activation(
                            out=dst[:, g0:g1, :].rearrange("p a b -> p (a b)"), in_=self.psb[:, 0:(g1 - g0) * 128], func=AF.Copy))
                P.op('dve', [Ak[1], f'vecs{l}'], [Ak[3]], lambda e: e.tensor_scalar(
                    out=kk32[:], in0=k32[:], scalar1=kkv[:, hp:hp + 1], scalar2=None, op0=ALU.mult))
                P.op('dve', [Ak[0], Ak[1], f'vecs{l}'], [Ak[6]], lambda e: e.scalar_tensor_tensor(
                    out=A[6][:], in0=r32[:], scalar=rkv[:, hp:hp + 1], in1=k32[:], op0=ALU.mult, op1=ALU.mult))
                for (t0, n) in TILES:
                    P.op('act', [Ak[3]], ['rw_sq5'], lambda e, t0=t0, n=n: e.activation(out=sq5[:, :n], in_=kk32[:, t0:t0 + n], func=AF.Square))
                    P.op('pe', ['rw_sq5', 'bd_bf'], ['ps4'], lambda e, n=n: e.matmul(self.ps[4][:, :n], lhsT=self.bd_bf[:], rhs=sq5[:, :n], start=True, stop=True))
                    P.op('act', ['ps4'], ['rw_rt'], lambda e, n=n: e.activation(out=rt[:, :n], in_=self.ps[4][:, :n], func=AF.Sqrt, scale=1.0, bias=self.eps_t[:, 0:1]))
                    P.op('dve', ['rw_rt'], ['rw_rt'], lambda e, n=n: e.reciprocal(out=rt[:, :n], in_=rt[:, :n]))
                    P.op('dve', [Ak[3], 'rw_rt'], [Ak[3]], lambda e, t0=t0, n=n: e.tensor_tensor(out=kk32[:, t0:t0 + n], in0=kk32[:, t0:t0 + n], in1=rt[:, :n], op=ALU.mult))
                    P.op('act', [Ak[6]], ['rw_sq5'], lambda e, t0=t0, n=n: e.activation(out=sq5[:, :n], in_=A[6][:, t0:t0 + n], func=AF.Copy))
                    P.op('pe', ['rw_sq5', 'bd_bf'], ['ps5'], lambda e, n=n: e.matmul(self.ps[5][:, :n], lhsT=self.bd_bf[:], rhs=sq5[:, :n], start=True, stop=True))
                    P.op('dve', ['ps5', Ak[2]], ['rw_bon'], lambda e, t0=t0, n=n: e.tensor_tensor(out=bon[:, t0:t0 + n], in0=self.ps[5][:, :n], in1=v32[:, t0:t0 + n], op=ALU.mult))
                for d in range(2):
                    ld, a_, cin, E = A[4], A[5], A[2], A[6]
                    ldk, ak_, cink, Ek = Ak[4], Ak[5], Ak[2], Ak[6]
                    P.dma(ld[:], self.ldT[d, cs, :], ['ldT'], [ldk])
                    P.dma(a_[:], self.aT[d, cs, :], ['aT'], [ak_])
                    P.op('dve', ['rmsk', ldk], [cink], lambda e: e.tensor_tensor_scan(
                        out=cin[:], data0=msk[:], data1=ld[:], initial=0.0, op0=ALU.mult, op1=ALU.add))
                    if d == 1:
                        P.op('dve', [cink], [Ek], lambda e: e.tensor_tensor(
                            out=c3(E), in0=c3(cin)[:, :, 63:64].to_broadcast([128, NCH, 64]), in1=c3(cin), op=ALU.subtract))
                        P.op('dve', [Ek, ldk], [cink], lambda e: e.tensor_tensor(out=cin[:], in0=E[:], in1=ld[:], op=ALU.add))
                    tot = c3(cin)[:, :, 63:64] if d == 0 else c3(cin)[:, :, 0:1]
                    P.op('act', [cink], ['rw_etot'], lambda e, tot=tot: e.activation(out=etot[:].unsqueeze(2), in_=tot, func=AF.Exp))
                    P.op('dve', [cink, ldk], [ldk], lambda e: e.tensor_tensor(out=ld[:], in0=cin[:], in1=ld[:], op=ALU.subtract))
                    P.op('act', [ldk], [Ek], lambda e: e.activation(out=E[:], in_=ld[:], func=AF.Exp))
                    P.op('dve', [Ak[3], Ek], ['rw_al'], lambda e: e.tensor_tensor(out=al[:], in0=kk32[:], in1=E[:], op=ALU.mult))
                    P.op('act', [cink], [Ek], lambda e: e.activation(out=E[:], in_=cin[:], func=AF.Exp))
                    rho32 = ld
                    P.op('dve', [Ak[0], Ek], [ldk], lambda e: e.tensor_tensor(out=rho32[:], in0=r32[:], in1=E[:], op=ALU.mult))
                    P.op('act', [ldk], ['rw_rho'], lambda e: e.activation(out=rho[:], in_=rho32[:], func=AF.Copy))
                    P.op('act', [cink], [Ek], lambda e: e.activation(out=E[:], in_=cin[:], func=AF.Exp, scale=-1.0))
                    P.op('dve', [Ek, ak_], [Ek], lambda e: e.tensor_tensor(out=E[:], in0=E[:], in1=a_[:], op=ALU.mult))
                    P.op('dve', [Ak[3], Ek], ['rw_be'], lambda e: e.tensor_tensor(out=be[:], in0=kk32[:], in1=E[:], op=ALU.mult))
                    P.op('act', [cink], [Ek], lambda e: e.activation(out=E[:], in_=cin[:], func=AF.Exp, scale=-1.0))
                    P.op('dve', [ak_, f'vecs{l}', 'rw_ka1'], [ak_], lambda e: e.tensor_scalar(
                        out=a_[:], in0=a_[:], scalar1=kav[:, hp:hp + 1], scalar2=ka1[:, hp:hp + 1], op0=ALU.mult, op1=ALU.add))
                    P.op('dve', [ak_, Ak[1]], [ak_], lambda e: e.tensor_tensor(out=a_[:], in0=a_[:], in1=k32[:], op=ALU.mult))
                    P.op('dve', [ak_, Ek], ['rw_ka'], lambda e: e.tensor_tensor(out=ka[:], in0=a_[:], in1=E[:], op=ALU.mult))
                    eb = etot[:].unsqueeze(2).to_broadcast([128, NCH, 64])
                    P.op('dve', ['rw_be', 'rw_etot'], ['rw_Bp'], lambda e: e.tensor_tensor(out=c3(Bp), in0=c3(be), in1=eb, op=ALU.mult))
                    P.op('dve', ['rw_ka', 'rw_etot'], ['rw_Kp'], lambda e: e.tensor_tensor(out=c3(Kp), in0=c3(ka), in1=eb, op=ALU.mult))
                    self.stopat(1)
                    for (srcb, srck, dst, dstk) in ((al, 'rw_al', al_tm, 'rw_altm'), (Bp, 'rw_Bp', Bp_tm, 'rw_Bptm'), (Kp, 'rw_Kp', Kp_tm, 'rw_Kptm')):
                        for g0 in range(0, NT, 4):
                            g1 = min(NT, g0 + 4)
                            for tt in range(g0, g1):
                                P.op('pe', [srck, 'cst_bf'], ['psb'], lambda e, tt=tt, g0=g0, srcb=srcb: e.transpose(
                                    self.psb[:, (tt - g0) * 128:(tt - g0 + 1) * 128], srcb[:, tt * 128:(tt + 1) * 128], self.ident_bf))
                            P.op('act', ['psb'], [dstk], lambda e, g0=g0, g1=g1, dst=dst: e.activation(
                                out=dst[:, g0:g1, :].rearrange("p a b -> p (a b)"), in_=self.psb[:, 0:(g1 - g0) * 128], func=AF.Copy))
                    P.op('dve', [], ['rw_alm'], lambda e: e.memset(a5b, 0.0))
                    P.op('dve', [], ['rw_rhom'], lambda e: e.memset(a6b, 0.0))
                    for e_ in range(2):
                        pr = slice(e_ * 64, (e_ + 1) * 64)
                        P.op('act', ['rw_al'], ['rw_alm'], lambda e, e_=e_, pr=pr: e.activation(out=al_m[e_][pr, :], in_=al[pr, :], func=AF.Copy))
                        P.op('act', ['rw_rho'], ['rw_rhom'], lambda e, e_=e_, pr=pr: e.activation(out=rho_m[e_][pr, :], in_=rho[pr, :], func=AF.Copy))
                    self.stopat(2)
                    m_st = self.mask_fs if d == 0 else self.mask_bs
                    m_in = self.mask_f if d == 0 else self.mask_b
                    m_ts = self.mask_bs if d == 0 else self.mask_fs
                    bc4 = lambda m: m.unsqueeze(1).to_broadcast([128, 4, 128])
                    v4 = lambda t: t[:].rearrange("p (a b) -> p a b", b=128)
                    def grp(g0, sl):
                        B_ = slots[sl]
                        pa, pb, pc = B_['banks']
                        pak, pbk, pck = (f'ps{i}' for i in B_['bank_ids'])
                        XNs, Qs, LkT, MbT, MkT, Hh, P1n, Gt, ptmp = B_['XN'], B_['Q'], B_['LkT'], B_['MbT'], B_['MkT'], B_['H'], B_['P1n'], B_['G'], B_['ptmp']
                        kx = lambda nm: f'{nm}_s{sl}'
                        probs = [(tt, e_) for tt in (g0, g0 + 1) for e_ in range(2)]
                        X, N_ = XNs[0]

                        def gram(specs):
                            for pi_, (tt, e_) in enumerate(probs):
                                tsl = slice(tt * 128, (tt + 1) * 128); osl = slice(pi_ * 128, (pi_ + 1) * 128)
                                for (pst, pk, lh, rh, rks) in specs:
                                    lh_ = lh[e_] if isinstance(lh, list) else lh
                                    rh_ = rh[e_] if isinstance(rh, list) else rh
                                    P.op('pe', rks, [pk], lambda e, pst=pst, lh_=lh_, rh_=rh_, tsl=tsl, osl=osl: e.matmul(
                                        pst[:, osl], lhsT=lh_[:, tsl], rhs=rh_[:, tsl], start=True, stop=True))
                        gram(((pa, pak, be, al_m, ['rw_be', 'rw_alm']), (pb, pbk, al_m, be, ['rw_alm', 'rw_be']), (pc, pck, ka, al_m, ['rw_ka', 'rw_alm'])))
                        P.op('dve', [pak, 'cst_f'], [kx('rw_X0')], lambda e: e.scalar_tensor_tensor(
                            out=v4(X), in0=v4(pa), scalar=-1.0, in1=bc4(m_st), op0=ALU.mult, op1=ALU.mult))
                        P.op('dve', [pbk, 'cst_f'], [kx('rw_N0')], lambda e: e.scalar_tensor_tensor(
                            out=v4(N_), in0=v4(pb), scalar=-1.0, in1=bc4(m_ts), op0=ALU.mult, op1=ALU.mult))
                        P.op('dve', [pck, 'cst_f'], [kx('rw_LkT')], lambda e: e.tensor_tensor(out=v4(LkT), in0=v4(pc), in1=bc4(m_st), op=ALU.mult))
                        yield
                        pa2, pa2k = pa, pak
                        gram(((pa2, pa2k, be, rho_m, ['rw_be', 'rw_rhom']), (pb, pbk, ka, rho_m, ['rw_ka', 'rw_rhom'])))
                        P.op('dve', [pa2k, 'cst_f'], [kx('rw_MbT')], lambda e: e.tensor_tensor(out=v4(MbT), in0=v4(pa2), in1=bc4(m_in), op=ALU.mult))
                        P.op('dve', [pbk, 'cst_f'], [kx('rw_MkT')], lambda e: e.tensor_tensor(out=v4(MkT), in0=v4(pb), in1=bc4(m_in), op=ALU.mult))
                        P.op('dve', [kx('rw_X0'), 'cst_bf'], [kx('rw_Q0')], lambda e: e.tensor_tensor(
                            out=v4(Qs[0]), in0=v4(X), in1=self.ident_bf.unsqueeze(1).to_broadcast([128, 4, 128]), op=ALU.add))
                        yield
                        qi_ = 0
                        for j in range(1, 6):
                            Xo, No = XNs[(j - 1) % 2]
                            Xn, Nn = XNs[j % 2]
                            xo_k, no_k = kx(f'rw_X{(j - 1) % 2}'), kx(f'rw_N{(j - 1) % 2}')
                            xn_k, nn_k = kx(f'rw_X{j % 2}'), kx(f'rw_N{j % 2}')
                            for pi_ in range(4):
                                osl = slice(pi_ * 128, (pi_ + 1) * 128)
                                P.op('pe', [xo_k, no_k], [pak], lambda e, osl=osl, Xo=Xo, No=No: e.matmul(
                                    pa[:, osl], lhsT=No[:, osl], rhs=Xo[:, osl], start=True, stop=True))
                                P.op('pe', [xo_k, no_k], [pbk], lambda e, osl=osl, Xo=Xo, No=No: e.matmul(
                                    pb[:, osl], lhsT=Xo[:, osl], rhs=No[:, osl], start=True, stop=True))
                            P.op('act', [pak], [xn_k], lambda e, Xn=Xn: e.activation(out=Xn[:], in_=pa[:], func=AF.Copy))
                            P.op('act', [pbk], [nn_k], lambda e, Nn=Nn: e.activation(out=Nn[:], in_=pb[:], func=AF.Copy))
                            yield
                            Qo, Qn = Qs[qi_ % 2], Qs[(qi_ + 1) % 2]
                            qo_k, qn_k = kx(f'rw_Q{qi_ % 2}'), kx(f'rw_Q{(qi_ + 1) % 2}')
                            for pi_ in range(4):
                                osl = slice(pi_ * 128, (pi_ + 1) * 128)
                                P.op('pe', [nn_k, qo_k], [pck], lambda e, osl=osl, Nn=Nn, Qo=Qo: e.matmul(
                                    pc[:, osl], lhsT=Nn[:, osl], rhs=Qo[:, osl], start=True, stop=True))
                            P.op('dve', [pck, qo_k], [qn_k], lambda e, Qo=Qo, Qn=Qn: e.tensor_tensor(out=Qn[:], in0=pc[:], in1=Qo[:], op=ALU.add))
                            qi_ += 1
                            yield
                        Qf = Qs[qi_ % 2]; qf_k = kx(f'rw_Q{qi_ % 2}')
                        for pi_, (tt, e_) in enumerate(probs):
                            pr = slice(e_ * 64, (e_ + 1) * 64); osl = slice(pi_ * 128, (pi_ + 1) * 128)
                            P.op('pe', [kx('rw_LkT'), 'rw_Vtm'], [pak], lambda e, osl=osl, tt=tt, pr=pr, pi_=pi_: e.matmul(
                                pa[:, pi_ * 64:(pi_ + 1) * 64], lhsT=LkT[:, osl], rhs=V_tm[:, tt, pr], start=True, stop=True))
                            P.op('pe', [qf_k, 'rw_altm'], [pak], lambda e, osl=osl, tt=tt, pr=pr, pi_=pi_, Qf=Qf: e.matmul(
                                pa[:, 256 + pi_ * 64:256 + (pi_ + 1) * 64], lhsT=Qf[:, osl], rhs=al_tm[:, tt, pr], start=True, stop=True))
                        P.op('act', [pak], [kx('rw_H')], lambda e: e.activation(out=Hh, in_=pa[:, 0:256], func=AF.Copy))
                        P.op('act', [pak], [kx('rw_G')], lambda e: e.activation(out=Gt, in_=pa[:, 256:512], func=AF.Copy))
                        yield
                        H3 = Hh.rearrange("p (a b) -> p a b", b=64); G3 = Gt.rearrange("p (a b) -> p a b", b=64); P3 = P1n.rearrange("p (a b) -> p a b", b=64)
                        for pi_, (tt, e_) in enumerate(probs):
                            osl = slice(pi_ * 128, (pi_ + 1) * 128)
                            P.op('pe', [qf_k, kx('rw_H')], [pbk], lambda e, osl=osl, pi_=pi_, Qf=Qf: e.matmul(
                                pb[:, pi_ * 64:(pi_ + 1) * 64], lhsT=Qf[:, osl], rhs=H3[:, pi_, :], start=True, stop=True))
                        P.op('act', [pbk], [kx('rw_P1n')], lambda e: e.activation(out=P1n, in_=pb[:, 0:256], func=AF.Identity, scale=-1.0))
                        yield
                        for ti_, tt in enumerate((g0, g0 + 1)):
                            for e_ in range(2):
                                pi_ = ti_ * 2 + e_
                                pr = slice(e_ * 64, (e_ + 1) * 64); osl = slice(pi_ * 128, (pi_ + 1) * 128)
                                P.op('pe', [kx('rw_G'), kx('rw_MbT')], [pak], lambda e, pr=pr, osl=osl, pi_=pi_, ti_=ti_: e.matmul(
                                    pa[pr, ti_ * 128:(ti_ + 1) * 128], lhsT=G3[:, pi_, :], rhs=MbT[:, osl], start=True, stop=True))
                                P.op('pe', ['rw_Vtm', kx('rw_MkT')], [pbk], lambda e, pr=pr, osl=osl, tt=tt, ti_=ti_: e.matmul(
                                    pb[pr, ti_ * 128:(ti_ + 1) * 128], lhsT=V_tm[:, tt, pr], rhs=MkT[:, osl], start=True, stop=False))
                                P.op('pe', [kx('rw_P1n'), kx('rw_MbT')], [pbk], lambda e, pr=pr, osl=osl, pi_=pi_, ti_=ti_: e.matmul(
                                    pb[pr, ti_ * 128:(ti_ + 1) * 128], lhsT=P3[:, pi_, :], rhs=MbT[:, osl], start=False, stop=True))
                        t2 = slice(g0 * 128, (g0 + 2) * 128)
                        P.op('dve', [pak, ldk], ['rw_R32'], lambda e, t2=t2: e.tensor_tensor(out=R32[:, t2], in0=rho32[:, t2], in1=pa[:, 0:256], op=ALU.subtract))
                        P.op('act', [pbk], ['rw_yloc'], lambda e, t2=t2: e.activation(out=yloc[:, t2], in_=pb[:, 0:256], func=AF.Copy))
                        yield
                        for ti_, tt in enumerate((g0, g0 + 1)):
                            for hf in range(2):
                                csl = slice(hf * 64, (hf + 1) * 64)
                                gsl = G3[csl, ti_ * 2:ti_ * 2 + 2, :].rearrange("p a b -> p (a b)")
                                p1sl = P3[csl, ti_ * 2:ti_ * 2 + 2, :].rearrange("p a b -> p (a b)")
                                col = slice((ti_ * 2 + hf) * 128, (ti_ * 2 + hf + 1) * 128)
                                P.op('pe', [kx('rw_G'), 'rw_Bptm'], [pak], lambda e, gsl=gsl, csl=csl, tt=tt, col=col: e.matmul(
                                    pa[:, col], lhsT=gsl, rhs=Bp_tm[csl, tt, :], start=True, stop=True))
                                P.op('pe', ['rw_Kptm', 'rw_Vtm'], [pck], lambda e, csl=csl, tt=tt, col=col: e.matmul(
                                    pc[:, col], lhsT=Kp_tm[csl, tt, :], rhs=V_tm[csl, tt, :], start=True, stop=False))
                                P.op('pe', ['rw_Bptm', kx('rw_P1n')], [pck], lambda e, csl=csl, tt=tt, col=col, p1sl=p1sl: e.matmul(
                                    pc[:, col], lhsT=Bp_tm[csl, tt, :], rhs=p1sl, start=False, stop=True))
                        for q_ in range(4):
                            ch = g0 * 2 + q_
                            col = slice(q_ * 128, (q_ + 1) * 128)
                            P.op('dve', [pak, 'cst_f'], [kx('rw_ptmp')], lambda e, col=col: e.tensor_tensor(out=ptmp, in0=pa[:, col], in1=self.mask_bd, op=ALU.mult))
                            P.op('dve', [kx('rw_ptmp'), 'rw_etot', 'cst_f'], ['rw_Phi'], lambda e, ch=ch: e.scalar_tensor_tensor(
                                out=Phi[:, ch, :], in0=self.ident32, scalar=etot[:, ch:ch + 1], in1=ptmp, op0=ALU.mult, op1=ALU.subtract))
                        P.op('dve', [pck, 'cst_f'], ['rw_D'], lambda e, g0=g0: e.tensor_tensor(
                            out=Dd[:, g0 * 2:g0 * 2 + 4, :], in0=v4(pc), in1=bc4(self.mask_bd), op=ALU.mult))

                    pending = list(range(0, NT, 2))
                    active = {}
                    while pending or active:
                        for sl in range(self.rw_slots):
                            if sl not in active and pending:
                                active[sl] = grp(pending.pop(0), sl)
                        for sl in list(active):
                            try:
                                self._steps = getattr(self, '_steps', 0) + 1
                                if self._steps == self.rw_stop:
                                    P.halt = True
                                next(active[sl])
                            except StopIteration:
                                del active[sl]
                    self.stopat(7)
                    P.op('dve', [], [f'rw_S{si % 2}'], lambda e, si=si: e.memset(Sbd[si % 2][:], 0.0))
                    order = list(range(NCH)) if d == 0 else [3, 2, 1, 0] + list(range(NCH - 1, 3, -1))
                    for n_i, ch in enumerate(order):
                        S_ = Sbd[si % 2]; sk = f'rw_S{si % 2}'
                        tok = slice(ch * 64, (ch + 1) * 64)
                        yb = n_i % 2
                        need_y = with_ctx or ch >= 4
                        if need_y:
                            P.op('pe', [sk, 'rw_R32'], [f'ps{3 + yb}'], lambda e, S_=S_, tok=tok, yb=yb: e.matmul(
                                self.ps[3 + yb][:, 0:64], lhsT=S_[:], rhs=R32[:, tok], start=True, stop=True))
                            if d == 0:
                                P.op('dve', [f'ps{3 + yb}', 'rw_yloc'], ['rw_yacc'], lambda e, tok=tok, yb=yb: e.tensor_tensor(
                                    out=yacc[:, tok], in0=self.ps[3 + yb][:, 0:64], in1=yloc[:, tok], op=ALU.add))
                            else:
                                P.op('dve', [f'ps{3 + yb}', 'rw_yloc'], ['rw_yloc'], lambda e, tok=tok, yb=yb: e.tensor_tensor(
                                    out=yloc[:, tok], in0=self.ps[3 + yb][:, 0:64], in1=yloc[:, tok], op=ALU.add))
                                P.op('pool', ['rw_yloc', 'rw_yacc'], ['rw_yacc'], lambda e, tok=tok: e.tensor_tensor(
                                    out=yacc[:, tok], in0=yacc[:, tok], in1=yloc[:, tok], op=ALU.add))
                        P.op('pe', [sk, 'rw_Phi'], [f'ps{5 + yb}'], lambda e, S_=S_, ch=ch, yb=yb: e.matmul(
                            self.ps[5 + yb][:, 0:128], lhsT=Phi[:, ch, :], rhs=S_[:], start=True, stop=True))
                        si += 1
                        P.op('dve', [f'ps{5 + yb}', 'rw_D'], [f'rw_S{si % 2}'], lambda e, ch=ch, yb=yb, si=si: e.tensor_tensor(
                            out=Sbd[si % 2][:], in0=self.ps[5 + yb][:, 0:128], in1=Dd[:, ch, :], op=ALU.add))
                    self.stopat(8)
                for (t0, n) in TILES:
                    if not with_ctx and t0 < NCTX:
                        continue
                    ya = yacc[:, t0:t0 + n]
                    P.op('act', ['rw_yacc'], ['rw_sq5'], lambda e, ya=ya, n=n: e.activation(out=sq5[:, :n], in_=ya, func=AF.Copy))
                    P.op('pe', ['rw_sq5', 'bd_bf'], ['ps4'], lambda e, n=n: e.matmul(self.ps[4][:, :n], lhsT=self.bd_bf[:], rhs=sq5[:, :n], start=True, stop=True))
                    P.op('dve', ['ps4', 'rw_yacc'], ['rw_t5'], lambda e, ya=ya, n=n: e.scalar_tensor_tensor(
                        out=t5[:, :n], in0=self.ps[4][:, :n], scalar=-1.0 / 64, in1=ya, op0=ALU.mult, op1=ALU.add))
                    P.op('act', ['rw_t5'], ['rw_sq5'], lambda e, n=n: e.activation(out=sq5[:, :n], in_=t5[:, :n], func=AF.Square))
                    P.op('pe', ['rw_sq5', 'bd_bf'], ['ps4'], lambda e, n=n: e.matmul(self.ps[4][:, :n], lhsT=self.bd_bf[:], rhs=sq5[:, :n], start=True, stop=True))
                    P.op('act', ['ps4'], ['rw_rt'], lambda e, n=n: e.activation(out=rt[:, :n], in_=self.ps[4][:, :n], func=AF.Sqrt, scale=1.0 / 64, bias=geps[:, 0:1]))
                    P.op('dve', ['rw_rt'], ['rw_rt'], lambda e, n=n: e.reciprocal(out=rt[:, :n], in_=rt[:, :n]))
                    P.op('dve', ['rw_t5', 'rw_rt'], ['rw_t5'], lambda e, n=n: e.tensor_tensor(out=t5[:, :n], in0=t5[:, :n], in1=rt[:, :n], op=ALU.mult))
                    P.op('dve', ['rw_t5', f'vecs{l}'], ['rw_t5'], lambda e, n=n: e.tensor_scalar(
                        out=t5[:, :n], in0=t5[:, :n], scalar1=gng[:, hp:hp + 1], scalar2=gnb[:, hp:hp + 1], op0=ALU.mult, op1=ALU.add))
                    P.op('dve', ['rw_t5', 'rw_bon'], ['rw_t5'], lambda e, t0=t0, n=n: e.tensor_tensor(out=t5[:, :n], in0=t5[:, :n], in1=bon[:, t0:t0 + n], op=ALU.add))
                    P.dma(gsb[:, :n], self.g2T[cs, t0:t0 + n], ['g2T'], ['rw_g'])
                    P.op('dve', ['rw_t5', 'rw_g'], ['rw_vb'], lambda e, t0=t0, n=n: e.tensor_tensor(out=yo[:, t0:t0 + n], in0=t5[:, :n], in1=gsb[:, :n], op=ALU.mult))
                t_lo = 0 if with_ctx else NCTX
                P.dma(self.yT[2, cs, t_lo:], yo[:, t_lo:], ['rw_vb'], ['yT'])
            if 'rwkv' in self.debug:
                self.dbg(f'yrw{l}', lambda o: P.dma(o, self.yT[2], ['yT'], []), [1024, TT], BF16)


    def merge_moe(self, l):
        nc, P = self.nc, self.P
        with_ctx = l < DEPTH - 1
        last = l == DEPTH - 1
        src = (self.xT0 if l == 0 else self.xT).rearrange("(kc p) t -> p kc t", p=128)
        dstx = self.xT.rearrange("(kc p) t -> p kc t", p=128)
        dsto = self.outT.rearrange("(kc p) t -> p kc t", p=128)
        gTv = self.gT.rearrange("(i c p) t -> p i c t", i=3, p=128)
        yTv = self.yT.rearrange("i (c p) t -> p i c t", p=128)
        wbv = self.w_branch[l].rearrange("i (kc p) c -> p i kc c", p=128)
        wov = self.w_out[l].rearrange("(kc p) c -> p kc c", p=128)
        BIG = 1.0e4
        with contextlib.ExitStack() as st:
            xg = self.sb(st, "mm_xg", [128, KC, 512])
            rw32 = self.sb(st, "mm_rw32", [128, KC, 16])
            rbias = self.sb(st, "mm_rbias", [128, 16])
            sel = self.sb(st, "mm_sel", [16, 16, 128])
            P.dma(rw32[:], self.router_w.rearrange("(kc p) e -> p kc e", p=128), [], ['mm_rw32'])
            P.dma(rbias[:], self.router_b[0:1, :].partition_broadcast(128), [], ['mm_rbias'])
            P.op('dve', ['cst_f'], ['mm_sel'], lambda e: e.tensor_copy(
                out=sel[:], in_=self.ident32[0:16, 0:16].unsqueeze(2).to_broadcast([16, 16, 128])))
            wld = 0
            for (t0, n) in TILES:
                if t0 < NCTX and not with_ctx:
                    continue
                j = 1 if t0 < NCTX else 0
                nb = n // 128
                P.dma(xg[:, :, :n], src[:, :, t0:t0 + n], ['xT'], ['mm_xg'])
                with contextlib.ExitStack() as s2:
                    yb = self.sb(s2, "mm_y", [128, 3, 8, 512], BF16)
                    mg = self.sb(s2, "mm_mg", [128, KC, 512], BF16)
                    gb = [self.sb(s2, f"mm_g{i}", [128, 3, 512], BF16) for i in range(2)]
                    wb = [self.sb(s2, f"mm_wb{i}", [128, 3, 8, 512], BF16) for i in range(2)]
                    wo = [self.sb(s2, f"mm_wo{i}", [128, KC, 512], BF16) for i in range(2)]
                    m32 = self.sb(s2, "mm_m32", [128, 512])
                    tm = self.sb(s2, "mm_tm", [128, 512])
                    for i in range(3):
                        P.dma(yb[:, i, :, :n], yTv[:, i, :, t0:t0 + n], ['yT'], ['mm_y'])
                    pi = 0
                    for cb in range(4):
                        wk = f'mm_wb{cb % 2}'
                        for i in range(3):
                            P.dma(wb[cb % 2][:, i], wbv[:, i, :, cb * 512:(cb + 1) * 512], [], [wk], q='pool')
                        for dcl in range(4):
                            dc = cb * 4 + dcl
                            gk = f'mm_g{dc % 2}'
                            P.dma(gb[dc % 2][:, :, :n], gTv[:, :, dc, t0:t0 + n], ['gT'], [gk])
                            for i in range(3):
                                pst, pk = self.ps[pi % 4], f'ps{pi % 4}'
                                pi += 1
                                for kc in range(8):
                                    P.op('pe', [wk, 'mm_y'], [pk], lambda e, i=i, kc=kc, cb=cb, dcl=dcl, pst=pst: e.matmul(
                                        pst[:, :n], lhsT=wb[cb % 2][:, i, kc, dcl * 128:(dcl + 1) * 128], rhs=yb[:, i, kc, :n],
                                        start=(kc == 0), stop=(kc == 7)))
                                if i == 0:
                                    P.op('dve', [pk, gk], ['mm_m32'], lambda e, pst=pst, dc=dc: e.tensor_tensor(
                                        out=m32[:, :n], in0=pst[:, :n], in1=gb[dc % 2][:, 0, :n], op=ALU.mult))
                                else:
                                    P.op('dve', [pk, gk], ['mm_tm'], lambda e, pst=pst, dc=dc, i=i: e.tensor_tensor(
                                        out=tm[:, :n], in0=pst[:, :n], in1=gb[dc % 2][:, i, :n], op=ALU.mult))
                                    if i == 1:
                                        P.op('dve', ['mm_tm', 'mm_m32'], ['mm_m32'], lambda e: e.tensor_tensor(
                                            out=m32[:, :n], in0=m32[:, :n], in1=tm[:, :n], op=ALU.add))
                                    else:
                                        P.op('dve', ['mm_tm', 'mm_m32'], ['mm_mg'], lambda e, dc=dc: e.tensor_tensor(
                                            out=mg[:, dc, :n], in0=m32[:, :n], in1=tm[:, :n], op=ALU.add))
                    for cb in range(4):
                        wk = f'mm_wo{cb % 2}'
                        P.dma(wo[cb % 2][:], wov[:, :, cb * 512:(cb + 1) * 512], [], [wk], q='pool')
                        for dcl in range(4):
                            dc = cb * 4 + dcl
                            pst, pk = self.ps[pi % 4], f'ps{pi % 4}'
                            pi += 1
                            for kc in range(KC):
                                P.op('pe', [wk, 'mm_mg'], [pk], lambda e, kc=kc, cb=cb, dcl=dcl, pst=pst: e.matmul(
                                    pst[:, :n], lhsT=wo[cb % 2][:, kc, dcl * 128:(dcl + 1) * 128], rhs=mg[:, kc, :n],
                                    start=(kc == 0), stop=(kc == KC - 1)))
                            P.op('dve', [pk, 'mm_xg', f'mod{l}'], ['mm_xg'], lambda e, pst=pst, dc=dc: e.scalar_tensor_tensor(
                                out=xg[:, dc, :n], in0=pst[:, :n], scalar=self.mod[l][:, 32 + dc, j:j + 1], in1=xg[:, dc, :n],
                                op0=ALU.mult, op1=ALU.add))
                    if 'x1' in self.debug:
                        self.dbg(f'x1_{l}_{t0}', lambda o: P.dma(o.rearrange("(kc p) t -> p kc t", p=128), xg[:, :, :n], ['mm_xg'], []), [D, n])
                    P.barrier()
                with contextlib.ExitStack() as s2:
                    h2 = self.sb(s2, "mo_h2", [128, KC, 512], BF16)
                    sq = h2
                    rt = self.sb(s2, "mo_rt", [128, 512])
                    lg = self.sb(s2, "mo_lg", [16, 512])
                    R = {nm: self.sb(s2, "mo_" + nm, [128, 4, 16]) for nm in ('s', 'bz', 'eq', 'b2', 'mb', 'e1', 'w')}
                    r4 = {nm: self.sb(s2, "mo_" + nm, [128, 4, 4]) for nm in ('m1', 'm2', 'gsel')}
                    r1 = {nm: self.sb(s2, "mo_" + nm, [128, 4]) for nm in ('gmax', 't1', 't2', 'ws')}
                    cT = self.sb(s2, "mo_cT", [16, 512])
                    bce = [self.sb(s2, f"mo_bce{i}", [128, 512]) for i in range(2)]
                    sg = [self.sb(s2, f"mo_sg{i}", [128, 512]) for i in range(2)]
                    act = [self.sb(s2, f"mo_act{i}", [128, 4, 512], BF16) for i in range(2)]
                    wg = [self.sb(s2, f"mo_wg{i}", [128, KC, 512], BF16) for i in range(2)]
                    wu = [self.sb(s2, f"mo_wu{i}", [128, KC, 512], BF16) for i in range(2)]
                    s3 = contextlib.ExitStack()
                    xn = self.sb(s3, "mo_xn", [128, KC, 512])
                    P.op('act', ['mm_xg'], ['mo_h2'], lambda e: e.activation(out=sq[:, :, :n], in_=xg[:, :, :n], func=AF.Square))
                    for kc in range(KC):
                        P.op('pe', ['mo_h2', 'ones_bf'], ['ps6'], lambda e, kc=kc: e.matmul(
                            self.ps[6][:, :n], lhsT=self.ones_bf[:], rhs=sq[:, kc, :n], start=(kc == 0), stop=(kc == KC - 1)))
                    P.op('act', ['ps6'], ['mo_rt'], lambda e: e.activation(out=rt[:, :n], in_=self.ps[6][:, :n], func=AF.Sqrt,
                                                                      scale=1.0 / D, bias=self.eps_t[:, 0:1]))
                    P.op('dve', ['mo_rt'], ['mo_rt'], lambda e: e.reciprocal(out=rt[:, :n], in_=rt[:, :n]))
                    P.op('dve', ['mm_xg', 'mo_rt'], ['mo_xn'], lambda e: e.tensor_tensor(
                        out=xn[:, :, :n], in0=xg[:, :, :n], in1=rt[:, :n].unsqueeze(1).to_broadcast([128, KC, n]), op=ALU.mult))
                    for kc in range(KC):
                        P.op('act', ['mo_xn', f'gm2_{l}', f'mod{l}'], ['mo_xn'], lambda e, kc=kc: e.activation(
                            out=xn[:, kc, :n], in_=xn[:, kc, :n], func=AF.Identity,
                            scale=self.gm2[l][:, kc, j:j + 1], bias=self.mod[l][:, 48 + kc, j:j + 1]))
                    P.op('dve', ['mo_xn'], ['mo_h2'], lambda e: e.tensor_copy(out=h2[:, :, :n], in_=xn[:, :, :n]))
                    for kc in range(KC):
                        P.op('pe', ['mo_xn', 'mm_rw32'], ['ps5'], lambda e, kc=kc: e.matmul(
                            self.ps[5][0:16, :n], lhsT=rw32[:, kc, :], rhs=xn[:, kc, :n], start=(kc == 0), stop=(kc == KC - 1)))
                    P.op('act', ['ps5'], ['mo_lg'], lambda e: e.activation(out=lg[:, :n], in_=self.ps[5][0:16, :n], func=AF.Copy))
                    for b_ in range(nb):
                        P.op('pe', ['mo_lg', 'cst_f'], ['ps4'], lambda e, b_=b_: e.transpose(
                            self.ps[4][:, b_ * 16:(b_ + 1) * 16], lg[0:16, b_ * 128:(b_ + 1) * 128], self.ident32[0:16, 0:16]))
                    s_, bz, eq, b2, mb, e1, w_ = (R[k][:, :nb, :] for k in ('s', 'bz', 'eq', 'b2', 'mb', 'e1', 'w'))
                    m1, m2, gsel = (r4[k][:, :nb, :] for k in ('m1', 'm2', 'gsel'))
                    gmax, t1, t2, ws = (r1[k][:, :nb] for k in ('gmax', 't1', 't2', 'ws'))
                    g4 = lambda a: a.rearrange("p b (g k) -> p b g k", k=4)
                    V = lambda reads, writes, fn: P.op('dve', reads, writes, fn)
                    P.op('act', ['ps4'], ['mo_s'], lambda e: e.activation(
                        out=s_, in_=self.ps[4][:, 0:nb * 16].rearrange("p (b k) -> p b k", k=16), func=AF.Sigmoid))
                    V(['mo_s', 'mm_rbias'], ['mo_bz'], lambda e: e.tensor_tensor(out=bz, in0=s_, in1=rbias[:].unsqueeze(1).to_broadcast([128, nb, 16]), op=ALU.add))
                    V(['mo_bz'], ['mo_m1'], lambda e: e.tensor_reduce(out=m1, in_=g4(bz), axis=AX.X, op=ALU.max))
                    V(['mo_bz', 'mo_m1'], ['mo_eq'], lambda e: e.tensor_tensor(out=g4(eq), in0=g4(bz), in1=m1.unsqueeze(3).to_broadcast([128, nb, 4, 4]), op=ALU.is_equal))
                    V(['mo_eq', 'mo_bz'], ['mo_b2'], lambda e: e.scalar_tensor_tensor(out=b2, in0=eq, scalar=-BIG, in1=bz, op0=ALU.mult, op1=ALU.add))
                    V(['mo_b2'], ['mo_m2'], lambda e: e.tensor_reduce(out=m2, in_=g4(b2), axis=AX.X, op=ALU.max))
                    V(['mo_m1', 'mo_m2'], ['mo_m1'], lambda e: e.tensor_tensor(out=m1, in0=m1, in1=m2, op=ALU.add))
                    V(['mo_m1'], ['mo_gmax'], lambda e: e.tensor_reduce(out=gmax, in_=m1, axis=AX.X, op=ALU.max))
                    V(['mo_m1', 'mo_gmax'], ['mo_gsel'], lambda e: e.tensor_tensor(out=gsel, in0=m1, in1=gmax.unsqueeze(2).to_broadcast([128, nb, 4]), op=ALU.is_equal))
                    V(['mo_gsel'], ['mo_gsel'], lambda e: e.tensor_scalar(out=gsel, in0=gsel, scalar1=BIG, scalar2=-BIG, op0=ALU.mult, op1=ALU.add))
                    V(['mo_bz', 'mo_gsel'], ['mo_mb'], lambda e: e.tensor_tensor(out=g4(mb), in0=g4(bz), in1=gsel.unsqueeze(3).to_broadcast([128, nb, 4, 4]), op=ALU.add))
                    V(['mo_mb'], ['mo_t1'], lambda e: e.tensor_reduce(out=t1, in_=mb, axis=AX.X, op=ALU.max))
                    V(['mo_mb', 'mo_t1'], ['mo_e1'], lambda e: e.tensor_tensor(out=e1, in0=mb, in1=t1.unsqueeze(2).to_broadcast([128, nb, 16]), op=ALU.is_equal))
                    V(['mo_e1', 'mo_mb'], ['mo_b2'], lambda e: e.scalar_tensor_tensor(out=b2, in0=e1, scalar=-BIG, in1=mb, op0=ALU.mult, op1=ALU.add))
                    V(['mo_b2'], ['mo_t2'], lambda e: e.tensor_reduce(out=t2, in_=b2, axis=AX.X, op=ALU.max))
                    V(['mo_b2', 'mo_t2'], ['mo_eq'], lambda e: e.tensor_tensor(out=eq, in0=b2, in1=t2.unsqueeze(2).to_broadcast([128, nb, 16]), op=ALU.is_equal))
                    V(['mo_eq', 'mo_e1'], ['mo_e1'], lambda e: e.tensor_tensor(out=e1, in0=e1, in1=eq, op=ALU.add))
                    V(['mo_e1', 'mo_s'], ['mo_w'], lambda e: e.tensor_tensor(out=w_, in0=e1, in1=s_, op=ALU.mult))
                    V(['mo_w'], ['mo_ws'], lambda e: e.tensor_reduce(out=ws, in_=w_, axis=AX.X, op=ALU.add))
                    V(['mo_ws'], ['mo_ws'], lambda e: e.reciprocal(out=ws, in_=ws))
                    V(['mo_w', 'mo_ws'], ['mo_w'], lambda e: e.tensor_tensor(out=w_, in0=w_, in1=ws.unsqueeze(2).to_broadcast([128, nb, 16]), op=ALU.mult))
                    for b_ in range(nb):
                        P.op('pe', ['mo_w', 'cst_f'], ['ps5'], lambda e, b_=b_: e.transpose(
                            self.ps[5][0:16, b_ * 128:(b_ + 1) * 128], R['w'][:, b_, :], self.ident32))
                    P.op('act', ['ps5'], ['mo_cT'], lambda e: e.activation(out=cT[:, :n], in_=self.ps[5][0:16, :n], func=AF.Copy))
                    if 'comb' in self.debug:
                        self.dbg(f'comb_{l}_{t0}', lambda o: P.dma(o, cT[:, :n], ['mo_cT'], []), [16, n])
                    P.barrier()
                    s3.close()
                    s3 = contextlib.ExitStack()
                    wd = [self.sb(s3, f"mo_wd{i}", [128, 4, D], BF16) for i in range(2)]
                    pi = 0
                    for ex in range(16):
                        b = ex % 2
                        P.dma(wg[b][:], self.moe_g[l, ex].rearrange("(kc p) f -> p kc f", p=128), [], [f'mo_wg{b}'], q='pool')
                        P.dma(wu[b][:], self.moe_u[l, ex].rearrange("(kc p) f -> p kc f", p=128), [], [f'mo_wu{b}'], q='pool')
                        P.dma(wd[b][:], self.moe_d[l, ex].rearrange("(fc p) d -> p fc d", p=128), [], [f'mo_wd{b}'], q='pool')
                        P.op('pe', ['mm_sel', 'mo_cT'], ['ps6'], lambda e, ex=ex: e.matmul(
                            self.ps[6][:, :n], lhsT=sel[:, ex, :], rhs=cT[:, :n], start=True, stop=True), rg=0)
                        P.op('act', ['ps6'], [f'mo_bce{b}'], lambda e, b=b: e.activation(out=bce[b][:, :n], in_=self.ps[6][:, :n], func=AF.Copy))
                        for fc in range(4):
                            pg, pgk = self.ps[pi % 4], f'ps{pi % 4}'
                            pu, puk = self.ps[(pi + 1) % 4], f'ps{(pi + 1) % 4}'
                            pi += 2
                            for kc in range(KC):
                                P.op('pe', [f'mo_wg{b}', 'mo_h2'], [pgk], lambda e, kc=kc, fc=fc, b=b, pg=pg: e.matmul(
                                    pg[:, :n], lhsT=wg[b][:, kc, fc * 128:(fc + 1) * 128], rhs=h2[:, kc, :n], start=(kc == 0), stop=(kc == KC - 1)))
                            for kc in range(KC):
                                P.op('pe', [f'mo_wu{b}', 'mo_h2'], [puk], lambda e, kc=kc, fc=fc, b=b, pu=pu: e.matmul(
                                    pu[:, :n], lhsT=wu[b][:, kc, fc * 128:(fc + 1) * 128], rhs=h2[:, kc, :n], start=(kc == 0), stop=(kc == KC - 1)))
                            sb_ = fc % 2
                            P.op('act', [pgk], [f'mo_sg{sb_}'], lambda e, pg=pg, sb_=sb_: e.activation(out=sg[sb_][:, :n], in_=pg[:, :n], func=AF.Silu))
                            P.op('dve', [puk, f'mo_sg{sb_}'], [f'mo_sg{sb_}'], lambda e, pu=pu, sb_=sb_: e.tensor_tensor(
                                out=sg[sb_][:, :n], in0=pu[:, :n], in1=sg[sb_][:, :n], op=ALU.mult))
                            P.op('dve', [f'mo_sg{sb_}', f'mo_bce{b}'], [f'mo_act{b}'], lambda e, sb_=sb_, b=b, fc=fc: e.tensor_tensor(
                                out=act[b][:, fc, :n], in0=sg[sb_][:, :n], in1=bce[b][:, :n], op=ALU.mult))
                        for dc in range(KC):
                            pd, pdk = self.ps[pi % 4], f'ps{pi % 4}'
                            pi += 1
                            for fc in range(4):
                                P.op('pe', [f'mo_wd{b}', f'mo_act{b}'], [pdk], lambda e, fc=fc, dc=dc, b=b, pd=pd: e.matmul(
                                    pd[:, :n], lhsT=wd[b][:, fc, dc * 128:(dc + 1) * 128], rhs=act[b][:, fc, :n], start=(fc == 0), stop=(fc == 3)))
                            P.op('dve', [pdk, 'mm_xg', f'mod{l}'], ['mm_xg'], lambda e, pd=pd, dc=dc: e.scalar_tensor_tensor(
                                out=xg[:, dc, :n], in0=pd[:, :n], scalar=self.mod[l][:, 80 + dc, j:j + 1], in1=xg[:, dc, :n],
                                op0=ALU.mult, op1=ALU.add))
                    if last:
                        P.dma(dsto[:, :, t0 - NCTX:t0 - NCTX + n], xg[:, :, :n], ['mm_xg'], ['outT'])
                    else:
                        P.dma(dstx[:, :, t0:t0 + n], xg[:, :, :n], ['mm_xg'], ['xT'])
                    if 'x2' in self.debug:
                        self.dbg(f'x2_{l}_{t0}', lambda o: P.dma(o.rearrange("(kc p) t -> p kc t", p=128), xg[:, :, :n], ['mm_xg'], []), [D, n])
                    P.barrier()
                    s3.close()

    def final_out(self):
        P = self.P
        P.dma(self.outT, self.xT[:, NCTX:], ['xT'], [])


def na_tables(rpb):
    c = np.arange(64)
    cs = np.clip(c - 8, 0, 48)
    kc = np.arange(64)
    inwin = (kc[:, None] >= cs[None, :]) & (kc[:, None] < cs[None, :] + 16)
    dc = np.clip(kc[:, None] - c[None, :] + 15, 0, 30)
    out = np.full((16, 2, 64, 14, 64), NEG, np.float32)
    for jj in range(2):
        for dr in range(14):
            g = rpb[:, dr + jj][:, dc]
            out[:, jj, :, dr, :] = np.where(inwin[None], g, np.float32(NEG))
    return out.reshape(16, 128, 14 * 64)


NCONST = 512 + 2 * SEQ + 384


def make_consts():
    c = np.zeros((128, NCONST), np.float32)
    p = np.arange(128)
    c[p, p] = 1.0
    c[p, 128 + (p ^ 32)] = 1.0
    j = p[:, None]; i = p[None, :]
    same = (j // 64) == (i // 64)
    c[:, 256:384] = (same & (j <= i)).astype(np.float32)
    c[:, 384:512] = (same & (j >= i)).astype(np.float32)
    t = np.arange(SEQ)
    pos = np.where(p[:, None] < 64, (t // 64)[None, :], (t % 64)[None, :]).astype(np.float32)
    inv = (10000.0 ** (-np.arange(0, 64, 2, dtype=np.float32) / 64)).astype(np.float32)
    ang = pos * inv[(p % 32)][:, None]
    c[:, 512:512 + SEQ] = np.cos(ang)
    sgn = np.where((p % 64) < 32, -1.0, 1.0).astype(np.float32)
    c[:, 512 + SEQ:512 + 2 * SEQ] = np.sin(ang) * sgn[:, None]
    o = 512 + 2 * SEQ
    c[:, o:o + 128] = (same & (j < i)).astype(np.float32)
    c[:, o + 128:o + 256] = (same & (j > i)).astype(np.float32)
    c[:, o + 256:o + 384] = same.astype(np.float32)
    return c


def host_inputs(inp, b):
    xT0 = np.ascontiguousarray(np.concatenate([inp['ctx'][b], inp['x'][b]], axis=0).T)
    cT = np.stack([fm(inp['c'][b]), fm(inp['c_ctx'])], axis=-1).reshape(128, 32)
    return {
        'xT0': xT0, 'cT': np.ascontiguousarray(cT),
        'ada_w': inp['ada_w'], 'w_in': inp['w_in'],
        'vecs': np.stack([pack_vecs(inp, l) for l in range(DEPTH)]),
        'natab': np.stack([na_tables(inp['na_rpb'][l]) for l in range(DEPTH)]),
        'consts': make_consts(), 'gla_gate_w2': inp['gla_gate_w2'],
        'rw_w2': inp['rw_w2'], 'rw_a2': inp['rw_a2'], 'rw_g2': inp['rw_g2'],
        'w_branch': inp['w_branch'], 'w_out': inp['w_out'], 'router_w': inp['router_w'],
        'router_bias': inp['router_bias'].reshape(1, 16),
        'moe_w_gate': inp['moe_w_gate'], 'moe_w_up': inp['moe_w_up'], 'moe_w_down': inp['moe_w_down'],
    }


def kernel(**inputs):
    inp = {k: np.asarray(v) for k, v in inputs.items()}
    bld = Builder()
    nc = bld.build()
    in_maps = [host_inputs(inp, c % 4) for c in range(8)]
    res = run_bass_kernel_spmd(nc, in_maps, core_ids=list(range(8)))
    out = np.stack([np.ascontiguousarray(res.results[b]["outT"].T) for b in range(4)], axis=0)
    return out.astype(np.float32)
```

```python
import contextlib
import os
import numpy as np
import concourse.bass as bass
import concourse.mybir as mybir
from concourse.bass_utils import run_bass_kernel_spmd

F32 = mybir.dt.float32
BF16 = mybir.dt.bfloat16
AF = mybir.ActivationFunctionType
ALU = mybir.AluOpType
AX = mybir.AxisListType

D = 2048
KC = 16
NCTX = 256
SEQ = 2048
TT = NCTX + SEQ
D_IN = 16032
EPS = 1e-6
NEG = -30000.0
DEPTH = 2

C_NAQ, C_NAK, C_NAV = 0, 1024, 2048
C_GQ, C_GK, C_GV, C_GR, C_GGD = 3072, 3584, 4096, 5120, 6144
C_RW = 6176
C_RR, C_RK, C_RV, C_RWD, C_RAD, C_RGD = C_RW, C_RW + 1024, C_RW + 2048, C_RW + 3072, C_RW + 3264, C_RW + 3456
C_GATE = 9888

TILES = [(0, 256), (256, 512), (768, 512), (1280, 512), (1792, 512)]

NDS = 12


class PEProxy:
    def __init__(self, real):
        self._real = real
        self._last = None
        self._dummy = None

    def _sep(self, out, w):
        K = w.shape[0]
        M = 1
        for d_ in w.shape[1:]:
            M *= d_
        t = None if K == 128 else (w.base_partition(), K)
        if t is not None and self._last is not None and t != self._last and self._dummy is not None:
            self._dummy(self._real)
        self._last = t

    def matmul(self, out, lhsT=None, rhs=None, **kw):
        self._sep(out, lhsT)
        return self._real.matmul(out, lhsT=lhsT, rhs=rhs, **kw)

    def transpose(self, out, in_, identity, **kw):
        self._sep(out, in_)
        return self._real.transpose(out, in_, identity, **kw)

    def __getattr__(self, name):
        return getattr(self._real, name)


class Prog:
    def __init__(self, nc, es):
        self.nc = nc
        self.e = dict(pe=PEProxy(nc.tensor), act=nc.scalar, dve=nc.vector, pool=nc.gpsimd, sp=nc.sync)
        self.sem = {k: es.enter_context(nc.semaphore("s_" + k)) for k in self.e}
        self.cnt = {k: 0 for k in self.e}
        self.seen = {k: {} for k in self.e}
        self.lw = {}
        self.rd = {}
        self.dsem = [es.enter_context(nc.semaphore(f"dq{i}")) for i in range(NDS)]
        self.dcnt = [0] * NDS
        self.dnext = 0

    def semh(self, k):
        return self.dsem[k[1]] if isinstance(k, tuple) else self.sem[k]

    def _wait(self, eng, k, v):
        if self.seen[eng].get(k, 0) < v:
            self.e[eng].wait_ge(self.semh(k), v)
            self.seen[eng][k] = v

    def set_parent(self, child, parent):
        self.parent = getattr(self, 'parent', {})
        self.children = getattr(self, 'children', {})
        self.parent[child] = parent
        self.children.setdefault(parent, []).append(child)

    def _expand(self, bs):
        par = getattr(self, 'parent', {})
        chl = getattr(self, 'children', {})
        out = []
        for b in bs:
            out.append(b)
            if b in par:
                out.append(par[b])
            out.extend(chl.get(b, ()))
        return out

    def _deps(self, eng, reads, writes):
        reads = self._expand(reads)
        writes = self._expand(writes)
        deps = {}
        for b in reads:
            lw = self.lw.get(b)
            if lw:
                deps[lw[0]] = max(deps.get(lw[0], 0), lw[1])
        for b in writes:
            lw = self.lw.get(b)
            if lw:
                deps[lw[0]] = max(deps.get(lw[0], 0), lw[1])
            for k, v in self.rd.get(b, {}).items():
                deps[k] = max(deps.get(k, 0), v)
        for k, v in deps.items():
            if k == 'pe' and eng == 'pe':
                continue
            self._wait(eng, k, v)

    def _mark(self, pt, reads, writes):
        for b in writes:
            self.lw[b] = pt
            self.rd[b] = {}
        for b in reads:
            d = self.rd.setdefault(b, {})
            d[pt[0]] = max(d.get(pt[0], 0), pt[1])

    halt = False

    pe_rg = None
    dummy = None

    def op(self, eng, reads, writes, fn, rg=None):
        if self.halt:
            return None
        if eng == 'pe':
            pass
        self._deps(eng, reads, writes)
        ins = fn(self.e[eng])
        self.cnt[eng] += 1
        ins.then_inc(self.sem[eng], 1)
        self._mark((eng, self.cnt[eng]), reads, writes)
        return ins

    def dma(self, out, in_, reads, writes, q='sp', **kw):
        if self.halt:
            return None
        i = self.dnext % NDS
        self.dnext += 1
        k = ('d', i)
        if self.dcnt[i]:
            self._wait(q, k, self.dcnt[i])
        self._deps(q, reads, writes)
        ins = self.e[q].dma_start(out=out, in_=in_, **kw)
        self.dcnt[i] += 16
        ins.then_inc(self.dsem[i], 16)
        self._mark((k, self.dcnt[i]), reads, writes)
        return ins

    def barrier(self):
        for q in self.e:
            for i in range(NDS):
                if self.dcnt[i]:
                    self._wait(q, ('d', i), self.dcnt[i])
            for k in self.e:
                if k != q and self.cnt[k]:
                    self._wait(q, k, self.cnt[k])
        self.lw = {}
        self.rd = {}

    def finish(self, q='sp'):
        for i in range(NDS):
            if self.dcnt[i]:
                self.e[q].wait_ge(self.dsem[i], self.dcnt[i])
        for k in self.e:
            if k != q and self.cnt[k]:
                self.e[q].wait_ge(self.sem[k], self.cnt[k])


def fm(v):
    v = np.asarray(v, np.float32)
    return np.ascontiguousarray(v.reshape(-1, 128).T)


VEC_SLOTS = {}


def _vec_layout():
    off = 0
    def add(name, n):
        nonlocal off
        VEC_SLOTS[name] = (off, n)
        off += n
    add('n1g', 16); add('n2g', 16); add('adab', 96)
    add('naq', 1); add('nak', 1)
    add('ggb', 8)
    add('gng', 2)
    add('mu_r', 8); add('mu_k', 8); add('mu_v', 8); add('mu_wd', 2); add('mu_ad', 2); add('mu_gd', 2)
    add('w0', 16); add('a0', 16); add('kk', 8); add('ka', 8); add('rk', 8); add('gng_rw', 8); add('gnb_rw', 8)
    return off


NV = _vec_layout()


def pack_vecs(inp, l):
    v = np.zeros((128, NV), np.float32)
    def put(name, arr):
        o, n = VEC_SLOTS[name]
        assert arr.shape == (128, n), (name, arr.shape)
        v[:, o:o + n] = arr
    put('n1g', fm(inp['norm1_g'][l])); put('n2g', fm(inp['norm2_g'][l])); put('adab', fm(inp['ada_b'][l]))
    put('naq', np.tile(inp['na_q_norm'][l], 2)[:, None]); put('nak', np.tile(inp['na_k_norm'][l], 2)[:, None])
    put('ggb', fm(inp['gla_gate_b'][l].reshape(-1)))
    put('gng', fm(inp['gla_norm_g'][l]))
    mu = inp['rw_mu'][l]
    put('mu_r', fm(mu[0:1024])); put('mu_k', fm(mu[1024:2048])); put('mu_v', fm(mu[2048:3072]))
    def pad96(a):
        o = np.zeros((128, 2), np.float32); o[:96, 0] = a[:96]; o[:96, 1] = a[96:192]; return o
    put('mu_wd', pad96(mu[3072:3264])); put('mu_ad', pad96(mu[3264:3456])); put('mu_gd', fm(mu[3456:3712]))
    put('w0', fm(inp['rw_w0'][l].reshape(-1))); put('a0', fm(inp['rw_a0'][l].reshape(-1)))
    put('kk', fm(inp['rw_k_k'][l])); put('ka', fm(inp['rw_k_a'][l])); put('rk', fm(inp['rw_r_k'][l].reshape(-1)))
    put('gng_rw', fm(inp['rw_gn_g'][l])); put('gnb_rw', fm(inp['rw_gn_b'][l]))
    return v


class _Stop(Exception):
    pass


class Builder:
    rw_stop = 0
    rw_slots = 2

    def sc_in(self, name, on=True):
        self.sc_out()
        if on:
            self._sc = (name, self.nc.enter_named_scope(name, False)[0])

    def sc_out(self):
        c = getattr(self, '_sc', None)
        if c:
            self.nc.leave_named_scope(c[0], c[1], False)
        self._sc = None

    def stopat(self, k):
        if self.rw_stop == k:
            self.P.halt = True

    def __init__(self, layers=(0, 1), upto='all', debug=()):
        self.layers = layers
        self.upto = upto
        self.debug = set(debug)
        self.nc = bass.Bass("TRN2", target_bir_lowering=False)
        self.es = contextlib.ExitStack()
        self.dbg_outs = {}

    def din(self, name, shape, dt=F32):
        return self.nc.dram_tensor(name, list(shape), dt, kind="ExternalInput").ap()

    def dout(self, name, shape, dt=F32):
        return self.nc.dram_tensor(name, list(shape), dt, kind="ExternalOutput").ap()

    def dscr(self, name, shape, dt=F32):
        return self.nc.dram_tensor(name, list(shape), dt, kind="Internal").ap()

    def sb(self, st, name, shape, dt=F32):
        self._uid = getattr(self, '_uid', 0) + 1
        return st.enter_context(self.nc.sbuf_tensor(f"{name}_u{self._uid}", list(shape), dt))

    def vec(self, l, name):
        o, n = VEC_SLOTS[name]
        return self.vecs[l][:, o:o + n]

    def build(self):
        nc = self.nc
        es = self.es
        with es:
            self.P = P = Prog(nc, es)
            self.xT0 = self.din("xT0", [D, TT])
            self.cT = self.din("cT", [128, 32])
            self.ada_w = self.din("ada_w", [DEPTH, D, 6 * D])
            self.w_in = self.din("w_in", [DEPTH, D, D_IN])
            self.vecs_d = self.din("vecs", [DEPTH, 128, NV])
            self.outT = self.dout("outT", [D, SEQ])
            self.xT = self.dscr("xT_s", [D, TT])
            self.zT = self.dscr("zT_s", [C_GATE, TT])
            self.gT = self.dscr("gT_s", [3 * D, TT], BF16)
            self.vtm = self.dscr("vtm_s", [TT, 2048], BF16)
            self.yT = self.dscr("yT_s", [3, 1024, TT], BF16)
            self.natab = self.din("natab", [DEPTH, 16, 128, 14 * 64])
            self.consts = self.din("consts", [128, NCONST])
            self.gla_w2 = self.din("gla_gate_w2", [DEPTH, 2, 16, 512])
            self.rw_w2 = self.din("rw_w2", [DEPTH, 2, 96, 1024])
            self.rw_a2 = self.din("rw_a2", [DEPTH, 2, 96, 1024])
            self.rw_g2 = self.din("rw_g2", [DEPTH, 256, 1024])
            self.w_branch = self.din("w_branch", [DEPTH, 3, 1024, D])
            self.w_out = self.din("w_out", [DEPTH, D, D])
            self.router_w = self.din("router_w", [D, 16])
            self.router_b = self.din("router_bias", [1, 16])
            self.moe_g = self.din("moe_w_gate", [DEPTH, 16, D, 512])
            self.moe_u = self.din("moe_w_up", [DEPTH, 16, D, 512])
            self.moe_d = self.din("moe_w_down", [DEPTH, 16, 512, D])
            self.ldT = self.dscr("ldT_s", [2, 1024, TT])
            self.aT = self.dscr("aT_s", [2, 1024, TT])
            self.g2T = self.dscr("g2T_s", [1024, TT], BF16)
            self.ones_bf = self.sb(es, "ones_bf", [128, 128], BF16)
            self.vecs = [self.sb(es, f"vecs{l}", [128, NV]) for l in range(DEPTH)]
            self.mod = [self.sb(es, f"mod{l}", [128, 96, 2]) for l in range(DEPTH)]
            self.gm1 = [self.sb(es, f"gm1_{l}", [128, 16, 2]) for l in range(DEPTH)]
            self.gm2 = [self.sb(es, f"gm2_{l}", [128, 16, 2]) for l in range(DEPTH)]
            self.ps = [es.enter_context(nc.psum_tensor(f"ps{i}", [128, 512], F32)) for i in range(7)]
            self.psb = es.enter_context(nc.psum_tensor("psb", [128, 1024], BF16))
            psd = self.psb[:, 512:1024].bitcast(F32)
            P.e['pe']._dummy = lambda e: e.matmul(psd[:, 0:1], lhsT=self.ones_bf[:], rhs=self.ones_bf[:, 0:1], start=True, stop=True)
            cst = self.sb(es, "cst_f", [128, 4 * 128])
            self.cst_bf = self.sb(es, "cst_bf", [128, 4 * 128], BF16)
            P.dma(cst[:], self.consts[:, 0:512], [], ['cst_f'])
            P.op('dve', ['cst_f'], ['cst_bf'], lambda e: e.tensor_copy(out=self.cst_bf[:], in_=cst[:]))
            self.ident_bf = self.cst_bf[:, 0:128]
            self.perm_bf = self.cst_bf[:, 128:256]
            self.mask_f = cst[:, 256:384]
            self.mask_b = cst[:, 384:512]
            self.ident32 = cst[:, 0:128]
            cst2 = self.sb(es, "cst2", [128, 384])
            P.dma(cst2[:], self.consts[:, 512 + 2 * SEQ:512 + 2 * SEQ + 384], [], ['cst_f'])
            self.mask_fs = cst2[:, 0:128]
            self.mask_bs = cst2[:, 128:256]
            self.mask_bd = cst2[:, 256:384]
            P.op('pool', [], ['ones_bf'], lambda e: e.memset(self.ones_bf[:], 1.0))
            self.bd_bf = self.sb(es, "bd_bf", [128, 128], BF16)
            P.op('pool', [], ['bd_bf'], lambda e: e.memset(self.bd_bf[:], 0.0))
            P.op('pool', ['bd_bf'], ['bd_bf'], lambda e: e.memset(self.bd_bf[0:64, 0:64], 1.0))
            P.op('pool', ['bd_bf'], ['bd_bf'], lambda e: e.memset(self.bd_bf[64:128, 64:128], 1.0))
            self.eps_t = self.sb(es, "eps_t", [128, 1])
            P.op('pool', [], ['eps_t'], lambda e: e.memset(self.eps_t[:], EPS))
            for l in range(DEPTH):
                P.dma(self.vecs[l][:], self.vecs_d[l], [], [f'vecs{l}'])

            with nc.named_scope('prologue'):
                self.prologue()
            for l in self.layers:
                self.layer(l)
                if self.upto != 'all':
                    break
            P.finish()
        return nc

    def dbg(self, name, src_ap_fn, shape, dt=F32, reads=()):
        o = self.dout("dbg_" + name, shape, dt)
        self.dbg_outs[name] = o
        src_ap_fn(o)

    def prologue(self):
        nc, P = self.nc, self.P
        with contextlib.ExitStack() as st:
            sc = self.sb(st, "sc", [128, 32])
            sc2 = self.sb(st, "sc2", [128, 32])
            awb = [self.sb(st, f"awb{i}", [128, 16, 512]) for i in range(2)]
            P.dma(sc[:], self.cT, [], ['sc'])
            P.op('act', ['sc'], ['sc2'], lambda e: e.activation(out=sc2[:], in_=sc[:], func=AF.Silu))
            for l in range(DEPTH):
                aw = self.ada_w[l].rearrange("(kc p) c -> p kc c", p=128)
                adab = self.vec(l, 'adab')
                for g in range(24):
                    wt = awb[g % 2]
                    wk = f'awb{g % 2}'
                    P.dma(wt[:], aw[:, :, g * 512:(g + 1) * 512], [], [wk], q=('sp' if g % 2 == 0 else 'act'))
                    pst = self.ps[g % 2]
                    pk = f'ps{g % 2}'
                    for f in range(4):
                        for kc in range(KC):
                            P.op('pe', [wk, 'sc2'], [pk], lambda e, f=f, kc=kc: e.matmul(
                                pst[:, f * 2:(f + 1) * 2], lhsT=wt[:, kc, f * 128:(f + 1) * 128],
                                rhs=sc2[:, kc * 2:(kc + 1) * 2], start=(kc == 0), stop=(kc == KC - 1)))
                    P.op('dve', [pk, f'vecs{l}'], [f'mod{l}'], lambda e, g=g: e.tensor_tensor(
                        out=self.mod[l][:, g * 4:(g + 1) * 4, :],
                        in0=pst[:, 0:8].rearrange("p (f j) -> p f j", j=2),
                        in1=adab[:, g * 4:(g + 1) * 4].unsqueeze(2).to_broadcast([128, 4, 2]), op=ALU.add))
                for (gm, nm, sco, key) in ((self.gm1[l], 'n1g', 16, f'gm1_{l}'), (self.gm2[l], 'n2g', 64, f'gm2_{l}')):
                    P.op('dve', [f'mod{l}'], [key], lambda e, gm=gm, sco=sco: e.tensor_scalar(
                        out=gm[:], in0=self.mod[l][:, sco:sco + 16, :], scalar1=1.0, scalar2=None, op0=ALU.add))
                    P.op('dve', [key, f'vecs{l}'], [key], lambda e, gm=gm, nm=nm: e.tensor_tensor(
                        out=gm[:], in0=gm[:], in1=self.vec(l, nm).unsqueeze(2).to_broadcast([128, 16, 2]), op=ALU.mult))
            if 'mod' in self.debug:
                for l in range(DEPTH):
                    self.dbg(f'mod{l}', lambda o, l=l: P.dma(o, self.mod[l][:].rearrange("p a b -> p (a b)"), [f'mod{l}'], []), [128, 192])
            P.barrier()

    def norm_tile(self, st_bufs, x, xk, n, j, gm, gmk, shmod, shoff, modk, out_fn, outk, ps_i=6):
        P = self.P
        sq, rt = st_bufs
        pst = self.ps[ps_i]
        pk = f'ps{ps_i}'
        P.op('act', [xk], ['nsq'], lambda e: e.activation(out=sq[:, :, :n], in_=x, func=AF.Square))
        for kc in range(KC):
            P.op('pe', ['nsq', 'ones_bf'], [pk], lambda e, kc=kc: e.matmul(
                pst[:, :n], lhsT=self.ones_bf[:], rhs=sq[:, kc, :n], start=(kc == 0), stop=(kc == KC - 1)))
        P.op('act', [pk], ['nrt'], lambda e: e.activation(out=rt[:, :n], in_=pst[:, :n], func=AF.Sqrt,
                                                         scale=1.0 / D, bias=self.eps_t[:, 0:1]))
        P.op('dve', ['nrt'], ['nrt'], lambda e: e.reciprocal(out=rt[:, :n], in_=rt[:, :n]))
        P.op('dve', [xk, 'nrt'], [xk], lambda e: e.tensor_tensor(
            out=x, in0=x, in1=rt[:, :n].unsqueeze(1).to_broadcast([128, KC, n]), op=ALU.mult))
        for kc in range(KC):
            P.op('act', [xk, gmk, modk], [outk], lambda e, kc=kc: e.activation(
                out=out_fn(kc), in_=x[:, kc, :], func=AF.Identity,
                scale=gm[:, kc, j:j + 1], bias=shmod[:, shoff + kc, j:j + 1]))

    def layer(self, l):
        nc, P = self.nc, self.P
        src = self.xT0 if l == 0 else self.xT
        with contextlib.ExitStack() as st:
            hT = self.sb(st, "hT", [128, KC, TT], BF16)
            with contextlib.ExitStack() as st2:
                xb = [self.sb(st2, f"xb{i}", [128, KC, 512]) for i in range(2)]
                sq = self.sb(st2, "nsq", [128, KC, 512], BF16)
                rt = self.sb(st2, "nrt", [128, 512])
                for ti, (t0, n) in enumerate(TILES):
                    x = xb[ti % 2]
                    xk = f'xb{ti % 2}'
                    j = 1 if t0 < NCTX else 0
                    P.dma(x[:, :, :n], src.rearrange("(kc p) t -> p kc t", p=128)[:, :, t0:t0 + n], ['xT'], [xk])
                    self.norm_tile((sq, rt), x[:, :, :n], xk, n, j, self.gm1[l], f'gm1_{l}', self.mod[l], 0, f'mod{l}',
                                   lambda kc, t0=t0, n=n: hT[:, kc, t0:t0 + n], 'hT')
                if 'hT' in self.debug:
                    self.dbg(f'hT{l}', lambda o: P.dma(o.rearrange("(kc p) t -> p kc t", p=128), hT[:], ['hT'], []), [D, TT], BF16)
                P.barrier()
            if self.upto == 'norm':
                return
            with nc.named_scope(f'L{l}_inproj'):
                self.inproj(l, hT)
            P.barrier()
        if self.upto == 'inproj':
            return
        with nc.named_scope(f'L{l}_na'):
            self.na_mixer(l)
        P.barrier()
        if self.upto == 'na':
            return
        with nc.named_scope(f'L{l}_gla'):
            self.gla_mixer(l)
        P.barrier()
        if self.upto == 'gla':
            return
        with nc.named_scope(f'L{l}_rwpre'):
            self.rwkv_pre(l)
        P.barrier()
        if self.upto == 'rwpre':
            self.dbg(f'ld{l}', lambda o: P.dma(o, self.ldT, ['ldT'], []), [2, 1024, TT])
            self.dbg(f'a{l}', lambda o: P.dma(o, self.aT, ['aT'], []), [2, 1024, TT])
            self.dbg(f'g2{l}', lambda o: P.dma(o, self.g2T, ['g2T'], []), [1024, TT], BF16)
            return
        with nc.named_scope(f'L{l}_rwkv'):
            self.rwkv_mixer(l)
        P.halt = False
        P.barrier()
        if self.upto == 'rwkv':
            return
        with nc.named_scope(f'L{l}_mergemoe'):
            self.merge_moe(l)
        P.barrier()

    def inproj(self, l, hT):
        nc, P = self.nc, self.P
        w = self.w_in[l].rearrange("(kc p) c -> p kc c", p=128)
        with contextlib.ExitStack() as st:
            wsl = [self.sb(st, f"wsl{i}", [128, KC, 512], BF16) for i in range(3)]
            zst = [self.sb(st, f"zst{i}", [128, 512]) for i in range(4)]
            gst = [self.sb(st, f"gst{i}", [128, 512], BF16) for i in range(4)]
            segs = [(0, 2048), (C_GQ, C_GV), (C_GR, C_GGD), (C_GGD, C_GGD + 16), (C_GGD + 16, C_RW),
                    (C_RR, C_RWD), (C_RWD, C_RWD + 96), (C_RWD + 96, C_RAD), (C_RAD, C_RAD + 96), (C_RAD + 96, C_RGD),
                    (C_RGD, C_GATE), (C_GATE, D_IN)]
            nload = 0
            nev = 0
            npsum = 0
            for (s0, s1) in segs:
                for b0 in range(s0, s1, 512):
                    bw = min(512, s1 - b0)
                    si = nload % 3
                    nload += 1
                    wk = f'wsl{si}'
                    P.dma(wsl[si][:, :, :bw], w[:, :, b0:b0 + bw], [], [wk], q='pool')
                    for c0 in range(b0, b0 + bw, 128):
                        m = min(128, b0 + bw - c0)
                        for (t0, n) in TILES:
                            pi = npsum % 4
                            npsum += 1
                            pst = self.ps[pi]
                            pk = f'ps{pi}'
                            for kc in range(KC):
                                P.op('pe', [wk, 'hT'], [pk], lambda e, kc=kc, c0=c0, m=m, t0=t0, n=n, si=si, pst=pst: e.matmul(
                                    pst[:m, :n], lhsT=wsl[si][:, kc, c0 - b0:c0 - b0 + m], rhs=hT[:, kc, t0:t0 + n],
                                    start=(kc == 0), stop=(kc == KC - 1)))
                            ei = nev % 4
                            eng = 'act' if nev % 2 == 0 else 'dve'
                            nev += 1
                            if c0 >= C_GATE:
                                P.op('act', [pk], [f'gst{ei}'], lambda e, m=m, n=n, ei=ei, pst=pst: e.activation(
                                    out=gst[ei][:m, :n], in_=pst[:m, :n], func=AF.Sigmoid))
                                P.dma(self.gT[c0 - C_GATE:c0 - C_GATE + m, t0:t0 + n], gst[ei][:m, :n], [f'gst{ei}'], ['gT'])
                            else:
                                if eng == 'act':
                                    P.op('act', [pk], [f'zst{ei}'], lambda e, m=m, n=n, ei=ei, pst=pst: e.activation(
                                        out=zst[ei][:m, :n], in_=pst[:m, :n], func=AF.Copy))
                                else:
                                    P.op('dve', [pk], [f'zst{ei}'], lambda e, m=m, n=n, ei=ei, pst=pst: e.tensor_copy(
                                        out=zst[ei][:m, :n], in_=pst[:m, :n]))
                                P.dma(self.zT[c0:c0 + m, t0:t0 + n], zst[ei][:m, :n], [f'zst{ei}'], ['zT'])
            for (s0, vo) in ((C_NAV, 0), (C_GV, 1024)):
                for b0 in range(0, 1024, 512):
                    si = nload % 3
                    nload += 1
                    wk = f'wsl{si}'
                    P.dma(wsl[si][:, :, :], w[:, :, s0 + b0:s0 + b0 + 512], [], [wk], q='pool')
                    for tt in range(TT // 128):
                        pi = npsum % 4
                        npsum += 1
                        pst = self.ps[pi]
                        pk = f'ps{pi}'
                        for kc in range(KC):
                            P.op('pe', [wk, 'hT'], [pk], lambda e, kc=kc, tt=tt, si=si, pst=pst: e.matmul(
                                pst[:, :], lhsT=hT[:, kc, tt * 128:(tt + 1) * 128], rhs=wsl[si][:, kc, :],
                                start=(kc == 0), stop=(kc == KC - 1)))
                        ei = nev % 4
                        eng = 'act' if nev % 2 == 0 else 'dve'
                        nev += 1
                        if eng == 'act':
                            P.op('act', [pk], [f'gst{ei}'], lambda e, ei=ei, pst=pst: e.activation(
                                out=gst[ei][:, :], in_=pst[:, :], func=AF.Copy))
                        else:
                            P.op('dve', [pk], [f'gst{ei}'], lambda e, ei=ei, pst=pst: e.tensor_copy(
                                out=gst[ei][:, :], in_=pst[:, :]))
                        P.dma(self.vtm[tt * 128:(tt + 1) * 128, vo + b0:vo + b0 + 512], gst[ei][:, :], [f'gst{ei}'], ['vtm'])
            if 'z' in self.debug:
                self.dbg(f'z{l}', lambda o: P.dma(o, self.zT, ['zT'], []), [C_GATE, TT])
                self.dbg(f'g{l}', lambda o: P.dma(o, self.gT, ['gT'], []), [3 * D, TT], BF16)
                self.dbg(f'vtm{l}', lambda o: P.dma(o, self.vtm, ['vtm'], []), [TT, 2048], BF16)


    def na_mixer(self, l):
        nc, P = self.nc, self.P
        with_ctx = l < DEPTH - 1
        vt_all = self.vtm.rearrange("(tt p) c -> p tt c", p=128)
        vt_odd = self.vtm[64:64 + 17 * 128, :].rearrange("(tt p) c -> p tt c", p=128)
        with contextlib.ExitStack() as st:
            zq = [self.sb(st, f"naz{i}", [128, TT]) for i in range(2)]
            sqb = self.sb(st, "nasq", [128, 512], BF16)
            rtb = self.sb(st, "nart", [128, 512])
            qk = [self.sb(st, "qn", [128, TT], BF16), self.sb(st, "kn", [128, TT], BF16)]
            vte = self.sb(st, "vte", [128, 18, 128], BF16)
            vto = self.sb(st, "vto", [128, 17, 128], BF16)
            tbl = [self.sb(st, f"natbl{i}", [128, 14, 64]) for i in range(2)]
            sT = [self.sb(st, f"sT{i}", [128, 4, 64]) for i in range(2)]
            pT = [self.sb(st, f"pT{i}", [128, 6, 64], BF16) for i in range(2)]
            pTc = self.sb(st, "pTc", [128, 2, 256], BF16)
            rsb = [self.sb(st, f"nars{i}", [128, 256]) for i in range(2)]
            yna = [self.sb(st, f"yna{i}", [128, TT], BF16) for i in range(2)]
            g8 = self.sb(st, "g8", [128, 2])
            qm = [self.sb(st, f"qm{i}", [128, TT], BF16) for i in range(2)]
            P.op('dve', [f'vecs{l}'], ['g8'], lambda e: e.tensor_scalar(
                out=g8[:, 0:1], in0=self.vec(l, 'naq'), scalar1=0.125, scalar2=None, op0=ALU.mult))
            P.op('dve', [f'vecs{l}'], ['g8'], lambda e: e.tensor_copy(out=g8[:, 1:2], in_=self.vec(l, 'nak')))
            it = 0
            for hp in range(8):
                for w_, c0 in ((0, C_NAQ), (1, C_NAK)):
                    z = zq[w_]
                    zk = f'naz{w_}'
                    P.dma(z[:], self.zT[c0 + hp * 128:c0 + (hp + 1) * 128, :], ['zT'], [zk])
                    dst = qk[w_]
                    dk = 'qn' if w_ == 0 else 'kn'
                    for (t0, n) in TILES:
                        P.op('act', [zk], ['nasq'], lambda e, z=z, t0=t0, n=n: e.activation(
                            out=sqb[:, :n], in_=z[:, t0:t0 + n], func=AF.Square))
                        P.op('pe', ['nasq', 'bd_bf'], ['ps4'], lambda e, n=n: e.matmul(
                            self.ps[4][:, :n], lhsT=self.bd_bf[:], rhs=sqb[:, :n], start=True, stop=True))
                        P.op('act', ['ps4'], ['nart'], lambda e, n=n: e.activation(
                            out=rtb[:, :n], in_=self.ps[4][:, :n], func=AF.Sqrt, scale=1.0 / 64, bias=self.eps_t[:, 0:1]))
                        P.op('dve', ['nart'], ['nart'], lambda e, n=n: e.reciprocal(out=rtb[:, :n], in_=rtb[:, :n]))
                        P.op('dve', [zk, 'nart', 'g8'], [dk], lambda e, z=z, t0=t0, n=n, dst=dst, w_=w_: e.scalar_tensor_tensor(
                            out=dst[:, t0:t0 + n], in0=z[:, t0:t0 + n], scalar=g8[:, w_:w_ + 1], in1=rtb[:, :n],
                            op0=ALU.mult, op1=ALU.mult))
                for e_ in range(2):
                    P.op('pool', [], [f'qm{e_}'], lambda e, e_=e_: e.memset(qm[e_][:], 0.0))
                    pr = slice(e_ * 64, (e_ + 1) * 64)
                    P.op('act', ['qn', f'qm{e_}'], [f'qm{e_}'], lambda e, e_=e_, pr=pr: e.activation(out=qm[e_][pr, :], in_=qk[0][pr, :], func=AF.Copy))
                P.dma(vte[:], vt_all[:, :, hp * 128:(hp + 1) * 128], ['vtm'], ['vte'])
                P.dma(vto[:], vt_odd[:, :, hp * 128:(hp + 1) * 128], ['vtm'], ['vto'])
                qn, kn = qk
                y = yna[hp % 2]
                yk = f'yna{hp % 2}'
                stages = []
                for e_ in range(2):
                    h = 2 * hp + e_
                    tb = tbl[h % 2]
                    tk = f'natbl{h % 2}'
                    pr = slice(e_ * 64, (e_ + 1) * 64)
                    for r in range(32):
                        rs = min(max(r - 4, 0), 24)
                        dlt = rs - r
                        tq = NCTX + r * 64
                        b = it % 2
                        it += 1
                        psS, psSk = self.ps[b], f'ps{b}'
                        psO, psOk = self.ps[2 + b], f'ps{2 + b}'

                        def s1(e_=e_, h=h, tb=tb, tk=tk, pr=pr, r=r, rs=rs, dlt=dlt, tq=tq, b=b, psS=psS, psSk=psSk):
                            if r == 0:
                                P.dma(tb[:].rearrange("p a b -> p (a b)"), self.natab[l, h], [], [tk])
                            for j in range(6):
                                kt = NCTX + (rs + 2 * j) * 64 if j < 4 else (j - 4) * 128
                                P.op('pe', [f'qm{e_}', 'kn'], [psSk], lambda e, j=j, kt=kt: e.matmul(
                                    psS[:, j * 64:(j + 1) * 64], lhsT=kn[:, kt:kt + 128], rhs=qm[e_][:, tq:tq + 64],
                                    start=True, stop=True))
                            d0 = dlt + 7
                            tv = tb[:].rearrange("p (u two) c -> p u two c", two=2)[:, d0 // 2:d0 // 2 + 4, d0 % 2, :]
                            P.op('dve', [psSk, tk], [f'sT{b}'], lambda e: e.tensor_tensor(
                                out=sT[b][:], in0=psS[:, 0:256].rearrange("p (j c) -> p j c", c=64), in1=tv, op=ALU.add))
                            P.op('act', [f'sT{b}'], [f'pT{b}'], lambda e: e.activation(
                                out=pT[b][:, 0:4, :], in_=sT[b][:], func=AF.Exp))
                            P.op('act', [psSk], [f'pT{b}'], lambda e: e.activation(
                                out=pT[b][:, 4:6, :], in_=psS[:, 256:384].rearrange("p (j c) -> p j c", c=64), func=AF.Exp))

                        def s2(pr=pr, rs=rs, tq=tq, b=b, psO=psO, psOk=psOk):
                            for part in range(2):
                                for j in range(6):
                                    if part == 1:
                                        lhs = self.ones_bf[:, :]
                                        rk_ = 'ones_bf'
                                    elif j >= 4:
                                        lhs = vte[:, j - 4, :]
                                        rk_ = 'vte'
                                    elif rs % 2 == 0:
                                        lhs = vte[:, 2 + rs // 2 + j, :]
                                        rk_ = 'vte'
                                    else:
                                        lhs = vto[:, (rs + 1) // 2 + 1 + j, :]
                                        rk_ = 'vto'
                                    P.op('pe', [rk_, f'pT{b}'], [psOk], lambda e, lhs=lhs, j=j, part=part: e.matmul(
                                        psO[:, part * 64:(part + 1) * 64], lhsT=lhs, rhs=pT[b][:, j, :],
                                        start=(j == 0), stop=(j == 5)))
                            P.op('dve', [psOk], [f'nars{b}'], lambda e: e.reciprocal(
                                out=rsb[b][pr, 0:64], in_=psO[pr, 64:128]))
                            P.op('dve', [psOk, f'nars{b}'], [yk], lambda e: e.tensor_tensor(
                                out=y[pr, tq:tq + 64], in0=psO[pr, 0:64], in1=rsb[b][pr, 0:64], op=ALU.mult))
                        stages.append((s1, s2))
                    if with_ctx:
                        b = it % 2
                        it += 1
                        psS, psSk = self.ps[b], f'ps{b}'
                        psO, psOk = self.ps[2 + b], f'ps{2 + b}'

                        def s1(e_=e_, pr=pr, psS=psS, psSk=psSk):
                            for j in range(2):
                                P.op('pe', [f'qm{e_}', 'kn'], [psSk], lambda e, j=j: e.matmul(
                                    psS[:, j * 256:(j + 1) * 256], lhsT=kn[:, j * 128:(j + 1) * 128], rhs=qm[e_][:, 0:256],
                                    start=True, stop=True))
                            P.op('act', [psSk], ['pTc'], lambda e: e.activation(
                                out=pTc[:].rearrange("p j c -> p (j c)"), in_=psS[:, :], func=AF.Exp))

                        def s2(pr=pr, b=b, psO=psO, psOk=psOk):
                            for part in range(2):
                                for j in range(2):
                                    lhs = self.ones_bf[:, :] if part == 1 else vte[:, j, :]
                                    P.op('pe', ['vte', 'ones_bf', 'pTc'], [psOk], lambda e, lhs=lhs, j=j, part=part: e.matmul(
                                        psO[:, part * 256:(part + 1) * 256], lhsT=lhs, rhs=pTc[:, j, :],
                                        start=(j == 0), stop=(j == 1)))
                            P.op('dve', [psOk], [f'nars{b}'], lambda e: e.reciprocal(
                                out=rsb[b][pr, :], in_=psO[pr, 256:512]))
                            P.op('dve', [psOk, f'nars{b}'], [yk], lambda e: e.tensor_tensor(
                                out=y[pr, 0:256], in0=psO[pr, 0:256], in1=rsb[b][pr, :], op=ALU.mult))
                        stages.append((s1, s2))
                for k_ in range(len(stages) + 1):
                    if k_ < len(stages):
                        stages[k_][0]()
                    if k_ >= 1:
                        stages[k_ - 1][1]()
                t_lo = 0 if with_ctx else NCTX
                P.dma(self.yT[0, hp * 128:(hp + 1) * 128, t_lo:], y[:, t_lo:], [yk], ['yT'])
            if 'na' in self.debug:
                self.dbg(f'yna{l}', lambda o: P.dma(o, self.yT[0], ['yT'], []), [1024, TT], BF16)


    def gla_mixer(self, l):
        nc, P = self.nc, self.P
        with_ctx = l < DEPTH - 1
        NCH = TT // 64
        NT = TT // 128
        qscale = 128 ** -0.5
        vt_all = self.vtm.rearrange("(tt p) c -> p tt c", p=128)
        with contextlib.ExitStack() as st:
            cos = self.sb(st, "cos", [128, SEQ])
            sin = self.sb(st, "sin", [128, SEQ])
            msk = self.sb(st, "cmsk", [128, TT])
            qk32 = [self.sb(st, "gq32", [128, TT]), self.sb(st, "gk32", [128, TT])]
            zb = self.sb(st, "gzb", [128, 512], BF16)
            rz = self.sb(st, "grz", [128, 2, TT])
            gd = [self.sb(st, f"ggd{d}", [16, TT]) for d in range(2)]
            gw2 = self.sb(st, "gw2", [16, 2, 512])
            nb = self.sb(st, "gnb", [128, 8])
            T1 = self.sb(st, "gT1", [128, TT]); T2 = self.sb(st, "gT2", [128, TT]); T3 = self.sb(st, "gT3", [128, TT])
            qi = self.sb(st, "gqi", [128, TT], BF16); kj = self.sb(st, "gkj", [128, TT], BF16)
            kd = self.sb(st, "gkd", [128, TT], BF16); qb = self.sb(st, "gqb", [128, TT], BF16)
            dec = self.sb(st, "gdec", [128, NCH])
            Vt = self.sb(st, "gVt", [128, NT, 256], BF16)
            kdT = self.sb(st, "gkdT", [128, NT, 128], BF16)
            Abf = [self.sb(st, f"gA{i}", [128, 128], BF16) for i in range(2)]
            S32 = self.sb(st, "gS32", [128, 256])
            Sbf = [self.sb(st, f"gSbf{i}", [128, 256], BF16) for i in range(2)]
            yf = self.sb(st, "gyf", [128, 2, TT], BF16)
            yo = self.sb(st, "gyo", [128, 2, TT], BF16)
            yt = [self.sb(st, f"gyt{i}", [128, 2, 128]) for i in range(2)]
            ysq = self.sb(st, "gysq", [128, 2, 128], BF16)
            yrt = self.sb(st, "gyrt", [128, 128])
            P.dma(cos[:], self.consts[:, 512:512 + SEQ], [], ['cos'])
            P.dma(sin[:], self.consts[:, 512 + SEQ:512 + 2 * SEQ], [], ['sin'])
            P.op('pool', [], ['cmsk'], lambda e: e.memset(msk[:], 1.0))
            P.op('pool', ['cmsk'], ['cmsk'], lambda e: e.memset(msk[:].rearrange("p (c j) -> p c j", j=64)[:, :, 0:1], 0.0))
            for d in range(2):
                P.dma(gd[d][:], self.zT[C_GGD + 16 * d:C_GGD + 16 * (d + 1), :], ['zT'], [f'ggd{d}'])
            P.dma(gw2[:], self.gla_w2[l].rearrange("d k c -> k d c"), [], ['gw2'])
            P.op('dve', [f'vecs{l}'], ['gnb'], lambda e: e.tensor_scalar(
                out=nb[:], in0=self.vec(l, 'ggb'), scalar1=-1.0, scalar2=None, op0=ALU.mult))
            gng = self.vec(l, 'gng')
            c3 = lambda t: t[:].rearrange("p (c j) -> p c j", j=64)
            sbi = 0
            for h in range(4):
                for w_, c0 in ((0, C_GQ), (1, C_GK)):
                    z = qk32[w_]
                    zk = 'gq32' if w_ == 0 else 'gk32'
                    P.dma(z[:], self.zT[c0 + h * 128:c0 + (h + 1) * 128, :], ['zT'], [zk])
                    for ti in range(4):
                        t0 = NCTX + ti * 512
                        P.op('act', [zk], ['gzb'], lambda e, z=z, t0=t0: e.activation(out=zb[:], in_=z[:, t0:t0 + 512], func=AF.Copy))
                        P.op('pe', ['gzb', 'cst_bf'], ['ps4'], lambda e: e.matmul(
                            self.ps[4][:, :], lhsT=self.perm_bf, rhs=zb[:], start=True, stop=True))
                        P.op('dve', ['ps4', 'sin'], ['gT3'], lambda e, ti=ti: e.tensor_tensor(
                            out=T3[:, 0:512], in0=self.ps[4][:, :], in1=sin[:, ti * 512:(ti + 1) * 512], op=ALU.mult))
                        P.op('dve', [zk, 'cos'], [zk], lambda e, z=z, t0=t0, ti=ti: e.tensor_tensor(
                            out=z[:, t0:t0 + 512], in0=z[:, t0:t0 + 512], in1=cos[:, ti * 512:(ti + 1) * 512], op=ALU.mult))
                        P.op('dve', [zk, 'gT3'], [zk], lambda e, z=z, t0=t0: e.tensor_tensor(
                            out=z[:, t0:t0 + 512], in0=z[:, t0:t0 + 512], in1=T3[:, 0:512], op=ALU.add))
                q32, k32 = qk32
                P.dma(rz[:], self.zT[C_GR + h * 256:C_GR + (h + 1) * 256, :].rearrange("(c p) t -> p c t", p=128), ['zT'], ['grz'])
                P.op('act', ['grz'], ['grz'], lambda e: e.activation(out=rz[:], in_=rz[:], func=AF.Silu))
                P.dma(Vt[:], vt_all[:, :, 1024 + h * 256:1024 + (h + 1) * 256], ['vtm'], ['gVt'])
                for d in range(2):
                    for (t0, n) in TILES:
                        P.op('pe', ['gw2', f'ggd{d}'], ['ps4'], lambda e, t0=t0, n=n, d=d, h=h: e.matmul(
                            self.ps[4][:, :n], lhsT=gw2[:, d, h * 128:(h + 1) * 128], rhs=gd[d][:, t0:t0 + n], start=True, stop=True), rg=0)
                        P.op('act', ['ps4', 'gnb'], ['gT1'], lambda e, t0=t0, n=n, d=d, h=h: e.activation(
                            out=T1[:, t0:t0 + n], in_=self.ps[4][:, :n], func=AF.Exp, scale=-1.0, bias=nb[:, d * 4 + h:d * 4 + h + 1]))
                    P.op('act', ['gT1'], ['gT1'], lambda e: e.activation(out=T1[:], in_=T1[:], func=AF.Ln, bias=1.0, scale=1.0))
                    P.op('dve', ['cmsk', 'gT1'], ['gT2'], lambda e: e.tensor_tensor_scan(
                        out=T2[:], data0=msk[:], data1=T1[:], initial=0.0, op0=ALU.mult, op1=ALU.add))
                    if d == 1:
                        P.op('dve', ['gT2'], ['gT3'], lambda e: e.tensor_tensor(
                            out=c3(T3), in0=c3(T2)[:, :, 63:64].to_broadcast([128, NCH, 64]), in1=c3(T2), op=ALU.subtract))
                        P.op('dve', ['gT3', 'gT1'], ['gT2'], lambda e: e.tensor_tensor(out=T2[:], in0=T3[:], in1=T1[:], op=ALU.add))
                    tot = c3(T2)[:, :, 63:64] if d == 0 else c3(T2)[:, :, 0:1]
                    cref = c3(T2)[:, :, 32:33] if d == 0 else c3(T2)[:, :, 31:32]
                    P.op('act', ['gT2'], ['gdec'], lambda e, tot=tot: e.activation(
                        out=dec[:].unsqueeze(2), in_=tot, func=AF.Exp, scale=-1.0 / 16))
                    P.op('dve', ['gT2'], ['gT3'], lambda e, cref=cref: e.tensor_tensor(
                        out=c3(T3), in0=c3(T2), in1=cref.to_broadcast([128, NCH, 64]), op=ALU.subtract))
                    P.op('act', ['gT3'], ['gqi'], lambda e: e.activation(out=qi[:], in_=T3[:], func=AF.Exp, scale=-1.0 / 16))
                    P.op('act', ['gT3'], ['gkj'], lambda e: e.activation(out=kj[:], in_=T3[:], func=AF.Exp, scale=1.0 / 16))
                    P.op('act', ['gT2'], ['gqb'], lambda e: e.activation(out=qb[:], in_=T2[:], func=AF.Exp, scale=-1.0 / 16))
                    P.op('dve', ['gT2'], ['gT1'], lambda e, tot=tot: e.tensor_tensor(
                        out=c3(T1), in0=tot.to_broadcast([128, NCH, 64]), in1=c3(T2), op=ALU.subtract))
                    P.op('act', ['gT1'], ['gkd'], lambda e: e.activation(out=kd[:], in_=T1[:], func=AF.Exp, scale=-1.0 / 16))
                    P.op('dve', ['gq32', 'gqi'], ['gqi'], lambda e: e.scalar_tensor_tensor(
                        out=qi[:], in0=qi[:], scalar=qscale, in1=q32[:], op0=ALU.mult, op1=ALU.mult))
                    P.op('dve', ['gk32', 'gkj'], ['gkj'], lambda e: e.tensor_tensor(out=kj[:], in0=kj[:], in1=k32[:], op=ALU.mult))
                    P.op('dve', ['gq32', 'gqb'], ['gqb'], lambda e: e.scalar_tensor_tensor(
                        out=qb[:], in0=qb[:], scalar=qscale, in1=q32[:], op0=ALU.mult, op1=ALU.mult))
                    P.op('dve', ['gk32', 'gkd'], ['gkd'], lambda e: e.tensor_tensor(out=kd[:], in0=kd[:], in1=k32[:], op=ALU.mult))
                    for g0 in range(0, NT, 4):
                        g1 = min(NT, g0 + 4)
                        for tt in range(g0, g1):
                            P.op('pe', ['gkd', 'cst_bf'], ['psb'], lambda e, tt=tt, g0=g0: e.transpose(
                                self.psb[:, (tt - g0) * 128:(tt - g0 + 1) * 128], kd[:, tt * 128:(tt + 1) * 128], self.ident_bf))
                        P.op('act', ['psb'], ['gkdT'], lambda e, g0=g0, g1=g1: e.activation(
                            out=kdT[:, g0:g1, :].rearrange("p a b -> p (a b)"), in_=self.psb[:, 0:(g1 - g0) * 128], func=AF.Copy))
                    P.op('dve', [], ['gS32'], lambda e: e.memset(S32[:], 0.0))
                    P.op('dve', [], [f'gSbf{sbi % 2}'], lambda e, sbi=sbi: e.memset(Sbf[sbi % 2][:], 0.0))
                    order = list(range(NT)) if d == 0 else [1, 0] + list(range(NT - 1, 1, -1))
                    maskd = self.mask_f if d == 0 else self.mask_b
                    for it_, tt in enumerate(order):
                        tsl = slice(tt * 128, (tt + 1) * 128)
                        ab = it_ % 2
                        P.op('pe', ['gkj', 'gqi'], ['ps5'], lambda e, tsl=tsl: e.matmul(
                            self.ps[5][:, 0:128], lhsT=kj[:, tsl], rhs=qi[:, tsl], start=True, stop=True))
                        P.op('dve', ['ps5', 'cst_f'], [f'gA{ab}'], lambda e, ab=ab, maskd=maskd: e.tensor_tensor(
                            out=Abf[ab][:], in0=self.ps[5][:, 0:128], in1=maskd, op=ALU.mult))
                        halves = (0, 1) if d == 0 else (1, 0)
                        psy = [self.ps[0 + 2 * (it_ % 2)], self.ps[1 + 2 * (it_ % 2)]]
                        psyk = [f'ps{0 + 2 * (it_ % 2)}', f'ps{1 + 2 * (it_ % 2)}']
                        for hi, hf in enumerate(halves):
                            csl = slice(hf * 64, (hf + 1) * 64)
                            tok = slice(tt * 128 + hf * 64, tt * 128 + (hf + 1) * 64)
                            sk = f'gSbf{sbi % 2}'
                            Sb = Sbf[sbi % 2]
                            for vc in range(2):
                                if hi == 0:
                                    P.op('pe', ['gVt', f'gA{ab}'], [psyk[vc]], lambda e, vc=vc, tt=tt, ab=ab, psy=psy: e.matmul(
                                        psy[vc][:, 0:128], lhsT=Vt[:, tt, vc * 128:(vc + 1) * 128], rhs=Abf[ab][:], start=True, stop=False))
                                P.op('pe', [sk, 'gqb'], [psyk[vc]], lambda e, vc=vc, Sb=Sb, csl=csl, tok=tok, hi=hi, psy=psy: e.matmul(
                                    psy[vc][:, csl], lhsT=Sb[:, vc * 128:(vc + 1) * 128], rhs=qb[:, tok], start=False, stop=(hi == 1)))
                            ch = tt * 2 + hf
                            P.op('pe', ['gkdT', 'gVt'], ['ps6'], lambda e, csl=csl, tt=tt: e.matmul(
                                self.ps[6][:, 0:256], lhsT=kdT[csl, tt, :], rhs=Vt[csl, tt, :], start=True, stop=True), rg=csl.start)
                            P.op('dve', ['ps6', 'gS32', 'gdec'], ['gS32'], lambda e, ch=ch: e.scalar_tensor_tensor(
                                out=S32[:], in0=S32[:], scalar=dec[:, ch:ch + 1], in1=self.ps[6][:, 0:256], op0=ALU.mult, op1=ALU.add))
                            sbi += 1
                            P.op('act', ['gS32'], [f'gSbf{sbi % 2}'], lambda e, sbi=sbi: e.activation(
                                out=Sbf[sbi % 2][:], in_=S32[:], func=AF.Copy))
                        if d == 0:
                            for vc in range(2):
                                P.op('act', [psyk[vc]], ['gyf'], lambda e, vc=vc, tsl=tsl, psy=psy: e.activation(
                                    out=yf[:, vc, tsl], in_=psy[vc][:, 0:128], func=AF.Copy))
                        elif with_ctx or tt >= 2:
                            ytb = yt[it_ % 2]
                            ytk = f'gyt{it_ % 2}'
                            for vc in range(2):
                                P.op('dve', [psyk[vc], 'gyf'], [ytk], lambda e, vc=vc, tsl=tsl, psy=psy, ytb=ytb: e.tensor_tensor(
                                    out=ytb[:, vc, :], in0=psy[vc][:, 0:128], in1=yf[:, vc, tsl], op=ALU.add))
                            P.op('act', [ytk], ['gysq'], lambda e, ytb=ytb: e.activation(out=ysq[:], in_=ytb[:], func=AF.Square))
                            for vc in range(2):
                                P.op('pe', ['gysq', 'ones_bf'], ['ps4'], lambda e, vc=vc: e.matmul(
                                    self.ps[4][:, 0:128], lhsT=self.ones_bf[:], rhs=ysq[:, vc, :], start=(vc == 0), stop=(vc == 1)))
                            P.op('act', ['ps4'], ['gyrt'], lambda e: e.activation(
                                out=yrt[:], in_=self.ps[4][:, 0:128], func=AF.Sqrt, scale=1.0 / 256, bias=self.eps_t[:, 0:1]))
                            P.op('dve', ['gyrt'], ['gyrt'], lambda e: e.reciprocal(out=yrt[:], in_=yrt[:]))
                            for vc in range(2):
                                P.op('dve', [ytk, 'gyrt', f'vecs{l}'], [ytk], lambda e, vc=vc, ytb=ytb: e.scalar_tensor_tensor(
                                    out=ytb[:, vc, :], in0=ytb[:, vc, :], scalar=gng[:, vc:vc + 1], in1=yrt[:], op0=ALU.mult, op1=ALU.mult))
                                P.op('dve', [ytk, 'grz'], ['gyo'], lambda e, vc=vc, ytb=ytb, tsl=tsl: e.tensor_tensor(
                                    out=yo[:, vc, tsl], in0=ytb[:, vc, :], in1=rz[:, vc, tsl], op=ALU.mult))
                t_lo = 0 if with_ctx else NCTX
                P.dma(self.yT[1, h * 256:(h + 1) * 256, t_lo:].rearrange("(c p) t -> p c t", p=128), yo[:, :, t_lo:], ['gyo'], ['yT'])
            if 'gla' in self.debug:
                self.dbg(f'ygla{l}', lambda o: P.dma(o, self.yT[1], ['yT'], []), [1024, TT], BF16)


    def _shift(self, z, zk, tmp, tmpk, om, hm, m):
        P = self.P
        P.op('dve', [zk], [tmpk], lambda e: e.tensor_tensor(out=tmp[:m, 1:TT - 1], in0=z[:m, 0:TT - 2], in1=z[:m, 2:TT], op=ALU.add))
        for (dst, src_) in ((0, 1), (NCTX - 1, NCTX - 2), (NCTX, NCTX + 1), (TT - 1, TT - 2)):
            P.op('dve', [zk, tmpk], [tmpk], lambda e, dst=dst, src_=src_: e.tensor_copy(out=tmp[:m, dst:dst + 1], in_=z[:m, src_:src_ + 1]))
        P.op('dve', [tmpk], [tmpk], lambda e: e.tensor_scalar(out=tmp[:m, :], in0=tmp[:m, :], scalar1=hm, scalar2=None, op0=ALU.mult))
        P.op('dve', [zk, tmpk], [zk], lambda e: e.scalar_tensor_tensor(out=z[:m, :], in0=z[:m, :], scalar=om, in1=tmp[:m, :], op0=ALU.mult, op1=ALU.add))

    def _mu_prep(self, st, l):
        P = self.P
        o0, _ = VEC_SLOTS['mu_r']
        mu = self.vecs[l][:, o0:o0 + 30]
        om = self.sb(st, "rw_om", [128, 30]); hm = self.sb(st, "rw_hm", [128, 30])
        P.op('dve', [f'vecs{l}'], ['rw_om'], lambda e: e.tensor_scalar(out=om[:], in0=mu, scalar1=-1.0, scalar2=1.0, op0=ALU.mult, op1=ALU.add))
        P.op('dve', [f'vecs{l}'], ['rw_hm'], lambda e: e.tensor_scalar(out=hm[:], in0=mu, scalar1=0.5, scalar2=None, op0=ALU.mult))
        return om, hm

    def rwkv_pre(self, l):
        nc, P = self.nc, self.P
        with contextlib.ExitStack() as st:
            om, hm = self._mu_prep(st, l)
            zt = self.sb(st, "rp_z", [128, TT]); tmp = self.sb(st, "rp_tmp", [128, TT])
            twd = [self.sb(st, f"rp_twd{d}", [96, TT], BF16) for d in range(2)]
            adb = [self.sb(st, f"rp_adb{d}", [96, TT], BF16) for d in range(2)]
            sgd = self.sb(st, "rp_sgd", [128, 2, TT], BF16)
            w2 = self.sb(st, "rp_w2", [96, 2, 1024], BF16); a2 = self.sb(st, "rp_a2", [96, 2, 1024], BF16)
            g2 = self.sb(st, "rp_g2", [128, 2, 1024], BF16)
            stg = [self.sb(st, f"rp_st{i}", [128, 512]) for i in range(4)]
            stb = [self.sb(st, f"rp_sb{i}", [128, 512], BF16) for i in range(2)]
            P.dma(w2[:], self.rw_w2[l].rearrange("d k c -> k d c"), [], ['rp_w2'], q='pool')
            P.dma(a2[:], self.rw_a2[l].rearrange("d k c -> k d c"), [], ['rp_a2'], q='pool')
            P.dma(g2[:], self.rw_g2[l].rearrange("(c p) n -> p c n", p=128), [], ['rp_g2'], q='pool')
            for d in range(2):
                P.dma(zt[:96, :], self.zT[C_RWD + 96 * d:C_RWD + 96 * (d + 1), :], ['zT'], ['rp_z'])
                self._shift(zt, 'rp_z', tmp, 'rp_tmp', om[:96, 24 + d:25 + d], hm[:96, 24 + d:25 + d], 96)
                P.op('act', ['rp_z'], [f'rp_twd{d}'], lambda e, d=d: e.activation(out=twd[d][:], in_=zt[:96, :], func=AF.Tanh))
                P.dma(zt[:96, :], self.zT[C_RAD + 96 * d:C_RAD + 96 * (d + 1), :], ['zT'], ['rp_z'])
                self._shift(zt, 'rp_z', tmp, 'rp_tmp', om[:96, 26 + d:27 + d], hm[:96, 26 + d:27 + d], 96)
                P.op('act', ['rp_z'], [f'rp_adb{d}'], lambda e, d=d: e.activation(out=adb[d][:], in_=zt[:96, :], func=AF.Copy))
            for c in range(2):
                P.dma(zt[:, :], self.zT[C_RGD + 128 * c:C_RGD + 128 * (c + 1), :], ['zT'], ['rp_z'])
                self._shift(zt, 'rp_z', tmp, 'rp_tmp', om[:, 28 + c:29 + c], hm[:, 28 + c:29 + c], 128)
                P.op('act', ['rp_z'], ['rp_sgd'], lambda e, c=c: e.activation(out=sgd[:, c, :], in_=zt[:, :], func=AF.Sigmoid))
            w0 = self.vec(l, 'w0'); a0 = self.vec(l, 'a0')
            n_ = 0
            for hp in range(8):
                cs = slice(hp * 128, (hp + 1) * 128)
                for (t0, n) in TILES:
                    for d in range(2):
                        for which in range(2):
                            pi = n_ % 4; n_ += 1
                            pst, pk = self.ps[pi], f'ps{pi}'
                            wm, src_, bias_ = ((w2, twd[d], w0), (a2, adb[d], a0))[which]
                            rk_ = [('rp_w2', f'rp_twd{d}'), ('rp_a2', f'rp_adb{d}')][which]
                            P.op('pe', list(rk_), [pk], lambda e, wm=wm, src_=src_, d=d, t0=t0, n=n, pst=pst, cs=cs: e.matmul(
                                pst[:, :n], lhsT=wm[:, d, cs], rhs=src_[:, t0:t0 + n], start=True, stop=True), rg=0)
                            sg = stg[pi]
                            P.op('act', [pk, f'vecs{l}'], [f'rp_st{pi}'], lambda e, pst=pst, sg=sg, n=n, bias_=bias_, d=d, hp=hp: e.activation(
                                out=sg[:, :n], in_=pst[:, :n], func=AF.Sigmoid, bias=bias_[:, d * 8 + hp:d * 8 + hp + 1], scale=1.0))
                            if which == 0:
                                P.op('dve', [f'rp_st{pi}'], [f'rp_st{pi}'], lambda e, sg=sg, n=n: e.tensor_scalar(
                                    out=sg[:, :n], in0=sg[:, :n], scalar1=-0.6065306597126334, scalar2=None, op0=ALU.mult))
                                P.dma(self.ldT[d, cs, t0:t0 + n], sg[:, :n], [f'rp_st{pi}'], ['ldT'])
                            else:
                                P.dma(self.aT[d, cs, t0:t0 + n], sg[:, :n], [f'rp_st{pi}'], ['aT'])
                    pi = n_ % 4; n_ += 1
                    pst, pk = self.ps[pi], f'ps{pi}'
                    for c in range(2):
                        P.op('pe', ['rp_g2', 'rp_sgd'], [pk], lambda e, c=c, t0=t0, n=n, pst=pst, cs=cs: e.matmul(
                            pst[:, :n], lhsT=g2[:, c, cs], rhs=sgd[:, c, t0:t0 + n], start=(c == 0), stop=(c == 1)))
                    bi = n_ % 2
                    P.op('act', [pk], [f'rp_sb{bi}'], lambda e, pst=pst, n=n, bi=bi: e.activation(out=stb[bi][:, :n], in_=pst[:, :n], func=AF.Copy))
                    P.dma(self.g2T[cs, t0:t0 + n], stb[bi][:, :n], [f'rp_sb{bi}'], ['g2T'])

    def rwkv_mixer(self, l):
        nc, P = self.nc, self.P
        with_ctx = l < DEPTH - 1
        NCH = TT // 64
        NT = TT // 128
        with contextlib.ExitStack() as st:
            om, hm = self._mu_prep(st, l)
            A = [self.sb(st, f"rwA{i}", [128, TT]) for i in range(7)]
            Ak = [f'rwA{i}' for i in range(7)]
            msk = self.sb(st, "rmsk", [128, TT])
            vb = self.sb(st, "rw_vb", [128, TT], BF16)
            bon = self.sb(st, "rw_bon", [128, TT], BF16)
            gsb = self.sb(st, "rw_g", [128, 512], BF16)
            al = self.sb(st, "rw_al", [128, TT], BF16); rho = self.sb(st, "rw_rho", [128, TT], BF16)
            be = self.sb(st, "rw_be", [128, TT], BF16); ka = self.sb(st, "rw_ka", [128, TT], BF16)
            Bp = self.sb(st, "rw_Bp", [128, TT], BF16); Kp = self.sb(st, "rw_Kp", [128, TT], BF16)
            al_tm = self.sb(st, "rw_altm", [128, NT, 128], BF16); Bp_tm = self.sb(st, "rw_Bptm", [128, NT, 128], BF16)
            Kp_tm = self.sb(st, "rw_Kptm", [128, NT, 128], BF16); V_tm = self.sb(st, "rw_Vtm", [128, NT, 128], BF16)
            etot = self.sb(st, "rw_etot", [128, NCH])
            slots = []
            s0 = dict(XN=[[self.sb(st, f"rw_X{i}", [128, 512], BF16), self.sb(st, f"rw_N{i}", [128, 512], BF16)] for i in range(2)],
                      Q=[self.sb(st, f"rw_Q{i}", [128, 512], BF16) for i in range(2)],
                      LkT=self.sb(st, "rw_LkT", [128, 512], BF16), MbT=self.sb(st, "rw_MbT", [128, 512], BF16), MkT=self.sb(st, "rw_MkT", [128, 512], BF16),
                      H=self.sb(st, "rw_H", [128, 256], BF16)[:], P1n=self.sb(st, "rw_P1n", [128, 256], BF16)[:], G=self.sb(st, "rw_G", [128, 256], BF16)[:],
                      ptmp=self.sb(st, "rw_ptmp", [128, 128])[:], banks=(self.ps[0], self.ps[1], self.ps[2]), bank_ids=(0, 1, 2))
            slots.append(s0)
            a2b = A[2][:].bitcast(BF16)
            cut = lambda i: a2b[:, i * 512:(i + 1) * 512]
            rt = self.sb(st, "rw_rt", [128, 512]); t5 = self.sb(st, "rw_t5", [128, 512])
            t5b = t5[:].bitcast(BF16)
            s1_ = dict(XN=[[cut(0), cut(1)], [cut(2), cut(3)]], Q=[cut(4), cut(5)], LkT=cut(6), MbT=cut(7), MkT=cut(8),
                       H=t5b[:, 0:256], P1n=t5b[:, 256:512], G=t5b[:, 512:768], ptmp=rt[:, 0:128],
                       banks=(self.ps[3], self.ps[5], self.ps[6]), bank_ids=(3, 5, 6))
            slots.append(s1_)
            for nm in ('rw_X0', 'rw_N0', 'rw_X1', 'rw_N1', 'rw_Q0', 'rw_Q1', 'rw_LkT', 'rw_MbT', 'rw_MkT'):
                P.set_parent(nm + '_s1', Ak[2])
            for nm in ('rw_H', 'rw_P1n', 'rw_G'):
                P.set_parent(nm + '_s1', 'rw_t5')
            P.set_parent('rw_ptmp_s1', 'rw_rt')
            a5b = A[5][:].bitcast(BF16); a6b = A[6][:].bitcast(BF16)
            al_m = [a5b[:, 0:TT], a5b[:, TT:2 * TT]]
            rho_m = [a6b[:, 0:TT], a6b[:, TT:2 * TT]]
            P.set_parent('rw_alm', Ak[5]); P.set_parent('rw_rhom', Ak[6])
            R32 = self.sb(st, "rw_R32", [128, TT]); yloc = self.sb(st, "rw_yloc", [128, TT]); yacc = self.sb(st, "rw_yacc", [128, TT])
            Phi = self.sb(st, "rw_Phi", [128, NCH, 128]); Dd = self.sb(st, "rw_D", [128, NCH, 128], BF16)
            Sbd = [self.sb(st, f"rw_S{i}", [128, 128]) for i in range(2)]
            ka1 = self.sb(st, "rw_ka1", [128, 8])
            sq5 = self.sb(st, "rw_sq5", [128, 512], BF16)
            yo = vb
            geps = self.sb(st, "rw_geps", [128, 1])
            P.op('pool', [], ['rw_geps'], lambda e: e.memset(geps[:], 64e-5))
            P.op('pool', [], ['rmsk'], lambda e: e.memset(msk[:], 1.0))
            P.op('pool', ['rmsk'], ['rmsk'], lambda e: e.memset(msk[:].rearrange("p (c j) -> p c j", j=64)[:, :, 0:1], 0.0))
            P.op('dve', [f'vecs{l}'], ['rw_ka1'], lambda e: e.tensor_scalar(
                out=ka1[:], in0=self.vec(l, 'ka'), scalar1=-1.0, scalar2=1.0, op0=ALU.mult, op1=ALU.add))
            c3 = lambda t: t[:].rearrange("p (c j) -> p c j", j=64)
            kkv = self.vec(l, 'kk'); kav = self.vec(l, 'ka'); rkv = self.vec(l, 'rk')
            gng = self.vec(l, 'gng_rw'); gnb = self.vec(l, 'gnb_rw')
            si = 0
            for hp in range(8):
                cs = slice(hp * 128, (hp + 1) * 128)
                self.sc_in('rw_load', hp == 0 and l == 0)
                r32, k32, v32, kk32 = A[0], A[1], A[2], A[3]
                for i_, c0 in enumerate((C_RR, C_RK, C_RV)):
                    P.dma(A[i_][:], self.zT[c0 + hp * 128:c0 + (hp + 1) * 128, :], ['zT'], [Ak[i_]])
                    ci = i_ * 8 + hp
                    self._shift(A[i_], Ak[i_], A[6], Ak[6], om[:, ci:ci + 1], hm[:, ci:ci + 1], 128)
                P.op('act', [Ak[2]], ['rw_vb'], lambda e: e.activation(out=vb[:], in_=v32[:], func=AF.Copy))
                for (srcb, srck, dst, dstk) in ((vb, 'rw_vb', V_tm, 'rw_Vtm'),):
                    for g0 in range(0, NT, 4):
                        g1 = min(NT, g0 + 4)
                        for tt in range(g0, g1):
                            P.op('pe', [srck, 'cst_bf'], ['psb'], lambda e, tt=tt, g0=g0, srcb=srcb: e.transpose(
                                self.psb[:, (tt - g0) * 128:(tt - g0 + 1) * 128], srcb[:, tt * 128:(tt + 1) * 128], self.ident_bf))
                        P.op('act', ['psb'], [dstk], lambda e, g0=g0, g1=g1, dst=dst: e.activation(
                            out=dst[:, g0:g1, :].rearrange("p a b -> p (a b)"), in_=self.psb[:, 0:(g1 - g0) * 128], func=AF.Copy))
                P.op('dve', [Ak[1], f'vecs{l}'], [Ak[3]], lambda e: e.tensor_scalar(
                    out=kk32[:], in0=k32[:], scalar1=kkv[:, hp:hp + 1], scalar2=None, op0=ALU.mult))
                P.op('dve', [Ak[0], Ak[1], f'vecs{l}'], [Ak[6]], lambda e: e.scalar_tensor_tensor(
                    out=A[6][:], in0=r32[:], scalar=rkv[:, hp:hp + 1], in1=k32[:], op0=ALU.mult, op1=ALU.mult))
                for (t0, n) in TILES:
                    P.op('act', [Ak[3]], ['rw_sq5'], lambda e, t0=t0, n=n: e.activation(out=sq5[:, :n], in_=kk32[:, t0:t0 + n], func=AF.Square))
                    P.op('pe', ['rw_sq5', 'bd_bf'], ['ps4'], lambda e, n=n: e.matmul(self.ps[4][:, :n], lhsT=self.bd_bf[:], rhs=sq5[:, :n], start=True, stop=True))
                    P.op('act', ['ps4'], ['rw_rt'], lambda e, n=n: e.activation(out=rt[:, :n], in_=self.ps[4][:, :n], func=AF.Sqrt, scale=1.0, bias=self.eps_t[:, 0:1]))
                    P.op('dve', ['rw_rt'], ['rw_rt'], lambda e, n=n: e.reciprocal(out=rt[:, :n], in_=rt[:, :n]))
                    P.op('dve', [Ak[3], 'rw_rt'], [Ak[3]], lambda e, t0=t0, n=n: e.tensor_tensor(out=kk32[:, t0:t0 + n], in0=kk32[:, t0:t0 + n], in1=rt[:, :n], op=ALU.mult))
                    P.op('act', [Ak[6]], ['rw_sq5'], lambda e, t0=t0, n=n: e.activation(out=sq5[:, :n], in_=A[6][:, t0:t0 + n], func=AF.Copy))
                    P.op('pe', ['rw_sq5', 'bd_bf'], ['ps5'], lambda e, n=n: e.matmul(self.ps[5][:, :n], lhsT=self.bd_bf[:], rhs=sq5[:, :n], start=True, stop=True))
                    P.op('dve', ['ps5', Ak[2]], ['rw_bon'], lambda e, t0=t0, n=n: e.tensor_tensor(out=bon[:, t0:t0 + n], in0=self.ps[5][:, :n], in1=v32[:, t0:t0 + n], op=ALU.mult))
                for d in range(2):
                    self.sc_in('rw_prep', hp == 0 and d == 0 and l == 0)
                    ld, a_, cin, E = A[4], A[5], A[2], A[6]
                    ldk, ak_, cink, Ek = Ak[4], Ak[5], Ak[2], Ak[6]
                    P.dma(ld[:], self.ldT[d, cs, :], ['ldT'], [ldk])
                    P.dma(a_[:], self.aT[d, cs, :], ['aT'], [ak_])
                    P.op('dve', ['rmsk', ldk], [cink], lambda e: e.tensor_tensor_scan(
                        out=cin[:], data0=msk[:], data1=ld[:], initial=0.0, op0=ALU.mult, op1=ALU.add))
                    if d == 1:
                        P.op('dve', [cink], [Ek], lambda e: e.tensor_tensor(
                            out=c3(E), in0=c3(cin)[:, :, 63:64].to_broadcast([128, NCH, 64]), in1=c3(cin), op=ALU.subtract))
                        P.op('dve', [Ek, ldk], [cink], lambda e: e.tensor_tensor(out=cin[:], in0=E[:], in1=ld[:], op=ALU.add))
                    tot = c3(cin)[:, :, 63:64] if d == 0 else c3(cin)[:, :, 0:1]
                    P.op('act', [cink], ['rw_etot'], lambda e, tot=tot: e.activation(out=etot[:].unsqueeze(2), in_=tot, func=AF.Exp))
                    P.op('dve', [cink, ldk], [ldk], lambda e: e.tensor_tensor(out=ld[:], in0=cin[:], in1=ld[:], op=ALU.subtract))
                    P.op('act', [ldk], ['rw_al'], lambda e: e.activation(out=al[:], in_=ld[:], func=AF.Exp))
                    P.op('act', [cink], [Ek], lambda e: e.activation(out=E[:], in_=cin[:], func=AF.Exp))
                    P.op('act', [cink], ['rw_be'], lambda e: e.activation(out=be[:], in_=cin[:], func=AF.Exp, scale=-1.0))
                    P.op('act', [cink], ['rw_ka'], lambda e: e.activation(out=ka[:], in_=cin[:], func=AF.Exp, scale=-1.0))
                    P.op('dve', [Ak[3], 'rw_al'], ['rw_al'], lambda e: e.tensor_tensor(out=al[:], in0=al[:], in1=kk32[:], op=ALU.mult))
                    rho32 = ld
                    P.op('dve', [Ak[0], Ek], [ldk], lambda e: e.tensor_tensor(out=rho32[:], in0=r32[:], in1=E[:], op=ALU.mult))
                    P.op('act', [ldk], ['rw_rho'], lambda e: e.activation(out=rho[:], in_=rho32[:], func=AF.Copy))
                    P.op('dve', ['rw_be', ak_], ['rw_be'], lambda e: e.tensor_tensor(out=be[:], in0=be[:], in1=a_[:], op=ALU.mult))
                    P.op('dve', ['rw_be', Ak[3]], ['rw_be'], lambda e: e.tensor_tensor(out=be[:], in0=be[:], in1=kk32[:], op=ALU.mult))
                    P.op('dve', [ak_, f'vecs{l}', 'rw_ka1'], [ak_], lambda e: e.tensor_scalar(
                        out=a_[:], in0=a_[:], scalar1=kav[:, hp:hp + 1], scalar2=ka1[:, hp:hp + 1], op0=ALU.mult, op1=ALU.add))
                    P.op('dve', [ak_, Ak[1]], [ak_], lambda e: e.tensor_tensor(out=a_[:], in0=a_[:], in1=k32[:], op=ALU.mult))
                    P.op('dve', [ak_, 'rw_ka'], ['rw_ka'], lambda e: e.tensor_tensor(out=ka[:], in0=ka[:], in1=a_[:], op=ALU.mult))
                    eb = etot[:].unsqueeze(2).to_broadcast([128, NCH, 64])
                    P.op('dve', ['rw_be', 'rw_etot'], ['rw_Bp'], lambda e: e.tensor_tensor(out=c3(Bp), in0=c3(be), in1=eb, op=ALU.mult))
                    P.op('dve', ['rw_ka', 'rw_etot'], ['rw_Kp'], lambda e: e.tensor_tensor(out=c3(Kp), in0=c3(ka), in1=eb, op=ALU.mult))
                    self.stopat(1)
                    self.sc_in('rw_tm', hp == 0 and d == 0 and l == 0)
                    for (srcb, srck, dst, dstk) in ((al, 'rw_al', al_tm, 'rw_altm'), (Bp, 'rw_Bp', Bp_tm, 'rw_Bptm'), (Kp, 'rw_Kp', Kp_tm, 'rw_Kptm')):
                        for g0 in range(0, NT, 4):
                            g1 = min(NT, g0 + 4)
                            for tt in range(g0, g1):
                                P.op('pe', [srck, 'cst_bf'], ['psb'], lambda e, tt=tt, g0=g0, srcb=srcb: e.transpose(
                                    self.psb[:, (tt - g0) * 128:(tt - g0 + 1) * 128], srcb[:, tt * 128:(tt + 1) * 128], self.ident_bf))
                            P.op('act', ['psb'], [dstk], lambda e, g0=g0, g1=g1, dst=dst: e.activation(
                                out=dst[:, g0:g1, :].rearrange("p a b -> p (a b)"), in_=self.psb[:, 0:(g1 - g0) * 128], func=AF.Copy))
                    P.op('dve', [], ['rw_alm'], lambda e: e.memset(a5b, 0.0))
                    P.op('dve', [], ['rw_rhom'], lambda e: e.memset(a6b, 0.0))
                    for e_ in range(2):
                        pr = slice(e_ * 64, (e_ + 1) * 64)
                        P.op('act', ['rw_al'], ['rw_alm'], lambda e, e_=e_, pr=pr: e.activation(out=al_m[e_][pr, :], in_=al[pr, :], func=AF.Copy))
                        P.op('act', ['rw_rho'], ['rw_rhom'], lambda e, e_=e_, pr=pr: e.activation(out=rho_m[e_][pr, :], in_=rho[pr, :], func=AF.Copy))
                    self.stopat(2)
                    m_st = self.mask_fs if d == 0 else self.mask_bs
                    m_in = self.mask_f if d == 0 else self.mask_b
                    m_ts = self.mask_bs if d == 0 else self.mask_fs
                    bc4 = lambda m: m.unsqueeze(1).to_broadcast([128, 4, 128])
                    v4 = lambda t: t[:].rearrange("p (a b) -> p a b", b=128)
                    def grp(g0, sl):
                        B_ = slots[sl]
                        pa, pb, pc = B_['banks']
                        pak, pbk, pck = (f'ps{i}' for i in B_['bank_ids'])
                        XNs, Qs, LkT, MbT, MkT, Hh, P1n, Gt, ptmp = B_['XN'], B_['Q'], B_['LkT'], B_['MbT'], B_['MkT'], B_['H'], B_['P1n'], B_['G'], B_['ptmp']
                        kx = lambda nm: f'{nm}_s{sl}'
                        probs = [(tt, e_) for tt in (g0, g0 + 1) for e_ in range(2)]
                        X, N_ = XNs[0]

                        def gram(specs):
                            for pi_, (tt, e_) in enumerate(probs):
                                tsl = slice(tt * 128, (tt + 1) * 128); osl = slice(pi_ * 128, (pi_ + 1) * 128)
                                for (pst, pk, lh, rh, rks) in specs:
                                    lh_ = lh[e_] if isinstance(lh, list) else lh
                                    rh_ = rh[e_] if isinstance(rh, list) else rh
                                    P.op('pe', rks, [pk], lambda e, pst=pst, lh_=lh_, rh_=rh_, tsl=tsl, osl=osl: e.matmul(
                                        pst[:, osl], lhsT=lh_[:, tsl], rhs=rh_[:, tsl], start=True, stop=True))
                        gram(((pa, pak, be, al_m, ['rw_be', 'rw_alm']), (pb, pbk, al_m, be, ['rw_alm', 'rw_be']), (pc, pck, ka, al_m, ['rw_ka', 'rw_alm'])))
                        P.op('dve', [pak, 'cst_f'], [kx('rw_X0')], lambda e: e.scalar_tensor_tensor(
                            out=v4(X), in0=v4(pa), scalar=-1.0, in1=bc4(m_st), op0=ALU.mult, op1=ALU.mult))
                        P.op('dve', [pbk, 'cst_f'], [kx('rw_N0')], lambda e: e.scalar_tensor_tensor(
                            out=v4(N_), in0=v4(pb), scalar=-1.0, in1=bc4(m_ts), op0=ALU.mult, op1=ALU.mult))
                        P.op('dve', [pck, 'cst_f'], [kx('rw_LkT')], lambda e: e.tensor_tensor(out=v4(LkT), in0=v4(pc), in1=bc4(m_st), op=ALU.mult))
                        yield
                        pa2, pa2k = pa, pak
                        gram(((pa2, pa2k, be, rho_m, ['rw_be', 'rw_rhom']), (pb, pbk, ka, rho_m, ['rw_ka', 'rw_rhom'])))
                        P.op('dve', [pa2k, 'cst_f'], [kx('rw_MbT')], lambda e: e.tensor_tensor(out=v4(MbT), in0=v4(pa2), in1=bc4(m_in), op=ALU.mult))
                        P.op('dve', [pbk, 'cst_f'], [kx('rw_MkT')], lambda e: e.tensor_tensor(out=v4(MkT), in0=v4(pb), in1=bc4(m_in), op=ALU.mult))
                        P.op('dve', [kx('rw_X0'), 'cst_bf'], [kx('rw_Q0')], lambda e: e.tensor_tensor(
                            out=v4(Qs[0]), in0=v4(X), in1=self.ident_bf.unsqueeze(1).to_broadcast([128, 4, 128]), op=ALU.add))
                        yield
                        qi_ = 0
                        for j in range(1, 6):
                            Xo, No = XNs[(j - 1) % 2]
                            Xn, Nn = XNs[j % 2]
                            xo_k, no_k = kx(f'rw_X{(j - 1) % 2}'), kx(f'rw_N{(j - 1) % 2}')
                            xn_k, nn_k = kx(f'rw_X{j % 2}'), kx(f'rw_N{j % 2}')
                            for pi_ in range(4):
                                osl = slice(pi_ * 128, (pi_ + 1) * 128)
                                P.op('pe', [xo_k, no_k], [pak], lambda e, osl=osl, Xo=Xo, No=No: e.matmul(
                                    pa[:, osl], lhsT=No[:, osl], rhs=Xo[:, osl], start=True, stop=True))
                                P.op('pe', [xo_k, no_k], [pbk], lambda e, osl=osl, Xo=Xo, No=No: e.matmul(
                                    pb[:, osl], lhsT=Xo[:, osl], rhs=No[:, osl], start=True, stop=True))
                            P.op('act', [pak], [xn_k], lambda e, Xn=Xn: e.activation(out=Xn[:], in_=pa[:], func=AF.Copy))
                            P.op('act', [pbk], [nn_k], lambda e, Nn=Nn: e.activation(out=Nn[:], in_=pb[:], func=AF.Copy))
                            yield
                            Qo, Qn = Qs[qi_ % 2], Qs[(qi_ + 1) % 2]
                            qo_k, qn_k = kx(f'rw_Q{qi_ % 2}'), kx(f'rw_Q{(qi_ + 1) % 2}')
                            for pi_ in range(4):
                                osl = slice(pi_ * 128, (pi_ + 1) * 128)
                                P.op('pe', [nn_k, qo_k], [pck], lambda e, osl=osl, Nn=Nn, Qo=Qo: e.matmul(
                                    pc[:, osl], lhsT=Nn[:, osl], rhs=Qo[:, osl], start=True, stop=True))
                            P.op('dve', [pck, qo_k], [qn_k], lambda e, Qo=Qo, Qn=Qn: e.tensor_tensor(out=Qn[:], in0=pc[:], in1=Qo[:], op=ALU.add))
                            qi_ += 1
                            yield
                        Qf = Qs[qi_ % 2]; qf_k = kx(f'rw_Q{qi_ % 2}')
                        for pi_, (tt, e_) in enumerate(probs):
                            pr = slice(e_ * 64, (e_ + 1) * 64); osl = slice(pi_ * 128, (pi_ + 1) * 128)
                            P.op('pe', [kx('rw_LkT'), 'rw_Vtm'], [pak], lambda e, osl=osl, tt=tt, pr=pr, pi_=pi_: e.matmul(
                                pa[:, pi_ * 64:(pi_ + 1) * 64], lhsT=LkT[:, osl], rhs=V_tm[:, tt, pr], start=True, stop=True))
                            P.op('pe', [qf_k, 'rw_altm'], [pak], lambda e, osl=osl, tt=tt, pr=pr, pi_=pi_, Qf=Qf: e.matmul(
                                pa[:, 256 + pi_ * 64:256 + (pi_ + 1) * 64], lhsT=Qf[:, osl], rhs=al_tm[:, tt, pr], start=True, stop=True))
                        P.op('act', [pak], [kx('rw_H')], lambda e: e.activation(out=Hh, in_=pa[:, 0:256], func=AF.Copy))
                        P.op('act', [pak], [kx('rw_G')], lambda e: e.activation(out=Gt, in_=pa[:, 256:512], func=AF.Copy))
                        yield
                        H3 = Hh.rearrange("p (a b) -> p a b", b=64); G3 = Gt.rearrange("p (a b) -> p a b", b=64); P3 = P1n.rearrange("p (a b) -> p a b", b=64)
                        for pi_, (tt, e_) in enumerate(probs):
                            osl = slice(pi_ * 128, (pi_ + 1) * 128)
                            P.op('pe', [qf_k, kx('rw_H')], [pbk], lambda e, osl=osl, pi_=pi_, Qf=Qf: e.matmul(
                                pb[:, pi_ * 64:(pi_ + 1) * 64], lhsT=Qf[:, osl], rhs=H3[:, pi_, :], start=True, stop=True))
                        P.op('act', [pbk], [kx('rw_P1n')], lambda e: e.activation(out=P1n, in_=pb[:, 0:256], func=AF.Identity, scale=-1.0))
                        yield
                        for ti_, tt in enumerate((g0, g0 + 1)):
                            for e_ in range(2):
                                pi_ = ti_ * 2 + e_
                                pr = slice(e_ * 64, (e_ + 1) * 64); osl = slice(pi_ * 128, (pi_ + 1) * 128)
                                P.op('pe', [kx('rw_G'), kx('rw_MbT')], [pak], lambda e, pr=pr, osl=osl, pi_=pi_, ti_=ti_: e.matmul(
                                    pa[pr, ti_ * 128:(ti_ + 1) * 128], lhsT=G3[:, pi_, :], rhs=MbT[:, osl], start=True, stop=True))
                                P.op('pe', ['rw_Vtm', kx('rw_MkT')], [pbk], lambda e, pr=pr, osl=osl, tt=tt, ti_=ti_: e.matmul(
                                    pb[pr, ti_ * 128:(ti_ + 1) * 128], lhsT=V_tm[:, tt, pr], rhs=MkT[:, osl], start=True, stop=False))
                                P.op('pe', [kx('rw_P1n'), kx('rw_MbT')], [pbk], lambda e, pr=pr, osl=osl, pi_=pi_, ti_=ti_: e.matmul(
                                    pb[pr, ti_ * 128:(ti_ + 1) * 128], lhsT=P3[:, pi_, :], rhs=MbT[:, osl], start=False, stop=True))
                        t2 = slice(g0 * 128, (g0 + 2) * 128)
                        P.op('dve', [pak, ldk], ['rw_R32'], lambda e, t2=t2: e.tensor_tensor(out=R32[:, t2], in0=rho32[:, t2], in1=pa[:, 0:256], op=ALU.subtract))
                        P.op('act', [pbk], ['rw_yloc'], lambda e, t2=t2: e.activation(out=yloc[:, t2], in_=pb[:, 0:256], func=AF.Copy))
                        yield
                        for ti_, tt in enumerate((g0, g0 + 1)):
                            for hf in range(2):
                                csl = slice(hf * 64, (hf + 1) * 64)
                                gsl = G3[csl, ti_ * 2:ti_ * 2 + 2, :].rearrange("p a b -> p (a b)")
                                p1sl = P3[csl, ti_ * 2:ti_ * 2 + 2, :].rearrange("p a b -> p (a b)")
                                col = slice((ti_ * 2 + hf) * 128, (ti_ * 2 + hf + 1) * 128)
                                P.op('pe', [kx('rw_G'), 'rw_Bptm'], [pak], lambda e, gsl=gsl, csl=csl, tt=tt, col=col: e.matmul(
                                    pa[:, col], lhsT=gsl, rhs=Bp_tm[csl, tt, :], start=True, stop=True))
                                P.op('pe', ['rw_Kptm', 'rw_Vtm'], [pck], lambda e, csl=csl, tt=tt, col=col: e.matmul(
                                    pc[:, col], lhsT=Kp_tm[csl, tt, :], rhs=V_tm[csl, tt, :], start=True, stop=False))
                                P.op('pe', ['rw_Bptm', kx('rw_P1n')], [pck], lambda e, csl=csl, tt=tt, col=col, p1sl=p1sl: e.matmul(
                                    pc[:, col], lhsT=Bp_tm[csl, tt, :], rhs=p1sl, start=False, stop=True))
                        for q_ in range(4):
                            ch = g0 * 2 + q_
                            col = slice(q_ * 128, (q_ + 1) * 128)
                            P.op('dve', [pak, 'cst_f'], [kx('rw_ptmp')], lambda e, col=col: e.tensor_tensor(out=ptmp, in0=pa[:, col], in1=self.mask_bd, op=ALU.mult))
                            P.op('dve', [kx('rw_ptmp'), 'rw_etot', 'cst_f'], ['rw_Phi'], lambda e, ch=ch: e.scalar_tensor_tensor(
                                out=Phi[:, ch, :], in0=self.ident32, scalar=etot[:, ch:ch + 1], in1=ptmp, op0=ALU.mult, op1=ALU.subtract))
                        P.op('dve', [pck, 'cst_f'], ['rw_D'], lambda e, g0=g0: e.tensor_tensor(
                            out=Dd[:, g0 * 2:g0 * 2 + 4, :], in0=v4(pc), in1=bc4(self.mask_bd), op=ALU.mult))

                    self.sc_in('rw_grp', hp == 0 and d == 0 and l == 0)
                    pending = list(range(0, NT, 2))
                    active = {}
                    while pending or active:
                        for sl in range(self.rw_slots):
                            if sl not in active and pending:
                                active[sl] = grp(pending.pop(0), sl)
                        for sl in list(active):
                            try:
                                self._steps = getattr(self, '_steps', 0) + 1
                                if self._steps == self.rw_stop:
                                    P.halt = True
                                next(active[sl])
                            except StopIteration:
                                del active[sl]
                    self.stopat(7)
                    self.sc_in('rw_rec', hp == 0 and d == 0 and l == 0)
                    P.op('dve', [], [f'rw_S{si % 2}'], lambda e, si=si: e.memset(Sbd[si % 2][:], 0.0))
                    order = list(range(NCH)) if d == 0 else [3, 2, 1, 0] + list(range(NCH - 1, 3, -1))
                    for n_i, ch in enumerate(order):
                        S_ = Sbd[si % 2]; sk = f'rw_S{si % 2}'
                        tok = slice(ch * 64, (ch + 1) * 64)
                        yb = n_i % 2
                        need_y = with_ctx or ch >= 4
                        if need_y:
                            P.op('pe', [sk, 'rw_R32'], [f'ps{3 + yb}'], lambda e, S_=S_, tok=tok, yb=yb: e.matmul(
                                self.ps[3 + yb][:, 0:64], lhsT=S_[:], rhs=R32[:, tok], start=True, stop=True))
                            if d == 0:
                                P.op('dve', [f'ps{3 + yb}', 'rw_yloc'], ['rw_yacc'], lambda e, tok=tok, yb=yb: e.tensor_tensor(
                                    out=yacc[:, tok], in0=self.ps[3 + yb][:, 0:64], in1=yloc[:, tok], op=ALU.add))
                            else:
                                P.op('dve', [f'ps{3 + yb}', 'rw_yloc'], ['rw_yloc'], lambda e, tok=tok, yb=yb: e.tensor_tensor(
                                    out=yloc[:, tok], in0=self.ps[3 + yb][:, 0:64], in1=yloc[:, tok], op=ALU.add))
                                P.op('pool', ['rw_yloc', 'rw_yacc'], ['rw_yacc'], lambda e, tok=tok: e.tensor_tensor(
                                    out=yacc[:, tok], in0=yacc[:, tok], in1=yloc[:, tok], op=ALU.add))
                        P.op('pe', [sk, 'rw_Phi'], [f'ps{5 + yb}'], lambda e, S_=S_, ch=ch, yb=yb: e.matmul(
                            self.ps[5 + yb][:, 0:128], lhsT=Phi[:, ch, :], rhs=S_[:], start=True, stop=True))
                        si += 1
                        P.op('dve', [f'ps{5 + yb}', 'rw_D'], [f'rw_S{si % 2}'], lambda e, ch=ch, yb=yb, si=si: e.tensor_tensor(
                            out=Sbd[si % 2][:], in0=self.ps[5 + yb][:, 0:128], in1=Dd[:, ch, :], op=ALU.add))
                    self.stopat(8)
                self.sc_in('rw_fin', hp == 0 and l == 0)
                for (t0, n) in TILES:
                    if not with_ctx and t0 < NCTX:
                        continue
                    ya = yacc[:, t0:t0 + n]
                    P.op('act', ['rw_yacc'], ['rw_sq5'], lambda e, ya=ya, n=n: e.activation(out=sq5[:, :n], in_=ya, func=AF.Copy))
                    P.op('pe', ['rw_sq5', 'bd_bf'], ['ps4'], lambda e, n=n: e.matmul(self.ps[4][:, :n], lhsT=self.bd_bf[:], rhs=sq5[:, :n], start=True, stop=True))
                    P.op('dve', ['ps4', 'rw_yacc'], ['rw_t5'], lambda e, ya=ya, n=n: e.scalar_tensor_tensor(
                        out=t5[:, :n], in0=self.ps[4][:, :n], scalar=-1.0 / 64, in1=ya, op0=ALU.mult, op1=ALU.add))
                    P.op('act', ['rw_t5'], ['rw_sq5'], lambda e, n=n: e.activation(out=sq5[:, :n], in_=t5[:, :n], func=AF.Square))
                    P.op('pe', ['rw_sq5', 'bd_bf'], ['ps4'], lambda e, n=n: e.matmul(self.ps[4][:, :n], lhsT=self.bd_bf[:], rhs=sq5[:, :n], start=True, stop=True))
                    P.op('act', ['ps4'], ['rw_rt'], lambda e, n=n: e.activation(out=rt[:, :n], in_=self.ps[4][:, :n], func=AF.Sqrt, scale=1.0 / 64, bias=geps[:, 0:1]))
                    P.op('dve', ['rw_rt'], ['rw_rt'], lambda e, n=n: e.reciprocal(out=rt[:, :n], in_=rt[:, :n]))
                    P.op('dve', ['rw_t5', 'rw_rt'], ['rw_t5'], lambda e, n=n: e.tensor_tensor(out=t5[:, :n], in0=t5[:, :n], in1=rt[:, :n], op=ALU.mult))
                    P.op('dve', ['rw_t5', f'vecs{l}'], ['rw_t5'], lambda e, n=n: e.tensor_scalar(
                        out=t5[:, :n], in0=t5[:, :n], scalar1=gng[:, hp:hp + 1], scalar2=gnb[:, hp:hp + 1], op0=ALU.mult, op1=ALU.add))
                    P.op('dve', ['rw_t5', 'rw_bon'], ['rw_t5'], lambda e, t0=t0, n=n: e.tensor_tensor(out=t5[:, :n], in0=t5[:, :n], in1=bon[:, t0:t0 + n], op=ALU.add))
                    P.dma(gsb[:, :n], self.g2T[cs, t0:t0 + n], ['g2T'], ['rw_g'])
                    P.op('dve', ['rw_t5', 'rw_g'], ['rw_vb'], lambda e, t0=t0, n=n: e.tensor_tensor(out=yo[:, t0:t0 + n], in0=t5[:, :n], in1=gsb[:, :n], op=ALU.mult))
                self.sc_out()
                t_lo = 0 if with_ctx else NCTX
                P.dma(self.yT[2, cs, t_lo:], yo[:, t_lo:], ['rw_vb'], ['yT'])
            if 'rwkv' in self.debug:
                self.dbg(f'yrw{l}', lambda o: P.dma(o, self.yT[2], ['yT'], []), [1024, TT], BF16)


    def merge_moe(self, l):
        nc, P = self.nc, self.P
        with_ctx = l < DEPTH - 1
        last = l == DEPTH - 1
        src = (self.xT0 if l == 0 else self.xT).rearrange("(kc p) t -> p kc t", p=128)
        dstx = self.xT.rearrange("(kc p) t -> p kc t", p=128)
        dsto = self.outT.rearrange("(kc p) t -> p kc t", p=128)
        gTv = self.gT.rearrange("(i c p) t -> p i c t", i=3, p=128)
        yTv = self.yT.rearrange("i (c p) t -> p i c t", p=128)
        wbv = self.w_branch[l].rearrange("i (kc p) c -> p i kc c", p=128)
        wov = self.w_out[l].rearrange("(kc p) c -> p kc c", p=128)
        BIG = 1.0e4
        with contextlib.ExitStack() as st:
            xg = self.sb(st, "mm_xg", [128, KC, 512])
            rw32 = self.sb(st, "mm_rw32", [128, KC, 16])
            rbias = self.sb(st, "mm_rbias", [128, 16])
            sel = self.sb(st, "mm_sel", [16, 16, 128])
            P.dma(rw32[:], self.router_w.rearrange("(kc p) e -> p kc e", p=128), [], ['mm_rw32'])
            P.dma(rbias[:], self.router_b[0:1, :].partition_broadcast(128), [], ['mm_rbias'])
            P.op('dve', ['cst_f'], ['mm_sel'], lambda e: e.tensor_copy(
                out=sel[:], in_=self.ident32[0:16, 0:16].unsqueeze(2).to_broadcast([16, 16, 128])))
            wld = 0
            for (t0, n) in TILES:
                if t0 < NCTX and not with_ctx:
                    continue
                j = 1 if t0 < NCTX else 0
                nb = n // 128
                P.dma(xg[:, :, :n], src[:, :, t0:t0 + n], ['xT'], ['mm_xg'])
                with contextlib.ExitStack() as s2:
                    yb = self.sb(s2, "mm_y", [128, 3, 8, 512], BF16)
                    mg = self.sb(s2, "mm_mg", [128, KC, 512], BF16)
                    gb = [self.sb(s2, f"mm_g{i}", [128, 3, 512], BF16) for i in range(2)]
                    wb = [self.sb(s2, f"mm_wb{i}", [128, 3, 8, 512], BF16) for i in range(2)]
                    wo = [self.sb(s2, f"mm_wo{i}", [128, KC, 512], BF16) for i in range(2)]
                    m32 = self.sb(s2, "mm_m32", [128, 512])
                    tm = self.sb(s2, "mm_tm", [128, 512])
                    for i in range(3):
                        P.dma(yb[:, i, :, :n], yTv[:, i, :, t0:t0 + n], ['yT'], ['mm_y'])
                    pi = 0
                    for cb in range(4):
                        wk = f'mm_wb{cb % 2}'
                        for i in range(3):
                            P.dma(wb[cb % 2][:, i], wbv[:, i, :, cb * 512:(cb + 1) * 512], [], [wk], q='pool')
                        for dcl in range(4):
                            dc = cb * 4 + dcl
                            gk = f'mm_g{dc % 2}'
                            P.dma(gb[dc % 2][:, :, :n], gTv[:, :, dc, t0:t0 + n], ['gT'], [gk])
                            for i in range(3):
                                pst, pk = self.ps[pi % 4], f'ps{pi % 4}'
                                pi += 1
                                for kc in range(8):
                                    P.op('pe', [wk, 'mm_y'], [pk], lambda e, i=i, kc=kc, cb=cb, dcl=dcl, pst=pst: e.matmul(
                                        pst[:, :n], lhsT=wb[cb % 2][:, i, kc, dcl * 128:(dcl + 1) * 128], rhs=yb[:, i, kc, :n],
                                        start=(kc == 0), stop=(kc == 7)))
                                if i == 0:
                                    P.op('dve', [pk, gk], ['mm_m32'], lambda e, pst=pst, dc=dc: e.tensor_tensor(
                                        out=m32[:, :n], in0=pst[:, :n], in1=gb[dc % 2][:, 0, :n], op=ALU.mult))
                                else:
                                    P.op('dve', [pk, gk], ['mm_tm'], lambda e, pst=pst, dc=dc, i=i: e.tensor_tensor(
                                        out=tm[:, :n], in0=pst[:, :n], in1=gb[dc % 2][:, i, :n], op=ALU.mult))
                                    if i == 1:
                                        P.op('dve', ['mm_tm', 'mm_m32'], ['mm_m32'], lambda e: e.tensor_tensor(
                                            out=m32[:, :n], in0=m32[:, :n], in1=tm[:, :n], op=ALU.add))
                                    else:
                                        P.op('dve', ['mm_tm', 'mm_m32'], ['mm_mg'], lambda e, dc=dc: e.tensor_tensor(
                                            out=mg[:, dc, :n], in0=m32[:, :n], in1=tm[:, :n], op=ALU.add))
                    for cb in range(4):
                        wk = f'mm_wo{cb % 2}'
                        P.dma(wo[cb % 2][:], wov[:, :, cb * 512:(cb + 1) * 512], [], [wk], q='pool')
                        for dcl in range(4):
                            dc = cb * 4 + dcl
                            pst, pk = self.ps[pi % 4], f'ps{pi % 4}'
                            pi += 1
                            for kc in range(KC):
                                P.op('pe', [wk, 'mm_mg'], [pk], lambda e, kc=kc, cb=cb, dcl=dcl, pst=pst: e.matmul(
                                    pst[:, :n], lhsT=wo[cb % 2][:, kc, dcl * 128:(dcl + 1) * 128], rhs=mg[:, kc, :n],
                                    start=(kc == 0), stop=(kc == KC - 1)))
                            P.op('dve', [pk, 'mm_xg', f'mod{l}'], ['mm_xg'], lambda e, pst=pst, dc=dc: e.scalar_tensor_tensor(
                                out=xg[:, dc, :n], in0=pst[:, :n], scalar=self.mod[l][:, 32 + dc, j:j + 1], in1=xg[:, dc, :n],
                                op0=ALU.mult, op1=ALU.add))
                    if 'x1' in self.debug:
                        self.dbg(f'x1_{l}_{t0}', lambda o: P.dma(o.rearrange("(kc p) t -> p kc t", p=128), xg[:, :, :n], ['mm_xg'], []), [D, n])
                    P.barrier()
                with contextlib.ExitStack() as s2:
                    h2 = self.sb(s2, "mo_h2", [128, KC, 512], BF16)
                    sq = h2
                    rt = self.sb(s2, "mo_rt", [128, 512])
                    lg = self.sb(s2, "mo_lg", [16, 512])
                    R = {nm: self.sb(s2, "mo_" + nm, [128, 4, 16]) for nm in ('s', 'bz', 'eq', 'b2', 'mb', 'e1', 'w')}
                    r4 = {nm: self.sb(s2, "mo_" + nm, [128, 4, 4]) for nm in ('m1', 'm2', 'gsel')}
                    r1 = {nm: self.sb(s2, "mo_" + nm, [128, 4]) for nm in ('gmax', 't1', 't2', 'ws')}
                    cT = self.sb(s2, "mo_cT", [16, 512])
                    bce = [self.sb(s2, f"mo_bce{i}", [128, 512]) for i in range(2)]
                    sg = [self.sb(s2, f"mo_sg{i}", [128, 512]) for i in range(2)]
                    act = [self.sb(s2, f"mo_act{i}", [128, 4, 512], BF16) for i in range(2)]
                    wg = [self.sb(s2, f"mo_wg{i}", [128, KC, 512], BF16) for i in range(2)]
                    wu = [self.sb(s2, f"mo_wu{i}", [128, KC, 512], BF16) for i in range(2)]
                    s3 = contextlib.ExitStack()
                    xn = self.sb(s3, "mo_xn", [128, KC, 512])
                    P.op('act', ['mm_xg'], ['mo_h2'], lambda e: e.activation(out=sq[:, :, :n], in_=xg[:, :, :n], func=AF.Square))
                    for kc in range(KC):
                        P.op('pe', ['mo_h2', 'ones_bf'], ['ps6'], lambda e, kc=kc: e.matmul(
                            self.ps[6][:, :n], lhsT=self.ones_bf[:], rhs=sq[:, kc, :n], start=(kc == 0), stop=(kc == KC - 1)))
                    P.op('act', ['ps6'], ['mo_rt'], lambda e: e.activation(out=rt[:, :n], in_=self.ps[6][:, :n], func=AF.Sqrt,
                                                                      scale=1.0 / D, bias=self.eps_t[:, 0:1]))
                    P.op('dve', ['mo_rt'], ['mo_rt'], lambda e: e.reciprocal(out=rt[:, :n], in_=rt[:, :n]))
                    P.op('dve', ['mm_xg', 'mo_rt'], ['mo_xn'], lambda e: e.tensor_tensor(
                        out=xn[:, :, :n], in0=xg[:, :, :n], in1=rt[:, :n].unsqueeze(1).to_broadcast([128, KC, n]), op=ALU.mult))
                    for kc in range(KC):
                        P.op('act', ['mo_xn', f'gm2_{l}', f'mod{l}'], ['mo_xn'], lambda e, kc=kc: e.activation(
                            out=xn[:, kc, :n], in_=xn[:, kc, :n], func=AF.Identity,
                            scale=self.gm2[l][:, kc, j:j + 1], bias=self.mod[l][:, 48 + kc, j:j + 1]))
                    P.op('dve', ['mo_xn'], ['mo_h2'], lambda e: e.tensor_copy(out=h2[:, :, :n], in_=xn[:, :, :n]))
                    for kc in range(KC):
                        P.op('pe', ['mo_xn', 'mm_rw32'], ['ps5'], lambda e, kc=kc: e.matmul(
                            self.ps[5][0:16, :n], lhsT=rw32[:, kc, :], rhs=xn[:, kc, :n], start=(kc == 0), stop=(kc == KC - 1)))
                    P.op('act', ['ps5'], ['mo_lg'], lambda e: e.activation(out=lg[:, :n], in_=self.ps[5][0:16, :n], func=AF.Copy))
                    for b_ in range(nb):
                        P.op('pe', ['mo_lg', 'cst_f'], ['ps4'], lambda e, b_=b_: e.transpose(
                            self.ps[4][:, b_ * 16:(b_ + 1) * 16], lg[0:16, b_ * 128:(b_ + 1) * 128], self.ident32[0:16, 0:16]))
                    s_, bz, eq, b2, mb, e1, w_ = (R[k][:, :nb, :] for k in ('s', 'bz', 'eq', 'b2', 'mb', 'e1', 'w'))
                    m1, m2, gsel = (r4[k][:, :nb, :] for k in ('m1', 'm2', 'gsel'))
                    gmax, t1, t2, ws = (r1[k][:, :nb] for k in ('gmax', 't1', 't2', 'ws'))
                    g4 = lambda a: a.rearrange("p b (g k) -> p b g k", k=4)
                    V = lambda reads, writes, fn: P.op('dve', reads, writes, fn)
                    P.op('act', ['ps4'], ['mo_s'], lambda e: e.activation(
                        out=s_, in_=self.ps[4][:, 0:nb * 16].rearrange("p (b k) -> p b k", k=16), func=AF.Sigmoid))
                    V(['mo_s', 'mm_rbias'], ['mo_bz'], lambda e: e.tensor_tensor(out=bz, in0=s_, in1=rbias[:].unsqueeze(1).to_broadcast([128, nb, 16]), op=ALU.add))
                    V(['mo_bz'], ['mo_m1'], lambda e: e.tensor_reduce(out=m1, in_=g4(bz), axis=AX.X, op=ALU.max))
                    V(['mo_bz', 'mo_m1'], ['mo_eq'], lambda e: e.tensor_tensor(out=g4(eq), in0=g4(bz), in1=m1.unsqueeze(3).to_broadcast([128, nb, 4, 4]), op=ALU.is_equal))
                    V(['mo_eq', 'mo_bz'], ['mo_b2'], lambda e: e.scalar_tensor_tensor(out=b2, in0=eq, scalar=-BIG, in1=bz, op0=ALU.mult, op1=ALU.add))
                    V(['mo_b2'], ['mo_m2'], lambda e: e.tensor_reduce(out=m2, in_=g4(b2), axis=AX.X, op=ALU.max))
                    V(['mo_m1', 'mo_m2'], ['mo_m1'], lambda e: e.tensor_tensor(out=m1, in0=m1, in1=m2, op=ALU.add))
                    V(['mo_m1'], ['mo_gmax'], lambda e: e.tensor_reduce(out=gmax, in_=m1, axis=AX.X, op=ALU.max))
                    V(['mo_m1', 'mo_gmax'], ['mo_gsel'], lambda e: e.tensor_tensor(out=gsel, in0=m1, in1=gmax.unsqueeze(2).to_broadcast([128, nb, 4]), op=ALU.is_equal))
                    V(['mo_gsel'], ['mo_gsel'], lambda e: e.tensor_scalar(out=gsel, in0=gsel, scalar1=BIG, scalar2=-BIG, op0=ALU.mult, op1=ALU.add))
                    V(['mo_bz', 'mo_gsel'], ['mo_mb'], lambda e: e.tensor_tensor(out=g4(mb), in0=g4(bz), in1=gsel.unsqueeze(3).to_broadcast([128, nb, 4, 4]), op=ALU.add))
                    V(['mo_mb'], ['mo_t1'], lambda e: e.tensor_reduce(out=t1, in_=mb, axis=AX.X, op=ALU.max))
                    V(['mo_mb', 'mo_t1'], ['mo_e1'], lambda e: e.tensor_tensor(out=e1, in0=mb, in1=t1.unsqueeze(2).to_broadcast([128, nb, 16]), op=ALU.is_equal))
                    V(['mo_e1', 'mo_mb'], ['mo_b2'], lambda e: e.scalar_tensor_tensor(out=b2, in0=e1, scalar=-BIG, in1=mb, op0=ALU.mult, op1=ALU.add))
                    V(['mo_b2'], ['mo_t2'], lambda e: e.tensor_reduce(out=t2, in_=b2, axis=AX.X, op=ALU.max))
                    V(['mo_b2', 'mo_t2'], ['mo_eq'], lambda e: e.tensor_tensor(out=eq, in0=b2, in1=t2.unsqueeze(2).to_broadcast([128, nb, 16]), op=ALU.is_equal))
                    V(['mo_eq', 'mo_e1'], ['mo_e1'], lambda e: e.tensor_tensor(out=e1, in0=e1, in1=eq, op=ALU.add))
                    V(['mo_e1', 'mo_s'], ['mo_w'], lambda e: e.tensor_tensor(out=w_, in0=e1, in1=s_, op=ALU.mult))
                    V(['mo_w'], ['mo_ws'], lambda e: e.tensor_reduce(out=ws, in_=w_, axis=AX.X, op=ALU.add))
                    V(['mo_ws'], ['mo_ws'], lambda e: e.reciprocal(out=ws, in_=ws))
                    V(['mo_w', 'mo_ws'], ['mo_w'], lambda e: e.tensor_tensor(out=w_, in0=w_, in1=ws.unsqueeze(2).to_broadcast([128, nb, 16]), op=ALU.mult))
                    for b_ in range(nb):
                        P.op('pe', ['mo_w', 'cst_f'], ['ps5'], lambda e, b_=b_: e.transpose(
                            self.ps[5][0:16, b_ * 128:(b_ + 1) * 128], R['w'][:, b_, :], self.ident32))
                    P.op('act', ['ps5'], ['mo_cT'], lambda e: e.activation(out=cT[:, :n], in_=self.ps[5][0:16, :n], func=AF.Copy))
                    if 'comb' in self.debug:
                        self.dbg(f'comb_{l}_{t0}', lambda o: P.dma(o, cT[:, :n], ['mo_cT'], []), [16, n])
                    P.barrier()
                    s3.close()
                    s3 = contextlib.ExitStack()
                    wd = [self.sb(s3, f"mo_wd{i}", [128, 4, D], BF16) for i in range(2)]
                    pi = 0
                    for ex in range(16):
                        b = ex % 2
                        P.dma(wg[b][:], self.moe_g[l, ex].rearrange("(kc p) f -> p kc f", p=128), [], [f'mo_wg{b}'], q='pool')
                        P.dma(wu[b][:], self.moe_u[l, ex].rearrange("(kc p) f -> p kc f", p=128), [], [f'mo_wu{b}'], q='pool')
                        P.dma(wd[b][:], self.moe_d[l, ex].rearrange("(fc p) d -> p fc d", p=128), [], [f'mo_wd{b}'], q='pool')
                        P.op('pe', ['mm_sel', 'mo_cT'], ['ps6'], lambda e, ex=ex: e.matmul(
                            self.ps[6][:, :n], lhsT=sel[:, ex, :], rhs=cT[:, :n], start=True, stop=True), rg=0)
                        P.op('act', ['ps6'], [f'mo_bce{b}'], lambda e, b=b: e.activation(out=bce[b][:, :n], in_=self.ps[6][:, :n], func=AF.Copy))
                        for fc in range(4):
                            pg, pgk = self.ps[pi % 4], f'ps{pi % 4}'
                            pu, puk = self.ps[(pi + 1) % 4], f'ps{(pi + 1) % 4}'
                            pi += 2
                            for kc in range(KC):
                                P.op('pe', [f'mo_wg{b}', 'mo_h2'], [pgk], lambda e, kc=kc, fc=fc, b=b, pg=pg: e.matmul(
                                    pg[:, :n], lhsT=wg[b][:, kc, fc * 128:(fc + 1) * 128], rhs=h2[:, kc, :n], start=(kc == 0), stop=(kc == KC - 1)))
                            for kc in range(KC):
                                P.op('pe', [f'mo_wu{b}', 'mo_h2'], [puk], lambda e, kc=kc, fc=fc, b=b, pu=pu: e.matmul(
                                    pu[:, :n], lhsT=wu[b][:, kc, fc * 128:(fc + 1) * 128], rhs=h2[:, kc, :n], start=(kc == 0), stop=(kc == KC - 1)))
                            sb_ = fc % 2
                            P.op('act', [pgk], [f'mo_sg{sb_}'], lambda e, pg=pg, sb_=sb_: e.activation(out=sg[sb_][:, :n], in_=pg[:, :n], func=AF.Silu))
                            P.op('dve', [puk, f'mo_sg{sb_}'], [f'mo_sg{sb_}'], lambda e, pu=pu, sb_=sb_: e.tensor_tensor(
                                out=sg[sb_][:, :n], in0=pu[:, :n], in1=sg[sb_][:, :n], op=ALU.mult))
                            P.op('dve', [f'mo_sg{sb_}', f'mo_bce{b}'], [f'mo_act{b}'], lambda e, sb_=sb_, b=b, fc=fc: e.tensor_tensor(
                                out=act[b][:, fc, :n], in0=sg[sb_][:, :n], in1=bce[b][:, :n], op=ALU.mult))
                        for dc in range(KC):
                            pd, pdk = self.ps[pi % 4], f'ps{pi % 4}'
                            pi += 1
                            for fc in range(4):
                                P.op('pe', [f'mo_wd{b}', f'mo_act{b}'], [pdk], lambda e, fc=fc, dc=dc, b=b, pd=pd: e.matmul(
                                    pd[:, :n], lhsT=wd[b][:, fc, dc * 128:(dc + 1) * 128], rhs=act[b][:, fc, :n], start=(fc == 0), stop=(fc == 3)))
                            P.op('dve', [pdk, 'mm_xg', f'mod{l}'], ['mm_xg'], lambda e, pd=pd, dc=dc: e.scalar_tensor_tensor(
                                out=xg[:, dc, :n], in0=pd[:, :n], scalar=self.mod[l][:, 80 + dc, j:j + 1], in1=xg[:, dc, :n],
                                op0=ALU.mult, op1=ALU.add))
                    if last:
                        P.dma(dsto[:, :, t0 - NCTX:t0 - NCTX + n], xg[:, :, :n], ['mm_xg'], ['outT'])
                    else:
                        P.dma(dstx[:, :, t0:t0 + n], xg[:, :, :n], ['mm_xg'], ['xT'])
                    if 'x2' in self.debug:
                        self.dbg(f'x2_{l}_{t0}', lambda o: P.dma(o.rearrange("(kc p) t -> p kc t", p=128), xg[:, :, :n], ['mm_xg'], []), [D, n])
                    P.barrier()
                    s3.close()

    def final_out(self):
        P = self.P
        P.dma(self.outT, self.xT[:, NCTX:], ['xT'], [])


def na_tables(rpb):
    c = np.arange(64)
    cs = np.clip(c - 8, 0, 48)
    kc = np.arange(64)
    inwin = (kc[:, None] >= cs[None, :]) & (kc[:, None] < cs[None, :] + 16)
    dc = np.clip(kc[:, None] - c[None, :] + 15, 0, 30)
    out = np.full((16, 2, 64, 14, 64), NEG, np.float32)
    for jj in range(2):
        for dr in range(14):
            g = rpb[:, dr + jj][:, dc]
            out[:, jj, :, dr, :] = np.where(inwin[None], g, np.float32(NEG))
    return out.reshape(16, 128, 14 * 64)


NCONST = 512 + 2 * SEQ + 384


def make_consts():
    c = np.zeros((128, NCONST), np.float32)
    p = np.arange(128)
    c[p, p] = 1.0
    c[p, 128 + (p ^ 32)] = 1.0
    j = p[:, None]; i = p[None, :]
    same = (j // 64) == (i // 64)
    c[:, 256:384] = (same & (j <= i)).astype(np.float32)
    c[:, 384:512] = (same & (j >= i)).astype(np.float32)
    t = np.arange(SEQ)
    pos = np.where(p[:, None] < 64, (t // 64)[None, :], (t % 64)[None, :]).astype(np.float32)
    inv = (10000.0 ** (-np.arange(0, 64, 2, dtype=np.float32) / 64)).astype(np.float32)
    ang = pos * inv[(p % 32)][:, None]
    c[:, 512:512 + SEQ] = np.cos(ang)
    sgn = np.where((p % 64) < 32, -1.0, 1.0).astype(np.float32)
    c[:, 512 + SEQ:512 + 2 * SEQ] = np.sin(ang) * sgn[:, None]
    o = 512 + 2 * SEQ
    c[:, o:o + 128] = (same & (j < i)).astype(np.float32)
    c[:, o + 128:o + 256] = (same & (j > i)).astype(np.float32)
    c[:, o + 256:o + 384] = same.astype(np.float32)
    return c


def host_inputs(inp, b):
    xT0 = np.ascontiguousarray(np.concatenate([inp['ctx'][b], inp['x'][b]], axis=0).T)
    cT = np.stack([fm(inp['c'][b]), fm(inp['c_ctx'])], axis=-1).reshape(128, 32)
    return {
        'xT0': xT0, 'cT': np.ascontiguousarray(cT),
        'ada_w': inp['ada_w'], 'w_in': inp['w_in'],
        'vecs': np.stack([pack_vecs(inp, l) for l in range(DEPTH)]),
        'natab': np.stack([na_tables(inp['na_rpb'][l]) for l in range(DEPTH)]),
        'consts': make_consts(), 'gla_gate_w2': inp['gla_gate_w2'],
        'rw_w2': inp['rw_w2'], 'rw_a2': inp['rw_a2'], 'rw_g2': inp['rw_g2'],
        'w_branch': inp['w_branch'], 'w_out': inp['w_out'], 'router_w': inp['router_w'],
        'router_bias': inp['router_bias'].reshape(1, 16),
        'moe_w_gate': inp['moe_w_gate'], 'moe_w_up': inp['moe_w_up'], 'moe_w_down': inp['moe_w_down'],
    }


def kernel(**inputs):
    inp = {k: np.asarray(v) for k, v in inputs.items()}
    bld = Builder()
    nc = bld.build()
    in_maps = [host_inputs(inp, c % 4) for c in range(8)]
    res = run_bass_kernel_spmd(nc, in_maps, core_ids=list(range(8)))
    out = np.stack([np.ascontiguousarray(res.results[b]["outT"].T) for b in range(4)], axis=0)
    return out.astype(np.float32)
```

```python
import contextlib
import os
import numpy as np
import concourse.bass as bass
import concourse.mybir as mybir
from concourse.bass_utils import run_bass_kernel_spmd

F32 = mybir.dt.float32
BF16 = mybir.dt.bfloat16
AF = mybir.ActivationFunctionType
ALU = mybir.AluOpType
AX = mybir.AxisListType

D = 2048
KC = 16
NCTX = 256
SEQ = 2048
TT = NCTX + SEQ
D_IN = 16032
EPS = 1e-6
NEG = -30000.0
DEPTH = 2

C_NAQ, C_NAK, C_NAV = 0, 1024, 2048
C_GQ, C_GK, C_GV, C_GR, C_GGD = 3072, 3584, 4096, 5120, 6144
C_RW = 6176
C_RR, C_RK, C_RV, C_RWD, C_RAD, C_RGD = C_RW, C_RW + 1024, C_RW + 2048, C_RW + 3072, C_RW + 3264, C_RW + 3456
C_GATE = 9888

TILES = [(0, 256), (256, 512), (768, 512), (1280, 512), (1792, 512)]

NDS = 12


class PEProxy:
    def __init__(self, real):
        self._real = real
        self._last = None
        self._dummy = None

    def _sep(self, out, w):
        K = w.shape[0]
        M = 1
        for d_ in w.shape[1:]:
            M *= d_
        t = None if K == 128 else (w.base_partition(), K)
        if t is not None and self._last is not None and t != self._last and self._dummy is not None:
            self._dummy(self._real)
        self._last = t

    def matmul(self, out, lhsT=None, rhs=None, **kw):
        self._sep(out, lhsT)
        return self._real.matmul(out, lhsT=lhsT, rhs=rhs, **kw)

    def transpose(self, out, in_, identity, **kw):
        self._sep(out, in_)
        return self._real.transpose(out, in_, identity, **kw)

    def __getattr__(self, name):
        return getattr(self._real, name)


class Prog:
    def __init__(self, nc, es):
        self.nc = nc
        self.e = dict(pe=PEProxy(nc.tensor), act=nc.scalar, dve=nc.vector, pool=nc.gpsimd, sp=nc.sync)
        self.sem = {k: es.enter_context(nc.semaphore("s_" + k)) for k in self.e}
        self.cnt = {k: 0 for k in self.e}
        self.seen = {k: {} for k in self.e}
        self.lw = {}
        self.rd = {}
        self.dsem = [es.enter_context(nc.semaphore(f"dq{i}")) for i in range(NDS)]
        self.dcnt = [0] * NDS
        self.dnext = 0

    def semh(self, k):
        return self.dsem[k[1]] if isinstance(k, tuple) else self.sem[k]

    def _wait(self, eng, k, v):
        if self.seen[eng].get(k, 0) < v:
            self.e[eng].wait_ge(self.semh(k), v)
            self.seen[eng][k] = v

    def set_parent(self, child, parent):
        self.parent = getattr(self, 'parent', {})
        self.children = getattr(self, 'children', {})
        self.parent[child] = parent
        self.children.setdefault(parent, []).append(child)

    def _expand(self, bs):
        par = getattr(self, 'parent', {})
        chl = getattr(self, 'children', {})
        out = []
        for b in bs:
            out.append(b)
            if b in par:
                out.append(par[b])
            out.extend(chl.get(b, ()))
        return out

    def _deps(self, eng, reads, writes):
        reads = self._expand(reads)
        writes = self._expand(writes)
        deps = {}
        for b in reads:
            lw = self.lw.get(b)
            if lw:
                deps[lw[0]] = max(deps.get(lw[0], 0), lw[1])
        for b in writes:
            lw = self.lw.get(b)
            if lw:
                deps[lw[0]] = max(deps.get(lw[0], 0), lw[1])
            for k, v in self.rd.get(b, {}).items():
                deps[k] = max(deps.get(k, 0), v)
        for k, v in deps.items():
            if k == 'pe' and eng == 'pe':
                continue
            self._wait(eng, k, v)

    def _mark(self, pt, reads, writes):
        for b in writes:
            self.lw[b] = pt
            self.rd[b] = {}
        for b in reads:
            d = self.rd.setdefault(b, {})
            d[pt[0]] = max(d.get(pt[0], 0), pt[1])

    halt = False

    pe_rg = None
    dummy = None

    def op(self, eng, reads, writes, fn, rg=None):
        if self.halt:
            return None
        if eng == 'pe':
            pass
        self._deps(eng, reads, writes)
        ins = fn(self.e[eng])
        self.cnt[eng] += 1
        ins.then_inc(self.sem[eng], 1)
        self._mark((eng, self.cnt[eng]), reads, writes)
        return ins

    def dma(self, out, in_, reads, writes, q='sp', **kw):
        if self.halt:
            return None
        i = self.dnext % NDS
        self.dnext += 1
        k = ('d', i)
        if self.dcnt[i]:
            self._wait(q, k, self.dcnt[i])
        self._deps(q, reads, writes)
        ins = self.e[q].dma_start(out=out, in_=in_, **kw)
        self.dcnt[i] += 16
        ins.then_inc(self.dsem[i], 16)
        self._mark((k, self.dcnt[i]), reads, writes)
        return ins

    def barrier(self):
        for q in self.e:
            for i in range(NDS):
                if self.dcnt[i]:
                    self._wait(q, ('d', i), self.dcnt[i])
            for k in self.e:
                if k != q and self.cnt[k]:
                    self._wait(q, k, self.cnt[k])
        self.lw = {}
        self.rd = {}

    def finish(self, q='sp'):
        for i in range(NDS):
            if self.dcnt[i]:
                self.e[q].wait_ge(self.dsem[i], self.dcnt[i])
        for k in self.e:
            if k != q and self.cnt[k]:
                self.e[q].wait_ge(self.sem[k], self.cnt[k])


def fm(v):
    v = np.asarray(v, np.float32)
    return np.ascontiguousarray(v.reshape(-1, 128).T)


VEC_SLOTS = {}


def _vec_layout():
    off = 0
    def add(name, n):
        nonlocal off
        VEC_SLOTS[name] = (off, n)
        off += n
    add('n1g', 16); add('n2g', 16); add('adab', 96)
    add('naq', 1); add('nak', 1)
    add('ggb', 8)
    add('gng', 2)
    add('mu_r', 8); add('mu_k', 8); add('mu_v', 8); add('mu_wd', 2); add('mu_ad', 2); add('mu_gd', 2)
    add('w0', 16); add('a0', 16); add('kk', 8); add('ka', 8); add('rk', 8); add('gng_rw', 8); add('gnb_rw', 8)
    return off


NV = _vec_layout()


def pack_vecs(inp, l):
    v = np.zeros((128, NV), np.float32)
    def put(name, arr):
        o, n = VEC_SLOTS[name]
        assert arr.shape == (128, n), (name, arr.shape)
        v[:, o:o + n] = arr
    put('n1g', fm(inp['norm1_g'][l])); put('n2g', fm(inp['norm2_g'][l])); put('adab', fm(inp['ada_b'][l]))
    put('naq', np.tile(inp['na_q_norm'][l], 2)[:, None]); put('nak', np.tile(inp['na_k_norm'][l], 2)[:, None])
    put('ggb', fm(inp['gla_gate_b'][l].reshape(-1)))
    put('gng', fm(inp['gla_norm_g'][l]))
    mu = inp['rw_mu'][l]
    put('mu_r', fm(mu[0:1024])); put('mu_k', fm(mu[1024:2048])); put('mu_v', fm(mu[2048:3072]))
    def pad96(a):
        o = np.zeros((128, 2), np.float32); o[:96, 0] = a[:96]; o[:96, 1] = a[96:192]; return o
    put('mu_wd', pad96(mu[3072:3264])); put('mu_ad', pad96(mu[3264:3456])); put('mu_gd', fm(mu[3456:3712]))
    put('w0', fm(inp['rw_w0'][l].reshape(-1))); put('a0', fm(inp['rw_a0'][l].reshape(-1)))
    put('kk', fm(inp['rw_k_k'][l])); put('ka', fm(inp['rw_k_a'][l])); put('rk', fm(inp['rw_r_k'][l].reshape(-1)))
    put('gng_rw', fm(inp['rw_gn_g'][l])); put('gnb_rw', fm(inp['rw_gn_b'][l]))
    return v


class _Stop(Exception):
    pass


class Builder:
    rw_stop = 0
    rw_slots = 2

    def sc_in(self, name, on=True):
        self.sc_out()
        if on:
            self._sc = (name, self.nc.enter_named_scope(name, False)[0])

    def sc_out(self):
        c = getattr(self, '_sc', None)
        if c:
            self.nc.leave_named_scope(c[0], c[1], False)
        self._sc = None

    def stopat(self, k):
        if self.rw_stop == k:
            self.P.halt = True

    def __init__(self, layers=(0, 1), upto='all', debug=()):
        self.layers = layers
        self.upto = upto
        self.debug = set(debug)
        self.nc = bass.Bass("TRN2", target_bir_lowering=False)
        self.es = contextlib.ExitStack()
        self.dbg_outs = {}

    def din(self, name, shape, dt=F32):
        return self.nc.dram_tensor(name, list(shape), dt, kind="ExternalInput").ap()

    def dout(self, name, shape, dt=F32):
        return self.nc.dram_tensor(name, list(shape), dt, kind="ExternalOutput").ap()

    def dscr(self, name, shape, dt=F32):
        return self.nc.dram_tensor(name, list(shape), dt, kind="Internal").ap()

    def sb(self, st, name, shape, dt=F32):
        self._uid = getattr(self, '_uid', 0) + 1
        return st.enter_context(self.nc.sbuf_tensor(f"{name}_u{self._uid}", list(shape), dt))

    def vec(self, l, name):
        o, n = VEC_SLOTS[name]
        return self.vecs[l][:, o:o + n]

    def build(self):
        nc = self.nc
        es = self.es
        with es:
            self.P = P = Prog(nc, es)
            self.xT0 = self.din("xT0", [D, TT])
            self.cT = self.din("cT", [128, 32])
            self.ada_w = self.din("ada_w", [DEPTH, D, 6 * D])
            self.w_in = self.din("w_in", [DEPTH, D, D_IN])
            self.vecs_d = self.din("vecs", [DEPTH, 128, NV])
            self.outT = self.dout("outT", [D, SEQ // 2])
            self.selw = self.din("selw", [128, 2])
            self.xsel = self.dscr("xsel_s", [D, SEQ // 2])
            self.ysel = self.dscr("ysel_s", [3, 1024, SEQ // 2], BF16)
            self.gsel = self.dscr("gsel_s", [3 * D, SEQ // 2], BF16)
            self.xT = self.dscr("xT_s", [D, TT])
            self.zT = self.dscr("zT_s", [C_GATE, TT])
            self.gT = self.dscr("gT_s", [3 * D, TT], BF16)
            self.vtm = self.dscr("vtm_s", [TT, 2048], BF16)
            self.yT = self.dscr("yT_s", [3, 1024, TT], BF16)
            self.natab = self.din("natab", [DEPTH, 16, 128, 14 * 64])
            self.consts = self.din("consts", [128, NCONST])
            self.gla_w2 = self.din("gla_gate_w2", [DEPTH, 2, 16, 512])
            self.rw_w2 = self.din("rw_w2", [DEPTH, 2, 96, 1024])
            self.rw_a2 = self.din("rw_a2", [DEPTH, 2, 96, 1024])
            self.rw_g2 = self.din("rw_g2", [DEPTH, 256, 1024])
            self.w_branch = self.din("w_branch", [DEPTH, 3, 1024, D])
            self.w_out = self.din("w_out", [DEPTH, D, D])
            self.router_w = self.din("router_w", [D, 16])
            self.router_b = self.din("router_bias", [1, 16])
            self.moe_g = self.din("moe_w_gate", [DEPTH, 16, D, 512])
            self.moe_u = self.din("moe_w_up", [DEPTH, 16, D, 512])
            self.moe_d = self.din("moe_w_down", [DEPTH, 16, 512, D])
            self.ldT = self.dscr("ldT_s", [2, 1024, TT])
            self.aT = self.dscr("aT_s", [2, 1024, TT])
            self.g2T = self.dscr("g2T_s", [1024, TT], BF16)
            self.ones_bf = self.sb(es, "ones_bf", [128, 128], BF16)
            self.vecs = [self.sb(es, f"vecs{l}", [128, NV]) for l in range(DEPTH)]
            self.mod = [self.sb(es, f"mod{l}", [128, 96, 2]) for l in range(DEPTH)]
            self.gm1 = [self.sb(es, f"gm1_{l}", [128, 16, 2]) for l in range(DEPTH)]
            self.gm2 = [self.sb(es, f"gm2_{l}", [128, 16, 2]) for l in range(DEPTH)]
            self.ps = [es.enter_context(nc.psum_tensor(f"ps{i}", [128, 512], F32)) for i in range(7)]
            self.psb = es.enter_context(nc.psum_tensor("psb", [128, 1024], BF16))
            psd = self.psb[:, 512:1024].bitcast(F32)
            P.e['pe']._dummy = lambda e: e.matmul(psd[:, 0:1], lhsT=self.ones_bf[:], rhs=self.ones_bf[:, 0:1], start=True, stop=True)
            cst = self.sb(es, "cst_f", [128, 4 * 128])
            self.cst_bf = self.sb(es, "cst_bf", [128, 4 * 128], BF16)
            P.dma(cst[:], self.consts[:, 0:512], [], ['cst_f'])
            P.op('dve', ['cst_f'], ['cst_bf'], lambda e: e.tensor_copy(out=self.cst_bf[:], in_=cst[:]))
            self.ident_bf = self.cst_bf[:, 0:128]
            self.perm_bf = self.cst_bf[:, 128:256]
            self.mask_f = cst[:, 256:384]
            self.mask_b = cst[:, 384:512]
            self.ident32 = cst[:, 0:128]
            cst2 = self.sb(es, "cst2", [128, 384])
            P.dma(cst2[:], self.consts[:, 512 + 2 * SEQ:512 + 2 * SEQ + 384], [], ['cst_f'])
            self.mask_fs = cst2[:, 0:128]
            self.mask_bs = cst2[:, 128:256]
            self.mask_bd = cst2[:, 256:384]
            P.op('pool', [], ['ones_bf'], lambda e: e.memset(self.ones_bf[:], 1.0))
            self.bd_bf = self.sb(es, "bd_bf", [128, 128], BF16)
            P.op('pool', [], ['bd_bf'], lambda e: e.memset(self.bd_bf[:], 0.0))
            P.op('pool', ['bd_bf'], ['bd_bf'], lambda e: e.memset(self.bd_bf[0:64, 0:64], 1.0))
            P.op('pool', ['bd_bf'], ['bd_bf'], lambda e: e.memset(self.bd_bf[64:128, 64:128], 1.0))
            self.eps_t = self.sb(es, "eps_t", [128, 1])
            P.op('pool', [], ['eps_t'], lambda e: e.memset(self.eps_t[:], EPS))
            for l in range(DEPTH):
                P.dma(self.vecs[l][:], self.vecs_d[l], [], [f'vecs{l}'])

            with nc.named_scope('prologue'):
                self.prologue()
            for l in self.layers:
                self.layer(l)
                if self.upto != 'all':
                    break
            P.finish()
        return nc

    def dbg(self, name, src_ap_fn, shape, dt=F32, reads=()):
        o = self.dout("dbg_" + name, shape, dt)
        self.dbg_outs[name] = o
        src_ap_fn(o)

    def prologue(self):
        nc, P = self.nc, self.P
        with contextlib.ExitStack() as st:
            sc = self.sb(st, "sc", [128, 32])
            sc2 = self.sb(st, "sc2", [128, 32])
            awb = [self.sb(st, f"awb{i}", [128, 16, 512]) for i in range(2)]
            P.dma(sc[:], self.cT, [], ['sc'])
            P.op('act', ['sc'], ['sc2'], lambda e: e.activation(out=sc2[:], in_=sc[:], func=AF.Silu))
            for l in range(DEPTH):
                aw = self.ada_w[l].rearrange("(kc p) c -> p kc c", p=128)
                adab = self.vec(l, 'adab')
                for g in range(24):
                    wt = awb[g % 2]
                    wk = f'awb{g % 2}'
                    P.dma(wt[:], aw[:, :, g * 512:(g + 1) * 512], [], [wk], q=('sp' if g % 2 == 0 else 'act'))
                    pst = self.ps[g % 2]
                    pk = f'ps{g % 2}'
                    for f in range(4):
                        for kc in range(KC):
                            P.op('pe', [wk, 'sc2'], [pk], lambda e, f=f, kc=kc: e.matmul(
                                pst[:, f * 2:(f + 1) * 2], lhsT=wt[:, kc, f * 128:(f + 1) * 128],
                                rhs=sc2[:, kc * 2:(kc + 1) * 2], start=(kc == 0), stop=(kc == KC - 1)))
                    P.op('dve', [pk, f'vecs{l}'], [f'mod{l}'], lambda e, g=g: e.tensor_tensor(
                        out=self.mod[l][:, g * 4:(g + 1) * 4, :],
                        in0=pst[:, 0:8].rearrange("p (f j) -> p f j", j=2),
                        in1=adab[:, g * 4:(g + 1) * 4].unsqueeze(2).to_broadcast([128, 4, 2]), op=ALU.add))
                for (gm, nm, sco, key) in ((self.gm1[l], 'n1g', 16, f'gm1_{l}'), (self.gm2[l], 'n2g', 64, f'gm2_{l}')):
                    P.op('dve', [f'mod{l}'], [key], lambda e, gm=gm, sco=sco: e.tensor_scalar(
                        out=gm[:], in0=self.mod[l][:, sco:sco + 16, :], scalar1=1.0, scalar2=None, op0=ALU.add))
                    P.op('dve', [key, f'vecs{l}'], [key], lambda e, gm=gm, nm=nm: e.tensor_tensor(
                        out=gm[:], in0=gm[:], in1=self.vec(l, nm).unsqueeze(2).to_broadcast([128, 16, 2]), op=ALU.mult))
            if 'mod' in self.debug:
                for l in range(DEPTH):
                    self.dbg(f'mod{l}', lambda o, l=l: P.dma(o, self.mod[l][:].rearrange("p a b -> p (a b)"), [f'mod{l}'], []), [128, 192])
            P.barrier()

    def norm_tile(self, st_bufs, x, xk, n, j, gm, gmk, shmod, shoff, modk, out_fn, outk, ps_i=6):
        P = self.P
        sq, rt = st_bufs
        pst = self.ps[ps_i]
        pk = f'ps{ps_i}'
        P.op('act', [xk], ['nsq'], lambda e: e.activation(out=sq[:, :, :n], in_=x, func=AF.Square))
        for kc in range(KC):
            P.op('pe', ['nsq', 'ones_bf'], [pk], lambda e, kc=kc: e.matmul(
                pst[:, :n], lhsT=self.ones_bf[:], rhs=sq[:, kc, :n], start=(kc == 0), stop=(kc == KC - 1)))
        P.op('act', [pk], ['nrt'], lambda e: e.activation(out=rt[:, :n], in_=pst[:, :n], func=AF.Sqrt,
                                                         scale=1.0 / D, bias=self.eps_t[:, 0:1]))
        P.op('dve', ['nrt'], ['nrt'], lambda e: e.reciprocal(out=rt[:, :n], in_=rt[:, :n]))
        P.op('dve', [xk, 'nrt'], [xk], lambda e: e.tensor_tensor(
            out=x, in0=x, in1=rt[:, :n].unsqueeze(1).to_broadcast([128, KC, n]), op=ALU.mult))
        for kc in range(KC):
            P.op('act', [xk, gmk, modk], [outk], lambda e, kc=kc: e.activation(
                out=out_fn(kc), in_=x[:, kc, :], func=AF.Identity,
                scale=gm[:, kc, j:j + 1], bias=shmod[:, shoff + kc, j:j + 1]))

    def layer(self, l):
        nc, P = self.nc, self.P
        src = self.xT0 if l == 0 else self.xT
        with contextlib.ExitStack() as st:
            hT = self.sb(st, "hT", [128, KC, TT], BF16)
            with contextlib.ExitStack() as st2:
                xb = [self.sb(st2, f"xb{i}", [128, KC, 512]) for i in range(2)]
                sq = self.sb(st2, "nsq", [128, KC, 512], BF16)
                rt = self.sb(st2, "nrt", [128, 512])
                for ti, (t0, n) in enumerate(TILES):
                    x = xb[ti % 2]
                    xk = f'xb{ti % 2}'
                    j = 1 if t0 < NCTX else 0
                    P.dma(x[:, :, :n], src.rearrange("(kc p) t -> p kc t", p=128)[:, :, t0:t0 + n], ['xT'], [xk])
                    self.norm_tile((sq, rt), x[:, :, :n], xk, n, j, self.gm1[l], f'gm1_{l}', self.mod[l], 0, f'mod{l}',
                                   lambda kc, t0=t0, n=n: hT[:, kc, t0:t0 + n], 'hT')
                if 'hT' in self.debug:
                    self.dbg(f'hT{l}', lambda o: P.dma(o.rearrange("(kc p) t -> p kc t", p=128), hT[:], ['hT'], []), [D, TT], BF16)
                P.barrier()
            if self.upto == 'norm':
                return
            with nc.named_scope(f'L{l}_inproj'):
                self.inproj(l, hT)
            P.barrier()
        if self.upto == 'inproj':
            return
        with nc.named_scope(f'L{l}_na'):
            self.na_mixer(l)
        P.barrier()
        if self.upto == 'na':
            return
        with nc.named_scope(f'L{l}_gla'):
            self.gla_mixer(l)
        P.barrier()
        if self.upto == 'gla':
            return
        with nc.named_scope(f'L{l}_rwpre'):
            self.rwkv_pre(l)
        P.barrier()
        if self.upto == 'rwpre':
            self.dbg(f'ld{l}', lambda o: P.dma(o, self.ldT, ['ldT'], []), [2, 1024, TT])
            self.dbg(f'a{l}', lambda o: P.dma(o, self.aT, ['aT'], []), [2, 1024, TT])
            self.dbg(f'g2{l}', lambda o: P.dma(o, self.g2T, ['g2T'], []), [1024, TT], BF16)
            return
        with nc.named_scope(f'L{l}_rwkv'):
            self.rwkv_mixer(l)
        P.halt = False
        P.barrier()
        if self.upto == 'rwkv':
            return
        with nc.named_scope(f'L{l}_mergemoe'):
            self.merge_moe(l)
        P.barrier()

    def inproj(self, l, hT):
        nc, P = self.nc, self.P
        w = self.w_in[l].rearrange("(kc p) c -> p kc c", p=128)
        with contextlib.ExitStack() as st:
            wsl = [self.sb(st, f"wsl{i}", [128, KC, 512], BF16) for i in range(3)]
            zst = [self.sb(st, f"zst{i}", [128, 512]) for i in range(4)]
            gst = [self.sb(st, f"gst{i}", [128, 512], BF16) for i in range(4)]
            segs = [(0, 2048), (C_GQ, C_GV), (C_GR, C_GGD), (C_GGD, C_GGD + 16), (C_GGD + 16, C_RW),
                    (C_RR, C_RWD), (C_RWD, C_RWD + 96), (C_RWD + 96, C_RAD), (C_RAD, C_RAD + 96), (C_RAD + 96, C_RGD),
                    (C_RGD, C_GATE), (C_GATE, D_IN)]
            nload = 0
            nev = 0
            npsum = 0
            for (s0, s1) in segs:
                for b0 in range(s0, s1, 512):
                    bw = min(512, s1 - b0)
                    si = nload % 3
                    nload += 1
                    wk = f'wsl{si}'
                    P.dma(wsl[si][:, :, :bw], w[:, :, b0:b0 + bw], [], [wk], q='pool')
                    for c0 in range(b0, b0 + bw, 128):
                        m = min(128, b0 + bw - c0)
                        for (t0, n) in TILES:
                            pi = npsum % 4
                            npsum += 1
                            pst = self.ps[pi]
                            pk = f'ps{pi}'
                            for kc in range(KC):
                                P.op('pe', [wk, 'hT'], [pk], lambda e, kc=kc, c0=c0, m=m, t0=t0, n=n, si=si, pst=pst: e.matmul(
                                    pst[:m, :n], lhsT=wsl[si][:, kc, c0 - b0:c0 - b0 + m], rhs=hT[:, kc, t0:t0 + n],
                                    start=(kc == 0), stop=(kc == KC - 1)))
                            ei = nev % 4
                            eng = 'act' if nev % 2 == 0 else 'dve'
                            nev += 1
                            if c0 >= C_GATE:
                                P.op('act', [pk], [f'gst{ei}'], lambda e, m=m, n=n, ei=ei, pst=pst: e.activation(
                                    out=gst[ei][:m, :n], in_=pst[:m, :n], func=AF.Sigmoid))
                                P.dma(self.gT[c0 - C_GATE:c0 - C_GATE + m, t0:t0 + n], gst[ei][:m, :n], [f'gst{ei}'], ['gT'])
                            else:
                                if eng == 'act':
                                    P.op('act', [pk], [f'zst{ei}'], lambda e, m=m, n=n, ei=ei, pst=pst: e.activation(
                                        out=zst[ei][:m, :n], in_=pst[:m, :n], func=AF.Copy))
                                else:
                                    P.op('dve', [pk], [f'zst{ei}'], lambda e, m=m, n=n, ei=ei, pst=pst: e.tensor_copy(
                                        out=zst[ei][:m, :n], in_=pst[:m, :n]))
                                P.dma(self.zT[c0:c0 + m, t0:t0 + n], zst[ei][:m, :n], [f'zst{ei}'], ['zT'])
            for (s0, vo) in ((C_NAV, 0), (C_GV, 1024)):
                for b0 in range(0, 1024, 512):
                    si = nload % 3
                    nload += 1
                    wk = f'wsl{si}'
                    P.dma(wsl[si][:, :, :], w[:, :, s0 + b0:s0 + b0 + 512], [], [wk], q='pool')
                    for tt in range(TT // 128):
                        pi = npsum % 4
                        npsum += 1
                        pst = self.ps[pi]
                        pk = f'ps{pi}'
                        for kc in range(KC):
                            P.op('pe', [wk, 'hT'], [pk], lambda e, kc=kc, tt=tt, si=si, pst=pst: e.matmul(
                                pst[:, :], lhsT=hT[:, kc, tt * 128:(tt + 1) * 128], rhs=wsl[si][:, kc, :],
                                start=(kc == 0), stop=(kc == KC - 1)))
                        ei = nev % 4
                        eng = 'act' if nev % 2 == 0 else 'dve'
                        nev += 1
                        if eng == 'act':
                            P.op('act', [pk], [f'gst{ei}'], lambda e, ei=ei, pst=pst: e.activation(
                                out=gst[ei][:, :], in_=pst[:, :], func=AF.Copy))
                        else:
                            P.op('dve', [pk], [f'gst{ei}'], lambda e, ei=ei, pst=pst: e.tensor_copy(
                                out=gst[ei][:, :], in_=pst[:, :]))
                        P.dma(self.vtm[tt * 128:(tt + 1) * 128, vo + b0:vo + b0 + 512], gst[ei][:, :], [f'gst{ei}'], ['vtm'])
            if 'z' in self.debug:
                self.dbg(f'z{l}', lambda o: P.dma(o, self.zT, ['zT'], []), [C_GATE, TT])
                self.dbg(f'g{l}', lambda o: P.dma(o, self.gT, ['gT'], []), [3 * D, TT], BF16)
                self.dbg(f'vtm{l}', lambda o: P.dma(o, self.vtm, ['vtm'], []), [TT, 2048], BF16)


    def na_mixer(self, l):
        nc, P = self.nc, self.P
        with_ctx = l < DEPTH - 1
        vt_all = self.vtm.rearrange("(tt p) c -> p tt c", p=128)
        vt_odd = self.vtm[64:64 + 17 * 128, :].rearrange("(tt p) c -> p tt c", p=128)
        with contextlib.ExitStack() as st:
            zq = [self.sb(st, f"naz{i}", [128, TT]) for i in range(2)]
            sqb = self.sb(st, "nasq", [128, 512], BF16)
            rtb = self.sb(st, "nart", [128, 512])
            qk = [self.sb(st, "qn", [128, TT], BF16), self.sb(st, "kn", [128, TT], BF16)]
            vte = self.sb(st, "vte", [128, 18, 128], BF16)
            vto = self.sb(st, "vto", [128, 17, 128], BF16)
            tbl = [self.sb(st, f"natbl{i}", [128, 14, 64]) for i in range(2)]
            sT = [self.sb(st, f"sT{i}", [128, 4, 64]) for i in range(2)]
            pT = [self.sb(st, f"pT{i}", [128, 6, 64], BF16) for i in range(2)]
            pTc = self.sb(st, "pTc", [128, 2, 256], BF16)
            rsb = [self.sb(st, f"nars{i}", [128, 256]) for i in range(2)]
            yna = [self.sb(st, f"yna{i}", [128, TT], BF16) for i in range(2)]
            g8 = self.sb(st, "g8", [128, 2])
            qm = [self.sb(st, f"qm{i}", [128, TT], BF16) for i in range(2)]
            P.op('dve', [f'vecs{l}'], ['g8'], lambda e: e.tensor_scalar(
                out=g8[:, 0:1], in0=self.vec(l, 'naq'), scalar1=0.125, scalar2=None, op0=ALU.mult))
            P.op('dve', [f'vecs{l}'], ['g8'], lambda e: e.tensor_copy(out=g8[:, 1:2], in_=self.vec(l, 'nak')))
            it = 0
            for hp in range(8):
                for w_, c0 in ((0, C_NAQ), (1, C_NAK)):
                    z = zq[w_]
                    zk = f'naz{w_}'
                    P.dma(z[:], self.zT[c0 + hp * 128:c0 + (hp + 1) * 128, :], ['zT'], [zk])
                    dst = qk[w_]
                    dk = 'qn' if w_ == 0 else 'kn'
                    for (t0, n) in TILES:
                        P.op('act', [zk], ['nasq'], lambda e, z=z, t0=t0, n=n: e.activation(
                            out=sqb[:, :n], in_=z[:, t0:t0 + n], func=AF.Square))
                        P.op('pe', ['nasq', 'bd_bf'], ['ps4'], lambda e, n=n: e.matmul(
                            self.ps[4][:, :n], lhsT=self.bd_bf[:], rhs=sqb[:, :n], start=True, stop=True))
                        P.op('act', ['ps4'], ['nart'], lambda e, n=n: e.activation(
                            out=rtb[:, :n], in_=self.ps[4][:, :n], func=AF.Sqrt, scale=1.0 / 64, bias=self.eps_t[:, 0:1]))
                        P.op('dve', ['nart'], ['nart'], lambda e, n=n: e.reciprocal(out=rtb[:, :n], in_=rtb[:, :n]))
                        P.op('dve', [zk, 'nart', 'g8'], [dk], lambda e, z=z, t0=t0, n=n, dst=dst, w_=w_: e.scalar_tensor_tensor(
                            out=dst[:, t0:t0 + n], in0=z[:, t0:t0 + n], scalar=g8[:, w_:w_ + 1], in1=rtb[:, :n],
                            op0=ALU.mult, op1=ALU.mult))
                for e_ in range(2):
                    P.op('pool', [], [f'qm{e_}'], lambda e, e_=e_: e.memset(qm[e_][:], 0.0))
                    pr = slice(e_ * 64, (e_ + 1) * 64)
                    P.op('act', ['qn', f'qm{e_}'], [f'qm{e_}'], lambda e, e_=e_, pr=pr: e.activation(out=qm[e_][pr, :], in_=qk[0][pr, :], func=AF.Copy))
                P.dma(vte[:], vt_all[:, :, hp * 128:(hp + 1) * 128], ['vtm'], ['vte'])
                P.dma(vto[:], vt_odd[:, :, hp * 128:(hp + 1) * 128], ['vtm'], ['vto'])
                qn, kn = qk
                y = yna[hp % 2]
                yk = f'yna{hp % 2}'
                stages = []
                for e_ in range(2):
                    h = 2 * hp + e_
                    tb = tbl[h % 2]
                    tk = f'natbl{h % 2}'
                    pr = slice(e_ * 64, (e_ + 1) * 64)
                    for r in range(32):
                        rs = min(max(r - 4, 0), 24)
                        dlt = rs - r
                        tq = NCTX + r * 64
                        b = it % 2
                        it += 1
                        psS, psSk = self.ps[b], f'ps{b}'
                        psO, psOk = self.ps[2 + b], f'ps{2 + b}'

                        def s1(e_=e_, h=h, tb=tb, tk=tk, pr=pr, r=r, rs=rs, dlt=dlt, tq=tq, b=b, psS=psS, psSk=psSk):
                            if r == 0:
                                P.dma(tb[:].rearrange("p a b -> p (a b)"), self.natab[l, h], [], [tk])
                            for j in range(6):
                                kt = NCTX + (rs + 2 * j) * 64 if j < 4 else (j - 4) * 128
                                P.op('pe', [f'qm{e_}', 'kn'], [psSk], lambda e, j=j, kt=kt: e.matmul(
                                    psS[:, j * 64:(j + 1) * 64], lhsT=kn[:, kt:kt + 128], rhs=qm[e_][:, tq:tq + 64],
                                    start=True, stop=True))
                            d0 = dlt + 7
                            tv = tb[:].rearrange("p (u two) c -> p u two c", two=2)[:, d0 // 2:d0 // 2 + 4, d0 % 2, :]
                            P.op('dve', [psSk, tk], [f'sT{b}'], lambda e: e.tensor_tensor(
                                out=sT[b][:], in0=psS[:, 0:256].rearrange("p (j c) -> p j c", c=64), in1=tv, op=ALU.add))
                            P.op('act', [f'sT{b}'], [f'pT{b}'], lambda e: e.activation(
                                out=pT[b][:, 0:4, :], in_=sT[b][:], func=AF.Exp))
                            P.op('act', [psSk], [f'pT{b}'], lambda e: e.activation(
                                out=pT[b][:, 4:6, :], in_=psS[:, 256:384].rearrange("p (j c) -> p j c", c=64), func=AF.Exp))

                        def s2(pr=pr, rs=rs, tq=tq, b=b, psO=psO, psOk=psOk):
                            for part in range(2):
                                for j in range(6):
                                    if part == 1:
                                        lhs = self.ones_bf[:, :]
                                        rk_ = 'ones_bf'
                                    elif j >= 4:
                                        lhs = vte[:, j - 4, :]
                                        rk_ = 'vte'
                                    elif rs % 2 == 0:
                                        lhs = vte[:, 2 + rs // 2 + j, :]
                                        rk_ = 'vte'
                                    else:
                                        lhs = vto[:, (rs + 1) // 2 + 1 + j, :]
                                        rk_ = 'vto'
                                    P.op('pe', [rk_, f'pT{b}'], [psOk], lambda e, lhs=lhs, j=j, part=part: e.matmul(
                                        psO[:, part * 64:(part + 1) * 64], lhsT=lhs, rhs=pT[b][:, j, :],
                                        start=(j == 0), stop=(j == 5)))
                            P.op('dve', [psOk], [f'nars{b}'], lambda e: e.reciprocal(
                                out=rsb[b][pr, 0:64], in_=psO[pr, 64:128]))
                            P.op('dve', [psOk, f'nars{b}'], [yk], lambda e: e.tensor_tensor(
                                out=y[pr, tq:tq + 64], in0=psO[pr, 0:64], in1=rsb[b][pr, 0:64], op=ALU.mult))
                        stages.append((s1, s2))
                    if with_ctx:
                        b = it % 2
                        it += 1
                        psS, psSk = self.ps[b], f'ps{b}'
                        psO, psOk = self.ps[2 + b], f'ps{2 + b}'

                        def s1(e_=e_, pr=pr, psS=psS, psSk=psSk):
                            for j in range(2):
                                P.op('pe', [f'qm{e_}', 'kn'], [psSk], lambda e, j=j: e.matmul(
                                    psS[:, j * 256:(j + 1) * 256], lhsT=kn[:, j * 128:(j + 1) * 128], rhs=qm[e_][:, 0:256],
                                    start=True, stop=True))
                            P.op('act', [psSk], ['pTc'], lambda e: e.activation(
                                out=pTc[:].rearrange("p j c -> p (j c)"), in_=psS[:, :], func=AF.Exp))

                        def s2(pr=pr, b=b, psO=psO, psOk=psOk):
                            for part in range(2):
                                for j in range(2):
                                    lhs = self.ones_bf[:, :] if part == 1 else vte[:, j, :]
                                    P.op('pe', ['vte', 'ones_bf', 'pTc'], [psOk], lambda e, lhs=lhs, j=j, part=part: e.matmul(
                                        psO[:, part * 256:(part + 1) * 256], lhsT=lhs, rhs=pTc[:, j, :],
                                        start=(j == 0), stop=(j == 1)))
                            P.op('dve', [psOk], [f'nars{b}'], lambda e: e.reciprocal(
                                out=rsb[b][pr, :], in_=psO[pr, 256:512]))
                            P.op('dve', [psOk, f'nars{b}'], [yk], lambda e: e.tensor_tensor(
                                out=y[pr, 0:256], in0=psO[pr, 0:256], in1=rsb[b][pr, :], op=ALU.mult))
                        stages.append((s1, s2))
                for k_ in range(len(stages) + 1):
                    if k_ < len(stages):
                        stages[k_][0]()
                    if k_ >= 1:
                        stages[k_ - 1][1]()
                t_lo = 0 if with_ctx else NCTX
                P.dma(self.yT[0, hp * 128:(hp + 1) * 128, t_lo:], y[:, t_lo:], [yk], ['yT'])
            if 'na' in self.debug:
                self.dbg(f'yna{l}', lambda o: P.dma(o, self.yT[0], ['yT'], []), [1024, TT], BF16)


    def gla_mixer(self, l):
        nc, P = self.nc, self.P
        with_ctx = l < DEPTH - 1
        NCH = TT // 64
        NT = TT // 128
        qscale = 128 ** -0.5
        vt_all = self.vtm.rearrange("(tt p) c -> p tt c", p=128)
        with contextlib.ExitStack() as st:
            cos = self.sb(st, "cos", [128, SEQ])
            sin = self.sb(st, "sin", [128, SEQ])
            msk = self.sb(st, "cmsk", [128, TT])
            qk32 = [self.sb(st, "gq32", [128, TT]), self.sb(st, "gk32", [128, TT])]
            zb = self.sb(st, "gzb", [128, 512], BF16)
            rz = self.sb(st, "grz", [128, 2, TT])
            gd = [self.sb(st, f"ggd{d}", [16, TT]) for d in range(2)]
            gw2 = self.sb(st, "gw2", [16, 2, 512])
            nb = self.sb(st, "gnb", [128, 8])
            T1 = self.sb(st, "gT1", [128, TT]); T2 = self.sb(st, "gT2", [128, TT]); T3 = self.sb(st, "gT3", [128, TT])
            qi = self.sb(st, "gqi", [128, TT], BF16); kj = self.sb(st, "gkj", [128, TT], BF16)
            kd = self.sb(st, "gkd", [128, TT], BF16); qb = self.sb(st, "gqb", [128, TT], BF16)
            dec = self.sb(st, "gdec", [128, NCH])
            Vt = self.sb(st, "gVt", [128, NT, 256], BF16)
            kdT = self.sb(st, "gkdT", [128, NT, 128], BF16)
            Abf = [self.sb(st, f"gA{i}", [128, 128], BF16) for i in range(2)]
            S32 = self.sb(st, "gS32", [128, 256])
            Sbf = [self.sb(st, f"gSbf{i}", [128, 256], BF16) for i in range(2)]
            yf = self.sb(st, "gyf", [128, 2, TT], BF16)
            yo = self.sb(st, "gyo", [128, 2, TT], BF16)
            yt = [self.sb(st, f"gyt{i}", [128, 2, 128]) for i in range(2)]
            ysq = self.sb(st, "gysq", [128, 2, 128], BF16)
            yrt = self.sb(st, "gyrt", [128, 128])
            P.dma(cos[:], self.consts[:, 512:512 + SEQ], [], ['cos'])
            P.dma(sin[:], self.consts[:, 512 + SEQ:512 + 2 * SEQ], [], ['sin'])
            P.op('pool', [], ['cmsk'], lambda e: e.memset(msk[:], 1.0))
            P.op('pool', ['cmsk'], ['cmsk'], lambda e: e.memset(msk[:].rearrange("p (c j) -> p c j", j=64)[:, :, 0:1], 0.0))
            for d in range(2):
                P.dma(gd[d][:], self.zT[C_GGD + 16 * d:C_GGD + 16 * (d + 1), :], ['zT'], [f'ggd{d}'])
            P.dma(gw2[:], self.gla_w2[l].rearrange("d k c -> k d c"), [], ['gw2'])
            P.op('dve', [f'vecs{l}'], ['gnb'], lambda e: e.tensor_scalar(
                out=nb[:], in0=self.vec(l, 'ggb'), scalar1=-1.0, scalar2=None, op0=ALU.mult))
            gng = self.vec(l, 'gng')
            c3 = lambda t: t[:].rearrange("p (c j) -> p c j", j=64)
            sbi = 0
            for h in range(4):
                for w_, c0 in ((0, C_GQ), (1, C_GK)):
                    z = qk32[w_]
                    zk = 'gq32' if w_ == 0 else 'gk32'
                    P.dma(z[:], self.zT[c0 + h * 128:c0 + (h + 1) * 128, :], ['zT'], [zk])
                    for ti in range(4):
                        t0 = NCTX + ti * 512
                        P.op('act', [zk], ['gzb'], lambda e, z=z, t0=t0: e.activation(out=zb[:], in_=z[:, t0:t0 + 512], func=AF.Copy))
                        P.op('pe', ['gzb', 'cst_bf'], ['ps4'], lambda e: e.matmul(
                            self.ps[4][:, :], lhsT=self.perm_bf, rhs=zb[:], start=True, stop=True))
                        P.op('dve', ['ps4', 'sin'], ['gT3'], lambda e, ti=ti: e.tensor_tensor(
                            out=T3[:, 0:512], in0=self.ps[4][:, :], in1=sin[:, ti * 512:(ti + 1) * 512], op=ALU.mult))
                        P.op('dve', [zk, 'cos'], [zk], lambda e, z=z, t0=t0, ti=ti: e.tensor_tensor(
                            out=z[:, t0:t0 + 512], in0=z[:, t0:t0 + 512], in1=cos[:, ti * 512:(ti + 1) * 512], op=ALU.mult))
                        P.op('dve', [zk, 'gT3'], [zk], lambda e, z=z, t0=t0: e.tensor_tensor(
                            out=z[:, t0:t0 + 512], in0=z[:, t0:t0 + 512], in1=T3[:, 0:512], op=ALU.add))
                q32, k32 = qk32
                P.dma(rz[:], self.zT[C_GR + h * 256:C_GR + (h + 1) * 256, :].rearrange("(c p) t -> p c t", p=128), ['zT'], ['grz'])
                P.op('act', ['grz'], ['grz'], lambda e: e.activation(out=rz[:], in_=rz[:], func=AF.Silu))
                P.dma(Vt[:], vt_all[:, :, 1024 + h * 256:1024 + (h + 1) * 256], ['vtm'], ['gVt'])
                for d in range(2):
                    for (t0, n) in TILES:
                        P.op('pe', ['gw2', f'ggd{d}'], ['ps4'], lambda e, t0=t0, n=n, d=d, h=h: e.matmul(
                            self.ps[4][:, :n], lhsT=gw2[:, d, h * 128:(h + 1) * 128], rhs=gd[d][:, t0:t0 + n], start=True, stop=True), rg=0)
                        P.op('act', ['ps4', 'gnb'], ['gT1'], lambda e, t0=t0, n=n, d=d, h=h: e.activation(
                            out=T1[:, t0:t0 + n], in_=self.ps[4][:, :n], func=AF.Exp, scale=-1.0, bias=nb[:, d * 4 + h:d * 4 + h + 1]))
                    P.op('act', ['gT1'], ['gT1'], lambda e: e.activation(out=T1[:], in_=T1[:], func=AF.Ln, bias=1.0, scale=1.0))
                    P.op('dve', ['cmsk', 'gT1'], ['gT2'], lambda e: e.tensor_tensor_scan(
                        out=T2[:], data0=msk[:], data1=T1[:], initial=0.0, op0=ALU.mult, op1=ALU.add))
                    if d == 1:
                        P.op('dve', ['gT2'], ['gT3'], lambda e: e.tensor_tensor(
                            out=c3(T3), in0=c3(T2)[:, :, 63:64].to_broadcast([128, NCH, 64]), in1=c3(T2), op=ALU.subtract))
                        P.op('dve', ['gT3', 'gT1'], ['gT2'], lambda e: e.tensor_tensor(out=T2[:], in0=T3[:], in1=T1[:], op=ALU.add))
                    tot = c3(T2)[:, :, 63:64] if d == 0 else c3(T2)[:, :, 0:1]
                    cref = c3(T2)[:, :, 32:33] if d == 0 else c3(T2)[:, :, 31:32]
                    P.op('act', ['gT2'], ['gdec'], lambda e, tot=tot: e.activation(
                        out=dec[:].unsqueeze(2), in_=tot, func=AF.Exp, scale=-1.0 / 16))
                    P.op('dve', ['gT2'], ['gT3'], lambda e, cref=cref: e.tensor_tensor(
                        out=c3(T3), in0=c3(T2), in1=cref.to_broadcast([128, NCH, 64]), op=ALU.subtract))
                    P.op('act', ['gT3'], ['gqi'], lambda e: e.activation(out=qi[:], in_=T3[:], func=AF.Exp, scale=-1.0 / 16))
                    P.op('act', ['gT3'], ['gkj'], lambda e: e.activation(out=kj[:], in_=T3[:], func=AF.Exp, scale=1.0 / 16))
                    P.op('act', ['gT2'], ['gqb'], lambda e: e.activation(out=qb[:], in_=T2[:], func=AF.Exp, scale=-1.0 / 16))
                    P.op('dve', ['gT2'], ['gT1'], lambda e, tot=tot: e.tensor_tensor(
                        out=c3(T1), in0=tot.to_broadcast([128, NCH, 64]), in1=c3(T2), op=ALU.subtract))
                    P.op('act', ['gT1'], ['gkd'], lambda e: e.activation(out=kd[:], in_=T1[:], func=AF.Exp, scale=-1.0 / 16))
                    P.op('dve', ['gq32', 'gqi'], ['gqi'], lambda e: e.scalar_tensor_tensor(
                        out=qi[:], in0=qi[:], scalar=qscale, in1=q32[:], op0=ALU.mult, op1=ALU.mult))
                    P.op('dve', ['gk32', 'gkj'], ['gkj'], lambda e: e.tensor_tensor(out=kj[:], in0=kj[:], in1=k32[:], op=ALU.mult))
                    P.op('dve', ['gq32', 'gqb'], ['gqb'], lambda e: e.scalar_tensor_tensor(
                        out=qb[:], in0=qb[:], scalar=qscale, in1=q32[:], op0=ALU.mult, op1=ALU.mult))
                    P.op('dve', ['gk32', 'gkd'], ['gkd'], lambda e: e.tensor_tensor(out=kd[:], in0=kd[:], in1=k32[:], op=ALU.mult))
                    for g0 in range(0, NT, 4):
                        g1 = min(NT, g0 + 4)
                        for tt in range(g0, g1):
                            P.op('pe', ['gkd', 'cst_bf'], ['psb'], lambda e, tt=tt, g0=g0: e.transpose(
                                self.psb[:, (tt - g0) * 128:(tt - g0 + 1) * 128], kd[:, tt * 128:(tt + 1) * 128], self.ident_bf))
                        P.op('act', ['psb'], ['gkdT'], lambda e, g0=g0, g1=g1: e.activation(
                            out=kdT[:, g0:g1, :].rearrange("p a b -> p (a b)"), in_=self.psb[:, 0:(g1 - g0) * 128], func=AF.Copy))
                    P.op('dve', [], ['gS32'], lambda e: e.memset(S32[:], 0.0))
                    P.op('dve', [], [f'gSbf{sbi % 2}'], lambda e, sbi=sbi: e.memset(Sbf[sbi % 2][:], 0.0))
                    order = list(range(NT)) if d == 0 else [1, 0] + list(range(NT - 1, 1, -1))
                    maskd = self.mask_f if d == 0 else self.mask_b
                    for it_, tt in enumerate(order):
                        tsl = slice(tt * 128, (tt + 1) * 128)
                        ab = it_ % 2
                        P.op('pe', ['gkj', 'gqi'], ['ps5'], lambda e, tsl=tsl: e.matmul(
                            self.ps[5][:, 0:128], lhsT=kj[:, tsl], rhs=qi[:, tsl], start=True, stop=True))
                        P.op('dve', ['ps5', 'cst_f'], [f'gA{ab}'], lambda e, ab=ab, maskd=maskd: e.tensor_tensor(
                            out=Abf[ab][:], in0=self.ps[5][:, 0:128], in1=maskd, op=ALU.mult))
                        halves = (0, 1) if d == 0 else (1, 0)
                        psy = [self.ps[0 + 2 * (it_ % 2)], self.ps[1 + 2 * (it_ % 2)]]
                        psyk = [f'ps{0 + 2 * (it_ % 2)}', f'ps{1 + 2 * (it_ % 2)}']
                        for hi, hf in enumerate(halves):
                            csl = slice(hf * 64, (hf + 1) * 64)
                            tok = slice(tt * 128 + hf * 64, tt * 128 + (hf + 1) * 64)
                            sk = f'gSbf{sbi % 2}'
                            Sb = Sbf[sbi % 2]
                            for vc in range(2):
                                if hi == 0:
                                    P.op('pe', ['gVt', f'gA{ab}'], [psyk[vc]], lambda e, vc=vc, tt=tt, ab=ab, psy=psy: e.matmul(
                                        psy[vc][:, 0:128], lhsT=Vt[:, tt, vc * 128:(vc + 1) * 128], rhs=Abf[ab][:], start=True, stop=False))
                                P.op('pe', [sk, 'gqb'], [psyk[vc]], lambda e, vc=vc, Sb=Sb, csl=csl, tok=tok, hi=hi, psy=psy: e.matmul(
                                    psy[vc][:, csl], lhsT=Sb[:, vc * 128:(vc + 1) * 128], rhs=qb[:, tok], start=False, stop=(hi == 1)))
                            ch = tt * 2 + hf
                            P.op('pe', ['gkdT', 'gVt'], ['ps6'], lambda e, csl=csl, tt=tt: e.matmul(
                                self.ps[6][:, 0:256], lhsT=kdT[csl, tt, :], rhs=Vt[csl, tt, :], start=True, stop=True), rg=csl.start)
                            P.op('dve', ['ps6', 'gS32', 'gdec'], ['gS32'], lambda e, ch=ch: e.scalar_tensor_tensor(
                                out=S32[:], in0=S32[:], scalar=dec[:, ch:ch + 1], in1=self.ps[6][:, 0:256], op0=ALU.mult, op1=ALU.add))
                            sbi += 1
                            P.op('act', ['gS32'], [f'gSbf{sbi % 2}'], lambda e, sbi=sbi: e.activation(
                                out=Sbf[sbi % 2][:], in_=S32[:], func=AF.Copy))
                        if d == 0:
                            for vc in range(2):
                                P.op('act', [psyk[vc]], ['gyf'], lambda e, vc=vc, tsl=tsl, psy=psy: e.activation(
                                    out=yf[:, vc, tsl], in_=psy[vc][:, 0:128], func=AF.Copy))
                        elif with_ctx or tt >= 2:
                            ytb = yt[it_ % 2]
                            ytk = f'gyt{it_ % 2}'
                            for vc in range(2):
                                P.op('dve', [psyk[vc], 'gyf'], [ytk], lambda e, vc=vc, tsl=tsl, psy=psy, ytb=ytb: e.tensor_tensor(
                                    out=ytb[:, vc, :], in0=psy[vc][:, 0:128], in1=yf[:, vc, tsl], op=ALU.add))
                            P.op('act', [ytk], ['gysq'], lambda e, ytb=ytb: e.activation(out=ysq[:], in_=ytb[:], func=AF.Square))
                            for vc in range(2):
                                P.op('pe', ['gysq', 'ones_bf'], ['ps4'], lambda e, vc=vc: e.matmul(
                                    self.ps[4][:, 0:128], lhsT=self.ones_bf[:], rhs=ysq[:, vc, :], start=(vc == 0), stop=(vc == 1)))
                            P.op('act', ['ps4'], ['gyrt'], lambda e: e.activation(
                                out=yrt[:], in_=self.ps[4][:, 0:128], func=AF.Sqrt, scale=1.0 / 256, bias=self.eps_t[:, 0:1]))
                            P.op('dve', ['gyrt'], ['gyrt'], lambda e: e.reciprocal(out=yrt[:], in_=yrt[:]))
                            for vc in range(2):
                                P.op('dve', [ytk, 'gyrt', f'vecs{l}'], [ytk], lambda e, vc=vc, ytb=ytb: e.scalar_tensor_tensor(
                                    out=ytb[:, vc, :], in0=ytb[:, vc, :], scalar=gng[:, vc:vc + 1], in1=yrt[:], op0=ALU.mult, op1=ALU.mult))
                                P.op('dve', [ytk, 'grz'], ['gyo'], lambda e, vc=vc, ytb=ytb, tsl=tsl: e.tensor_tensor(
                                    out=yo[:, vc, tsl], in0=ytb[:, vc, :], in1=rz[:, vc, tsl], op=ALU.mult))
                t_lo = 0 if with_ctx else NCTX
                P.dma(self.yT[1, h * 256:(h + 1) * 256, t_lo:].rearrange("(c p) t -> p c t", p=128), yo[:, :, t_lo:], ['gyo'], ['yT'])
            if 'gla' in self.debug:
                self.dbg(f'ygla{l}', lambda o: P.dma(o, self.yT[1], ['yT'], []), [1024, TT], BF16)


    def _shift(self, z, zk, tmp, tmpk, om, hm, m):
        P = self.P
        P.op('dve', [zk], [tmpk], lambda e: e.tensor_tensor(out=tmp[:m, 1:TT - 1], in0=z[:m, 0:TT - 2], in1=z[:m, 2:TT], op=ALU.add))
        for (dst, src_) in ((0, 1), (NCTX - 1, NCTX - 2), (NCTX, NCTX + 1), (TT - 1, TT - 2)):
            P.op('dve', [zk, tmpk], [tmpk], lambda e, dst=dst, src_=src_: e.tensor_copy(out=tmp[:m, dst:dst + 1], in_=z[:m, src_:src_ + 1]))
        P.op('dve', [tmpk], [tmpk], lambda e: e.tensor_scalar(out=tmp[:m, :], in0=tmp[:m, :], scalar1=hm, scalar2=None, op0=ALU.mult))
        P.op('dve', [zk, tmpk], [zk], lambda e: e.scalar_tensor_tensor(out=z[:m, :], in0=z[:m, :], scalar=om, in1=tmp[:m, :], op0=ALU.mult, op1=ALU.add))

    def _mu_prep(self, st, l):
        P = self.P
        o0, _ = VEC_SLOTS['mu_r']
        mu = self.vecs[l][:, o0:o0 + 30]
        om = self.sb(st, "rw_om", [128, 30]); hm = self.sb(st, "rw_hm", [128, 30])
        P.op('dve', [f'vecs{l}'], ['rw_om'], lambda e: e.tensor_scalar(out=om[:], in0=mu, scalar1=-1.0, scalar2=1.0, op0=ALU.mult, op1=ALU.add))
        P.op('dve', [f'vecs{l}'], ['rw_hm'], lambda e: e.tensor_scalar(out=hm[:], in0=mu, scalar1=0.5, scalar2=None, op0=ALU.mult))
        return om, hm

    def rwkv_pre(self, l):
        nc, P = self.nc, self.P
        with contextlib.ExitStack() as st:
            om, hm = self._mu_prep(st, l)
            zt = self.sb(st, "rp_z", [128, TT]); tmp = self.sb(st, "rp_tmp", [128, TT])
            twd = [self.sb(st, f"rp_twd{d}", [96, TT], BF16) for d in range(2)]
            adb = [self.sb(st, f"rp_adb{d}", [96, TT], BF16) for d in range(2)]
            sgd = self.sb(st, "rp_sgd", [128, 2, TT], BF16)
            w2 = self.sb(st, "rp_w2", [96, 2, 1024], BF16); a2 = self.sb(st, "rp_a2", [96, 2, 1024], BF16)
            g2 = self.sb(st, "rp_g2", [128, 2, 1024], BF16)
            stg = [self.sb(st, f"rp_st{i}", [128, 512]) for i in range(4)]
            stb = [self.sb(st, f"rp_sb{i}", [128, 512], BF16) for i in range(2)]
            P.dma(w2[:], self.rw_w2[l].rearrange("d k c -> k d c"), [], ['rp_w2'], q='pool')
            P.dma(a2[:], self.rw_a2[l].rearrange("d k c -> k d c"), [], ['rp_a2'], q='pool')
            P.dma(g2[:], self.rw_g2[l].rearrange("(c p) n -> p c n", p=128), [], ['rp_g2'], q='pool')
            for d in range(2):
                P.dma(zt[:96, :], self.zT[C_RWD + 96 * d:C_RWD + 96 * (d + 1), :], ['zT'], ['rp_z'])
                self._shift(zt, 'rp_z', tmp, 'rp_tmp', om[:96, 24 + d:25 + d], hm[:96, 24 + d:25 + d], 96)
                P.op('act', ['rp_z'], [f'rp_twd{d}'], lambda e, d=d: e.activation(out=twd[d][:], in_=zt[:96, :], func=AF.Tanh))
                P.dma(zt[:96, :], self.zT[C_RAD + 96 * d:C_RAD + 96 * (d + 1), :], ['zT'], ['rp_z'])
                self._shift(zt, 'rp_z', tmp, 'rp_tmp', om[:96, 26 + d:27 + d], hm[:96, 26 + d:27 + d], 96)
                P.op('act', ['rp_z'], [f'rp_adb{d}'], lambda e, d=d: e.activation(out=adb[d][:], in_=zt[:96, :], func=AF.Copy))
            for c in range(2):
                P.dma(zt[:, :], self.zT[C_RGD + 128 * c:C_RGD + 128 * (c + 1), :], ['zT'], ['rp_z'])
                self._shift(zt, 'rp_z', tmp, 'rp_tmp', om[:, 28 + c:29 + c], hm[:, 28 + c:29 + c], 128)
                P.op('act', ['rp_z'], ['rp_sgd'], lambda e, c=c: e.activation(out=sgd[:, c, :], in_=zt[:, :], func=AF.Sigmoid))
            w0 = self.vec(l, 'w0'); a0 = self.vec(l, 'a0')
            n_ = 0
            for hp in range(8):
                cs = slice(hp * 128, (hp + 1) * 128)
                for (t0, n) in TILES:
                    for d in range(2):
                        for which in range(2):
                            pi = n_ % 4; n_ += 1
                            pst, pk = self.ps[pi], f'ps{pi}'
                            wm, src_, bias_ = ((w2, twd[d], w0), (a2, adb[d], a0))[which]
                            rk_ = [('rp_w2', f'rp_twd{d}'), ('rp_a2', f'rp_adb{d}')][which]
                            P.op('pe', list(rk_), [pk], lambda e, wm=wm, src_=src_, d=d, t0=t0, n=n, pst=pst, cs=cs: e.matmul(
                                pst[:, :n], lhsT=wm[:, d, cs], rhs=src_[:, t0:t0 + n], start=True, stop=True), rg=0)
                            sg = stg[pi]
                            P.op('act', [pk, f'vecs{l}'], [f'rp_st{pi}'], lambda e, pst=pst, sg=sg, n=n, bias_=bias_, d=d, hp=hp: e.activation(
                                out=sg[:, :n], in_=pst[:, :n], func=AF.Sigmoid, bias=bias_[:, d * 8 + hp:d * 8 + hp + 1], scale=1.0))
                            if which == 0:
                                P.op('dve', [f'rp_st{pi}'], [f'rp_st{pi}'], lambda e, sg=sg, n=n: e.tensor_scalar(
                                    out=sg[:, :n], in0=sg[:, :n], scalar1=-0.6065306597126334, scalar2=None, op0=ALU.mult))
                                P.dma(self.ldT[d, cs, t0:t0 + n], sg[:, :n], [f'rp_st{pi}'], ['ldT'])
                            else:
                                P.dma(self.aT[d, cs, t0:t0 + n], sg[:, :n], [f'rp_st{pi}'], ['aT'])
                    pi = n_ % 4; n_ += 1
                    pst, pk = self.ps[pi], f'ps{pi}'
                    for c in range(2):
                        P.op('pe', ['rp_g2', 'rp_sgd'], [pk], lambda e, c=c, t0=t0, n=n, pst=pst, cs=cs: e.matmul(
                            pst[:, :n], lhsT=g2[:, c, cs], rhs=sgd[:, c, t0:t0 + n], start=(c == 0), stop=(c == 1)))
                    bi = n_ % 2
                    P.op('act', [pk], [f'rp_sb{bi}'], lambda e, pst=pst, n=n, bi=bi: e.activation(out=stb[bi][:, :n], in_=pst[:, :n], func=AF.Copy))
                    P.dma(self.g2T[cs, t0:t0 + n], stb[bi][:, :n], [f'rp_sb{bi}'], ['g2T'])

    def rwkv_mixer(self, l):
        nc, P = self.nc, self.P
        with_ctx = l < DEPTH - 1
        NCH = TT // 64
        NT = TT // 128
        with contextlib.ExitStack() as st:
            om, hm = self._mu_prep(st, l)
            A = [self.sb(st, f"rwA{i}", [128, TT]) for i in range(7)]
            Ak = [f'rwA{i}' for i in range(7)]
            msk = self.sb(st, "rmsk", [128, TT])
            vb = self.sb(st, "rw_vb", [128, TT], BF16)
            bon = self.sb(st, "rw_bon", [128, TT], BF16)
            gsb = self.sb(st, "rw_g", [128, 512], BF16)
            al = self.sb(st, "rw_al", [128, TT], BF16); rho = self.sb(st, "rw_rho", [128, TT], BF16)
            be = self.sb(st, "rw_be", [128, TT], BF16); ka = self.sb(st, "rw_ka", [128, TT], BF16)
            Bp = self.sb(st, "rw_Bp", [128, TT], BF16); Kp = self.sb(st, "rw_Kp", [128, TT], BF16)
            al_tm = self.sb(st, "rw_altm", [128, NT, 128], BF16); Bp_tm = self.sb(st, "rw_Bptm", [128, NT, 128], BF16)
            Kp_tm = self.sb(st, "rw_Kptm", [128, NT, 128], BF16); V_tm = self.sb(st, "rw_Vtm", [128, NT, 128], BF16)
            etot = self.sb(st, "rw_etot", [128, NCH])
            slots = []
            s0 = dict(XN=[[self.sb(st, f"rw_X{i}", [128, 512], BF16), self.sb(st, f"rw_N{i}", [128, 512], BF16)] for i in range(2)],
                      Q=[self.sb(st, f"rw_Q{i}", [128, 512], BF16) for i in range(2)],
                      LkT=self.sb(st, "rw_LkT", [128, 512], BF16), MbT=self.sb(st, "rw_MbT", [128, 512], BF16), MkT=self.sb(st, "rw_MkT", [128, 512], BF16),
                      H=self.sb(st, "rw_H", [128, 256], BF16)[:], P1n=self.sb(st, "rw_P1n", [128, 256], BF16)[:], G=self.sb(st, "rw_G", [128, 256], BF16)[:],
                      ptmp=self.sb(st, "rw_ptmp", [128, 128])[:], banks=(self.ps[0], self.ps[1], self.ps[2]), bank_ids=(0, 1, 2))
            slots.append(s0)
            a2b = A[2][:].bitcast(BF16)
            cut = lambda i: a2b[:, i * 512:(i + 1) * 512]
            rt = self.sb(st, "rw_rt", [128, 512]); t5 = self.sb(st, "rw_t5", [128, 512])
            t5b = t5[:].bitcast(BF16)
            s1_ = dict(XN=[[cut(0), cut(1)], [cut(2), cut(3)]], Q=[cut(4), cut(5)], LkT=cut(6), MbT=cut(7), MkT=cut(8),
                       H=t5b[:, 0:256], P1n=t5b[:, 256:512], G=t5b[:, 512:768], ptmp=rt[:, 0:128],
                       banks=(self.ps[3], self.ps[5], self.ps[6]), bank_ids=(3, 5, 6))
            slots.append(s1_)
            for nm in ('rw_X0', 'rw_N0', 'rw_X1', 'rw_N1', 'rw_Q0', 'rw_Q1', 'rw_LkT', 'rw_MbT', 'rw_MkT'):
                P.set_parent(nm + '_s1', Ak[2])
            for nm in ('rw_H', 'rw_P1n', 'rw_G'):
                P.set_parent(nm + '_s1', 'rw_t5')
            P.set_parent('rw_ptmp_s1', 'rw_rt')
            a5b = A[5][:].bitcast(BF16); a6b = A[6][:].bitcast(BF16)
            al_m = [a5b[:, 0:TT], a5b[:, TT:2 * TT]]
            rho_m = [a6b[:, 0:TT], a6b[:, TT:2 * TT]]
            P.set_parent('rw_alm', Ak[5]); P.set_parent('rw_rhom', Ak[6])
            R32 = self.sb(st, "rw_R32", [128, TT]); yloc = self.sb(st, "rw_yloc", [128, TT]); yacc = self.sb(st, "rw_yacc", [128, TT])
            Phi = self.sb(st, "rw_Phi", [128, NCH, 128]); Dd = self.sb(st, "rw_D", [128, NCH, 128], BF16)
            Sbd = [self.sb(st, f"rw_S{i}", [128, 128]) for i in range(2)]
            ka1 = self.sb(st, "rw_ka1", [128, 8])
            sq5 = self.sb(st, "rw_sq5", [128, 512], BF16)
            yo = vb
            geps = self.sb(st, "rw_geps", [128, 1])
            P.op('pool', [], ['rw_geps'], lambda e: e.memset(geps[:], 64e-5))
            P.op('pool', [], ['rmsk'], lambda e: e.memset(msk[:], 1.0))
            P.op('pool', ['rmsk'], ['rmsk'], lambda e: e.memset(msk[:].rearrange("p (c j) -> p c j", j=64)[:, :, 0:1], 0.0))
            P.op('dve', [f'vecs{l}'], ['rw_ka1'], lambda e: e.tensor_scalar(
                out=ka1[:], in0=self.vec(l, 'ka'), scalar1=-1.0, scalar2=1.0, op0=ALU.mult, op1=ALU.add))
            c3 = lambda t: t[:].rearrange("p (c j) -> p c j", j=64)
            kkv = self.vec(l, 'kk'); kav = self.vec(l, 'ka'); rkv = self.vec(l, 'rk')
            gng = self.vec(l, 'gng_rw'); gnb = self.vec(l, 'gnb_rw')
            si = 0
            for hp in range(8):
                cs = slice(hp * 128, (hp + 1) * 128)
                self.sc_in('rw_load', hp == 0 and l == 0)
                r32, k32, v32, kk32 = A[0], A[1], A[2], A[3]
                for i_, c0 in enumerate((C_RR, C_RK, C_RV)):
                    P.dma(A[i_][:], self.zT[c0 + hp * 128:c0 + (hp + 1) * 128, :], ['zT'], [Ak[i_]])
                    ci = i_ * 8 + hp
                    self._shift(A[i_], Ak[i_], A[6], Ak[6], om[:, ci:ci + 1], hm[:, ci:ci + 1], 128)
                P.op('act', [Ak[2]], ['rw_vb'], lambda e: e.activation(out=vb[:], in_=v32[:], func=AF.Copy))
                for (srcb, srck, dst, dstk) in ((vb, 'rw_vb', V_tm, 'rw_Vtm'),):
                    for g0 in range(0, NT, 4):
                        g1 = min(NT, g0 + 4)
                        for tt in range(g0, g1):
                            P.op('pe', [srck, 'cst_bf'], ['psb'], lambda e, tt=tt, g0=g0, srcb=srcb: e.transpose(
                                self.psb[:, (tt - g0) * 128:(tt - g0 + 1) * 128], srcb[:, tt * 128:(tt + 1) * 128], self.ident_bf))
                        P.op('act', ['psb'], [dstk], lambda e, g0=g0, g1=g1, dst=dst: e.activation(
                            out=dst[:, g0:g1, :].rearrange("p a b -> p (a b)"), in_=self.psb[:, 0:(g1 - g0) * 128], func=AF.Copy))
                P.op('dve', [Ak[1], f'vecs{l}'], [Ak[3]], lambda e: e.tensor_scalar(
                    out=kk32[:], in0=k32[:], scalar1=kkv[:, hp:hp + 1], scalar2=None, op0=ALU.mult))
                P.op('dve', [Ak[0], Ak[1], f'vecs{l}'], [Ak[6]], lambda e: e.scalar_tensor_tensor(
                    out=A[6][:], in0=r32[:], scalar=rkv[:, hp:hp + 1], in1=k32[:], op0=ALU.mult, op1=ALU.mult))
                for (t0, n) in TILES:
                    P.op('act', [Ak[3]], ['rw_sq5'], lambda e, t0=t0, n=n: e.activation(out=sq5[:, :n], in_=kk32[:, t0:t0 + n], func=AF.Square))
                    P.op('pe', ['rw_sq5', 'bd_bf'], ['ps4'], lambda e, n=n: e.matmul(self.ps[4][:, :n], lhsT=self.bd_bf[:], rhs=sq5[:, :n], start=True, stop=True))
                    P.op('act', ['ps4'], ['rw_rt'], lambda e, n=n: e.activation(out=rt[:, :n], in_=self.ps[4][:, :n], func=AF.Sqrt, scale=1.0, bias=self.eps_t[:, 0:1]))
                    P.op('dve', ['rw_rt'], ['rw_rt'], lambda e, n=n: e.reciprocal(out=rt[:, :n], in_=rt[:, :n]))
                    P.op('dve', [Ak[3], 'rw_rt'], [Ak[3]], lambda e, t0=t0, n=n: e.tensor_tensor(out=kk32[:, t0:t0 + n], in0=kk32[:, t0:t0 + n], in1=rt[:, :n], op=ALU.mult))
                    P.op('act', [Ak[6]], ['rw_sq5'], lambda e, t0=t0, n=n: e.activation(out=sq5[:, :n], in_=A[6][:, t0:t0 + n], func=AF.Copy))
                    P.op('pe', ['rw_sq5', 'bd_bf'], ['ps5'], lambda e, n=n: e.matmul(self.ps[5][:, :n], lhsT=self.bd_bf[:], rhs=sq5[:, :n], start=True, stop=True))
                    P.op('dve', ['ps5', Ak[2]], ['rw_bon'], lambda e, t0=t0, n=n: e.tensor_tensor(out=bon[:, t0:t0 + n], in0=self.ps[5][:, :n], in1=v32[:, t0:t0 + n], op=ALU.mult))
                for d in range(2):
                    self.sc_in('rw_prep', hp == 0 and d == 0 and l == 0)
                    ld, a_, cin, E = A[4], A[5], A[2], A[6]
                    ldk, ak_, cink, Ek = Ak[4], Ak[5], Ak[2], Ak[6]
                    P.dma(ld[:], self.ldT[d, cs, :], ['ldT'], [ldk])
                    P.dma(a_[:], self.aT[d, cs, :], ['aT'], [ak_])
                    P.op('dve', ['rmsk', ldk], [cink], lambda e: e.tensor_tensor_scan(
                        out=cin[:], data0=msk[:], data1=ld[:], initial=0.0, op0=ALU.mult, op1=ALU.add))
                    if d == 1:
                        P.op('dve', [cink], [Ek], lambda e: e.tensor_tensor(
                            out=c3(E), in0=c3(cin)[:, :, 63:64].to_broadcast([128, NCH, 64]), in1=c3(cin), op=ALU.subtract))
                        P.op('dve', [Ek, ldk], [cink], lambda e: e.tensor_tensor(out=cin[:], in0=E[:], in1=ld[:], op=ALU.add))
                    tot = c3(cin)[:, :, 63:64] if d == 0 else c3(cin)[:, :, 0:1]
                    P.op('act', [cink], ['rw_etot'], lambda e, tot=tot: e.activation(out=etot[:].unsqueeze(2), in_=tot, func=AF.Exp))
                    P.op('dve', [cink, ldk], [ldk], lambda e: e.tensor_tensor(out=ld[:], in0=cin[:], in1=ld[:], op=ALU.subtract))
                    P.op('act', [ldk], ['rw_al'], lambda e: e.activation(out=al[:], in_=ld[:], func=AF.Exp))
                    P.op('act', [cink], [Ek], lambda e: e.activation(out=E[:], in_=cin[:], func=AF.Exp))
                    P.op('act', [cink], ['rw_be'], lambda e: e.activation(out=be[:], in_=cin[:], func=AF.Exp, scale=-1.0))
                    P.op('act', [cink], ['rw_ka'], lambda e: e.activation(out=ka[:], in_=cin[:], func=AF.Exp, scale=-1.0))
                    P.op('dve', [Ak[3], 'rw_al'], ['rw_al'], lambda e: e.tensor_tensor(out=al[:], in0=al[:], in1=kk32[:], op=ALU.mult))
                    rho32 = ld
                    P.op('dve', [Ak[0], Ek], [ldk], lambda e: e.tensor_tensor(out=rho32[:], in0=r32[:], in1=E[:], op=ALU.mult))
                    P.op('act', [ldk], ['rw_rho'], lambda e: e.activation(out=rho[:], in_=rho32[:], func=AF.Copy))
                    P.op('dve', ['rw_be', ak_], ['rw_be'], lambda e: e.tensor_tensor(out=be[:], in0=be[:], in1=a_[:], op=ALU.mult))
                    P.op('dve', ['rw_be', Ak[3]], ['rw_be'], lambda e: e.tensor_tensor(out=be[:], in0=be[:], in1=kk32[:], op=ALU.mult))
                    P.op('dve', [ak_, f'vecs{l}', 'rw_ka1'], [ak_], lambda e: e.tensor_scalar(
                        out=a_[:], in0=a_[:], scalar1=kav[:, hp:hp + 1], scalar2=ka1[:, hp:hp + 1], op0=ALU.mult, op1=ALU.add))
                    P.op('dve', [ak_, Ak[1]], [ak_], lambda e: e.tensor_tensor(out=a_[:], in0=a_[:], in1=k32[:], op=ALU.mult))
                    P.op('dve', [ak_, 'rw_ka'], ['rw_ka'], lambda e: e.tensor_tensor(out=ka[:], in0=ka[:], in1=a_[:], op=ALU.mult))
                    eb = etot[:].unsqueeze(2).to_broadcast([128, NCH, 64])
                    P.op('dve', ['rw_be', 'rw_etot'], ['rw_Bp'], lambda e: e.tensor_tensor(out=c3(Bp), in0=c3(be), in1=eb, op=ALU.mult))
                    P.op('dve', ['rw_ka', 'rw_etot'], ['rw_Kp'], lambda e: e.tensor_tensor(out=c3(Kp), in0=c3(ka), in1=eb, op=ALU.mult))
                    self.stopat(1)
                    self.sc_in('rw_tm', hp == 0 and d == 0 and l == 0)
                    for (srcb, srck, dst, dstk) in ((al, 'rw_al', al_tm, 'rw_altm'), (Bp, 'rw_Bp', Bp_tm, 'rw_Bptm'), (Kp, 'rw_Kp', Kp_tm, 'rw_Kptm')):
                        for g0 in range(0, NT, 4):
                            g1 = min(NT, g0 + 4)
                            for tt in range(g0, g1):
                                P.op('pe', [srck, 'cst_bf'], ['psb'], lambda e, tt=tt, g0=g0, srcb=srcb: e.transpose(
                                    self.psb[:, (tt - g0) * 128:(tt - g0 + 1) * 128], srcb[:, tt * 128:(tt + 1) * 128], self.ident_bf))
                            P.op('act', ['psb'], [dstk], lambda e, g0=g0, g1=g1, dst=dst: e.activation(
                                out=dst[:, g0:g1, :].rearrange("p a b -> p (a b)"), in_=self.psb[:, 0:(g1 - g0) * 128], func=AF.Copy))
                    P.op('dve', [], ['rw_alm'], lambda e: e.memset(a5b, 0.0))
                    P.op('dve', [], ['rw_rhom'], lambda e: e.memset(a6b, 0.0))
                    for e_ in range(2):
                        pr = slice(e_ * 64, (e_ + 1) * 64)
                        P.op('act', ['rw_al'], ['rw_alm'], lambda e, e_=e_, pr=pr: e.activation(out=al_m[e_][pr, :], in_=al[pr, :], func=AF.Copy))
                        P.op('act', ['rw_rho'], ['rw_rhom'], lambda e, e_=e_, pr=pr: e.activation(out=rho_m[e_][pr, :], in_=rho[pr, :], func=AF.Copy))
                    self.stopat(2)
                    m_st = self.mask_fs if d == 0 else self.mask_bs
                    m_in = self.mask_f if d == 0 else self.mask_b
                    m_ts = self.mask_bs if d == 0 else self.mask_fs
                    bc4 = lambda m: m.unsqueeze(1).to_broadcast([128, 4, 128])
                    v4 = lambda t: t[:].rearrange("p (a b) -> p a b", b=128)
                    def grp(g0, sl):
                        B_ = slots[sl]
                        pa, pb, pc = B_['banks']
                        pak, pbk, pck = (f'ps{i}' for i in B_['bank_ids'])
                        XNs, Qs, LkT, MbT, MkT, Hh, P1n, Gt, ptmp = B_['XN'], B_['Q'], B_['LkT'], B_['MbT'], B_['MkT'], B_['H'], B_['P1n'], B_['G'], B_['ptmp']
                        kx = lambda nm: f'{nm}_s{sl}'
                        probs = [(tt, e_) for tt in (g0, g0 + 1) for e_ in range(2)]
                        X, N_ = XNs[0]

                        def gram(specs):
                            for pi_, (tt, e_) in enumerate(probs):
                                tsl = slice(tt * 128, (tt + 1) * 128); osl = slice(pi_ * 128, (pi_ + 1) * 128)
                                for (pst, pk, lh, rh, rks) in specs:
                                    lh_ = lh[e_] if isinstance(lh, list) else lh
                                    rh_ = rh[e_] if isinstance(rh, list) else rh
                                    P.op('pe', rks, [pk], lambda e, pst=pst, lh_=lh_, rh_=rh_, tsl=tsl, osl=osl: e.matmul(
                                        pst[:, osl], lhsT=lh_[:, tsl], rhs=rh_[:, tsl], start=True, stop=True))
                        gram(((pa, pak, be, al_m, ['rw_be', 'rw_alm']), (pb, pbk, al_m, be, ['rw_alm', 'rw_be']), (pc, pck, ka, al_m, ['rw_ka', 'rw_alm'])))
                        P.op('dve', [pak, 'cst_f'], [kx('rw_X0')], lambda e: e.scalar_tensor_tensor(
                            out=v4(X), in0=v4(pa), scalar=-1.0, in1=bc4(m_st), op0=ALU.mult, op1=ALU.mult))
                        P.op('dve', [pbk, 'cst_f'], [kx('rw_N0')], lambda e: e.scalar_tensor_tensor(
                            out=v4(N_), in0=v4(pb), scalar=-1.0, in1=bc4(m_ts), op0=ALU.mult, op1=ALU.mult))
                        P.op('dve', [pck, 'cst_f'], [kx('rw_LkT')], lambda e: e.tensor_tensor(out=v4(LkT), in0=v4(pc), in1=bc4(m_st), op=ALU.mult))
                        yield
                        pa2, pa2k = pa, pak
                        gram(((pa2, pa2k, be, rho_m, ['rw_be', 'rw_rhom']), (pb, pbk, ka, rho_m, ['rw_ka', 'rw_rhom'])))
                        P.op('dve', [pa2k, 'cst_f'], [kx('rw_MbT')], lambda e: e.tensor_tensor(out=v4(MbT), in0=v4(pa2), in1=bc4(m_in), op=ALU.mult))
                        P.op('dve', [pbk, 'cst_f'], [kx('rw_MkT')], lambda e: e.tensor_tensor(out=v4(MkT), in0=v4(pb), in1=bc4(m_in), op=ALU.mult))
                        P.op('dve', [kx('rw_X0'), 'cst_bf'], [kx('rw_Q0')], lambda e: e.tensor_tensor(
                            out=v4(Qs[0]), in0=v4(X), in1=self.ident_bf.unsqueeze(1).to_broadcast([128, 4, 128]), op=ALU.add))
                        yield
                        qi_ = 0
                        for j in range(1, 6):
                            Xo, No = XNs[(j - 1) % 2]
                            Xn, Nn = XNs[j % 2]
                            xo_k, no_k = kx(f'rw_X{(j - 1) % 2}'), kx(f'rw_N{(j - 1) % 2}')
                            xn_k, nn_k = kx(f'rw_X{j % 2}'), kx(f'rw_N{j % 2}')
                            for pi_ in range(4):
                                osl = slice(pi_ * 128, (pi_ + 1) * 128)
                                P.op('pe', [xo_k, no_k], [pak], lambda e, osl=osl, Xo=Xo, No=No: e.matmul(
                                    pa[:, osl], lhsT=No[:, osl], rhs=Xo[:, osl], start=True, stop=True))
                                P.op('pe', [xo_k, no_k], [pbk], lambda e, osl=osl, Xo=Xo, No=No: e.matmul(
                                    pb[:, osl], lhsT=Xo[:, osl], rhs=No[:, osl], start=True, stop=True))
                            P.op('act', [pak], [xn_k], lambda e, Xn=Xn: e.activation(out=Xn[:], in_=pa[:], func=AF.Copy))
                            P.op('act', [pbk], [nn_k], lambda e, Nn=Nn: e.activation(out=Nn[:], in_=pb[:], func=AF.Copy))
                            yield
                            Qo, Qn = Qs[qi_ % 2], Qs[(qi_ + 1) % 2]
                            qo_k, qn_k = kx(f'rw_Q{qi_ % 2}'), kx(f'rw_Q{(qi_ + 1) % 2}')
                            for pi_ in range(4):
                                osl = slice(pi_ * 128, (pi_ + 1) * 128)
                                P.op('pe', [nn_k, qo_k], [pck], lambda e, osl=osl, Nn=Nn, Qo=Qo: e.matmul(
                                    pc[:, osl], lhsT=Nn[:, osl], rhs=Qo[:, osl], start=True, stop=True))
                            P.op('dve', [pck, qo_k], [qn_k], lambda e, Qo=Qo, Qn=Qn: e.tensor_tensor(out=Qn[:], in0=pc[:], in1=Qo[:], op=ALU.add))
                            qi_ += 1
                            yield
                        Qf = Qs[qi_ % 2]; qf_k = kx(f'rw_Q{qi_ % 2}')
                        for pi_, (tt, e_) in enumerate(probs):
                            pr = slice(e_ * 64, (e_ + 1) * 64); osl = slice(pi_ * 128, (pi_ + 1) * 128)
                            P.op('pe', [kx('rw_LkT'), 'rw_Vtm'], [pak], lambda e, osl=osl, tt=tt, pr=pr, pi_=pi_: e.matmul(
                                pa[:, pi_ * 64:(pi_ + 1) * 64], lhsT=LkT[:, osl], rhs=V_tm[:, tt, pr], start=True, stop=True))
                            P.op('pe', [qf_k, 'rw_altm'], [pak], lambda e, osl=osl, tt=tt, pr=pr, pi_=pi_, Qf=Qf: e.matmul(
                                pa[:, 256 + pi_ * 64:256 + (pi_ + 1) * 64], lhsT=Qf[:, osl], rhs=al_tm[:, tt, pr], start=True, stop=True))
                        P.op('act', [pak], [kx('rw_H')], lambda e: e.activation(out=Hh, in_=pa[:, 0:256], func=AF.Copy))
                        P.op('act', [pak], [kx('rw_G')], lambda e: e.activation(out=Gt, in_=pa[:, 256:512], func=AF.Copy))
                        yield
                        H3 = Hh.rearrange("p (a b) -> p a b", b=64); G3 = Gt.rearrange("p (a b) -> p a b", b=64); P3 = P1n.rearrange("p (a b) -> p a b", b=64)
                        for pi_, (tt, e_) in enumerate(probs):
                            osl = slice(pi_ * 128, (pi_ + 1) * 128)
                            P.op('pe', [qf_k, kx('rw_H')], [pbk], lambda e, osl=osl, pi_=pi_, Qf=Qf: e.matmul(
                                pb[:, pi_ * 64:(pi_ + 1) * 64], lhsT=Qf[:, osl], rhs=H3[:, pi_, :], start=True, stop=True))
                        P.op('act', [pbk], [kx('rw_P1n')], lambda e: e.activation(out=P1n, in_=pb[:, 0:256], func=AF.Identity, scale=-1.0))
                        yield
                        for ti_, tt in enumerate((g0, g0 + 1)):
                            for e_ in range(2):
                                pi_ = ti_ * 2 + e_
                                pr = slice(e_ * 64, (e_ + 1) * 64); osl = slice(pi_ * 128, (pi_ + 1) * 128)
                                P.op('pe', [kx('rw_G'), kx('rw_MbT')], [pak], lambda e, pr=pr, osl=osl, pi_=pi_, ti_=ti_: e.matmul(
                                    pa[pr, ti_ * 128:(ti_ + 1) * 128], lhsT=G3[:, pi_, :], rhs=MbT[:, osl], start=True, stop=True))
                                P.op('pe', ['rw_Vtm', kx('rw_MkT')], [pbk], lambda e, pr=pr, osl=osl, tt=tt, ti_=ti_: e.matmul(
                                    pb[pr, ti_ * 128:(ti_ + 1) * 128], lhsT=V_tm[:, tt, pr], rhs=MkT[:, osl], start=True, stop=False))
                                P.op('pe', [kx('rw_P1n'), kx('rw_MbT')], [pbk], lambda e, pr=pr, osl=osl, pi_=pi_, ti_=ti_: e.matmul(
                                    pb[pr, ti_ * 128:(ti_ + 1) * 128], lhsT=P3[:, pi_, :], rhs=MbT[:, osl], start=False, stop=True))
                        t2 = slice(g0 * 128, (g0 + 2) * 128)
                        P.op('dve', [pak, ldk], ['rw_R32'], lambda e, t2=t2: e.tensor_tensor(out=R32[:, t2], in0=rho32[:, t2], in1=pa[:, 0:256], op=ALU.subtract))
                        P.op('act', [pbk], ['rw_yloc'], lambda e, t2=t2: e.activation(out=yloc[:, t2], in_=pb[:, 0:256], func=AF.Copy))
                        yield
                        for ti_, tt in enumerate((g0, g0 + 1)):
                            for hf in range(2):
                                csl = slice(hf * 64, (hf + 1) * 64)
                                gsl = G3[csl, ti_ * 2:ti_ * 2 + 2, :].rearrange("p a b -> p (a b)")
                                p1sl = P3[csl, ti_ * 2:ti_ * 2 + 2, :].rearrange("p a b -> p (a b)")
                                col = slice((ti_ * 2 + hf) * 128, (ti_ * 2 + hf + 1) * 128)
                                P.op('pe', [kx('rw_G'), 'rw_Bptm'], [pak], lambda e, gsl=gsl, csl=csl, tt=tt, col=col: e.matmul(
                                    pa[:, col], lhsT=gsl, rhs=Bp_tm[csl, tt, :], start=True, stop=True))
                                P.op('pe', ['rw_Kptm', 'rw_Vtm'], [pck], lambda e, csl=csl, tt=tt, col=col: e.matmul(
                                    pc[:, col], lhsT=Kp_tm[csl, tt, :], rhs=V_tm[csl, tt, :], start=True, stop=False))
                                P.op('pe', ['rw_Bptm', kx('rw_P1n')], [pck], lambda e, csl=csl, tt=tt, col=col, p1sl=p1sl: e.matmul(
                                    pc[:, col], lhsT=Bp_tm[csl, tt, :], rhs=p1sl, start=False, stop=True))
                        for q_ in range(4):
                            ch = g0 * 2 + q_
                            col = slice(q_ * 128, (q_ + 1) * 128)
                            P.op('dve', [pak, 'cst_f'], [kx('rw_ptmp')], lambda e, col=col: e.tensor_tensor(out=ptmp, in0=pa[:, col], in1=self.mask_bd, op=ALU.mult))
                            P.op('dve', [kx('rw_ptmp'), 'rw_etot', 'cst_f'], ['rw_Phi'], lambda e, ch=ch: e.scalar_tensor_tensor(
                                out=Phi[:, ch, :], in0=self.ident32, scalar=etot[:, ch:ch + 1], in1=ptmp, op0=ALU.mult, op1=ALU.subtract))
                        P.op('dve', [pck, 'cst_f'], ['rw_D'], lambda e, g0=g0: e.tensor_tensor(
                            out=Dd[:, g0 * 2:g0 * 2 + 4, :], in0=v4(pc), in1=bc4(self.mask_bd), op=ALU.mult))

                    self.sc_in('rw_grp', hp == 0 and d == 0 and l == 0)
                    pending = list(range(0, NT, 2))
                    active = {}
                    while pending or active:
                        for sl in range(self.rw_slots):
                            if sl not in active and pending:
                                active[sl] = grp(pending.pop(0), sl)
                        for sl in list(active):
                            try:
                                self._steps = getattr(self, '_steps', 0) + 1
                                if self._steps == self.rw_stop:
                                    P.halt = True
                                next(active[sl])
                            except StopIteration:
                                del active[sl]
                    self.stopat(7)
                    self.sc_in('rw_rec', hp == 0 and d == 0 and l == 0)
                    P.op('dve', [], [f'rw_S{si % 2}'], lambda e, si=si: e.memset(Sbd[si % 2][:], 0.0))
                    order = list(range(NCH)) if d == 0 else [3, 2, 1, 0] + list(range(NCH - 1, 3, -1))
                    for n_i, ch in enumerate(order):
                        S_ = Sbd[si % 2]; sk = f'rw_S{si % 2}'
                        tok = slice(ch * 64, (ch + 1) * 64)
                        yb = n_i % 2
                        need_y = with_ctx or ch >= 4
                        if need_y:
                            P.op('pe', [sk, 'rw_R32'], [f'ps{3 + yb}'], lambda e, S_=S_, tok=tok, yb=yb: e.matmul(
                                self.ps[3 + yb][:, 0:64], lhsT=S_[:], rhs=R32[:, tok], start=True, stop=True))
                            if d == 0:
                                P.op('dve', [f'ps{3 + yb}', 'rw_yloc'], ['rw_yacc'], lambda e, tok=tok, yb=yb: e.tensor_tensor(
                                    out=yacc[:, tok], in0=self.ps[3 + yb][:, 0:64], in1=yloc[:, tok], op=ALU.add))
                            else:
                                P.op('dve', [f'ps{3 + yb}', 'rw_yloc'], ['rw_yloc'], lambda e, tok=tok, yb=yb: e.tensor_tensor(
                                    out=yloc[:, tok], in0=self.ps[3 + yb][:, 0:64], in1=yloc[:, tok], op=ALU.add))
                                P.op('pool', ['rw_yloc', 'rw_yacc'], ['rw_yacc'], lambda e, tok=tok: e.tensor_tensor(
                                    out=yacc[:, tok], in0=yacc[:, tok], in1=yloc[:, tok], op=ALU.add))
                        P.op('pe', [sk, 'rw_Phi'], [f'ps{5 + yb}'], lambda e, S_=S_, ch=ch, yb=yb: e.matmul(
                            self.ps[5 + yb][:, 0:128], lhsT=Phi[:, ch, :], rhs=S_[:], start=True, stop=True))
                        si += 1
                        P.op('dve', [f'ps{5 + yb}', 'rw_D'], [f'rw_S{si % 2}'], lambda e, ch=ch, yb=yb, si=si: e.tensor_tensor(
                            out=Sbd[si % 2][:], in0=self.ps[5 + yb][:, 0:128], in1=Dd[:, ch, :], op=ALU.add))
                    self.stopat(8)
                self.sc_in('rw_fin', hp == 0 and l == 0)
                for (t0, n) in TILES:
                    if not with_ctx and t0 < NCTX:
                        continue
                    ya = yacc[:, t0:t0 + n]
                    P.op('act', ['rw_yacc'], ['rw_sq5'], lambda e, ya=ya, n=n: e.activation(out=sq5[:, :n], in_=ya, func=AF.Copy))
                    P.op('pe', ['rw_sq5', 'bd_bf'], ['ps4'], lambda e, n=n: e.matmul(self.ps[4][:, :n], lhsT=self.bd_bf[:], rhs=sq5[:, :n], start=True, stop=True))
                    P.op('dve', ['ps4', 'rw_yacc'], ['rw_t5'], lambda e, ya=ya, n=n: e.scalar_tensor_tensor(
                        out=t5[:, :n], in0=self.ps[4][:, :n], scalar=-1.0 / 64, in1=ya, op0=ALU.mult, op1=ALU.add))
                    P.op('act', ['rw_t5'], ['rw_sq5'], lambda e, n=n: e.activation(out=sq5[:, :n], in_=t5[:, :n], func=AF.Square))
                    P.op('pe', ['rw_sq5', 'bd_bf'], ['ps4'], lambda e, n=n: e.matmul(self.ps[4][:, :n], lhsT=self.bd_bf[:], rhs=sq5[:, :n], start=True, stop=True))
                    P.op('act', ['ps4'], ['rw_rt'], lambda e, n=n: e.activation(out=rt[:, :n], in_=self.ps[4][:, :n], func=AF.Sqrt, scale=1.0 / 64, bias=geps[:, 0:1]))
                    P.op('dve', ['rw_rt'], ['rw_rt'], lambda e, n=n: e.reciprocal(out=rt[:, :n], in_=rt[:, :n]))
                    P.op('dve', ['rw_t5', 'rw_rt'], ['rw_t5'], lambda e, n=n: e.tensor_tensor(out=t5[:, :n], in0=t5[:, :n], in1=rt[:, :n], op=ALU.mult))
                    P.op('dve', ['rw_t5', f'vecs{l}'], ['rw_t5'], lambda e, n=n: e.tensor_scalar(
                        out=t5[:, :n], in0=t5[:, :n], scalar1=gng[:, hp:hp + 1], scalar2=gnb[:, hp:hp + 1], op0=ALU.mult, op1=ALU.add))
                    P.op('dve', ['rw_t5', 'rw_bon'], ['rw_t5'], lambda e, t0=t0, n=n: e.tensor_tensor(out=t5[:, :n], in0=t5[:, :n], in1=bon[:, t0:t0 + n], op=ALU.add))
                    P.dma(gsb[:, :n], self.g2T[cs, t0:t0 + n], ['g2T'], ['rw_g'])
                    P.op('dve', ['rw_t5', 'rw_g'], ['rw_vb'], lambda e, t0=t0, n=n: e.tensor_tensor(out=yo[:, t0:t0 + n], in0=t5[:, :n], in1=gsb[:, :n], op=ALU.mult))
                self.sc_out()
                t_lo = 0 if with_ctx else NCTX
                P.dma(self.yT[2, cs, t_lo:], yo[:, t_lo:], ['rw_vb'], ['yT'])
            if 'rwkv' in self.debug:
                self.dbg(f'yrw{l}', lambda o: P.dma(o, self.yT[2], ['yT'], []), [1024, TT], BF16)


    def select_pass(self):
        nc, P = self.nc, self.P
        H = SEQ // 2
        with contextlib.ExitStack() as st:
            sw = self.sb(st, "sp_w", [128, 2])
            P.dma(sw[:], self.selw, [], ['sp_w'])
            fa = [self.sb(st, f"sp_fa{i}", [128, 4, H]) for i in range(2)]
            fb = [self.sb(st, f"sp_fb{i}", [128, 4, H]) for i in range(2)]
            ha = [self.sb(st, f"sp_ha{i}", [128, 8, H], BF16) for i in range(2)]
            hb = [self.sb(st, f"sp_hb{i}", [128, 8, H], BF16) for i in range(2)]
            jobs = []
            xv = self.xT.rearrange("(c p) t -> p c t", p=128)
            xo = self.xsel.rearrange("(c p) t -> p c t", p=128)
            for c0 in range(0, KC, 4):
                jobs.append((fa, fb, 'f', xv[:, c0:c0 + 4, :], xo[:, c0:c0 + 4, :], 'xT'))
            yv = self.yT.rearrange("i (c p) t -> p (i c) t", p=128)
            yo = self.ysel.rearrange("i (c p) t -> p (i c) t", p=128)
            for c0 in range(0, 24, 8):
                jobs.append((ha, hb, 'h', yv[:, c0:c0 + 8, :], yo[:, c0:c0 + 8, :], 'yT'))
            gv = self.gT.rearrange("(c p) t -> p c t", p=128)
            go = self.gsel.rearrange("(c p) t -> p c t", p=128)
            for c0 in range(0, 48, 8):
                jobs.append((ha, hb, 'h', gv[:, c0:c0 + 8, :], go[:, c0:c0 + 8, :], 'gT'))
            cnt = {'f': 0, 'h': 0}
            for (ta, tb, kind, sv, dv, rk_) in jobs:
                i = cnt[kind] % 2
                cnt[kind] += 1
                a, b = ta[i], tb[i]
                ak, bk = f'sp_{kind}a{i}', f'sp_{kind}b{i}'
                P.dma(a[:], sv[:, :, NCTX:NCTX + H], [rk_], [ak])
                P.dma(b[:], sv[:, :, NCTX + H:NCTX + 2 * H], [rk_], [bk], q='act')
                P.op('dve', [ak, 'sp_w'], [ak], lambda e, a=a: e.tensor_scalar(
                    out=a[:], in0=a[:], scalar1=sw[:, 0:1], scalar2=None, op0=ALU.mult))
                P.op('dve', [ak, bk, 'sp_w'], [ak], lambda e, a=a, b=b: e.scalar_tensor_tensor(
                    out=a[:], in0=b[:], scalar=sw[:, 1:2], in1=a[:], op0=ALU.mult, op1=ALU.add))
                P.dma(dv, a[:], [ak], ['sel_out'])

    def merge_moe(self, l):
        nc, P = self.nc, self.P
        with_ctx = l < DEPTH - 1
        last = l == DEPTH - 1
        src = (self.xT0 if l == 0 else self.xT).rearrange("(kc p) t -> p kc t", p=128)
        dstx = self.xT.rearrange("(kc p) t -> p kc t", p=128)
        dsto = self.outT.rearrange("(kc p) t -> p kc t", p=128)
        gTv = self.gT.rearrange("(i c p) t -> p i c t", i=3, p=128)
        yTv = self.yT.rearrange("i (c p) t -> p i c t", p=128)
        tiles = TILES
        if last:
            self.select_pass()
            P.barrier()
            src = self.xsel.rearrange("(kc p) t -> p kc t", p=128)
            gTv = self.gsel.rearrange("(i c p) t -> p i c t", i=3, p=128)
            yTv = self.ysel.rearrange("i (c p) t -> p i c t", p=128)
            tiles = [(0, 512), (512, 512)]
        wbv = self.w_branch[l].rearrange("i (kc p) c -> p i kc c", p=128)
        wov = self.w_out[l].rearrange("(kc p) c -> p kc c", p=128)
        BIG = 1.0e4
        with contextlib.ExitStack() as st:
            xg = self.sb(st, "mm_xg", [128, KC, 512])
            rw32 = self.sb(st, "mm_rw32", [128, KC, 16])
            rbias = self.sb(st, "mm_rbias", [128, 16])
            sel = self.sb(st, "mm_sel", [16, 16, 128])
            P.dma(rw32[:], self.router_w.rearrange("(kc p) e -> p kc e", p=128), [], ['mm_rw32'])
            P.dma(rbias[:], self.router_b[0:1, :].partition_broadcast(128), [], ['mm_rbias'])
            P.op('dve', ['cst_f'], ['mm_sel'], lambda e: e.tensor_copy(
                out=sel[:], in_=self.ident32[0:16, 0:16].unsqueeze(2).to_broadcast([16, 16, 128])))
            wld = 0
            for (t0, n) in tiles:
                if not last and t0 < NCTX and not with_ctx:
                    continue
                j = 1 if (t0 < NCTX and not last) else 0
                nb = n // 128
                P.dma(xg[:, :, :n], src[:, :, t0:t0 + n], ['xT'], ['mm_xg'])
                with contextlib.ExitStack() as s2:
                    yb = self.sb(s2, "mm_y", [128, 3, 8, 512], BF16)
                    mg = self.sb(s2, "mm_mg", [128, KC, 512], BF16)
                    gb = [self.sb(s2, f"mm_g{i}", [128, 3, 512], BF16) for i in range(2)]
                    wb = [self.sb(s2, f"mm_wb{i}", [128, 3, 8, 512], BF16) for i in range(2)]
                    wo = [self.sb(s2, f"mm_wo{i}", [128, KC, 512], BF16) for i in range(2)]
                    m32 = self.sb(s2, "mm_m32", [128, 512])
                    tm = self.sb(s2, "mm_tm", [128, 512])
                    for i in range(3):
                        P.dma(yb[:, i, :, :n], yTv[:, i, :, t0:t0 + n], ['yT'], ['mm_y'])
                    pi = 0
                    for cb in range(4):
                        wk = f'mm_wb{cb % 2}'
                        for i in range(3):
                            P.dma(wb[cb % 2][:, i], wbv[:, i, :, cb * 512:(cb + 1) * 512], [], [wk], q='pool')
                        for dcl in range(4):
                            dc = cb * 4 + dcl
                            gk = f'mm_g{dc % 2}'
                            P.dma(gb[dc % 2][:, :, :n], gTv[:, :, dc, t0:t0 + n], ['gT'], [gk])
                            for i in range(3):
                                pst, pk = self.ps[pi % 4], f'ps{pi % 4}'
                                pi += 1
                                for kc in range(8):
                                    P.op('pe', [wk, 'mm_y'], [pk], lambda e, i=i, kc=kc, cb=cb, dcl=dcl, pst=pst: e.matmul(
                                        pst[:, :n], lhsT=wb[cb % 2][:, i, kc, dcl * 128:(dcl + 1) * 128], rhs=yb[:, i, kc, :n],
                                        start=(kc == 0), stop=(kc == 7)))
                                if i == 0:
                                    P.op('dve', [pk, gk], ['mm_m32'], lambda e, pst=pst, dc=dc: e.tensor_tensor(
                                        out=m32[:, :n], in0=pst[:, :n], in1=gb[dc % 2][:, 0, :n], op=ALU.mult))
                                else:
                                    P.op('dve', [pk, gk], ['mm_tm'], lambda e, pst=pst, dc=dc, i=i: e.tensor_tensor(
                                        out=tm[:, :n], in0=pst[:, :n], in1=gb[dc % 2][:, i, :n], op=ALU.mult))
                                    if i == 1:
                                        P.op('dve', ['mm_tm', 'mm_m32'], ['mm_m32'], lambda e: e.tensor_tensor(
                                            out=m32[:, :n], in0=m32[:, :n], in1=tm[:, :n], op=ALU.add))
                                    else:
                                        P.op('dve', ['mm_tm', 'mm_m32'], ['mm_mg'], lambda e, dc=dc: e.tensor_tensor(
                                            out=mg[:, dc, :n], in0=m32[:, :n], in1=tm[:, :n], op=ALU.add))
                    for cb in range(4):
                        wk = f'mm_wo{cb % 2}'
                        P.dma(wo[cb % 2][:], wov[:, :, cb * 512:(cb + 1) * 512], [], [wk], q='pool')
                        for dcl in range(4):
                            dc = cb * 4 + dcl
                            pst, pk = self.ps[pi % 4], f'ps{pi % 4}'
                            pi += 1
                            for kc in range(KC):
                                P.op('pe', [wk, 'mm_mg'], [pk], lambda e, kc=kc, cb=cb, dcl=dcl, pst=pst: e.matmul(
                                    pst[:, :n], lhsT=wo[cb % 2][:, kc, dcl * 128:(dcl + 1) * 128], rhs=mg[:, kc, :n],
                                    start=(kc == 0), stop=(kc == KC - 1)))
                            P.op('dve', [pk, 'mm_xg', f'mod{l}'], ['mm_xg'], lambda e, pst=pst, dc=dc: e.scalar_tensor_tensor(
                                out=xg[:, dc, :n], in0=pst[:, :n], scalar=self.mod[l][:, 32 + dc, j:j + 1], in1=xg[:, dc, :n],
                                op0=ALU.mult, op1=ALU.add))
                    if 'x1' in self.debug:
                        self.dbg(f'x1_{l}_{t0}', lambda o: P.dma(o.rearrange("(kc p) t -> p kc t", p=128), xg[:, :, :n], ['mm_xg'], []), [D, n])
                    P.barrier()
                with contextlib.ExitStack() as s2:
                    h2 = self.sb(s2, "mo_h2", [128, KC, 512], BF16)
                    sq = h2
                    rt = self.sb(s2, "mo_rt", [128, 512])
                    lg = self.sb(s2, "mo_lg", [16, 512])
                    R = {nm: self.sb(s2, "mo_" + nm, [128, 4, 16]) for nm in ('s', 'bz', 'eq', 'b2', 'mb', 'e1', 'w')}
                    r4 = {nm: self.sb(s2, "mo_" + nm, [128, 4, 4]) for nm in ('m1', 'm2', 'gsel')}
                    r1 = {nm: self.sb(s2, "mo_" + nm, [128, 4]) for nm in ('gmax', 't1', 't2', 'ws')}
                    cT = self.sb(s2, "mo_cT", [16, 512])
                    bce = [self.sb(s2, f"mo_bce{i}", [128, 512]) for i in range(2)]
                    sg = [self.sb(s2, f"mo_sg{i}", [128, 512]) for i in range(2)]
                    act = [self.sb(s2, f"mo_act{i}", [128, 4, 512], BF16) for i in range(2)]
                    wg = [self.sb(s2, f"mo_wg{i}", [128, KC, 512], BF16) for i in range(2)]
                    wu = [self.sb(s2, f"mo_wu{i}", [128, KC, 512], BF16) for i in range(2)]
                    s3 = contextlib.ExitStack()
                    xn = self.sb(s3, "mo_xn", [128, KC, 512])
                    P.op('act', ['mm_xg'], ['mo_h2'], lambda e: e.activation(out=sq[:, :, :n], in_=xg[:, :, :n], func=AF.Square))
                    for kc in range(KC):
                        P.op('pe', ['mo_h2', 'ones_bf'], ['ps6'], lambda e, kc=kc: e.matmul(
                            self.ps[6][:, :n], lhsT=self.ones_bf[:], rhs=sq[:, kc, :n], start=(kc == 0), stop=(kc == KC - 1)))
                    P.op('act', ['ps6'], ['mo_rt'], lambda e: e.activation(out=rt[:, :n], in_=self.ps[6][:, :n], func=AF.Sqrt,
                                                                      scale=1.0 / D, bias=self.eps_t[:, 0:1]))
                    P.op('dve', ['mo_rt'], ['mo_rt'], lambda e: e.reciprocal(out=rt[:, :n], in_=rt[:, :n]))
                    P.op('dve', ['mm_xg', 'mo_rt'], ['mo_xn'], lambda e: e.tensor_tensor(
                        out=xn[:, :, :n], in0=xg[:, :, :n], in1=rt[:, :n].unsqueeze(1).to_broadcast([128, KC, n]), op=ALU.mult))
                    for kc in range(KC):
                        P.op('act', ['mo_xn', f'gm2_{l}', f'mod{l}'], ['mo_xn'], lambda e, kc=kc: e.activation(
                            out=xn[:, kc, :n], in_=xn[:, kc, :n], func=AF.Identity,
                            scale=self.gm2[l][:, kc, j:j + 1], bias=self.mod[l][:, 48 + kc, j:j + 1]))
                    P.op('dve', ['mo_xn'], ['mo_h2'], lambda e: e.tensor_copy(out=h2[:, :, :n], in_=xn[:, :, :n]))
                    for kc in range(KC):
                        P.op('pe', ['mo_xn', 'mm_rw32'], ['ps5'], lambda e, kc=kc: e.matmul(
                            self.ps[5][0:16, :n], lhsT=rw32[:, kc, :], rhs=xn[:, kc, :n], start=(kc == 0), stop=(kc == KC - 1)))
                    P.op('act', ['ps5'], ['mo_lg'], lambda e: e.activation(out=lg[:, :n], in_=self.ps[5][0:16, :n], func=AF.Copy))
                    for b_ in range(nb):
                        P.op('pe', ['mo_lg', 'cst_f'], ['ps4'], lambda e, b_=b_: e.transpose(
                            self.ps[4][:, b_ * 16:(b_ + 1) * 16], lg[0:16, b_ * 128:(b_ + 1) * 128], self.ident32[0:16, 0:16]))
                    s_, bz, eq, b2, mb, e1, w_ = (R[k][:, :nb, :] for k in ('s', 'bz', 'eq', 'b2', 'mb', 'e1', 'w'))
                    m1, m2, gsel = (r4[k][:, :nb, :] for k in ('m1', 'm2', 'gsel'))
                    gmax, t1, t2, ws = (r1[k][:, :nb] for k in ('gmax', 't1', 't2', 'ws'))
                    g4 = lambda a: a.rearrange("p b (g k) -> p b g k", k=4)
                    V = lambda reads, writes, fn: P.op('dve', reads, writes, fn)
                    P.op('act', ['ps4'], ['mo_s'], lambda e: e.activation(
                        out=s_, in_=self.ps[4][:, 0:nb * 16].rearrange("p (b k) -> p b k", k=16), func=AF.Sigmoid))
                    V(['mo_s', 'mm_rbias'], ['mo_bz'], lambda e: e.tensor_tensor(out=bz, in0=s_, in1=rbias[:].unsqueeze(1).to_broadcast([128, nb, 16]), op=ALU.add))
                    V(['mo_bz'], ['mo_m1'], lambda e: e.tensor_reduce(out=m1, in_=g4(bz), axis=AX.X, op=ALU.max))
                    V(['mo_bz', 'mo_m1'], ['mo_eq'], lambda e: e.tensor_tensor(out=g4(eq), in0=g4(bz), in1=m1.unsqueeze(3).to_broadcast([128, nb, 4, 4]), op=ALU.is_equal))
                    V(['mo_eq', 'mo_bz'], ['mo_b2'], lambda e: e.scalar_tensor_tensor(out=b2, in0=eq, scalar=-BIG, in1=bz, op0=ALU.mult, op1=ALU.add))
                    V(['mo_b2'], ['mo_m2'], lambda e: e.tensor_reduce(out=m2, in_=g4(b2), axis=AX.X, op=ALU.max))
                    V(['mo_m1', 'mo_m2'], ['mo_m1'], lambda e: e.tensor_tensor(out=m1, in0=m1, in1=m2, op=ALU.add))
                    V(['mo_m1'], ['mo_gmax'], lambda e: e.tensor_reduce(out=gmax, in_=m1, axis=AX.X, op=ALU.max))
                    V(['mo_m1', 'mo_gmax'], ['mo_gsel'], lambda e: e.tensor_tensor(out=gsel, in0=m1, in1=gmax.unsqueeze(2).to_broadcast([128, nb, 4]), op=ALU.is_equal))
                    V(['mo_gsel'], ['mo_gsel'], lambda e: e.tensor_scalar(out=gsel, in0=gsel, scalar1=BIG, scalar2=-BIG, op0=ALU.mult, op1=ALU.add))
                    V(['mo_bz', 'mo_gsel'], ['mo_mb'], lambda e: e.tensor_tensor(out=g4(mb), in0=g4(bz), in1=gsel.unsqueeze(3).to_broadcast([128, nb, 4, 4]), op=ALU.add))
                    V(['mo_mb'], ['mo_t1'], lambda e: e.tensor_reduce(out=t1, in_=mb, axis=AX.X, op=ALU.max))
                    V(['mo_mb', 'mo_t1'], ['mo_e1'], lambda e: e.tensor_tensor(out=e1, in0=mb, in1=t1.unsqueeze(2).to_broadcast([128, nb, 16]), op=ALU.is_equal))
                    V(['mo_e1', 'mo_mb'], ['mo_b2'], lambda e: e.scalar_tensor_tensor(out=b2, in0=e1, scalar=-BIG, in1=mb, op0=ALU.mult, op1=ALU.add))
                    V(['mo_b2'], ['mo_t2'], lambda e: e.tensor_reduce(out=t2, in_=b2, axis=AX.X, op=ALU.max))
                    V(['mo_b2', 'mo_t2'], ['mo_eq'], lambda e: e.tensor_tensor(out=eq, in0=b2, in1=t2.unsqueeze(2).to_broadcast([128, nb, 16]), op=ALU.is_equal))
                    V(['mo_eq', 'mo_e1'], ['mo_e1'], lambda e: e.tensor_tensor(out=e1, in0=e1, in1=eq, op=ALU.add))
                    V(['mo_e1', 'mo_s'], ['mo_w'], lambda e: e.tensor_tensor(out=w_, in0=e1, in1=s_, op=ALU.mult))
                    V(['mo_w'], ['mo_ws'], lambda e: e.tensor_reduce(out=ws, in_=w_, axis=AX.X, op=ALU.add))
                    V(['mo_ws'], ['mo_ws'], lambda e: e.reciprocal(out=ws, in_=ws))
                    V(['mo_w', 'mo_ws'], ['mo_w'], lambda e: e.tensor_tensor(out=w_, in0=w_, in1=ws.unsqueeze(2).to_broadcast([128, nb, 16]), op=ALU.mult))
                    for b_ in range(nb):
                        P.op('pe', ['mo_w', 'cst_f'], ['ps5'], lambda e, b_=b_: e.transpose(
                            self.ps[5][0:16, b_ * 128:(b_ + 1) * 128], R['w'][:, b_, :], self.ident32))
                    P.op('act', ['ps5'], ['mo_cT'], lambda e: e.activation(out=cT[:, :n], in_=self.ps[5][0:16, :n], func=AF.Copy))
                    if 'comb' in self.debug:
                        self.dbg(f'comb_{l}_{t0}', lambda o: P.dma(o, cT[:, :n], ['mo_cT'], []), [16, n])
                    P.barrier()
                    s3.close()
                    s3 = contextlib.ExitStack()
                    wd = [self.sb(s3, f"mo_wd{i}", [128, 4, D], BF16) for i in range(2)]
                    pi = 0
                    for ex in range(16):
                        b = ex % 2
                        P.dma(wg[b][:], self.moe_g[l, ex].rearrange("(kc p) f -> p kc f", p=128), [], [f'mo_wg{b}'], q='pool')
                        P.dma(wu[b][:], self.moe_u[l, ex].rearrange("(kc p) f -> p kc f", p=128), [], [f'mo_wu{b}'], q='pool')
                        P.dma(wd[b][:], self.moe_d[l, ex].rearrange("(fc p) d -> p fc d", p=128), [], [f'mo_wd{b}'], q='pool')
                        P.op('pe', ['mm_sel', 'mo_cT'], ['ps6'], lambda e, ex=ex: e.matmul(
                            self.ps[6][:, :n], lhsT=sel[:, ex, :], rhs=cT[:, :n], start=True, stop=True), rg=0)
                        P.op('act', ['ps6'], [f'mo_bce{b}'], lambda e, b=b: e.activation(out=bce[b][:, :n], in_=self.ps[6][:, :n], func=AF.Copy))
                        for fc in range(4):
                            pg, pgk = self.ps[pi % 4], f'ps{pi % 4}'
                            pu, puk = self.ps[(pi + 1) % 4], f'ps{(pi + 1) % 4}'
                            pi += 2
                            for kc in range(KC):
                                P.op('pe', [f'mo_wg{b}', 'mo_h2'], [pgk], lambda e, kc=kc, fc=fc, b=b, pg=pg: e.matmul(
                                    pg[:, :n], lhsT=wg[b][:, kc, fc * 128:(fc + 1) * 128], rhs=h2[:, kc, :n], start=(kc == 0), stop=(kc == KC - 1)))
                            for kc in range(KC):
                                P.op('pe', [f'mo_wu{b}', 'mo_h2'], [puk], lambda e, kc=kc, fc=fc, b=b, pu=pu: e.matmul(
                                    pu[:, :n], lhsT=wu[b][:, kc, fc * 128:(fc + 1) * 128], rhs=h2[:, kc, :n], start=(kc == 0), stop=(kc == KC - 1)))
                            sb_ = fc % 2
                            P.op('act', [pgk], [f'mo_sg{sb_}'], lambda e, pg=pg, sb_=sb_: e.activation(out=sg[sb_][:, :n], in_=pg[:, :n], func=AF.Silu))
                            P.op('dve', [puk, f'mo_sg{sb_}'], [f'mo_sg{sb_}'], lambda e, pu=pu, sb_=sb_: e.tensor_tensor(
                                out=sg[sb_][:, :n], in0=pu[:, :n], in1=sg[sb_][:, :n], op=ALU.mult))
                            P.op('dve', [f'mo_sg{sb_}', f'mo_bce{b}'], [f'mo_act{b}'], lambda e, sb_=sb_, b=b, fc=fc: e.tensor_tensor(
                                out=act[b][:, fc, :n], in0=sg[sb_][:, :n], in1=bce[b][:, :n], op=ALU.mult))
                        for dc in range(KC):
                            pd, pdk = self.ps[pi % 4], f'ps{pi % 4}'
                            pi += 1
                            for fc in range(4):
                                P.op('pe', [f'mo_wd{b}', f'mo_act{b}'], [pdk], lambda e, fc=fc, dc=dc, b=b, pd=pd: e.matmul(
                                    pd[:, :n], lhsT=wd[b][:, fc, dc * 128:(dc + 1) * 128], rhs=act[b][:, fc, :n], start=(fc == 0), stop=(fc == 3)))
                            P.op('dve', [pdk, 'mm_xg', f'mod{l}'], ['mm_xg'], lambda e, pd=pd, dc=dc: e.scalar_tensor_tensor(
                                out=xg[:, dc, :n], in0=pd[:, :n], scalar=self.mod[l][:, 80 + dc, j:j + 1], in1=xg[:, dc, :n],
                                op0=ALU.mult, op1=ALU.add))
                    if last:
                        P.dma(dsto[:, :, t0:t0 + n], xg[:, :, :n], ['mm_xg'], ['outT'])
                    else:
                        P.dma(dstx[:, :, t0:t0 + n], xg[:, :, :n], ['mm_xg'], ['xT'])
                    if 'x2' in self.debug:
                        self.dbg(f'x2_{l}_{t0}', lambda o: P.dma(o.rearrange("(kc p) t -> p kc t", p=128), xg[:, :, :n], ['mm_xg'], []), [D, n])
                    P.barrier()
                    s3.close()

    def final_out(self):
        P = self.P
        P.dma(self.outT, self.xT[:, NCTX:], ['xT'], [])


def na_tables(rpb):
    c = np.arange(64)
    cs = np.clip(c - 8, 0, 48)
    kc = np.arange(64)
    inwin = (kc[:, None] >= cs[None, :]) & (kc[:, None] < cs[None, :] + 16)
    dc = np.clip(kc[:, None] - c[None, :] + 15, 0, 30)
    out = np.full((16, 2, 64, 14, 64), NEG, np.float32)
    for jj in range(2):
        for dr in range(14):
            g = rpb[:, dr + jj][:, dc]
            out[:, jj, :, dr, :] = np.where(inwin[None], g, np.float32(NEG))
    return out.reshape(16, 128, 14 * 64)


NCONST = 512 + 2 * SEQ + 384


def make_consts():
    c = np.zeros((128, NCONST), np.float32)
    p = np.arange(128)
    c[p, p] = 1.0
    c[p, 128 + (p ^ 32)] = 1.0
    j = p[:, None]; i = p[None, :]
    same = (j // 64) == (i // 64)
    c[:, 256:384] = (same & (j <= i)).astype(np.float32)
    c[:, 384:512] = (same & (j >= i)).astype(np.float32)
    t = np.arange(SEQ)
    pos = np.where(p[:, None] < 64, (t // 64)[None, :], (t % 64)[None, :]).astype(np.float32)
    inv = (10000.0 ** (-np.arange(0, 64, 2, dtype=np.float32) / 64)).astype(np.float32)
    ang = pos * inv[(p % 32)][:, None]
    c[:, 512:512 + SEQ] = np.cos(ang)
    sgn = np.where((p % 64) < 32, -1.0, 1.0).astype(np.float32)
    c[:, 512 + SEQ:512 + 2 * SEQ] = np.sin(ang) * sgn[:, None]
    o = 512 + 2 * SEQ
    c[:, o:o + 128] = (same & (j < i)).astype(np.float32)
    c[:, o + 128:o + 256] = (same & (j > i)).astype(np.float32)
    c[:, o + 256:o + 384] = same.astype(np.float32)
    return c


def host_inputs(inp, b, half=0):
    xT0 = np.ascontiguousarray(np.concatenate([inp['ctx'][b], inp['x'][b]], axis=0).T)
    cT = np.stack([fm(inp['c'][b]), fm(inp['c_ctx'])], axis=-1).reshape(128, 32)
    return {
        'xT0': xT0, 'cT': np.ascontiguousarray(cT),
        'ada_w': inp['ada_w'], 'w_in': inp['w_in'],
        'vecs': np.stack([pack_vecs(inp, l) for l in range(DEPTH)]),
        'natab': np.stack([na_tables(inp['na_rpb'][l]) for l in range(DEPTH)]),
        'consts': make_consts(), 'gla_gate_w2': inp['gla_gate_w2'],
        'rw_w2': inp['rw_w2'], 'rw_a2': inp['rw_a2'], 'rw_g2': inp['rw_g2'],
        'w_branch': inp['w_branch'], 'w_out': inp['w_out'], 'router_w': inp['router_w'],
        'router_bias': inp['router_bias'].reshape(1, 16),
        'moe_w_gate': inp['moe_w_gate'], 'moe_w_up': inp['moe_w_up'], 'moe_w_down': inp['moe_w_down'],
        'selw': np.tile(np.asarray([[1.0, 0.0]] if half == 0 else [[0.0, 1.0]], np.float32), (128, 1)),
    }


def kernel(**inputs):
    inp = {k: np.asarray(v) for k, v in inputs.items()}
    bld = Builder()
    nc = bld.build()
    in_maps = [host_inputs(inp, c % 4, c // 4) for c in range(8)]
    res = run_bass_kernel_spmd(nc, in_maps, core_ids=list(range(8)))
    out = np.stack([np.concatenate([res.results[b]["outT"].T, res.results[b + 4]["outT"].T], axis=0) for b in range(4)], axis=0)
    return np.ascontiguousarray(out).astype(np.float32)
```

```python
import contextlib
import os
import numpy as np
import concourse.bass as bass
import concourse.mybir as mybir
from concourse.bass_utils import run_bass_kernel_spmd

F32 = mybir.dt.float32
BF16 = mybir.dt.bfloat16
AF = mybir.ActivationFunctionType
ALU = mybir.AluOpType
AX = mybir.AxisListType

D = 2048
KC = 16
NCTX = 256
SEQ = 2048
TT = NCTX + SEQ
D_IN = 16032
EPS = 1e-6
NEG = -30000.0
DEPTH = 2

C_NAQ, C_NAK, C_NAV = 0, 1024, 2048
C_GQ, C_GK, C_GV, C_GR, C_GGD = 3072, 3584, 4096, 5120, 6144
C_RW = 6176
C_RR, C_RK, C_RV, C_RWD, C_RAD, C_RGD = C_RW, C_RW + 1024, C_RW + 2048, C_RW + 3072, C_RW + 3264, C_RW + 3456
C_GATE = 9888

TILES = [(0, 256), (256, 512), (768, 512), (1280, 512), (1792, 512)]

NDS = 12


class PEProxy:
    def __init__(self, real):
        self._real = real
        self._last = None
        self._dummy = None

    def _sep(self, out, w):
        K = w.shape[0]
        M = 1
        for d_ in w.shape[1:]:
            M *= d_
        t = None if K == 128 else (w.base_partition(), K)
        if t is not None and self._last is not None and t != self._last and self._dummy is not None:
            self._dummy(self._real)
        self._last = t

    def matmul(self, out, lhsT=None, rhs=None, **kw):
        self._sep(out, lhsT)
        return self._real.matmul(out, lhsT=lhsT, rhs=rhs, **kw)

    def transpose(self, out, in_, identity, **kw):
        self._sep(out, in_)
        return self._real.transpose(out, in_, identity, **kw)

    def __getattr__(self, name):
        return getattr(self._real, name)


class Prog:
    def __init__(self, nc, es):
        self.nc = nc
        self.e = dict(pe=PEProxy(nc.tensor), act=nc.scalar, dve=nc.vector, pool=nc.gpsimd, sp=nc.sync)
        self.sem = {k: es.enter_context(nc.semaphore("s_" + k)) for k in self.e}
        self.cnt = {k: 0 for k in self.e}
        self.seen = {k: {} for k in self.e}
        self.lw = {}
        self.rd = {}
        self.dsem = [es.enter_context(nc.semaphore(f"dq{i}")) for i in range(NDS)]
        self.dcnt = [0] * NDS
        self.dnext = 0

    def semh(self, k):
        return self.dsem[k[1]] if isinstance(k, tuple) else self.sem[k]

    def _wait(self, eng, k, v):
        if self.seen[eng].get(k, 0) < v:
            self.e[eng].wait_ge(self.semh(k), v)
            self.seen[eng][k] = v

    def set_parent(self, child, parent):
        self.parent = getattr(self, 'parent', {})
        self.children = getattr(self, 'children', {})
        self.parent[child] = parent
        self.children.setdefault(parent, []).append(child)

    def _expand(self, bs):
        par = getattr(self, 'parent', {})
        chl = getattr(self, 'children', {})
        out = []
        for b in bs:
            out.append(b)
            if b in par:
                out.append(par[b])
            out.extend(chl.get(b, ()))
        return out

    def _deps(self, eng, reads, writes):
        reads = self._expand(reads)
        writes = self._expand(writes)
        deps = {}
        for b in reads:
            lw = self.lw.get(b)
            if lw:
                deps[lw[0]] = max(deps.get(lw[0], 0), lw[1])
        for b in writes:
            lw = self.lw.get(b)
            if lw:
                deps[lw[0]] = max(deps.get(lw[0], 0), lw[1])
            for k, v in self.rd.get(b, {}).items():
                deps[k] = max(deps.get(k, 0), v)
        for k, v in deps.items():
            if k == 'pe' and eng == 'pe':
                continue
            self._wait(eng, k, v)

    def _mark(self, pt, reads, writes):
        for b in writes:
            self.lw[b] = pt
            self.rd[b] = {}
        for b in reads:
            d = self.rd.setdefault(b, {})
            d[pt[0]] = max(d.get(pt[0], 0), pt[1])

    halt = False

    pe_rg = None
    dummy = None

    def op(self, eng, reads, writes, fn, rg=None):
        if self.halt:
            return None
        if eng == 'pe':
            pass
        self._deps(eng, reads, writes)
        ins = fn(self.e[eng])
        self.cnt[eng] += 1
        ins.then_inc(self.sem[eng], 1)
        self._mark((eng, self.cnt[eng]), reads, writes)
        return ins

    def dma(self, out, in_, reads, writes, q='sp', **kw):
        if self.halt:
            return None
        i = self.dnext % NDS
        self.dnext += 1
        k = ('d', i)
        if self.dcnt[i]:
            self._wait(q, k, self.dcnt[i])
        self._deps(q, reads, writes)
        ins = self.e[q].dma_start(out=out, in_=in_, **kw)
        self.dcnt[i] += 16
        ins.then_inc(self.dsem[i], 16)
        self._mark((k, self.dcnt[i]), reads, writes)
        return ins

    def barrier(self):
        for q in self.e:
            for i in range(NDS):
                if self.dcnt[i]:
                    self._wait(q, ('d', i), self.dcnt[i])
            for k in self.e:
                if k != q and self.cnt[k]:
                    self._wait(q, k, self.cnt[k])
        self.lw = {}
        self.rd = {}

    def finish(self, q='sp'):
        for i in range(NDS):
            if self.dcnt[i]:
                self.e[q].wait_ge(self.dsem[i], self.dcnt[i])
        for k in self.e:
            if k != q and self.cnt[k]:
                self.e[q].wait_ge(self.sem[k], self.cnt[k])


def fm(v):
    v = np.asarray(v, np.float32)
    return np.ascontiguousarray(v.reshape(-1, 128).T)


VEC_SLOTS = {}


def _vec_layout():
    off = 0
    def add(name, n):
        nonlocal off
        VEC_SLOTS[name] = (off, n)
        off += n
    add('n1g', 16); add('n2g', 16); add('adab', 96)
    add('naq', 1); add('nak', 1)
    add('ggb', 8)
    add('gng', 2)
    add('mu_r', 8); add('mu_k', 8); add('mu_v', 8); add('mu_wd', 2); add('mu_ad', 2); add('mu_gd', 2)
    add('w0', 16); add('a0', 16); add('kk', 8); add('ka', 8); add('rk', 8); add('gng_rw', 8); add('gnb_rw', 8)
    return off


NV = _vec_layout()


def pack_vecs(inp, l):
    v = np.zeros((128, NV), np.float32)
    def put(name, arr):
        o, n = VEC_SLOTS[name]
        assert arr.shape == (128, n), (name, arr.shape)
        v[:, o:o + n] = arr
    put('n1g', fm(inp['norm1_g'][l])); put('n2g', fm(inp['norm2_g'][l])); put('adab', fm(inp['ada_b'][l]))
    put('naq', np.tile(inp['na_q_norm'][l], 2)[:, None]); put('nak', np.tile(inp['na_k_norm'][l], 2)[:, None])
    put('ggb', fm(inp['gla_gate_b'][l].reshape(-1)))
    put('gng', fm(inp['gla_norm_g'][l]))
    mu = inp['rw_mu'][l]
    put('mu_r', fm(mu[0:1024])); put('mu_k', fm(mu[1024:2048])); put('mu_v', fm(mu[2048:3072]))
    def pad96(a):
        o = np.zeros((128, 2), np.float32); o[:96, 0] = a[:96]; o[:96, 1] = a[96:192]; return o
    put('mu_wd', pad96(mu[3072:3264])); put('mu_ad', pad96(mu[3264:3456])); put('mu_gd', fm(mu[3456:3712]))
    put('w0', fm(inp['rw_w0'][l].reshape(-1))); put('a0', fm(inp['rw_a0'][l].reshape(-1)))
    put('kk', fm(inp['rw_k_k'][l])); put('ka', fm(inp['rw_k_a'][l])); put('rk', fm(inp['rw_r_k'][l].reshape(-1)))
    put('gng_rw', fm(inp['rw_gn_g'][l])); put('gnb_rw', fm(inp['rw_gn_b'][l]))
    return v


class _Stop(Exception):
    pass


class Builder:
    rw_stop = 0
    rw_slots = 2

    def sc_in(self, name, on=True):
        self.sc_out()
        if on:
            self._sc = (name, self.nc.enter_named_scope(name, False)[0])

    def sc_out(self):
        c = getattr(self, '_sc', None)
        if c:
            self.nc.leave_named_scope(c[0], c[1], False)
        self._sc = None

    def stopat(self, k):
        if self.rw_stop == k:
            self.P.halt = True

    def __init__(self, layers=(0, 1), upto='all', debug=()):
        self.layers = layers
        self.upto = upto
        self.debug = set(debug)
        self.nc = bass.Bass("TRN2", target_bir_lowering=False)
        self.es = contextlib.ExitStack()
        self.dbg_outs = {}

    def din(self, name, shape, dt=F32):
        return self.nc.dram_tensor(name, list(shape), dt, kind="ExternalInput").ap()

    def dout(self, name, shape, dt=F32):
        return self.nc.dram_tensor(name, list(shape), dt, kind="ExternalOutput").ap()

    def dscr(self, name, shape, dt=F32):
        return self.nc.dram_tensor(name, list(shape), dt, kind="Internal").ap()

    def sb(self, st, name, shape, dt=F32):
        self._uid = getattr(self, '_uid', 0) + 1
        return st.enter_context(self.nc.sbuf_tensor(f"{name}_u{self._uid}", list(shape), dt))

    def vec(self, l, name):
        o, n = VEC_SLOTS[name]
        return self.vecs[l][:, o:o + n]

    def build(self):
        nc = self.nc
        es = self.es
        with es:
            self.P = P = Prog(nc, es)
            self.xT0 = self.din("xT0", [D, TT])
            self.cT = self.din("cT", [128, 32])
            self.ada_w = self.din("ada_w", [DEPTH, D, 6 * D])
            self.w_in = self.din("w_in", [DEPTH, D, D_IN])
            self.vecs_d = self.din("vecs", [DEPTH, 128, NV])
            self.outT = self.dout("outT", [D, SEQ // 2])
            self.selw = self.din("selw", [128, 2])
            self.xsel = self.dscr("xsel_s", [D, SEQ // 2])
            self.ysel = self.dscr("ysel_s", [3, 1024, SEQ // 2], BF16)
            self.gsel = self.dscr("gsel_s", [3 * D, SEQ // 2], BF16)
            self.xT = self.dscr("xT_s", [D, TT])
            self.zT = self.dscr("zT_s", [C_GATE, TT])
            self.gT = self.dscr("gT_s", [3 * D, TT], BF16)
            self.vtm = self.dscr("vtm_s", [TT, 2048], BF16)
            self.yT = self.dscr("yT_s", [3, 1024, TT], BF16)
            self.natab = self.din("natab", [DEPTH, 16, 128, 14 * 64])
            self.consts = self.din("consts", [128, NCONST])
            self.gla_w2 = self.din("gla_gate_w2", [DEPTH, 2, 16, 512])
            self.rw_w2 = self.din("rw_w2", [DEPTH, 2, 96, 1024])
            self.rw_a2 = self.din("rw_a2", [DEPTH, 2, 96, 1024])
            self.rw_g2 = self.din("rw_g2", [DEPTH, 256, 1024])
            self.w_branch = self.din("w_branch", [DEPTH, 3, 1024, D])
            self.w_out = self.din("w_out", [DEPTH, D, D])
            self.router_w = self.din("router_w", [D, 16])
            self.router_b = self.din("router_bias", [1, 16])
            self.moe_g = self.din("moe_w_gate", [DEPTH, 16, D, 512])
            self.moe_u = self.din("moe_w_up", [DEPTH, 16, D, 512])
            self.moe_d = self.din("moe_w_down", [DEPTH, 16, 512, D])
            self.ldT = self.dscr("ldT_s", [2, 1024, TT])
            self.aT = self.dscr("aT_s", [2, 1024, TT])
            self.g2T = self.dscr("g2T_s", [1024, TT], BF16)
            self.ones_bf = self.sb(es, "ones_bf", [128, 128], BF16)
            self.vecs = [self.sb(es, f"vecs{l}", [128, NV]) for l in range(DEPTH)]
            self.mod = [self.sb(es, f"mod{l}", [128, 96, 2]) for l in range(DEPTH)]
            self.gm1 = [self.sb(es, f"gm1_{l}", [128, 16, 2]) for l in range(DEPTH)]
            self.gm2 = [self.sb(es, f"gm2_{l}", [128, 16, 2]) for l in range(DEPTH)]
            self.ps = [es.enter_context(nc.psum_tensor(f"ps{i}", [128, 512], F32)) for i in range(7)]
            self.psb = es.enter_context(nc.psum_tensor("psb", [128, 1024], BF16))
            psd = self.psb[:, 512:1024].bitcast(F32)
            P.e['pe']._dummy = lambda e: e.matmul(psd[:, 0:1], lhsT=self.ones_bf[:], rhs=self.ones_bf[:, 0:1], start=True, stop=True)
            cst = self.sb(es, "cst_f", [128, 4 * 128])
            self.cst_bf = self.sb(es, "cst_bf", [128, 4 * 128], BF16)
            P.dma(cst[:], self.consts[:, 0:512], [], ['cst_f'])
            P.op('dve', ['cst_f'], ['cst_bf'], lambda e: e.tensor_copy(out=self.cst_bf[:], in_=cst[:]))
            self.ident_bf = self.cst_bf[:, 0:128]
            self.perm_bf = self.cst_bf[:, 128:256]
            self.mask_f = cst[:, 256:384]
            self.mask_b = cst[:, 384:512]
            self.ident32 = cst[:, 0:128]
            cst2 = self.sb(es, "cst2", [128, 384])
            P.dma(cst2[:], self.consts[:, 512 + 2 * SEQ:512 + 2 * SEQ + 384], [], ['cst_f'])
            self.mask_fs = cst2[:, 0:128]
            self.mask_bs = cst2[:, 128:256]
            self.mask_bd = cst2[:, 256:384]
            P.op('pool', [], ['ones_bf'], lambda e: e.memset(self.ones_bf[:], 1.0))
            self.bd_bf = self.sb(es, "bd_bf", [128, 128], BF16)
            P.op('pool', [], ['bd_bf'], lambda e: e.memset(self.bd_bf[:], 0.0))
            P.op('pool', ['bd_bf'], ['bd_bf'], lambda e: e.memset(self.bd_bf[0:64, 0:64], 1.0))
            P.op('pool', ['bd_bf'], ['bd_bf'], lambda e: e.memset(self.bd_bf[64:128, 64:128], 1.0))
            self.eps_t = self.sb(es, "eps_t", [128, 1])
            self.selw_t = self.sb(es, "selw_t", [128, 2])
            P.dma(self.selw_t[:], self.selw, [], ['selw_t'])
            P.op('pool', [], ['eps_t'], lambda e: e.memset(self.eps_t[:], EPS))
            for l in range(DEPTH):
                P.dma(self.vecs[l][:], self.vecs_d[l], [], [f'vecs{l}'])

            with nc.named_scope('prologue'):
                self.prologue()
            for l in self.layers:
                self.layer(l)
                if self.upto != 'all':
                    break
            P.finish()
        return nc

    def dbg(self, name, src_ap_fn, shape, dt=F32, reads=()):
        o = self.dout("dbg_" + name, shape, dt)
        self.dbg_outs[name] = o
        src_ap_fn(o)

    def prologue(self):
        nc, P = self.nc, self.P
        with contextlib.ExitStack() as st:
            sc = self.sb(st, "sc", [128, 32])
            sc2 = self.sb(st, "sc2", [128, 32])
            awb = [self.sb(st, f"awb{i}", [128, 16, 512], BF16) for i in range(3)]
            sc2b = self.sb(st, "sc2b", [128, 32], BF16)
            P.dma(sc[:], self.cT, [], ['sc'])
            P.op('act', ['sc'], ['sc2'], lambda e: e.activation(out=sc2[:], in_=sc[:], func=AF.Silu))
            P.op('dve', ['sc2'], ['sc2'], lambda e: e.tensor_copy(out=sc2b[:], in_=sc2[:]))
            for l in range(DEPTH):
                aw = self.ada_w[l].rearrange("(kc p) c -> p kc c", p=128)
                adab = self.vec(l, 'adab')
                for g in range(24):
                    wt = awb[g % 3]
                    wk = f'awb{g % 3}'
                    P.dma(wt[:], aw[:, :, g * 512:(g + 1) * 512], [], [wk], q='pool')
                    pst = self.ps[g % 2]
                    pk = f'ps{g % 2}'
                    for f in range(4):
                        for kc in range(KC):
                            P.op('pe', [wk, 'sc2'], [pk], lambda e, f=f, kc=kc: e.matmul(
                                pst[:, f * 2:(f + 1) * 2], lhsT=wt[:, kc, f * 128:(f + 1) * 128],
                                rhs=sc2b[:, kc * 2:(kc + 1) * 2], start=(kc == 0), stop=(kc == KC - 1)))
                    P.op('dve', [pk, f'vecs{l}'], [f'mod{l}'], lambda e, g=g: e.tensor_tensor(
                        out=self.mod[l][:, g * 4:(g + 1) * 4, :],
                        in0=pst[:, 0:8].rearrange("p (f j) -> p f j", j=2),
                        in1=adab[:, g * 4:(g + 1) * 4].unsqueeze(2).to_broadcast([128, 4, 2]), op=ALU.add))
                for (gm, nm, sco, key) in ((self.gm1[l], 'n1g', 16, f'gm1_{l}'), (self.gm2[l], 'n2g', 64, f'gm2_{l}')):
                    P.op('dve', [f'mod{l}'], [key], lambda e, gm=gm, sco=sco: e.tensor_scalar(
                        out=gm[:], in0=self.mod[l][:, sco:sco + 16, :], scalar1=1.0, scalar2=None, op0=ALU.add))
                    P.op('dve', [key, f'vecs{l}'], [key], lambda e, gm=gm, nm=nm: e.tensor_tensor(
                        out=gm[:], in0=gm[:], in1=self.vec(l, nm).unsqueeze(2).to_broadcast([128, 16, 2]), op=ALU.mult))
            if 'mod' in self.debug:
                for l in range(DEPTH):
                    self.dbg(f'mod{l}', lambda o, l=l: P.dma(o, self.mod[l][:].rearrange("p a b -> p (a b)"), [f'mod{l}'], []), [128, 192])
            P.barrier()

    def norm_tile(self, st_bufs, x, xk, n, j, gm, gmk, shmod, shoff, modk, out_fn, outk, ps_i=6):
        P = self.P
        sq, rt = st_bufs
        pst = self.ps[ps_i]
        pk = f'ps{ps_i}'
        P.op('act', [xk], ['nsq'], lambda e: e.activation(out=sq[:, :, :n], in_=x, func=AF.Square))
        for kc in range(KC):
            P.op('pe', ['nsq', 'ones_bf'], [pk], lambda e, kc=kc: e.matmul(
                pst[:, :n], lhsT=self.ones_bf[:], rhs=sq[:, kc, :n], start=(kc == 0), stop=(kc == KC - 1)))
        P.op('act', [pk], ['nrt'], lambda e: e.activation(out=rt[:, :n], in_=pst[:, :n], func=AF.Sqrt,
                                                         scale=1.0 / D, bias=self.eps_t[:, 0:1]))
        P.op('dve', ['nrt'], ['nrt'], lambda e: e.reciprocal(out=rt[:, :n], in_=rt[:, :n]))
        P.op('dve', [xk, 'nrt'], [xk], lambda e: e.tensor_tensor(
            out=x, in0=x, in1=rt[:, :n].unsqueeze(1).to_broadcast([128, KC, n]), op=ALU.mult))
        for kc in range(KC):
            P.op('act', [xk, gmk, modk], [outk], lambda e, kc=kc: e.activation(
                out=out_fn(kc), in_=x[:, kc, :], func=AF.Identity,
                scale=gm[:, kc, j:j + 1], bias=shmod[:, shoff + kc, j:j + 1]))

    def layer(self, l):
        nc, P = self.nc, self.P
        src = self.xT0 if l == 0 else self.xT
        with contextlib.ExitStack() as st:
            hT = self.sb(st, "hT", [128, KC, TT], BF16)
            with contextlib.ExitStack() as st2:
                xb = [self.sb(st2, f"xb{i}", [128, KC, 512]) for i in range(2)]
                sq = self.sb(st2, "nsq", [128, KC, 512], BF16)
                rt = self.sb(st2, "nrt", [128, 512])
                for ti, (t0, n) in enumerate(TILES):
                    x = xb[ti % 2]
                    xk = f'xb{ti % 2}'
                    j = 1 if t0 < NCTX else 0
                    P.dma(x[:, :, :n], src.rearrange("(kc p) t -> p kc t", p=128)[:, :, t0:t0 + n], ['xT'], [xk])
                    self.norm_tile((sq, rt), x[:, :, :n], xk, n, j, self.gm1[l], f'gm1_{l}', self.mod[l], 0, f'mod{l}',
                                   lambda kc, t0=t0, n=n: hT[:, kc, t0:t0 + n], 'hT')
                if 'hT' in self.debug:
                    self.dbg(f'hT{l}', lambda o: P.dma(o.rearrange("(kc p) t -> p kc t", p=128), hT[:], ['hT'], []), [D, TT], BF16)
                P.barrier()
            if self.upto == 'norm':
                return
            with nc.named_scope(f'L{l}_inproj'):
                self.inproj(l, hT)
            P.barrier()
        if self.upto == 'inproj':
            return
        with nc.named_scope(f'L{l}_na'):
            self.na_mixer(l)
        P.barrier()
        if self.upto == 'na':
            return
        with nc.named_scope(f'L{l}_gla'):
            self.gla_mixer(l)
        P.barrier()
        if self.upto == 'gla':
            return
        with nc.named_scope(f'L{l}_rwpre'):
            self.rwkv_pre(l)
        P.barrier()
        if self.upto == 'rwpre':
            self.dbg(f'ld{l}', lambda o: P.dma(o, self.ldT, ['ldT'], []), [2, 1024, TT])
            self.dbg(f'a{l}', lambda o: P.dma(o, self.aT, ['aT'], []), [2, 1024, TT])
            self.dbg(f'g2{l}', lambda o: P.dma(o, self.g2T, ['g2T'], []), [1024, TT], BF16)
            return
        with nc.named_scope(f'L{l}_rwkv'):
            self.rwkv_mixer(l)
        P.halt = False
        P.barrier()
        if self.upto == 'rwkv':
            return
        with nc.named_scope(f'L{l}_mergemoe'):
            self.merge_moe(l)
        P.barrier()

    def inproj(self, l, hT):
        nc, P = self.nc, self.P
        w = self.w_in[l].rearrange("(kc p) c -> p kc c", p=128)
        with contextlib.ExitStack() as st:
            wsl = [self.sb(st, f"wsl{i}", [128, KC, 512], BF16) for i in range(3)]
            zst = [self.sb(st, f"zst{i}", [128, 512]) for i in range(4)]
            gst = [self.sb(st, f"gst{i}", [128, 512], BF16) for i in range(4)]
            last = l == DEPTH - 1
            if last:
                H_ = SEQ // 2
                hsel = self.sb(st, "hsel", [128, KC, H_], BF16)
                P.op('dve', ['hT', 'selw_t'], ['hsel'], lambda e: e.tensor_scalar(
                    out=hsel[:], in0=hT[:, :, NCTX:NCTX + H_], scalar1=self.selw_t[:, 0:1], scalar2=None, op0=ALU.mult))
                P.op('dve', ['hT', 'hsel', 'selw_t'], ['hsel'], lambda e: e.scalar_tensor_tensor(
                    out=hsel[:], in0=hT[:, :, NCTX + H_:NCTX + 2 * H_], scalar=self.selw_t[:, 1:2], in1=hsel[:], op0=ALU.mult, op1=ALU.add))
            segs = [(0, 2048), (C_GQ, C_GV), (C_GR, C_GGD), (C_GGD, C_GGD + 16), (C_GGD + 16, C_RW),
                    (C_RR, C_RWD), (C_RWD, C_RWD + 96), (C_RWD + 96, C_RAD), (C_RAD, C_RAD + 96), (C_RAD + 96, C_RGD),
                    (C_RGD, C_GATE), (C_GATE, D_IN)]
            nload = 0
            nev = 0
            npsum = 0
            for (s0, s1) in segs:
                for b0 in range(s0, s1, 512):
                    bw = min(512, s1 - b0)
                    si = nload % 3
                    nload += 1
                    wk = f'wsl{si}'
                    P.dma(wsl[si][:, :, :bw], w[:, :, b0:b0 + bw], [], [wk], q='pool')
                    for c0 in range(b0, b0 + bw, 128):
                        m = min(128, b0 + bw - c0)
                        gsel_blk = last and c0 >= C_GATE
                        hsrc, hkey = (hsel, 'hsel') if gsel_blk else (hT, 'hT')
                        for (t0, n) in ([(0, 512), (512, 512)] if gsel_blk else TILES):
                            pi = npsum % 4
                            npsum += 1
                            pst = self.ps[pi]
                            pk = f'ps{pi}'
                            for kc in range(KC):
                                P.op('pe', [wk, hkey], [pk], lambda e, kc=kc, c0=c0, m=m, t0=t0, n=n, si=si, pst=pst, hsrc=hsrc: e.matmul(
                                    pst[:m, :n], lhsT=wsl[si][:, kc, c0 - b0:c0 - b0 + m], rhs=hsrc[:, kc, t0:t0 + n],
                                    start=(kc == 0), stop=(kc == KC - 1)))
                            ei = nev % 4
                            eng = 'act' if nev % 2 == 0 else 'dve'
                            nev += 1
                            if c0 >= C_GATE:
                                P.op('act', [pk], [f'gst{ei}'], lambda e, m=m, n=n, ei=ei, pst=pst: e.activation(
                                    out=gst[ei][:m, :n], in_=pst[:m, :n], func=AF.Sigmoid))
                                gdst = self.gsel if gsel_blk else self.gT
                                P.dma(gdst[c0 - C_GATE:c0 - C_GATE + m, t0:t0 + n], gst[ei][:m, :n], [f'gst{ei}'], ['gT'])
                            else:
                                if eng == 'act':
                                    P.op('act', [pk], [f'zst{ei}'], lambda e, m=m, n=n, ei=ei, pst=pst: e.activation(
                                        out=zst[ei][:m, :n], in_=pst[:m, :n], func=AF.Copy))
                                else:
                                    P.op('dve', [pk], [f'zst{ei}'], lambda e, m=m, n=n, ei=ei, pst=pst: e.tensor_copy(
                                        out=zst[ei][:m, :n], in_=pst[:m, :n]))
                                P.dma(self.zT[c0:c0 + m, t0:t0 + n], zst[ei][:m, :n], [f'zst{ei}'], ['zT'])
            for (s0, vo) in ((C_NAV, 0), (C_GV, 1024)):
                for b0 in range(0, 1024, 512):
                    si = nload % 3
                    nload += 1
                    wk = f'wsl{si}'
                    P.dma(wsl[si][:, :, :], w[:, :, s0 + b0:s0 + b0 + 512], [], [wk], q='pool')
                    for tt in range(TT // 128):
                        pi = npsum % 4
                        npsum += 1
                        pst = self.ps[pi]
                        pk = f'ps{pi}'
                        for kc in range(KC):
                            P.op('pe', [wk, 'hT'], [pk], lambda e, kc=kc, tt=tt, si=si, pst=pst: e.matmul(
                                pst[:, :], lhsT=hT[:, kc, tt * 128:(tt + 1) * 128], rhs=wsl[si][:, kc, :],
                                start=(kc == 0), stop=(kc == KC - 1)))
                        ei = nev % 4
                        eng = 'act' if nev % 2 == 0 else 'dve'
                        nev += 1
                        if eng == 'act':
                            P.op('act', [pk], [f'gst{ei}'], lambda e, ei=ei, pst=pst: e.activation(
                                out=gst[ei][:, :], in_=pst[:, :], func=AF.Copy))
                        else:
                            P.op('dve', [pk], [f'gst{ei}'], lambda e, ei=ei, pst=pst: e.tensor_copy(
                                out=gst[ei][:, :], in_=pst[:, :]))
                        P.dma(self.vtm[tt * 128:(tt + 1) * 128, vo + b0:vo + b0 + 512], gst[ei][:, :], [f'gst{ei}'], ['vtm'])
            if 'z' in self.debug:
                self.dbg(f'z{l}', lambda o: P.dma(o, self.zT, ['zT'], []), [C_GATE, TT])
                self.dbg(f'g{l}', lambda o: P.dma(o, self.gT, ['gT'], []), [3 * D, TT], BF16)
                self.dbg(f'vtm{l}', lambda o: P.dma(o, self.vtm, ['vtm'], []), [TT, 2048], BF16)


    def na_mixer(self, l):
        nc, P = self.nc, self.P
        with_ctx = l < DEPTH - 1
        vt_all = self.vtm.rearrange("(tt p) c -> p tt c", p=128)
        vt_odd = self.vtm[64:64 + 17 * 128, :].rearrange("(tt p) c -> p tt c", p=128)
        with contextlib.ExitStack() as st:
            zq = [self.sb(st, f"naz{i}", [128, TT]) for i in range(2)]
            sqb = self.sb(st, "nasq", [128, 512], BF16)
            rtb = self.sb(st, "nart", [128, 512])
            qk = [self.sb(st, "qn", [128, TT], BF16), self.sb(st, "kn", [128, TT], BF16)]
            vte = self.sb(st, "vte", [128, 18, 128], BF16)
            vto = self.sb(st, "vto", [128, 17, 128], BF16)
            tbl = [self.sb(st, f"natbl{i}", [128, 14, 64]) for i in range(2)]
            sT = [self.sb(st, f"sT{i}", [128, 4, 64]) for i in range(2)]
            pT = [self.sb(st, f"pT{i}", [128, 6, 64], BF16) for i in range(2)]
            pTc = self.sb(st, "pTc", [128, 2, 256], BF16)
            rsb = [self.sb(st, f"nars{i}", [128, 256]) for i in range(2)]
            yna = [self.sb(st, f"yna{i}", [128, TT], BF16) for i in range(2)]
            g8 = self.sb(st, "g8", [128, 2])
            qm = [self.sb(st, f"qm{i}", [128, TT], BF16) for i in range(2)]
            P.op('dve', [f'vecs{l}'], ['g8'], lambda e: e.tensor_scalar(
                out=g8[:, 0:1], in0=self.vec(l, 'naq'), scalar1=0.125, scalar2=None, op0=ALU.mult))
            P.op('dve', [f'vecs{l}'], ['g8'], lambda e: e.tensor_copy(out=g8[:, 1:2], in_=self.vec(l, 'nak')))
            it = 0
            for hp in range(8):
                for w_, c0 in ((0, C_NAQ), (1, C_NAK)):
                    z = zq[w_]
                    zk = f'naz{w_}'
                    P.dma(z[:], self.zT[c0 + hp * 128:c0 + (hp + 1) * 128, :], ['zT'], [zk])
                    dst = qk[w_]
                    dk = 'qn' if w_ == 0 else 'kn'
                    for (t0, n) in TILES:
                        P.op('act', [zk], ['nasq'], lambda e, z=z, t0=t0, n=n: e.activation(
                            out=sqb[:, :n], in_=z[:, t0:t0 + n], func=AF.Square))
                        P.op('pe', ['nasq', 'bd_bf'], ['ps4'], lambda e, n=n: e.matmul(
                            self.ps[4][:, :n], lhsT=self.bd_bf[:], rhs=sqb[:, :n], start=True, stop=True))
                        P.op('act', ['ps4'], ['nart'], lambda e, n=n: e.activation(
                            out=rtb[:, :n], in_=self.ps[4][:, :n], func=AF.Sqrt, scale=1.0 / 64, bias=self.eps_t[:, 0:1]))
                        P.op('dve', ['nart'], ['nart'], lambda e, n=n: e.reciprocal(out=rtb[:, :n], in_=rtb[:, :n]))
                        P.op('dve', [zk, 'nart', 'g8'], [dk], lambda e, z=z, t0=t0, n=n, dst=dst, w_=w_: e.scalar_tensor_tensor(
                            out=dst[:, t0:t0 + n], in0=z[:, t0:t0 + n], scalar=g8[:, w_:w_ + 1], in1=rtb[:, :n],
                            op0=ALU.mult, op1=ALU.mult))
                for e_ in range(2):
                    P.op('pool', [], [f'qm{e_}'], lambda e, e_=e_: e.memset(qm[e_][:], 0.0))
                    pr = slice(e_ * 64, (e_ + 1) * 64)
                    P.op('act', ['qn', f'qm{e_}'], [f'qm{e_}'], lambda e, e_=e_, pr=pr: e.activation(out=qm[e_][pr, :], in_=qk[0][pr, :], func=AF.Copy))
                P.dma(vte[:], vt_all[:, :, hp * 128:(hp + 1) * 128], ['vtm'], ['vte'])
                P.dma(vto[:], vt_odd[:, :, hp * 128:(hp + 1) * 128], ['vtm'], ['vto'])
                qn, kn = qk
                y = yna[hp % 2]
                yk = f'yna{hp % 2}'
                stages = []
                for e_ in range(2):
                    h = 2 * hp + e_
                    tb = tbl[h % 2]
                    tk = f'natbl{h % 2}'
                    pr = slice(e_ * 64, (e_ + 1) * 64)
                    for r in range(32):
                        rs = min(max(r - 4, 0), 24)
                        dlt = rs - r
                        tq = NCTX + r * 64
                        b = it % 2
                        it += 1
                        psS, psSk = self.ps[b], f'ps{b}'
                        psO, psOk = self.ps[2 + b], f'ps{2 + b}'

                        def s1(e_=e_, h=h, tb=tb, tk=tk, pr=pr, r=r, rs=rs, dlt=dlt, tq=tq, b=b, psS=psS, psSk=psSk):
                            if r == 0:
                                P.dma(tb[:].rearrange("p a b -> p (a b)"), self.natab[l, h], [], [tk])
                            for j in range(6):
                                kt = NCTX + (rs + 2 * j) * 64 if j < 4 else (j - 4) * 128
                                P.op('pe', [f'qm{e_}', 'kn'], [psSk], lambda e, j=j, kt=kt: e.matmul(
                                    psS[:, j * 64:(j + 1) * 64], lhsT=kn[:, kt:kt + 128], rhs=qm[e_][:, tq:tq + 64],
                                    start=True, stop=True))
                            d0 = dlt + 7
                            tv = tb[:].rearrange("p (u two) c -> p u two c", two=2)[:, d0 // 2:d0 // 2 + 4, d0 % 2, :]
                            P.op('dve', [psSk, tk], [f'sT{b}'], lambda e: e.tensor_tensor(
                                out=sT[b][:], in0=psS[:, 0:256].rearrange("p (j c) -> p j c", c=64), in1=tv, op=ALU.add))
                            P.op('act', [f'sT{b}'], [f'pT{b}'], lambda e: e.activation(
                                out=pT[b][:, 0:4, :], in_=sT[b][:], func=AF.Exp))
                            P.op('act', [psSk], [f'pT{b}'], lambda e: e.activation(
                                out=pT[b][:, 4:6, :], in_=psS[:, 256:384].rearrange("p (j c) -> p j c", c=64), func=AF.Exp))

                        def s2(pr=pr, rs=rs, tq=tq, b=b, psO=psO, psOk=psOk):
                            for part in range(2):
                                for j in range(6):
                                    if part == 1:
                                        lhs = self.ones_bf[:, :]
                                        rk_ = 'ones_bf'
                                    elif j >= 4:
                                        lhs = vte[:, j - 4, :]
                                        rk_ = 'vte'
                                    elif rs % 2 == 0:
                                        lhs = vte[:, 2 + rs // 2 + j, :]
                                        rk_ = 'vte'
                                    else:
                                        lhs = vto[:, (rs + 1) // 2 + 1 + j, :]
                                        rk_ = 'vto'
                                    P.op('pe', [rk_, f'pT{b}'], [psOk], lambda e, lhs=lhs, j=j, part=part: e.matmul(
                                        psO[:, part * 64:(part + 1) * 64], lhsT=lhs, rhs=pT[b][:, j, :],
                                        start=(j == 0), stop=(j == 5)))
                            P.op('dve', [psOk], [f'nars{b}'], lambda e: e.reciprocal(
                                out=rsb[b][pr, 0:64], in_=psO[pr, 64:128]))
                            P.op('dve', [psOk, f'nars{b}'], [yk], lambda e: e.tensor_tensor(
                                out=y[pr, tq:tq + 64], in0=psO[pr, 0:64], in1=rsb[b][pr, 0:64], op=ALU.mult))
                        stages.append((s1, s2))
                    if with_ctx:
                        b = it % 2
                        it += 1
                        psS, psSk = self.ps[b], f'ps{b}'
                        psO, psOk = self.ps[2 + b], f'ps{2 + b}'

                        def s1(e_=e_, pr=pr, psS=psS, psSk=psSk):
                            for j in range(2):
                                P.op('pe', [f'qm{e_}', 'kn'], [psSk], lambda e, j=j: e.matmul(
                                    psS[:, j * 256:(j + 1) * 256], lhsT=kn[:, j * 128:(j + 1) * 128], rhs=qm[e_][:, 0:256],
                                    start=True, stop=True))
                            P.op('act', [psSk], ['pTc'], lambda e: e.activation(
                                out=pTc[:].rearrange("p j c -> p (j c)"), in_=psS[:, :], func=AF.Exp))

                        def s2(pr=pr, b=b, psO=psO, psOk=psOk):
                            for part in range(2):
                                for j in range(2):
                                    lhs = self.ones_bf[:, :] if part == 1 else vte[:, j, :]
                                    P.op('pe', ['vte', 'ones_bf', 'pTc'], [psOk], lambda e, lhs=lhs, j=j, part=part: e.matmul(
                                        psO[:, part * 256:(part + 1) * 256], lhsT=lhs, rhs=pTc[:, j, :],
                                        start=(j == 0), stop=(j == 1)))
                            P.op('dve', [psOk], [f'nars{b}'], lambda e: e.reciprocal(
                                out=rsb[b][pr, :], in_=psO[pr, 256:512]))
                            P.op('dve', [psOk, f'nars{b}'], [yk], lambda e: e.tensor_tensor(
                                out=y[pr, 0:256], in0=psO[pr, 0:256], in1=rsb[b][pr, :], op=ALU.mult))
                        stages.append((s1, s2))
                for k_ in range(len(stages) + 1):
                    if k_ < len(stages):
                        stages[k_][0]()
                    if k_ >= 1:
                        stages[k_ - 1][1]()
                t_lo = 0 if with_ctx else NCTX
                P.dma(self.yT[0, hp * 128:(hp + 1) * 128, t_lo:], y[:, t_lo:], [yk], ['yT'])
            if 'na' in self.debug:
                self.dbg(f'yna{l}', lambda o: P.dma(o, self.yT[0], ['yT'], []), [1024, TT], BF16)


    def gla_mixer(self, l):
        nc, P = self.nc, self.P
        with_ctx = l < DEPTH - 1
        NCH = TT // 64
        NT = TT // 128
        qscale = 128 ** -0.5
        vt_all = self.vtm.rearrange("(tt p) c -> p tt c", p=128)
        with contextlib.ExitStack() as st:
            cos = self.sb(st, "cos", [128, SEQ])
            sin = self.sb(st, "sin", [128, SEQ])
            msk = self.sb(st, "cmsk", [128, TT])
            qk32 = [self.sb(st, "gq32", [128, TT]), self.sb(st, "gk32", [128, TT])]
            zb = self.sb(st, "gzb", [128, 512], BF16)
            rz = self.sb(st, "grz", [128, 2, TT])
            gd = [self.sb(st, f"ggd{d}", [16, TT]) for d in range(2)]
            gw2 = self.sb(st, "gw2", [16, 2, 512])
            nb = self.sb(st, "gnb", [128, 8])
            T1 = self.sb(st, "gT1", [128, TT]); T2 = self.sb(st, "gT2", [128, TT]); T3 = self.sb(st, "gT3", [128, TT])
            qi = self.sb(st, "gqi", [128, TT], BF16); kj = self.sb(st, "gkj", [128, TT], BF16)
            kd = self.sb(st, "gkd", [128, TT], BF16); qb = self.sb(st, "gqb", [128, TT], BF16)
            dec = self.sb(st, "gdec", [128, NCH])
            Vt = self.sb(st, "gVt", [128, NT, 256], BF16)
            kdT = self.sb(st, "gkdT", [128, NT, 128], BF16)
            Abf = [self.sb(st, f"gA{i}", [128, 128], BF16) for i in range(2)]
            S32 = self.sb(st, "gS32", [128, 256])
            Sbf = [self.sb(st, f"gSbf{i}", [128, 256], BF16) for i in range(2)]
            yf = self.sb(st, "gyf", [128, 2, TT], BF16)
            yo = self.sb(st, "gyo", [128, 2, TT], BF16)
            yt = [self.sb(st, f"gyt{i}", [128, 2, 128]) for i in range(2)]
            ysq = self.sb(st, "gysq", [128, 2, 128], BF16)
            yrt = self.sb(st, "gyrt", [128, 128])
            P.dma(cos[:], self.consts[:, 512:512 + SEQ], [], ['cos'])
            P.dma(sin[:], self.consts[:, 512 + SEQ:512 + 2 * SEQ], [], ['sin'])
            P.op('pool', [], ['cmsk'], lambda e: e.memset(msk[:], 1.0))
            P.op('pool', ['cmsk'], ['cmsk'], lambda e: e.memset(msk[:].rearrange("p (c j) -> p c j", j=64)[:, :, 0:1], 0.0))
            for d in range(2):
                P.dma(gd[d][:], self.zT[C_GGD + 16 * d:C_GGD + 16 * (d + 1), :], ['zT'], [f'ggd{d}'])
            P.dma(gw2[:], self.gla_w2[l].rearrange("d k c -> k d c"), [], ['gw2'])
            P.op('dve', [f'vecs{l}'], ['gnb'], lambda e: e.tensor_scalar(
                out=nb[:], in0=self.vec(l, 'ggb'), scalar1=-1.0, scalar2=None, op0=ALU.mult))
            gng = self.vec(l, 'gng')
            c3 = lambda t: t[:].rearrange("p (c j) -> p c j", j=64)
            sbi = 0
            for h in range(4):
                for w_, c0 in ((0, C_GQ), (1, C_GK)):
                    z = qk32[w_]
                    zk = 'gq32' if w_ == 0 else 'gk32'
                    P.dma(z[:], self.zT[c0 + h * 128:c0 + (h + 1) * 128, :], ['zT'], [zk])
                    for ti in range(4):
                        t0 = NCTX + ti * 512
                        P.op('act', [zk], ['gzb'], lambda e, z=z, t0=t0: e.activation(out=zb[:], in_=z[:, t0:t0 + 512], func=AF.Copy))
                        P.op('pe', ['gzb', 'cst_bf'], ['ps4'], lambda e: e.matmul(
                            self.ps[4][:, :], lhsT=self.perm_bf, rhs=zb[:], start=True, stop=True))
                        P.op('dve', ['ps4', 'sin'], ['gT3'], lambda e, ti=ti: e.tensor_tensor(
                            out=T3[:, 0:512], in0=self.ps[4][:, :], in1=sin[:, ti * 512:(ti + 1) * 512], op=ALU.mult))
                        P.op('dve', [zk, 'cos'], [zk], lambda e, z=z, t0=t0, ti=ti: e.tensor_tensor(
                            out=z[:, t0:t0 + 512], in0=z[:, t0:t0 + 512], in1=cos[:, ti * 512:(ti + 1) * 512], op=ALU.mult))
                        P.op('dve', [zk, 'gT3'], [zk], lambda e, z=z, t0=t0: e.tensor_tensor(
                            out=z[:, t0:t0 + 512], in0=z[:, t0:t0 + 512], in1=T3[:, 0:512], op=ALU.add))
                q32, k32 = qk32
                P.dma(rz[:], self.zT[C_GR + h * 256:C_GR + (h + 1) * 256, :].rearrange("(c p) t -> p c t", p=128), ['zT'], ['grz'])
                P.op('act', ['grz'], ['grz'], lambda e: e.activation(out=rz[:], in_=rz[:], func=AF.Silu))
                P.dma(Vt[:], vt_all[:, :, 1024 + h * 256:1024 + (h + 1) * 256], ['vtm'], ['gVt'])
                for d in range(2):
                    for (t0, n) in TILES:
                        P.op('pe', ['gw2', f'ggd{d}'], ['ps4'], lambda e, t0=t0, n=n, d=d, h=h: e.matmul(
                            self.ps[4][:, :n], lhsT=gw2[:, d, h * 128:(h + 1) * 128], rhs=gd[d][:, t0:t0 + n], start=True, stop=True), rg=0)
                        P.op('act', ['ps4', 'gnb'], ['gT1'], lambda e, t0=t0, n=n, d=d, h=h: e.activation(
                            out=T1[:, t0:t0 + n], in_=self.ps[4][:, :n], func=AF.Exp, scale=-1.0, bias=nb[:, d * 4 + h:d * 4 + h + 1]))
                    P.op('act', ['gT1'], ['gT1'], lambda e: e.activation(out=T1[:], in_=T1[:], func=AF.Ln, bias=1.0, scale=1.0))
                    P.op('dve', ['cmsk', 'gT1'], ['gT2'], lambda e: e.tensor_tensor_scan(
                        out=T2[:], data0=msk[:], data1=T1[:], initial=0.0, op0=ALU.mult, op1=ALU.add))
                    if d == 1:
                        P.op('dve', ['gT2'], ['gT3'], lambda e: e.tensor_tensor(
                            out=c3(T3), in0=c3(T2)[:, :, 63:64].to_broadcast([128, NCH, 64]), in1=c3(T2), op=ALU.subtract))
                        P.op('dve', ['gT3', 'gT1'], ['gT2'], lambda e: e.tensor_tensor(out=T2[:], in0=T3[:], in1=T1[:], op=ALU.add))
                    tot = c3(T2)[:, :, 63:64] if d == 0 else c3(T2)[:, :, 0:1]
                    cref = c3(T2)[:, :, 32:33] if d == 0 else c3(T2)[:, :, 31:32]
                    P.op('act', ['gT2'], ['gdec'], lambda e, tot=tot: e.activation(
                        out=dec[:].unsqueeze(2), in_=tot, func=AF.Exp, scale=-1.0 / 16))
                    P.op('dve', ['gT2'], ['gT3'], lambda e, cref=cref: e.tensor_tensor(
                        out=c3(T3), in0=c3(T2), in1=cref.to_broadcast([128, NCH, 64]), op=ALU.subtract))
                    P.op('act', ['gT3'], ['gqi'], lambda e: e.activation(out=qi[:], in_=T3[:], func=AF.Exp, scale=-1.0 / 16))
                    P.op('act', ['gT3'], ['gkj'], lambda e: e.activation(out=kj[:], in_=T3[:], func=AF.Exp, scale=1.0 / 16))
                    P.op('act', ['gT2'], ['gqb'], lambda e: e.activation(out=qb[:], in_=T2[:], func=AF.Exp, scale=-1.0 / 16))
                    P.op('dve', ['gT2'], ['gT1'], lambda e, tot=tot: e.tensor_tensor(
                        out=c3(T1), in0=tot.to_broadcast([128, NCH, 64]), in1=c3(T2), op=ALU.subtract))
                    P.op('act', ['gT1'], ['gkd'], lambda e: e.activation(out=kd[:], in_=T1[:], func=AF.Exp, scale=-1.0 / 16))
                    P.op('dve', ['gq32', 'gqi'], ['gqi'], lambda e: e.scalar_tensor_tensor(
                        out=qi[:], in0=qi[:], scalar=qscale, in1=q32[:], op0=ALU.mult, op1=ALU.mult))
                    P.op('dve', ['gk32', 'gkj'], ['gkj'], lambda e: e.tensor_tensor(out=kj[:], in0=kj[:], in1=k32[:], op=ALU.mult))
                    P.op('dve', ['gq32', 'gqb'], ['gqb'], lambda e: e.scalar_tensor_tensor(
                        out=qb[:], in0=qb[:], scalar=qscale, in1=q32[:], op0=ALU.mult, op1=ALU.mult))
                    P.op('dve', ['gk32', 'gkd'], ['gkd'], lambda e: e.tensor_tensor(out=kd[:], in0=kd[:], in1=k32[:], op=ALU.mult))
                    for g0 in range(0, NT, 4):
                        g1 = min(NT, g0 + 4)
                        for tt in range(g0, g1):
                            P.op('pe', ['gkd', 'cst_bf'], ['psb'], lambda e, tt=tt, g0=g0: e.transpose(
                                self.psb[:, (tt - g0) * 128:(tt - g0 + 1) * 128], kd[:, tt * 128:(tt + 1) * 128], self.ident_bf))
                        P.op('act', ['psb'], ['gkdT'], lambda e, g0=g0, g1=g1: e.activation(
                            out=kdT[:, g0:g1, :].rearrange("p a b -> p (a b)"), in_=self.psb[:, 0:(g1 - g0) * 128], func=AF.Copy))
                    P.op('dve', [], ['gS32'], lambda e: e.memset(S32[:], 0.0))
                    P.op('dve', [], [f'gSbf{sbi % 2}'], lambda e, sbi=sbi: e.memset(Sbf[sbi % 2][:], 0.0))
                    order = list(range(NT)) if d == 0 else [1, 0] + list(range(NT - 1, 1, -1))
                    maskd = self.mask_f if d == 0 else self.mask_b
                    for it_, tt in enumerate(order):
                        tsl = slice(tt * 128, (tt + 1) * 128)
                        ab = it_ % 2
                        P.op('pe', ['gkj', 'gqi'], ['ps5'], lambda e, tsl=tsl: e.matmul(
                            self.ps[5][:, 0:128], lhsT=kj[:, tsl], rhs=qi[:, tsl], start=True, stop=True))
                        P.op('dve', ['ps5', 'cst_f'], [f'gA{ab}'], lambda e, ab=ab, maskd=maskd: e.tensor_tensor(
                            out=Abf[ab][:], in0=self.ps[5][:, 0:128], in1=maskd, op=ALU.mult))
                        halves = (0, 1) if d == 0 else (1, 0)
                        psy = [self.ps[0 + 2 * (it_ % 2)], self.ps[1 + 2 * (it_ % 2)]]
                        psyk = [f'ps{0 + 2 * (it_ % 2)}', f'ps{1 + 2 * (it_ % 2)}']
                        for hi, hf in enumerate(halves):
                            csl = slice(hf * 64, (hf + 1) * 64)
                            tok = slice(tt * 128 + hf * 64, tt * 128 + (hf + 1) * 64)
                            sk = f'gSbf{sbi % 2}'
                            Sb = Sbf[sbi % 2]
                            for vc in range(2):
                                if hi == 0:
                                    P.op('pe', ['gVt', f'gA{ab}'], [psyk[vc]], lambda e, vc=vc, tt=tt, ab=ab, psy=psy: e.matmul(
                                        psy[vc][:, 0:128], lhsT=Vt[:, tt, vc * 128:(vc + 1) * 128], rhs=Abf[ab][:], start=True, stop=False))
                                P.op('pe', [sk, 'gqb'], [psyk[vc]], lambda e, vc=vc, Sb=Sb, csl=csl, tok=tok, hi=hi, psy=psy: e.matmul(
                                    psy[vc][:, csl], lhsT=Sb[:, vc * 128:(vc + 1) * 128], rhs=qb[:, tok], start=False, stop=(hi == 1)))
                            ch = tt * 2 + hf
                            P.op('pe', ['gkdT', 'gVt'], ['ps6'], lambda e, csl=csl, tt=tt: e.matmul(
                                self.ps[6][:, 0:256], lhsT=kdT[csl, tt, :], rhs=Vt[csl, tt, :], start=True, stop=True), rg=csl.start)
                            P.op('dve', ['ps6', 'gS32', 'gdec'], ['gS32'], lambda e, ch=ch: e.scalar_tensor_tensor(
                                out=S32[:], in0=S32[:], scalar=dec[:, ch:ch + 1], in1=self.ps[6][:, 0:256], op0=ALU.mult, op1=ALU.add))
                            sbi += 1
                            P.op('act', ['gS32'], [f'gSbf{sbi % 2}'], lambda e, sbi=sbi: e.activation(
                                out=Sbf[sbi % 2][:], in_=S32[:], func=AF.Copy))
                        if d == 0:
                            for vc in range(2):
                                P.op('act', [psyk[vc]], ['gyf'], lambda e, vc=vc, tsl=tsl, psy=psy: e.activation(
                                    out=yf[:, vc, tsl], in_=psy[vc][:, 0:128], func=AF.Copy))
                        elif with_ctx or tt >= 2:
                            ytb = yt[it_ % 2]
                            ytk = f'gyt{it_ % 2}'
                            for vc in range(2):
                                P.op('dve', [psyk[vc], 'gyf'], [ytk], lambda e, vc=vc, tsl=tsl, psy=psy, ytb=ytb: e.tensor_tensor(
                                    out=ytb[:, vc, :], in0=psy[vc][:, 0:128], in1=yf[:, vc, tsl], op=ALU.add))
                            P.op('act', [ytk], ['gysq'], lambda e, ytb=ytb: e.activation(out=ysq[:], in_=ytb[:], func=AF.Square))
                            for vc in range(2):
                                P.op('pe', ['gysq', 'ones_bf'], ['ps4'], lambda e, vc=vc: e.matmul(
                                    self.ps[4][:, 0:128], lhsT=self.ones_bf[:], rhs=ysq[:, vc, :], start=(vc == 0), stop=(vc == 1)))
                            P.op('act', ['ps4'], ['gyrt'], lambda e: e.activation(
                                out=yrt[:], in_=self.ps[4][:, 0:128], func=AF.Sqrt, scale=1.0 / 256, bias=self.eps_t[:, 0:1]))
                            P.op('dve', ['gyrt'], ['gyrt'], lambda e: e.reciprocal(out=yrt[:], in_=yrt[:]))
                            for vc in range(2):
                                P.op('dve', [ytk, 'gyrt', f'vecs{l}'], [ytk], lambda e, vc=vc, ytb=ytb: e.scalar_tensor_tensor(
                                    out=ytb[:, vc, :], in0=ytb[:, vc, :], scalar=gng[:, vc:vc + 1], in1=yrt[:], op0=ALU.mult, op1=ALU.mult))
                                P.op('dve', [ytk, 'grz'], ['gyo'], lambda e, vc=vc, ytb=ytb, tsl=tsl: e.tensor_tensor(
                                    out=yo[:, vc, tsl], in0=ytb[:, vc, :], in1=rz[:, vc, tsl], op=ALU.mult))
                t_lo = 0 if with_ctx else NCTX
                P.dma(self.yT[1, h * 256:(h + 1) * 256, t_lo:].rearrange("(c p) t -> p c t", p=128), yo[:, :, t_lo:], ['gyo'], ['yT'])
            if 'gla' in self.debug:
                self.dbg(f'ygla{l}', lambda o: P.dma(o, self.yT[1], ['yT'], []), [1024, TT], BF16)


    def _shift(self, z, zk, tmp, tmpk, om, hm, m):
        P = self.P
        P.op('dve', [zk], [tmpk], lambda e: e.tensor_tensor(out=tmp[:m, 1:TT - 1], in0=z[:m, 0:TT - 2], in1=z[:m, 2:TT], op=ALU.add))
        for (dst, src_) in ((0, 1), (NCTX - 1, NCTX - 2), (NCTX, NCTX + 1), (TT - 1, TT - 2)):
            P.op('dve', [zk, tmpk], [tmpk], lambda e, dst=dst, src_=src_: e.tensor_copy(out=tmp[:m, dst:dst + 1], in_=z[:m, src_:src_ + 1]))
        P.op('dve', [tmpk], [tmpk], lambda e: e.tensor_scalar(out=tmp[:m, :], in0=tmp[:m, :], scalar1=hm, scalar2=None, op0=ALU.mult))
        P.op('dve', [zk, tmpk], [zk], lambda e: e.scalar_tensor_tensor(out=z[:m, :], in0=z[:m, :], scalar=om, in1=tmp[:m, :], op0=ALU.mult, op1=ALU.add))

    def _mu_prep(self, st, l):
        P = self.P
        o0, _ = VEC_SLOTS['mu_r']
        mu = self.vecs[l][:, o0:o0 + 30]
        om = self.sb(st, "rw_om", [128, 30]); hm = self.sb(st, "rw_hm", [128, 30])
        P.op('dve', [f'vecs{l}'], ['rw_om'], lambda e: e.tensor_scalar(out=om[:], in0=mu, scalar1=-1.0, scalar2=1.0, op0=ALU.mult, op1=ALU.add))
        P.op('dve', [f'vecs{l}'], ['rw_hm'], lambda e: e.tensor_scalar(out=hm[:], in0=mu, scalar1=0.5, scalar2=None, op0=ALU.mult))
        return om, hm

    def rwkv_pre(self, l):
        nc, P = self.nc, self.P
        with contextlib.ExitStack() as st:
            om, hm = self._mu_prep(st, l)
            zt = self.sb(st, "rp_z", [128, TT]); tmp = self.sb(st, "rp_tmp", [128, TT])
            twd = [self.sb(st, f"rp_twd{d}", [96, TT], BF16) for d in range(2)]
            adb = [self.sb(st, f"rp_adb{d}", [96, TT], BF16) for d in range(2)]
            sgd = self.sb(st, "rp_sgd", [128, 2, TT], BF16)
            w2 = self.sb(st, "rp_w2", [96, 2, 1024], BF16); a2 = self.sb(st, "rp_a2", [96, 2, 1024], BF16)
            g2 = self.sb(st, "rp_g2", [128, 2, 1024], BF16)
            stg = [self.sb(st, f"rp_st{i}", [128, 512]) for i in range(4)]
            stb = [self.sb(st, f"rp_sb{i}", [128, 512], BF16) for i in range(2)]
            P.dma(w2[:], self.rw_w2[l].rearrange("d k c -> k d c"), [], ['rp_w2'], q='pool')
            P.dma(a2[:], self.rw_a2[l].rearrange("d k c -> k d c"), [], ['rp_a2'], q='pool')
            P.dma(g2[:], self.rw_g2[l].rearrange("(c p) n -> p c n", p=128), [], ['rp_g2'], q='pool')
            for d in range(2):
                P.dma(zt[:96, :], self.zT[C_RWD + 96 * d:C_RWD + 96 * (d + 1), :], ['zT'], ['rp_z'])
                self._shift(zt, 'rp_z', tmp, 'rp_tmp', om[:96, 24 + d:25 + d], hm[:96, 24 + d:25 + d], 96)
                P.op('act', ['rp_z'], [f'rp_twd{d}'], lambda e, d=d: e.activation(out=twd[d][:], in_=zt[:96, :], func=AF.Tanh))
                P.dma(zt[:96, :], self.zT[C_RAD + 96 * d:C_RAD + 96 * (d + 1), :], ['zT'], ['rp_z'])
                self._shift(zt, 'rp_z', tmp, 'rp_tmp', om[:96, 26 + d:27 + d], hm[:96, 26 + d:27 + d], 96)
                P.op('act', ['rp_z'], [f'rp_adb{d}'], lambda e, d=d: e.activation(out=adb[d][:], in_=zt[:96, :], func=AF.Copy))
            for c in range(2):
                P.dma(zt[:, :], self.zT[C_RGD + 128 * c:C_RGD + 128 * (c + 1), :], ['zT'], ['rp_z'])
                self._shift(zt, 'rp_z', tmp, 'rp_tmp', om[:, 28 + c:29 + c], hm[:, 28 + c:29 + c], 128)
                P.op('act', ['rp_z'], ['rp_sgd'], lambda e, c=c: e.activation(out=sgd[:, c, :], in_=zt[:, :], func=AF.Sigmoid))
            w0 = self.vec(l, 'w0'); a0 = self.vec(l, 'a0')
            n_ = 0
            for hp in range(8):
                cs = slice(hp * 128, (hp + 1) * 128)
                for (t0, n) in TILES:
                    for d in range(2):
                        for which in range(2):
                            pi = n_ % 4; n_ += 1
                            pst, pk = self.ps[pi], f'ps{pi}'
                            wm, src_, bias_ = ((w2, twd[d], w0), (a2, adb[d], a0))[which]
                            rk_ = [('rp_w2', f'rp_twd{d}'), ('rp_a2', f'rp_adb{d}')][which]
                            P.op('pe', list(rk_), [pk], lambda e, wm=wm, src_=src_, d=d, t0=t0, n=n, pst=pst, cs=cs: e.matmul(
                                pst[:, :n], lhsT=wm[:, d, cs], rhs=src_[:, t0:t0 + n], start=True, stop=True), rg=0)
                            sg = stg[pi]
                            P.op('act', [pk, f'vecs{l}'], [f'rp_st{pi}'], lambda e, pst=pst, sg=sg, n=n, bias_=bias_, d=d, hp=hp: e.activation(
                                out=sg[:, :n], in_=pst[:, :n], func=AF.Sigmoid, bias=bias_[:, d * 8 + hp:d * 8 + hp + 1], scale=1.0))
                            if which == 0:
                                P.op('dve', [f'rp_st{pi}'], [f'rp_st{pi}'], lambda e, sg=sg, n=n: e.tensor_scalar(
                                    out=sg[:, :n], in0=sg[:, :n], scalar1=-0.6065306597126334, scalar2=None, op0=ALU.mult))
                                P.dma(self.ldT[d, cs, t0:t0 + n], sg[:, :n], [f'rp_st{pi}'], ['ldT'])
                            else:
                                P.dma(self.aT[d, cs, t0:t0 + n], sg[:, :n], [f'rp_st{pi}'], ['aT'])
                    pi = n_ % 4; n_ += 1
                    pst, pk = self.ps[pi], f'ps{pi}'
                    for c in range(2):
                        P.op('pe', ['rp_g2', 'rp_sgd'], [pk], lambda e, c=c, t0=t0, n=n, pst=pst, cs=cs: e.matmul(
                            pst[:, :n], lhsT=g2[:, c, cs], rhs=sgd[:, c, t0:t0 + n], start=(c == 0), stop=(c == 1)))
                    bi = n_ % 2
                    P.op('act', [pk], [f'rp_sb{bi}'], lambda e, pst=pst, n=n, bi=bi: e.activation(out=stb[bi][:, :n], in_=pst[:, :n], func=AF.Copy))
                    P.dma(self.g2T[cs, t0:t0 + n], stb[bi][:, :n], [f'rp_sb{bi}'], ['g2T'])

    def rwkv_mixer(self, l):
        nc, P = self.nc, self.P
        with_ctx = l < DEPTH - 1
        NCH = TT // 64
        NT = TT // 128
        with contextlib.ExitStack() as st:
            om, hm = self._mu_prep(st, l)
            A = [self.sb(st, f"rwA{i}", [128, TT]) for i in range(7)]
            Ak = [f'rwA{i}' for i in range(7)]
            msk = self.sb(st, "rmsk", [128, TT])
            vb = self.sb(st, "rw_vb", [128, TT], BF16)
            bon = self.sb(st, "rw_bon", [128, TT], BF16)
            gsb = self.sb(st, "rw_g", [128, 512], BF16)
            al = self.sb(st, "rw_al", [128, TT], BF16); rho = self.sb(st, "rw_rho", [128, TT], BF16)
            be = self.sb(st, "rw_be", [128, TT], BF16); ka = self.sb(st, "rw_ka", [128, TT], BF16)
            Bp = self.sb(st, "rw_Bp", [128, TT], BF16); Kp = self.sb(st, "rw_Kp", [128, TT], BF16)
            al_tm = self.sb(st, "rw_altm", [128, NT, 128], BF16); Bp_tm = self.sb(st, "rw_Bptm", [128, NT, 128], BF16)
            Kp_tm = self.sb(st, "rw_Kptm", [128, NT, 128], BF16); V_tm = self.sb(st, "rw_Vtm", [128, NT, 128], BF16)
            etot = self.sb(st, "rw_etot", [128, NCH])
            slots = []
            s0 = dict(XN=[[self.sb(st, f"rw_X{i}", [128, 512], BF16), self.sb(st, f"rw_N{i}", [128, 512], BF16)] for i in range(2)],
                      Q=[self.sb(st, f"rw_Q{i}", [128, 512], BF16) for i in range(2)],
                      LkT=self.sb(st, "rw_LkT", [128, 512], BF16), MbT=self.sb(st, "rw_MbT", [128, 512], BF16), MkT=self.sb(st, "rw_MkT", [128, 512], BF16),
                      H=self.sb(st, "rw_H", [128, 256], BF16)[:], P1n=self.sb(st, "rw_P1n", [128, 256], BF16)[:], G=self.sb(st, "rw_G", [128, 256], BF16)[:],
                      ptmp=self.sb(st, "rw_ptmp", [128, 128])[:], banks=(self.ps[0], self.ps[1], self.ps[2]), bank_ids=(0, 1, 2))
            slots.append(s0)
            a2b = A[2][:].bitcast(BF16)
            cut = lambda i: a2b[:, i * 512:(i + 1) * 512]
            rt = self.sb(st, "rw_rt", [128, 512]); t5 = self.sb(st, "rw_t5", [128, 512])
            t5b = t5[:].bitcast(BF16)
            s1_ = dict(XN=[[cut(0), cut(1)], [cut(2), cut(3)]], Q=[cut(4), cut(5)], LkT=cut(6), MbT=cut(7), MkT=cut(8),
                       H=t5b[:, 0:256], P1n=t5b[:, 256:512], G=t5b[:, 512:768], ptmp=rt[:, 0:128],
                       banks=(self.ps[3], self.ps[5], self.ps[6]), bank_ids=(3, 5, 6))
            slots.append(s1_)
            for nm in ('rw_X0', 'rw_N0', 'rw_X1', 'rw_N1', 'rw_Q0', 'rw_Q1', 'rw_LkT', 'rw_MbT', 'rw_MkT'):
                P.set_parent(nm + '_s1', Ak[2])
            for nm in ('rw_H', 'rw_P1n', 'rw_G'):
                P.set_parent(nm + '_s1', 'rw_t5')
            P.set_parent('rw_ptmp_s1', 'rw_rt')
            a5b = A[5][:].bitcast(BF16); a6b = A[6][:].bitcast(BF16)
            al_m = [a5b[:, 0:TT], a5b[:, TT:2 * TT]]
            rho_m = [a6b[:, 0:TT], a6b[:, TT:2 * TT]]
            P.set_parent('rw_alm', Ak[5]); P.set_parent('rw_rhom', Ak[6])
            R32 = self.sb(st, "rw_R32", [128, TT]); yloc = self.sb(st, "rw_yloc", [128, TT]); yacc = self.sb(st, "rw_yacc", [128, TT])
            Phi = self.sb(st, "rw_Phi", [128, NCH, 128]); Dd = self.sb(st, "rw_D", [128, NCH, 128], BF16)
            Sbd = [self.sb(st, f"rw_S{i}", [128, 128]) for i in range(2)]
            ka1 = self.sb(st, "rw_ka1", [128, 8])
            sq5 = self.sb(st, "rw_sq5", [128, 512], BF16)
            yo = vb
            geps = self.sb(st, "rw_geps", [128, 1])
            P.op('pool', [], ['rw_geps'], lambda e: e.memset(geps[:], 64e-5))
            P.op('pool', [], ['rmsk'], lambda e: e.memset(msk[:], 1.0))
            P.op('pool', ['rmsk'], ['rmsk'], lambda e: e.memset(msk[:].rearrange("p (c j) -> p c j", j=64)[:, :, 0:1], 0.0))
            P.op('dve', [f'vecs{l}'], ['rw_ka1'], lambda e: e.tensor_scalar(
                out=ka1[:], in0=self.vec(l, 'ka'), scalar1=-1.0, scalar2=1.0, op0=ALU.mult, op1=ALU.add))
            c3 = lambda t: t[:].rearrange("p (c j) -> p c j", j=64)
            kkv = self.vec(l, 'kk'); kav = self.vec(l, 'ka'); rkv = self.vec(l, 'rk')
            gng = self.vec(l, 'gng_rw'); gnb = self.vec(l, 'gnb_rw')
            si = 0
            for hp in range(8):
                cs = slice(hp * 128, (hp + 1) * 128)
                self.sc_in('rw_load', hp == 0 and l == 0)
                r32, k32, v32, kk32 = A[0], A[1], A[2], A[3]
                for i_, c0 in enumerate((C_RR, C_RK, C_RV)):
                    P.dma(A[i_][:], self.zT[c0 + hp * 128:c0 + (hp + 1) * 128, :], ['zT'], [Ak[i_]])
                    ci = i_ * 8 + hp
                    self._shift(A[i_], Ak[i_], A[6], Ak[6], om[:, ci:ci + 1], hm[:, ci:ci + 1], 128)
                P.op('act', [Ak[2]], ['rw_vb'], lambda e: e.activation(out=vb[:], in_=v32[:], func=AF.Copy))
                for (srcb, srck, dst, dstk) in ((vb, 'rw_vb', V_tm, 'rw_Vtm'),):
                    for g0 in range(0, NT, 4):
                        g1 = min(NT, g0 + 4)
                        for tt in range(g0, g1):
                            P.op('pe', [srck, 'cst_bf'], ['psb'], lambda e, tt=tt, g0=g0, srcb=srcb: e.transpose(
                                self.psb[:, (tt - g0) * 128:(tt - g0 + 1) * 128], srcb[:, tt * 128:(tt + 1) * 128], self.ident_bf))
                        P.op('act', ['psb'], [dstk], lambda e, g0=g0, g1=g1, dst=dst: e.activation(
                            out=dst[:, g0:g1, :].rearrange("p a b -> p (a b)"), in_=self.psb[:, 0:(g1 - g0) * 128], func=AF.Copy))
                P.op('dve', [Ak[1], f'vecs{l}'], [Ak[3]], lambda e: e.tensor_scalar(
                    out=kk32[:], in0=k32[:], scalar1=kkv[:, hp:hp + 1], scalar2=None, op0=ALU.mult))
                P.op('dve', [Ak[0], Ak[1], f'vecs{l}'], [Ak[6]], lambda e: e.scalar_tensor_tensor(
                    out=A[6][:], in0=r32[:], scalar=rkv[:, hp:hp + 1], in1=k32[:], op0=ALU.mult, op1=ALU.mult))
                for (t0, n) in TILES:
                    P.op('act', [Ak[3]], ['rw_sq5'], lambda e, t0=t0, n=n: e.activation(out=sq5[:, :n], in_=kk32[:, t0:t0 + n], func=AF.Square))
                    P.op('pe', ['rw_sq5', 'bd_bf'], ['ps4'], lambda e, n=n: e.matmul(self.ps[4][:, :n], lhsT=self.bd_bf[:], rhs=sq5[:, :n], start=True, stop=True))
                    P.op('act', ['ps4'], ['rw_rt'], lambda e, n=n: e.activation(out=rt[:, :n], in_=self.ps[4][:, :n], func=AF.Sqrt, scale=1.0, bias=self.eps_t[:, 0:1]))
                    P.op('dve', ['rw_rt'], ['rw_rt'], lambda e, n=n: e.reciprocal(out=rt[:, :n], in_=rt[:, :n]))
                    P.op('dve', [Ak[3], 'rw_rt'], [Ak[3]], lambda e, t0=t0, n=n: e.tensor_tensor(out=kk32[:, t0:t0 + n], in0=kk32[:, t0:t0 + n], in1=rt[:, :n], op=ALU.mult))
                    P.op('act', [Ak[6]], ['rw_sq5'], lambda e, t0=t0, n=n: e.activation(out=sq5[:, :n], in_=A[6][:, t0:t0 + n], func=AF.Copy))
                    P.op('pe', ['rw_sq5', 'bd_bf'], ['ps5'], lambda e, n=n: e.matmul(self.ps[5][:, :n], lhsT=self.bd_bf[:], rhs=sq5[:, :n], start=True, stop=True))
                    P.op('dve', ['ps5', Ak[2]], ['rw_bon'], lambda e, t0=t0, n=n: e.tensor_tensor(out=bon[:, t0:t0 + n], in0=self.ps[5][:, :n], in1=v32[:, t0:t0 + n], op=ALU.mult))
                for d in range(2):
                    self.sc_in('rw_prep', hp == 0 and d == 0 and l == 0)
                    ld, a_, cin, E = A[4], A[5], A[2], A[6]
                    ldk, ak_, cink, Ek = Ak[4], Ak[5], Ak[2], Ak[6]
                    P.dma(ld[:], self.ldT[d, cs, :], ['ldT'], [ldk])
                    P.dma(a_[:], self.aT[d, cs, :], ['aT'], [ak_])
                    P.op('dve', ['rmsk', ldk], [cink], lambda e: e.tensor_tensor_scan(
                        out=cin[:], data0=msk[:], data1=ld[:], initial=0.0, op0=ALU.mult, op1=ALU.add))
                    if d == 1:
                        P.op('dve', [cink], [Ek], lambda e: e.tensor_tensor(
                            out=c3(E), in0=c3(cin)[:, :, 63:64].to_broadcast([128, NCH, 64]), in1=c3(cin), op=ALU.subtract))
                        P.op('dve', [Ek, ldk], [cink], lambda e: e.tensor_tensor(out=cin[:], in0=E[:], in1=ld[:], op=ALU.add))
                    tot = c3(cin)[:, :, 63:64] if d == 0 else c3(cin)[:, :, 0:1]
                    P.op('act', [cink], ['rw_etot'], lambda e, tot=tot: e.activation(out=etot[:].unsqueeze(2), in_=tot, func=AF.Exp))
                    P.op('dve', [cink, ldk], [ldk], lambda e: e.tensor_tensor(out=ld[:], in0=cin[:], in1=ld[:], op=ALU.subtract))
                    P.op('act', [ldk], ['rw_al'], lambda e: e.activation(out=al[:], in_=ld[:], func=AF.Exp))
                    P.op('act', [cink], [Ek], lambda e: e.activation(out=E[:], in_=cin[:], func=AF.Exp))
                    P.op('act', [cink], ['rw_be'], lambda e: e.activation(out=be[:], in_=cin[:], func=AF.Exp, scale=-1.0))
                    P.op('act', [cink], ['rw_ka'], lambda e: e.activation(out=ka[:], in_=cin[:], func=AF.Exp, scale=-1.0))
                    P.op('dve', [Ak[3], 'rw_al'], ['rw_al'], lambda e: e.tensor_tensor(out=al[:], in0=al[:], in1=kk32[:], op=ALU.mult))
                    rho32 = ld
                    P.op('dve', [Ak[0], Ek], [ldk], lambda e: e.tensor_tensor(out=rho32[:], in0=r32[:], in1=E[:], op=ALU.mult))
                    P.op('act', [ldk], ['rw_rho'], lambda e: e.activation(out=rho[:], in_=rho32[:], func=AF.Copy))
                    P.op('dve', ['rw_be', ak_], ['rw_be'], lambda e: e.tensor_tensor(out=be[:], in0=be[:], in1=a_[:], op=ALU.mult))
                    P.op('dve', ['rw_be', Ak[3]], ['rw_be'], lambda e: e.tensor_tensor(out=be[:], in0=be[:], in1=kk32[:], op=ALU.mult))
                    P.op('dve', [ak_, f'vecs{l}', 'rw_ka1'], [ak_], lambda e: e.tensor_scalar(
                        out=a_[:], in0=a_[:], scalar1=kav[:, hp:hp + 1], scalar2=ka1[:, hp:hp + 1], op0=ALU.mult, op1=ALU.add))
                    P.op('dve', [ak_, Ak[1]], [ak_], lambda e: e.tensor_tensor(out=a_[:], in0=a_[:], in1=k32[:], op=ALU.mult))
                    P.op('dve', [ak_, 'rw_ka'], ['rw_ka'], lambda e: e.tensor_tensor(out=ka[:], in0=ka[:], in1=a_[:], op=ALU.mult))
                    eb = etot[:].unsqueeze(2).to_broadcast([128, NCH, 64])
                    P.op('dve', ['rw_be', 'rw_etot'], ['rw_Bp'], lambda e: e.tensor_tensor(out=c3(Bp), in0=c3(be), in1=eb, op=ALU.mult))
                    P.op('dve', ['rw_ka', 'rw_etot'], ['rw_Kp'], lambda e: e.tensor_tensor(out=c3(Kp), in0=c3(ka), in1=eb, op=ALU.mult))
                    self.stopat(1)
                    self.sc_in('rw_tm', hp == 0 and d == 0 and l == 0)
                    for (srcb, srck, dst, dstk) in ((al, 'rw_al', al_tm, 'rw_altm'), (Bp, 'rw_Bp', Bp_tm, 'rw_Bptm'), (Kp, 'rw_Kp', Kp_tm, 'rw_Kptm')):
                        for g0 in range(0, NT, 4):
                            g1 = min(NT, g0 + 4)
                            for tt in range(g0, g1):
                                P.op('pe', [srck, 'cst_bf'], ['psb'], lambda e, tt=tt, g0=g0, srcb=srcb: e.transpose(
                                    self.psb[:, (tt - g0) * 128:(tt - g0 + 1) * 128], srcb[:, tt * 128:(tt + 1) * 128], self.ident_bf))
                            P.op('act', ['psb'], [dstk], lambda e, g0=g0, g1=g1, dst=dst: e.activation(
                                out=dst[:, g0:g1, :].rearrange("p a b -> p (a b)"), in_=self.psb[:, 0:(g1 - g0) * 128], func=AF.Copy))
                    P.op('pool', [], ['rw_alm'], lambda e: e.memset(a5b, 0.0))
                    P.op('pool', [], ['rw_rhom'], lambda e: e.memset(a6b, 0.0))
                    for e_ in range(2):
                        pr = slice(e_ * 64, (e_ + 1) * 64)
                        P.op('act', ['rw_al'], ['rw_alm'], lambda e, e_=e_, pr=pr: e.activation(out=al_m[e_][pr, :], in_=al[pr, :], func=AF.Copy))
                        P.op('act', ['rw_rho'], ['rw_rhom'], lambda e, e_=e_, pr=pr: e.activation(out=rho_m[e_][pr, :], in_=rho[pr, :], func=AF.Copy))
                    self.stopat(2)
                    m_st = self.mask_fs if d == 0 else self.mask_bs
                    m_in = self.mask_f if d == 0 else self.mask_b
                    m_ts = self.mask_bs if d == 0 else self.mask_fs
                    bc4 = lambda m: m.unsqueeze(1).to_broadcast([128, 4, 128])
                    v4 = lambda t: t[:].rearrange("p (a b) -> p a b", b=128)
                    def grp(g0, sl):
                        B_ = slots[sl]
                        pa, pb, pc = B_['banks']
                        pak, pbk, pck = (f'ps{i}' for i in B_['bank_ids'])
                        XNs, Qs, LkT, MbT, MkT, Hh, P1n, Gt, ptmp = B_['XN'], B_['Q'], B_['LkT'], B_['MbT'], B_['MkT'], B_['H'], B_['P1n'], B_['G'], B_['ptmp']
                        kx = lambda nm: f'{nm}_s{sl}'
                        probs = [(tt, e_) for tt in (g0, g0 + 1) for e_ in range(2)]
                        X, N_ = XNs[0]

                        def gram(specs):
                            for pi_, (tt, e_) in enumerate(probs):
                                tsl = slice(tt * 128, (tt + 1) * 128); osl = slice(pi_ * 128, (pi_ + 1) * 128)
                                for (pst, pk, lh, rh, rks) in specs:
                                    lh_ = lh[e_] if isinstance(lh, list) else lh
                                    rh_ = rh[e_] if isinstance(rh, list) else rh
                                    P.op('pe', rks, [pk], lambda e, pst=pst, lh_=lh_, rh_=rh_, tsl=tsl, osl=osl: e.matmul(
                                        pst[:, osl], lhsT=lh_[:, tsl], rhs=rh_[:, tsl], start=True, stop=True))
                        gram(((pa, pak, be, al_m, ['rw_be', 'rw_alm']), (pb, pbk, al_m, be, ['rw_alm', 'rw_be']), (pc, pck, ka, al_m, ['rw_ka', 'rw_alm'])))
                        P.op('dve', [pak, 'cst_f'], [kx('rw_X0')], lambda e: e.scalar_tensor_tensor(
                            out=v4(X), in0=v4(pa), scalar=-1.0, in1=bc4(m_st), op0=ALU.mult, op1=ALU.mult))
                        P.op('dve', [pbk, 'cst_f'], [kx('rw_N0')], lambda e: e.scalar_tensor_tensor(
                            out=v4(N_), in0=v4(pb), scalar=-1.0, in1=bc4(m_ts), op0=ALU.mult, op1=ALU.mult))
                        P.op('dve', [pck, 'cst_f'], [kx('rw_LkT')], lambda e: e.tensor_tensor(out=v4(LkT), in0=v4(pc), in1=bc4(m_st), op=ALU.mult))
                        yield
                        pa2, pa2k = pa, pak
                        gram(((pa2, pa2k, be, rho_m, ['rw_be', 'rw_rhom']), (pb, pbk, ka, rho_m, ['rw_ka', 'rw_rhom'])))
                        P.op('dve', [pa2k, 'cst_f'], [kx('rw_MbT')], lambda e: e.tensor_tensor(out=v4(MbT), in0=v4(pa2), in1=bc4(m_in), op=ALU.mult))
                        P.op('dve', [pbk, 'cst_f'], [kx('rw_MkT')], lambda e: e.tensor_tensor(out=v4(MkT), in0=v4(pb), in1=bc4(m_in), op=ALU.mult))
                        P.op('dve', [kx('rw_X0'), 'cst_bf'], [kx('rw_Q0')], lambda e: e.tensor_tensor(
                            out=v4(Qs[0]), in0=v4(X), in1=self.ident_bf.unsqueeze(1).to_broadcast([128, 4, 128]), op=ALU.add))
                        yield
                        qi_ = 0
                        for j in range(1, 6):
                            Xo, No = XNs[(j - 1) % 2]
                            Xn, Nn = XNs[j % 2]
                            xo_k, no_k = kx(f'rw_X{(j - 1) % 2}'), kx(f'rw_N{(j - 1) % 2}')
                            xn_k, nn_k = kx(f'rw_X{j % 2}'), kx(f'rw_N{j % 2}')
                            for pi_ in range(4):
                                osl = slice(pi_ * 128, (pi_ + 1) * 128)
                                P.op('pe', [xo_k, no_k], [pak], lambda e, osl=osl, Xo=Xo, No=No: e.matmul(
                                    pa[:, osl], lhsT=No[:, osl], rhs=Xo[:, osl], start=True, stop=True))
                                P.op('pe', [xo_k, no_k], [pbk], lambda e, osl=osl, Xo=Xo, No=No: e.matmul(
                                    pb[:, osl], lhsT=Xo[:, osl], rhs=No[:, osl], start=True, stop=True))
                            P.op('act', [pak], [xn_k], lambda e, Xn=Xn: e.activation(out=Xn[:], in_=pa[:], func=AF.Copy))
                            P.op('act', [pbk], [nn_k], lambda e, Nn=Nn: e.activation(out=Nn[:], in_=pb[:], func=AF.Copy))
                            yield
                            Qo, Qn = Qs[qi_ % 2], Qs[(qi_ + 1) % 2]
                            qo_k, qn_k = kx(f'rw_Q{qi_ % 2}'), kx(f'rw_Q{(qi_ + 1) % 2}')
                            for pi_ in range(4):
                                osl = slice(pi_ * 128, (pi_ + 1) * 128)
                                P.op('pe', [nn_k, qo_k], [pck], lambda e, osl=osl, Nn=Nn, Qo=Qo: e.matmul(
                                    pc[:, osl], lhsT=Nn[:, osl], rhs=Qo[:, osl], start=True, stop=True))
                            P.op('dve', [pck, qo_k], [qn_k], lambda e, Qo=Qo, Qn=Qn: e.tensor_tensor(out=Qn[:], in0=pc[:], in1=Qo[:], op=ALU.add))
                            qi_ += 1
                            yield
                        Qf = Qs[qi_ % 2]; qf_k = kx(f'rw_Q{qi_ % 2}')
                        for pi_, (tt, e_) in enumerate(probs):
                            pr = slice(e_ * 64, (e_ + 1) * 64); osl = slice(pi_ * 128, (pi_ + 1) * 128)
                            P.op('pe', [kx('rw_LkT'), 'rw_Vtm'], [pak], lambda e, osl=osl, tt=tt, pr=pr, pi_=pi_: e.matmul(
                                pa[:, pi_ * 64:(pi_ + 1) * 64], lhsT=LkT[:, osl], rhs=V_tm[:, tt, pr], start=True, stop=True))
                            P.op('pe', [qf_k, 'rw_altm'], [pak], lambda e, osl=osl, tt=tt, pr=pr, pi_=pi_, Qf=Qf: e.matmul(
                                pa[:, 256 + pi_ * 64:256 + (pi_ + 1) * 64], lhsT=Qf[:, osl], rhs=al_tm[:, tt, pr], start=True, stop=True))
                        P.op('act', [pak], [kx('rw_H')], lambda e: e.activation(out=Hh, in_=pa[:, 0:256], func=AF.Copy))
                        P.op('act', [pak], [kx('rw_G')], lambda e: e.activation(out=Gt, in_=pa[:, 256:512], func=AF.Copy))
                        yield
                        H3 = Hh.rearrange("p (a b) -> p a b", b=64); G3 = Gt.rearrange("p (a b) -> p a b", b=64); P3 = P1n.rearrange("p (a b) -> p a b", b=64)
                        for pi_, (tt, e_) in enumerate(probs):
                            osl = slice(pi_ * 128, (pi_ + 1) * 128)
                            P.op('pe', [qf_k, kx('rw_H')], [pbk], lambda e, osl=osl, pi_=pi_, Qf=Qf: e.matmul(
                                pb[:, pi_ * 64:(pi_ + 1) * 64], lhsT=Qf[:, osl], rhs=H3[:, pi_, :], start=True, stop=True))
                        P.op('act', [pbk], [kx('rw_P1n')], lambda e: e.activation(out=P1n, in_=pb[:, 0:256], func=AF.Identity, scale=-1.0))
                        yield
                        for ti_, tt in enumerate((g0, g0 + 1)):
                            for e_ in range(2):
                                pi_ = ti_ * 2 + e_
                                pr = slice(e_ * 64, (e_ + 1) * 64); osl = slice(pi_ * 128, (pi_ + 1) * 128)
                                P.op('pe', [kx('rw_G'), kx('rw_MbT')], [pak], lambda e, pr=pr, osl=osl, pi_=pi_, ti_=ti_: e.matmul(
                                    pa[pr, ti_ * 128:(ti_ + 1) * 128], lhsT=G3[:, pi_, :], rhs=MbT[:, osl], start=True, stop=True))
                                P.op('pe', ['rw_Vtm', kx('rw_MkT')], [pbk], lambda e, pr=pr, osl=osl, tt=tt, ti_=ti_: e.matmul(
                                    pb[pr, ti_ * 128:(ti_ + 1) * 128], lhsT=V_tm[:, tt, pr], rhs=MkT[:, osl], start=True, stop=False))
                                P.op('pe', [kx('rw_P1n'), kx('rw_MbT')], [pbk], lambda e, pr=pr, osl=osl, pi_=pi_, ti_=ti_: e.matmul(
                                    pb[pr, ti_ * 128:(ti_ + 1) * 128], lhsT=P3[:, pi_, :], rhs=MbT[:, osl], start=False, stop=True))
                        t2 = slice(g0 * 128, (g0 + 2) * 128)
                        P.op('dve', [pak, ldk], ['rw_R32'], lambda e, t2=t2: e.tensor_tensor(out=R32[:, t2], in0=rho32[:, t2], in1=pa[:, 0:256], op=ALU.subtract))
                        P.op('act', [pbk], ['rw_yloc'], lambda e, t2=t2: e.activation(out=yloc[:, t2], in_=pb[:, 0:256], func=AF.Copy))
                        yield
                        for ti_, tt in enumerate((g0, g0 + 1)):
                            for hf in range(2):
                                csl = slice(hf * 64, (hf + 1) * 64)
                                gsl = G3[csl, ti_ * 2:ti_ * 2 + 2, :].rearrange("p a b -> p (a b)")
                                p1sl = P3[csl, ti_ * 2:ti_ * 2 + 2, :].rearrange("p a b -> p (a b)")
                                col = slice((ti_ * 2 + hf) * 128, (ti_ * 2 + hf + 1) * 128)
                                P.op('pe', [kx('rw_G'), 'rw_Bptm'], [pak], lambda e, gsl=gsl, csl=csl, tt=tt, col=col: e.matmul(
                                    pa[:, col], lhsT=gsl, rhs=Bp_tm[csl, tt, :], start=True, stop=True))
                                P.op('pe', ['rw_Kptm', 'rw_Vtm'], [pck], lambda e, csl=csl, tt=tt, col=col: e.matmul(
                                    pc[:, col], lhsT=Kp_tm[csl, tt, :], rhs=V_tm[csl, tt, :], start=True, stop=False))
                                P.op('pe', ['rw_Bptm', kx('rw_P1n')], [pck], lambda e, csl=csl, tt=tt, col=col, p1sl=p1sl: e.matmul(
                                    pc[:, col], lhsT=Bp_tm[csl, tt, :], rhs=p1sl, start=False, stop=True))
                        for q_ in range(4):
                            ch = g0 * 2 + q_
                            col = slice(q_ * 128, (q_ + 1) * 128)
                            P.op('dve', [pak, 'cst_f'], [kx('rw_ptmp')], lambda e, col=col: e.tensor_tensor(out=ptmp, in0=pa[:, col], in1=self.mask_bd, op=ALU.mult))
                            P.op('dve', [kx('rw_ptmp'), 'rw_etot', 'cst_f'], ['rw_Phi'], lambda e, ch=ch: e.scalar_tensor_tensor(
                                out=Phi[:, ch, :], in0=self.ident32, scalar=etot[:, ch:ch + 1], in1=ptmp, op0=ALU.mult, op1=ALU.subtract))
                        P.op('dve', [pck, 'cst_f'], ['rw_D'], lambda e, g0=g0: e.tensor_tensor(
                            out=Dd[:, g0 * 2:g0 * 2 + 4, :], in0=v4(pc), in1=bc4(self.mask_bd), op=ALU.mult))

                    self.sc_in('rw_grp', hp == 0 and d == 0 and l == 0)
                    pending = list(range(0, NT, 2))
                    active = {}
                    while pending or active:
                        for sl in range(self.rw_slots):
                            if sl not in active and pending:
                                active[sl] = grp(pending.pop(0), sl)
                        for sl in list(active):
                            try:
                                self._steps = getattr(self, '_steps', 0) + 1
                                if self._steps == self.rw_stop:
                                    P.halt = True
                                next(active[sl])
                            except StopIteration:
                                del active[sl]
                    self.stopat(7)
                    self.sc_in('rw_rec', hp == 0 and d == 0 and l == 0)
                    P.op('dve', [], [f'rw_S{si % 2}'], lambda e, si=si: e.memset(Sbd[si % 2][:], 0.0))
                    order = list(range(NCH)) if d == 0 else [3, 2, 1, 0] + list(range(NCH - 1, 3, -1))
                    for n_i, ch in enumerate(order):
                        S_ = Sbd[si % 2]; sk = f'rw_S{si % 2}'
                        tok = slice(ch * 64, (ch + 1) * 64)
                        yb = n_i % 2
                        need_y = with_ctx or ch >= 4
                        if need_y:
                            P.op('pe', [sk, 'rw_R32'], [f'ps{3 + yb}'], lambda e, S_=S_, tok=tok, yb=yb: e.matmul(
                                self.ps[3 + yb][:, 0:64], lhsT=S_[:], rhs=R32[:, tok], start=True, stop=True))
                            if d == 0:
                                P.op('dve', [f'ps{3 + yb}', 'rw_yloc'], ['rw_yacc'], lambda e, tok=tok, yb=yb: e.tensor_tensor(
                                    out=yacc[:, tok], in0=self.ps[3 + yb][:, 0:64], in1=yloc[:, tok], op=ALU.add))
                            else:
                                P.op('dve', [f'ps{3 + yb}', 'rw_yloc'], ['rw_yloc'], lambda e, tok=tok, yb=yb: e.tensor_tensor(
                                    out=yloc[:, tok], in0=self.ps[3 + yb][:, 0:64], in1=yloc[:, tok], op=ALU.add))
                                P.op('pool', ['rw_yloc', 'rw_yacc'], ['rw_yacc'], lambda e, tok=tok: e.tensor_tensor(
                                    out=yacc[:, tok], in0=yacc[:, tok], in1=yloc[:, tok], op=ALU.add))
                        P.op('pe', [sk, 'rw_Phi'], [f'ps{5 + yb}'], lambda e, S_=S_, ch=ch, yb=yb: e.matmul(
                            self.ps[5 + yb][:, 0:128], lhsT=Phi[:, ch, :], rhs=S_[:], start=True, stop=True))
                        si += 1
                        P.op('dve', [f'ps{5 + yb}', 'rw_D'], [f'rw_S{si % 2}'], lambda e, ch=ch, yb=yb, si=si: e.tensor_tensor(
                            out=Sbd[si % 2][:], in0=self.ps[5 + yb][:, 0:128], in1=Dd[:, ch, :], op=ALU.add))
                    self.stopat(8)
                self.sc_in('rw_fin', hp == 0 and l == 0)
                for (t0, n) in TILES:
                    if not with_ctx and t0 < NCTX:
                        continue
                    ya = yacc[:, t0:t0 + n]
                    P.op('act', ['rw_yacc'], ['rw_sq5'], lambda e, ya=ya, n=n: e.activation(out=sq5[:, :n], in_=ya, func=AF.Copy))
                    P.op('pe', ['rw_sq5', 'bd_bf'], ['ps4'], lambda e, n=n: e.matmul(self.ps[4][:, :n], lhsT=self.bd_bf[:], rhs=sq5[:, :n], start=True, stop=True))
                    P.op('dve', ['ps4', 'rw_yacc'], ['rw_t5'], lambda e, ya=ya, n=n: e.scalar_tensor_tensor(
                        out=t5[:, :n], in0=self.ps[4][:, :n], scalar=-1.0 / 64, in1=ya, op0=ALU.mult, op1=ALU.add))
                    P.op('act', ['rw_t5'], ['rw_sq5'], lambda e, n=n: e.activation(out=sq5[:, :n], in_=t5[:, :n], func=AF.Square))
                    P.op('pe', ['rw_sq5', 'bd_bf'], ['ps4'], lambda e, n=n: e.matmul(self.ps[4][:, :n], lhsT=self.bd_bf[:], rhs=sq5[:, :n], start=True, stop=True))
                    P.op('act', ['ps4'], ['rw_rt'], lambda e, n=n: e.activation(out=rt[:, :n], in_=self.ps[4][:, :n], func=AF.Sqrt, scale=1.0 / 64, bias=geps[:, 0:1]))
                    P.op('dve', ['rw_rt'], ['rw_rt'], lambda e, n=n: e.reciprocal(out=rt[:, :n], in_=rt[:, :n]))
                    P.op('dve', ['rw_t5', 'rw_rt'], ['rw_t5'], lambda e, n=n: e.tensor_tensor(out=t5[:, :n], in0=t5[:, :n], in1=rt[:, :n], op=ALU.mult))
                    P.op('dve', ['rw_t5', f'vecs{l}'], ['rw_t5'], lambda e, n=n: e.tensor_scalar(
                        out=t5[:, :n], in0=t5[:, :n], scalar1=gng[:, hp:hp + 1], scalar2=gnb[:, hp:hp + 1], op0=ALU.mult, op1=ALU.add))
                    P.op('dve', ['rw_t5', 'rw_bon'], ['rw_t5'], lambda e, t0=t0, n=n: e.tensor_tensor(out=t5[:, :n], in0=t5[:, :n], in1=bon[:, t0:t0 + n], op=ALU.add))
                    P.dma(gsb[:, :n], self.g2T[cs, t0:t0 + n], ['g2T'], ['rw_g'])
                    P.op('dve', ['rw_t5', 'rw_g'], ['rw_vb'], lambda e, t0=t0, n=n: e.tensor_tensor(out=yo[:, t0:t0 + n], in0=t5[:, :n], in1=gsb[:, :n], op=ALU.mult))
                self.sc_out()
                t_lo = 0 if with_ctx else NCTX
                P.dma(self.yT[2, cs, t_lo:], yo[:, t_lo:], ['rw_vb'], ['yT'])
            if 'rwkv' in self.debug:
                self.dbg(f'yrw{l}', lambda o: P.dma(o, self.yT[2], ['yT'], []), [1024, TT], BF16)


    def select_pass(self):
        nc, P = self.nc, self.P
        H = SEQ // 2
        with contextlib.ExitStack() as st:
            sw = self.sb(st, "sp_w", [128, 2])
            P.dma(sw[:], self.selw, [], ['sp_w'])
            fa = [self.sb(st, f"sp_fa{i}", [128, 4, H]) for i in range(2)]
            fb = [self.sb(st, f"sp_fb{i}", [128, 4, H]) for i in range(2)]
            ha = [self.sb(st, f"sp_ha{i}", [128, 8, H], BF16) for i in range(2)]
            hb = [self.sb(st, f"sp_hb{i}", [128, 8, H], BF16) for i in range(2)]
            jobs = []
            xv = self.xT.rearrange("(c p) t -> p c t", p=128)
            xo = self.xsel.rearrange("(c p) t -> p c t", p=128)
            for c0 in range(0, KC, 4):
                jobs.append((fa, fb, 'f', xv[:, c0:c0 + 4, :], xo[:, c0:c0 + 4, :], 'xT'))
            yv = self.yT.rearrange("i (c p) t -> p (i c) t", p=128)
            yo = self.ysel.rearrange("i (c p) t -> p (i c) t", p=128)
            for c0 in range(0, 24, 8):
                jobs.append((ha, hb, 'h', yv[:, c0:c0 + 8, :], yo[:, c0:c0 + 8, :], 'yT'))
            cnt = {'f': 0, 'h': 0}
            for (ta, tb, kind, sv, dv, rk_) in jobs:
                i = cnt[kind] % 2
                cnt[kind] += 1
                a, b = ta[i], tb[i]
                ak, bk = f'sp_{kind}a{i}', f'sp_{kind}b{i}'
                P.dma(a[:], sv[:, :, NCTX:NCTX + H], [rk_], [ak])
                P.dma(b[:], sv[:, :, NCTX + H:NCTX + 2 * H], [rk_], [bk], q='act')
                P.op('dve', [ak, 'sp_w'], [ak], lambda e, a=a: e.tensor_scalar(
                    out=a[:], in0=a[:], scalar1=sw[:, 0:1], scalar2=None, op0=ALU.mult))
                P.op('dve', [ak, bk, 'sp_w'], [ak], lambda e, a=a, b=b: e.scalar_tensor_tensor(
                    out=a[:], in0=b[:], scalar=sw[:, 1:2], in1=a[:], op0=ALU.mult, op1=ALU.add))
                P.dma(dv, a[:], [ak], ['sel_out'])

    def merge_moe(self, l):
        nc, P = self.nc, self.P
        with_ctx = l < DEPTH - 1
        last = l == DEPTH - 1
        src = (self.xT0 if l == 0 else self.xT).rearrange("(kc p) t -> p kc t", p=128)
        dstx = self.xT.rearrange("(kc p) t -> p kc t", p=128)
        dsto = self.outT.rearrange("(kc p) t -> p kc t", p=128)
        gTv = self.gT.rearrange("(i c p) t -> p i c t", i=3, p=128)
        yTv = self.yT.rearrange("i (c p) t -> p i c t", p=128)
        tiles = TILES
        if last:
            self.select_pass()
            P.barrier()
            src = self.xsel.rearrange("(kc p) t -> p kc t", p=128)
            gTv = self.gsel.rearrange("(i c p) t -> p i c t", i=3, p=128)
            yTv = self.ysel.rearrange("i (c p) t -> p i c t", p=128)
            tiles = [(0, 512), (512, 512)]
        wbv = self.w_branch[l].rearrange("i (kc p) c -> p i kc c", p=128)
        wov = self.w_out[l].rearrange("(kc p) c -> p kc c", p=128)
        BIG = 1.0e4
        with contextlib.ExitStack() as st:
            xg = self.sb(st, "mm_xg", [128, KC, 512])
            rw32 = self.sb(st, "mm_rw32", [128, KC, 16])
            rbias = self.sb(st, "mm_rbias", [128, 16])
            sel = self.sb(st, "mm_sel", [16, 16, 128])
            P.dma(rw32[:], self.router_w.rearrange("(kc p) e -> p kc e", p=128), [], ['mm_rw32'])
            P.dma(rbias[:], self.router_b[0:1, :].partition_broadcast(128), [], ['mm_rbias'])
            P.op('dve', ['cst_f'], ['mm_sel'], lambda e: e.tensor_copy(
                out=sel[:], in_=self.ident32[0:16, 0:16].unsqueeze(2).to_broadcast([16, 16, 128])))
            wld = 0
            for (t0, n) in tiles:
                if not last and t0 < NCTX and not with_ctx:
                    continue
                j = 1 if (t0 < NCTX and not last) else 0
                nb = n // 128
                P.dma(xg[:, :, :n], src[:, :, t0:t0 + n], ['xT'], ['mm_xg'])
                with contextlib.ExitStack() as s2:
                    yb = self.sb(s2, "mm_y", [128, 3, 8, 512], BF16)
                    mg = self.sb(s2, "mm_mg", [128, KC, 512], BF16)
                    gb = [self.sb(s2, f"mm_g{i}", [128, 3, 512], BF16) for i in range(2)]
                    wb = [self.sb(s2, f"mm_wb{i}", [128, 3, 8, 512], BF16) for i in range(2)]
                    wo = [self.sb(s2, f"mm_wo{i}", [128, KC, 512], BF16) for i in range(2)]
                    m32 = self.sb(s2, "mm_m32", [128, 512])
                    tm = self.sb(s2, "mm_tm", [128, 512])
                    for i in range(3):
                        P.dma(yb[:, i, :, :n], yTv[:, i, :, t0:t0 + n], ['yT'], ['mm_y'])
                    pi = 0
                    for cb in range(4):
                        wk = f'mm_wb{cb % 2}'
                        for i in range(3):
                            P.dma(wb[cb % 2][:, i], wbv[:, i, :, cb * 512:(cb + 1) * 512], [], [wk], q='pool')
                        for dcl in range(4):
                            dc = cb * 4 + dcl
                            gk = f'mm_g{dc % 2}'
                            P.dma(gb[dc % 2][:, :, :n], gTv[:, :, dc, t0:t0 + n], ['gT'], [gk])
                            for i in range(3):
                                pst, pk = self.ps[pi % 4], f'ps{pi % 4}'
                                pi += 1
                                for kc in range(8):
                                    P.op('pe', [wk, 'mm_y'], [pk], lambda e, i=i, kc=kc, cb=cb, dcl=dcl, pst=pst: e.matmul(
                                        pst[:, :n], lhsT=wb[cb % 2][:, i, kc, dcl * 128:(dcl + 1) * 128], rhs=yb[:, i, kc, :n],
                                        start=(kc == 0), stop=(kc == 7)))
                                if i == 0:
                                    P.op('dve', [pk, gk], ['mm_m32'], lambda e, pst=pst, dc=dc: e.tensor_tensor(
                                        out=m32[:, :n], in0=pst[:, :n], in1=gb[dc % 2][:, 0, :n], op=ALU.mult))
                                else:
                                    P.op('dve', [pk, gk], ['mm_tm'], lambda e, pst=pst, dc=dc, i=i: e.tensor_tensor(
                                        out=tm[:, :n], in0=pst[:, :n], in1=gb[dc % 2][:, i, :n], op=ALU.mult))
                                    if i == 1:
                                        P.op('dve', ['mm_tm', 'mm_m32'], ['mm_m32'], lambda e: e.tensor_tensor(
                                            out=m32[:, :n], in0=m32[:, :n], in1=tm[:, :n], op=ALU.add))
                                    else:
                                        P.op('dve', ['mm_tm', 'mm_m32'], ['mm_mg'], lambda e, dc=dc: e.tensor_tensor(
                                            out=mg[:, dc, :n], in0=m32[:, :n], in1=tm[:, :n], op=ALU.add))
                    for cb in range(4):
                        wk = f'mm_wo{cb % 2}'
                        P.dma(wo[cb % 2][:], wov[:, :, cb * 512:(cb + 1) * 512], [], [wk], q='pool')
                        for dcl in range(4):
                            dc = cb * 4 + dcl
                            pst, pk = self.ps[pi % 4], f'ps{pi % 4}'
                            pi += 1
                            for kc in range(KC):
                                P.op('pe', [wk, 'mm_mg'], [pk], lambda e, kc=kc, cb=cb, dcl=dcl, pst=pst: e.matmul(
                                    pst[:, :n], lhsT=wo[cb % 2][:, kc, dcl * 128:(dcl + 1) * 128], rhs=mg[:, kc, :n],
                                    start=(kc == 0), stop=(kc == KC - 1)))
                            P.op('dve', [pk, 'mm_xg', f'mod{l}'], ['mm_xg'], lambda e, pst=pst, dc=dc: e.scalar_tensor_tensor(
                                out=xg[:, dc, :n], in0=pst[:, :n], scalar=self.mod[l][:, 32 + dc, j:j + 1], in1=xg[:, dc, :n],
                                op0=ALU.mult, op1=ALU.add))
                    if 'x1' in self.debug:
                        self.dbg(f'x1_{l}_{t0}', lambda o: P.dma(o.rearrange("(kc p) t -> p kc t", p=128), xg[:, :, :n], ['mm_xg'], []), [D, n])
                    P.barrier()
                with contextlib.ExitStack() as s2:
                    h2 = self.sb(s2, "mo_h2", [128, KC, 512], BF16)
                    sq = h2
                    rt = self.sb(s2, "mo_rt", [128, 512])
                    lg = self.sb(s2, "mo_lg", [16, 512])
                    R = {nm: self.sb(s2, "mo_" + nm, [128, 4, 16]) for nm in ('s', 'bz', 'eq', 'b2', 'mb', 'e1', 'w')}
                    r4 = {nm: self.sb(s2, "mo_" + nm, [128, 4, 4]) for nm in ('m1', 'm2', 'gsel')}
                    r1 = {nm: self.sb(s2, "mo_" + nm, [128, 4]) for nm in ('gmax', 't1', 't2', 'ws')}
                    cT = self.sb(s2, "mo_cT", [16, 512])
                    bce = [self.sb(s2, f"mo_bce{i}", [128, 512]) for i in range(2)]
                    sg = [self.sb(s2, f"mo_sg{i}", [128, 512]) for i in range(2)]
                    act = [self.sb(s2, f"mo_act{i}", [128, 4, 512], BF16) for i in range(2)]
                    wg = [self.sb(s2, f"mo_wg{i}", [128, KC, 512], BF16) for i in range(2)]
                    wu = [self.sb(s2, f"mo_wu{i}", [128, KC, 512], BF16) for i in range(2)]
                    s3 = contextlib.ExitStack()
                    xn = self.sb(s3, "mo_xn", [128, KC, 512])
                    P.dma(wg[0][:], self.moe_g[l, 0].rearrange("(kc p) f -> p kc f", p=128), [], ['mo_wg0'], q='pool')
                    P.dma(wu[0][:], self.moe_u[l, 0].rearrange("(kc p) f -> p kc f", p=128), [], ['mo_wu0'], q='pool')
                    P.op('act', ['mm_xg'], ['mo_h2'], lambda e: e.activation(out=sq[:, :, :n], in_=xg[:, :, :n], func=AF.Square))
                    for kc in range(KC):
                        P.op('pe', ['mo_h2', 'ones_bf'], ['ps6'], lambda e, kc=kc: e.matmul(
                            self.ps[6][:, :n], lhsT=self.ones_bf[:], rhs=sq[:, kc, :n], start=(kc == 0), stop=(kc == KC - 1)))
                    P.op('act', ['ps6'], ['mo_rt'], lambda e: e.activation(out=rt[:, :n], in_=self.ps[6][:, :n], func=AF.Sqrt,
                                                                      scale=1.0 / D, bias=self.eps_t[:, 0:1]))
                    P.op('dve', ['mo_rt'], ['mo_rt'], lambda e: e.reciprocal(out=rt[:, :n], in_=rt[:, :n]))
                    P.op('dve', ['mm_xg', 'mo_rt'], ['mo_xn'], lambda e: e.tensor_tensor(
                        out=xn[:, :, :n], in0=xg[:, :, :n], in1=rt[:, :n].unsqueeze(1).to_broadcast([128, KC, n]), op=ALU.mult))
                    for kc in range(KC):
                        P.op('act', ['mo_xn', f'gm2_{l}', f'mod{l}'], ['mo_xn'], lambda e, kc=kc: e.activation(
                            out=xn[:, kc, :n], in_=xn[:, kc, :n], func=AF.Identity,
                            scale=self.gm2[l][:, kc, j:j + 1], bias=self.mod[l][:, 48 + kc, j:j + 1]))
                    P.op('dve', ['mo_xn'], ['mo_h2'], lambda e: e.tensor_copy(out=h2[:, :, :n], in_=xn[:, :, :n]))
                    for kc in range(KC):
                        P.op('pe', ['mo_xn', 'mm_rw32'], ['ps5'], lambda e, kc=kc: e.matmul(
                            self.ps[5][0:16, :n], lhsT=rw32[:, kc, :], rhs=xn[:, kc, :n], start=(kc == 0), stop=(kc == KC - 1)))
                    P.op('act', ['ps5'], ['mo_lg'], lambda e: e.activation(out=lg[:, :n], in_=self.ps[5][0:16, :n], func=AF.Copy))
                    for b_ in range(nb):
                        P.op('pe', ['mo_lg', 'cst_f'], ['ps4'], lambda e, b_=b_: e.transpose(
                            self.ps[4][:, b_ * 16:(b_ + 1) * 16], lg[0:16, b_ * 128:(b_ + 1) * 128], self.ident32[0:16, 0:16]))
                    s_, bz, eq, b2, mb, e1, w_ = (R[k][:, :nb, :] for k in ('s', 'bz', 'eq', 'b2', 'mb', 'e1', 'w'))
                    m1, m2, gsel = (r4[k][:, :nb, :] for k in ('m1', 'm2', 'gsel'))
                    gmax, t1, t2, ws = (r1[k][:, :nb] for k in ('gmax', 't1', 't2', 'ws'))
                    g4 = lambda a: a.rearrange("p b (g k) -> p b g k", k=4)
                    V = lambda reads, writes, fn: P.op('dve', reads, writes, fn)
                    P.op('act', ['ps4'], ['mo_s'], lambda e: e.activation(
                        out=s_, in_=self.ps[4][:, 0:nb * 16].rearrange("p (b k) -> p b k", k=16), func=AF.Sigmoid))
                    V(['mo_s', 'mm_rbias'], ['mo_bz'], lambda e: e.tensor_tensor(out=bz, in0=s_, in1=rbias[:].unsqueeze(1).to_broadcast([128, nb, 16]), op=ALU.add))
                    V(['mo_bz'], ['mo_m1'], lambda e: e.tensor_reduce(out=m1, in_=g4(bz), axis=AX.X, op=ALU.max))
                    V(['mo_bz', 'mo_m1'], ['mo_eq'], lambda e: e.tensor_tensor(out=g4(eq), in0=g4(bz), in1=m1.unsqueeze(3).to_broadcast([128, nb, 4, 4]), op=ALU.is_equal))
                    V(['mo_eq', 'mo_bz'], ['mo_b2'], lambda e: e.scalar_tensor_tensor(out=b2, in0=eq, scalar=-BIG, in1=bz, op0=ALU.mult, op1=ALU.add))
                    V(['mo_b2'], ['mo_m2'], lambda e: e.tensor_reduce(out=m2, in_=g4(b2), axis=AX.X, op=ALU.max))
                    V(['mo_m1', 'mo_m2'], ['mo_m1'], lambda e: e.tensor_tensor(out=m1, in0=m1, in1=m2, op=ALU.add))
                    V(['mo_m1'], ['mo_gmax'], lambda e: e.tensor_reduce(out=gmax, in_=m1, axis=AX.X, op=ALU.max))
                    V(['mo_m1', 'mo_gmax'], ['mo_gsel'], lambda e: e.tensor_tensor(out=gsel, in0=m1, in1=gmax.unsqueeze(2).to_broadcast([128, nb, 4]), op=ALU.is_equal))
                    V(['mo_gsel'], ['mo_gsel'], lambda e: e.tensor_scalar(out=gsel, in0=gsel, scalar1=BIG, scalar2=-BIG, op0=ALU.mult, op1=ALU.add))
                    V(['mo_bz', 'mo_gsel'], ['mo_mb'], lambda e: e.tensor_tensor(out=g4(mb), in0=g4(bz), in1=gsel.unsqueeze(3).to_broadcast([128, nb, 4, 4]), op=ALU.add))
                    V(['mo_mb'], ['mo_t1'], lambda e: e.tensor_reduce(out=t1, in_=mb, axis=AX.X, op=ALU.max))
                    V(['mo_mb', 'mo_t1'], ['mo_e1'], lambda e: e.tensor_tensor(out=e1, in0=mb, in1=t1.unsqueeze(2).to_broadcast([128, nb, 16]), op=ALU.is_equal))
                    V(['mo_e1', 'mo_mb'], ['mo_b2'], lambda e: e.scalar_tensor_tensor(out=b2, in0=e1, scalar=-BIG, in1=mb, op0=ALU.mult, op1=ALU.add))
                    V(['mo_b2'], ['mo_t2'], lambda e: e.tensor_reduce(out=t2, in_=b2, axis=AX.X, op=ALU.max))
                    V(['mo_b2', 'mo_t2'], ['mo_eq'], lambda e: e.tensor_tensor(out=eq, in0=b2, in1=t2.unsqueeze(2).to_broadcast([128, nb, 16]), op=ALU.is_equal))
                    V(['mo_eq', 'mo_e1'], ['mo_e1'], lambda e: e.tensor_tensor(out=e1, in0=e1, in1=eq, op=ALU.add))
                    V(['mo_e1', 'mo_s'], ['mo_w'], lambda e: e.tensor_tensor(out=w_, in0=e1, in1=s_, op=ALU.mult))
                    V(['mo_w'], ['mo_ws'], lambda e: e.tensor_reduce(out=ws, in_=w_, axis=AX.X, op=ALU.add))
                    V(['mo_ws'], ['mo_ws'], lambda e: e.reciprocal(out=ws, in_=ws))
                    V(['mo_w', 'mo_ws'], ['mo_w'], lambda e: e.tensor_tensor(out=w_, in0=w_, in1=ws.unsqueeze(2).to_broadcast([128, nb, 16]), op=ALU.mult))
                    for b_ in range(nb):
                        P.op('pe', ['mo_w', 'cst_f'], ['ps5'], lambda e, b_=b_: e.transpose(
                            self.ps[5][0:16, b_ * 128:(b_ + 1) * 128], R['w'][:, b_, :], self.ident32))
                    P.op('act', ['ps5'], ['mo_cT'], lambda e: e.activation(out=cT[:, :n], in_=self.ps[5][0:16, :n], func=AF.Copy))
                    if 'comb' in self.debug:
                        self.dbg(f'comb_{l}_{t0}', lambda o: P.dma(o, cT[:, :n], ['mo_cT'], []), [16, n])
                    P.barrier()
                    s3.close()
                    s3 = contextlib.ExitStack()
                    wd = [self.sb(s3, f"mo_wd{i}", [128, 4, D], BF16) for i in range(2)]
                    pi = 0
                    for ex in range(16):
                        b = ex % 2
                        if ex > 0:
                            P.dma(wg[b][:], self.moe_g[l, ex].rearrange("(kc p) f -> p kc f", p=128), [], [f'mo_wg{b}'], q='pool')
                            P.dma(wu[b][:], self.moe_u[l, ex].rearrange("(kc p) f -> p kc f", p=128), [], [f'mo_wu{b}'], q='pool')
                        P.dma(wd[b][:], self.moe_d[l, ex].rearrange("(fc p) d -> p fc d", p=128), [], [f'mo_wd{b}'], q='pool')
                        P.op('pe', ['mm_sel', 'mo_cT'], ['ps6'], lambda e, ex=ex: e.matmul(
                            self.ps[6][:, :n], lhsT=sel[:, ex, :], rhs=cT[:, :n], start=True, stop=True), rg=0)
                        P.op('act', ['ps6'], [f'mo_bce{b}'], lambda e, b=b: e.activation(out=bce[b][:, :n], in_=self.ps[6][:, :n], func=AF.Copy))
                        for fc in range(4):
                            pg, pgk = self.ps[pi % 4], f'ps{pi % 4}'
                            pu, puk = self.ps[(pi + 1) % 4], f'ps{(pi + 1) % 4}'
                            pi += 2
                            for kc in range(KC):
                                P.op('pe', [f'mo_wg{b}', 'mo_h2'], [pgk], lambda e, kc=kc, fc=fc, b=b, pg=pg: e.matmul(
                                    pg[:, :n], lhsT=wg[b][:, kc, fc * 128:(fc + 1) * 128], rhs=h2[:, kc, :n], start=(kc == 0), stop=(kc == KC - 1)))
                            for kc in range(KC):
                                P.op('pe', [f'mo_wu{b}', 'mo_h2'], [puk], lambda e, kc=kc, fc=fc, b=b, pu=pu: e.matmul(
                                    pu[:, :n], lhsT=wu[b][:, kc, fc * 128:(fc + 1) * 128], rhs=h2[:, kc, :n], start=(kc == 0), stop=(kc == KC - 1)))
                            sb_ = fc % 2
                            P.op('act', [pgk], [f'mo_sg{sb_}'], lambda e, pg=pg, sb_=sb_: e.activation(out=sg[sb_][:, :n], in_=pg[:, :n], func=AF.Silu))
                            P.op('dve', [puk, f'mo_sg{sb_}'], [f'mo_sg{sb_}'], lambda e, pu=pu, sb_=sb_: e.tensor_tensor(
                                out=sg[sb_][:, :n], in0=pu[:, :n], in1=sg[sb_][:, :n], op=ALU.mult))
                            P.op('dve', [f'mo_sg{sb_}', f'mo_bce{b}'], [f'mo_act{b}'], lambda e, sb_=sb_, b=b, fc=fc: e.tensor_tensor(
                                out=act[b][:, fc, :n], in0=sg[sb_][:, :n], in1=bce[b][:, :n], op=ALU.mult))
                        for dc in range(KC):
                            pd, pdk = self.ps[pi % 4], f'ps{pi % 4}'
                            pi += 1
                            for fc in range(4):
                                P.op('pe', [f'mo_wd{b}', f'mo_act{b}'], [pdk], lambda e, fc=fc, dc=dc, b=b, pd=pd: e.matmul(
                                    pd[:, :n], lhsT=wd[b][:, fc, dc * 128:(dc + 1) * 128], rhs=act[b][:, fc, :n], start=(fc == 0), stop=(fc == 3)))
                            P.op('dve', [pdk, 'mm_xg', f'mod{l}'], ['mm_xg'], lambda e, pd=pd, dc=dc: e.scalar_tensor_tensor(
                                out=xg[:, dc, :n], in0=pd[:, :n], scalar=self.mod[l][:, 80 + dc, j:j + 1], in1=xg[:, dc, :n],
                                op0=ALU.mult, op1=ALU.add))
                    if last:
                        P.dma(dsto[:, :, t0:t0 + n], xg[:, :, :n], ['mm_xg'], ['outT'])
                    else:
                        P.dma(dstx[:, :, t0:t0 + n], xg[:, :, :n], ['mm_xg'], ['xT'])
                    if 'x2' in self.debug:
                        self.dbg(f'x2_{l}_{t0}', lambda o: P.dma(o.rearrange("(kc p) t -> p kc t", p=128), xg[:, :, :n], ['mm_xg'], []), [D, n])
                    P.barrier()
                    s3.close()

    def final_out(self):
        P = self.P
        P.dma(self.outT, self.xT[:, NCTX:], ['xT'], [])


def na_tables(rpb):
    c = np.arange(64)
    cs = np.clip(c - 8, 0, 48)
    kc = np.arange(64)
    inwin = (kc[:, None] >= cs[None, :]) & (kc[:, None] < cs[None, :] + 16)
    dc = np.clip(kc[:, None] - c[None, :] + 15, 0, 30)
    out = np.full((16, 2, 64, 14, 64), NEG, np.float32)
    for jj in range(2):
        for dr in range(14):
            g = rpb[:, dr + jj][:, dc]
            out[:, jj, :, dr, :] = np.where(inwin[None], g, np.float32(NEG))
    return out.reshape(16, 128, 14 * 64)


NCONST = 512 + 2 * SEQ + 384


def make_consts():
    c = np.zeros((128, NCONST), np.float32)
    p = np.arange(128)
    c[p, p] = 1.0
    c[p, 128 + (p ^ 32)] = 1.0
    j = p[:, None]; i = p[None, :]
    same = (j // 64) == (i // 64)
    c[:, 256:384] = (same & (j <= i)).astype(np.float32)
    c[:, 384:512] = (same & (j >= i)).astype(np.float32)
    t = np.arange(SEQ)
    pos = np.where(p[:, None] < 64, (t // 64)[None, :], (t % 64)[None, :]).astype(np.float32)
    inv = (10000.0 ** (-np.arange(0, 64, 2, dtype=np.float32) / 64)).astype(np.float32)
    ang = pos * inv[(p % 32)][:, None]
    c[:, 512:512 + SEQ] = np.cos(ang)
    sgn = np.where((p % 64) < 32, -1.0, 1.0).astype(np.float32)
    c[:, 512 + SEQ:512 + 2 * SEQ] = np.sin(ang) * sgn[:, None]
    o = 512 + 2 * SEQ
    c[:, o:o + 128] = (same & (j < i)).astype(np.float32)
    c[:, o + 128:o + 256] = (same & (j > i)).astype(np.float32)
    c[:, o + 256:o + 384] = same.astype(np.float32)
    return c


def host_inputs(inp, b, half=0):
    xT0 = np.ascontiguousarray(np.concatenate([inp['ctx'][b], inp['x'][b]], axis=0).T)
    cT = np.stack([fm(inp['c'][b]), fm(inp['c_ctx'])], axis=-1).reshape(128, 32)
    return {
        'xT0': xT0, 'cT': np.ascontiguousarray(cT),
        'ada_w': inp['ada_w'], 'w_in': inp['w_in'],
        'vecs': np.stack([pack_vecs(inp, l) for l in range(DEPTH)]),
        'natab': np.stack([na_tables(inp['na_rpb'][l]) for l in range(DEPTH)]),
        'consts': make_consts(), 'gla_gate_w2': inp['gla_gate_w2'],
        'rw_w2': inp['rw_w2'], 'rw_a2': inp['rw_a2'], 'rw_g2': inp['rw_g2'],
        'w_branch': inp['w_branch'], 'w_out': inp['w_out'], 'router_w': inp['router_w'],
        'router_bias': inp['router_bias'].reshape(1, 16),
        'moe_w_gate': inp['moe_w_gate'], 'moe_w_up': inp['moe_w_up'], 'moe_w_down': inp['moe_w_down'],
        'selw': np.tile(np.asarray([[1.0, 0.0]] if half == 0 else [[0.0, 1.0]], np.float32), (128, 1)),
    }


def kernel(**inputs):
    inp = {k: np.asarray(v) for k, v in inputs.items()}
    bld = Builder()
    nc = bld.build()
    in_maps = [host_inputs(inp, c % 4, c // 4) for c in range(8)]
    res = run_bass_kernel_spmd(nc, in_maps, core_ids=list(range(8)))
    out = np.stack([np.concatenate([res.results[b]["outT"].T, res.results[b + 4]["outT"].T], axis=0) for b in range(4)], axis=0)
    return np.ascontiguousarray(out).astype(np.float32)
```

```python
import contextlib
import os
import numpy as np
import concourse.bass as bass
import concourse.mybir as mybir
from concourse.bass_utils import run_bass_kernel_spmd

F32 = mybir.dt.float32
BF16 = mybir.dt.bfloat16
AF = mybir.ActivationFunctionType
ALU = mybir.AluOpType
AX = mybir.AxisListType

D = 2048
KC = 16
NCTX = 256
SEQ = 2048
TT = NCTX + SEQ
D_IN = 16032
EPS = 1e-6
NEG = -30000.0
DEPTH = 2

C_NAQ, C_NAK, C_NAV = 0, 1024, 2048
C_GQ, C_GK, C_GV, C_GR, C_GGD = 3072, 3584, 4096, 5120, 6144
C_RW = 6176
C_RR, C_RK, C_RV, C_RWD, C_RAD, C_RGD = C_RW, C_RW + 1024, C_RW + 2048, C_RW + 3072, C_RW + 3264, C_RW + 3456
C_GATE = 9888

TILES = [(0, 256), (256, 512), (768, 512), (1280, 512), (1792, 512)]

NDS = 12


class PEProxy:
    def __init__(self, real):
        self._real = real
        self._last = None
        self._dummy = None

    def _sep(self, out, w):
        K = w.shape[0]
        M = 1
        for d_ in w.shape[1:]:
            M *= d_
        t = None if K == 128 else (w.base_partition(), K)
        if t is not None and self._last is not None and t != self._last and self._dummy is not None:
            self._dummy(self._real)
        self._last = t

    def matmul(self, out, lhsT=None, rhs=None, **kw):
        self._sep(out, lhsT)
        return self._real.matmul(out, lhsT=lhsT, rhs=rhs, **kw)

    def transpose(self, out, in_, identity, **kw):
        self._sep(out, in_)
        return self._real.transpose(out, in_, identity, **kw)

    def __getattr__(self, name):
        return getattr(self._real, name)


class Prog:
    def __init__(self, nc, es):
        self.nc = nc
        self.e = dict(pe=PEProxy(nc.tensor), act=nc.scalar, dve=nc.vector, pool=nc.gpsimd, sp=nc.sync)
        self.sem = {k: es.enter_context(nc.semaphore("s_" + k)) for k in self.e}
        self.cnt = {k: 0 for k in self.e}
        self.seen = {k: {} for k in self.e}
        self.lw = {}
        self.rd = {}
        self.dsem = [es.enter_context(nc.semaphore(f"dq{i}")) for i in range(NDS)]
        self.dcnt = [0] * NDS
        self.dnext = 0

    def semh(self, k):
        return self.dsem[k[1]] if isinstance(k, tuple) else self.sem[k]

    def _wait(self, eng, k, v):
        if self.seen[eng].get(k, 0) < v:
            self.e[eng].wait_ge(self.semh(k), v)
            self.seen[eng][k] = v

    def set_parent(self, child, parent):
        self.parent = getattr(self, 'parent', {})
        self.children = getattr(self, 'children', {})
        self.parent[child] = parent
        self.children.setdefault(parent, []).append(child)

    def _expand(self, bs):
        par = getattr(self, 'parent', {})
        chl = getattr(self, 'children', {})
        out = []
        for b in bs:
            out.append(b)
            if b in par:
                out.append(par[b])
            out.extend(chl.get(b, ()))
        return out

    def _deps(self, eng, reads, writes):
        reads = self._expand(reads)
        writes = self._expand(writes)
        deps = {}
        for b in reads:
            lw = self.lw.get(b)
            if lw:
                deps[lw[0]] = max(deps.get(lw[0], 0), lw[1])
        for b in writes:
            lw = self.lw.get(b)
            if lw:
                deps[lw[0]] = max(deps.get(lw[0], 0), lw[1])
            for k, v in self.rd.get(b, {}).items():
                deps[k] = max(deps.get(k, 0), v)
        for k, v in deps.items():
            if k == 'pe' and eng == 'pe':
                continue
            self._wait(eng, k, v)

    def _mark(self, pt, reads, writes):
        for b in writes:
            self.lw[b] = pt
            self.rd[b] = {}
        for b in reads:
            d = self.rd.setdefault(b, {})
            d[pt[0]] = max(d.get(pt[0], 0), pt[1])

    halt = False

    pe_rg = None
    dummy = None

    def op(self, eng, reads, writes, fn, rg=None):
        if self.halt:
            return None
        if eng == 'pe':
            pass
        self._deps(eng, reads, writes)
        ins = fn(self.e[eng])
        self.cnt[eng] += 1
        ins.then_inc(self.sem[eng], 1)
        self._mark((eng, self.cnt[eng]), reads, writes)
        return ins

    def dma(self, out, in_, reads, writes, q='sp', **kw):
        if self.halt:
            return None
        i = self.dnext % NDS
        self.dnext += 1
        k = ('d', i)
        if self.dcnt[i]:
            self._wait(q, k, self.dcnt[i])
        self._deps(q, reads, writes)
        ins = self.e[q].dma_start(out=out, in_=in_, **kw)
        self.dcnt[i] += 16
        ins.then_inc(self.dsem[i], 16)
        self._mark((k, self.dcnt[i]), reads, writes)
        return ins

    def barrier(self):
        for q in self.e:
            for i in range(NDS):
                if self.dcnt[i]:
                    self._wait(q, ('d', i), self.dcnt[i])
            for k in self.e:
                if k != q and self.cnt[k]:
                    self._wait(q, k, self.cnt[k])
        self.lw = {}
        self.rd = {}

    def finish(self, q='sp'):
        for i in range(NDS):
            if self.dcnt[i]:
                self.e[q].wait_ge(self.dsem[i], self.dcnt[i])
        for k in self.e:
            if k != q and self.cnt[k]:
                self.e[q].wait_ge(self.sem[k], self.cnt[k])


def fm(v):
    v = np.asarray(v, np.float32)
    return np.ascontiguousarray(v.reshape(-1, 128).T)


VEC_SLOTS = {}


def _vec_layout():
    off = 0
    def add(name, n):
        nonlocal off
        VEC_SLOTS[name] = (off, n)
        off += n
    add('n1g', 16); add('n2g', 16); add('adab', 96)
    add('naq', 1); add('nak', 1)
    add('ggb', 8)
    add('gng', 2)
    add('mu_r', 8); add('mu_k', 8); add('mu_v', 8); add('mu_wd', 2); add('mu_ad', 2); add('mu_gd', 2)
    add('w0', 16); add('a0', 16); add('kk', 8); add('ka', 8); add('rk', 8); add('gng_rw', 8); add('gnb_rw', 8)
    return off


NV = _vec_layout()


def pack_vecs(inp, l):
    v = np.zeros((128, NV), np.float32)
    def put(name, arr):
        o, n = VEC_SLOTS[name]
        assert arr.shape == (128, n), (name, arr.shape)
        v[:, o:o + n] = arr
    put('n1g', fm(inp['norm1_g'][l])); put('n2g', fm(inp['norm2_g'][l])); put('adab', fm(inp['ada_b'][l]))
    put('naq', np.tile(inp['na_q_norm'][l], 2)[:, None]); put('nak', np.tile(inp['na_k_norm'][l], 2)[:, None])
    put('ggb', fm(inp['gla_gate_b'][l].reshape(-1)))
    put('gng', fm(inp['gla_norm_g'][l]))
    mu = inp['rw_mu'][l]
    put('mu_r', fm(mu[0:1024])); put('mu_k', fm(mu[1024:2048])); put('mu_v', fm(mu[2048:3072]))
    def pad96(a):
        o = np.zeros((128, 2), np.float32); o[:96, 0] = a[:96]; o[:96, 1] = a[96:192]; return o
    put('mu_wd', pad96(mu[3072:3264])); put('mu_ad', pad96(mu[3264:3456])); put('mu_gd', fm(mu[3456:3712]))
    put('w0', fm(inp['rw_w0'][l].reshape(-1))); put('a0', fm(inp['rw_a0'][l].reshape(-1)))
    put('kk', fm(inp['rw_k_k'][l])); put('ka', fm(inp['rw_k_a'][l])); put('rk', fm(inp['rw_r_k'][l].reshape(-1)))
    put('gng_rw', fm(inp['rw_gn_g'][l])); put('gnb_rw', fm(inp['rw_gn_b'][l]))
    return v


class _Stop(Exception):
    pass


class Builder:
    rw_stop = 0
    rw_slots = 2

    def sc_in(self, name, on=True):
        self.sc_out()
        if on:
            self._sc = (name, self.nc.enter_named_scope(name, False)[0])

    def sc_out(self):
        c = getattr(self, '_sc', None)
        if c:
            self.nc.leave_named_scope(c[0], c[1], False)
        self._sc = None

    def stopat(self, k):
        if self.rw_stop == k:
            self.P.halt = True

    def __init__(self, layers=(0, 1), upto='all', debug=()):
        self.layers = layers
        self.upto = upto
        self.debug = set(debug)
        self.nc = bass.Bass("TRN2", target_bir_lowering=False)
        self.es = contextlib.ExitStack()
        self.dbg_outs = {}

    def din(self, name, shape, dt=F32):
        return self.nc.dram_tensor(name, list(shape), dt, kind="ExternalInput").ap()

    def dout(self, name, shape, dt=F32):
        return self.nc.dram_tensor(name, list(shape), dt, kind="ExternalOutput").ap()

    def dscr(self, name, shape, dt=F32):
        return self.nc.dram_tensor(name, list(shape), dt, kind="Internal").ap()

    def sb(self, st, name, shape, dt=F32):
        self._uid = getattr(self, '_uid', 0) + 1
        return st.enter_context(self.nc.sbuf_tensor(f"{name}_u{self._uid}", list(shape), dt))

    def vec(self, l, name):
        o, n = VEC_SLOTS[name]
        return self.vecs[l][:, o:o + n]

    def build(self):
        nc = self.nc
        es = self.es
        with es:
            self.P = P = Prog(nc, es)
            self.xT0 = self.din("xT0", [D, TT])
            self.cT = self.din("cT", [128, 32])
            self.ada_w = self.din("ada_w", [DEPTH, D, 6 * D])
            self.w_in = self.din("w_in", [DEPTH, D, D_IN])
            self.vecs_d = self.din("vecs", [DEPTH, 128, NV])
            self.outT = self.dout("outT", [D, SEQ // 2])
            self.selw = self.din("selw", [128, 2])
            self.xsel = self.dscr("xsel_s", [D, SEQ // 2])
            self.ysel = self.dscr("ysel_s", [3, 1024, SEQ // 2], BF16)
            self.gsel = self.dscr("gsel_s", [3 * D, SEQ // 2], BF16)
            self.xT = self.dscr("xT_s", [D, TT])
            self.zT = self.dscr("zT_s", [C_GATE, TT])
            self.gT = self.dscr("gT_s", [3 * D, TT], BF16)
            self.vtm = self.dscr("vtm_s", [TT, 2048], BF16)
            self.yT = self.dscr("yT_s", [3, 1024, TT], BF16)
            self.natab = self.din("natab", [DEPTH, 16, 128, 14 * 64])
            self.consts = self.din("consts", [128, NCONST])
            self.gla_w2 = self.din("gla_gate_w2", [DEPTH, 2, 16, 512])
            self.rw_w2 = self.din("rw_w2", [DEPTH, 2, 96, 1024])
            self.rw_a2 = self.din("rw_a2", [DEPTH, 2, 96, 1024])
            self.rw_g2 = self.din("rw_g2", [DEPTH, 256, 1024])
            self.w_branch = self.din("w_branch", [DEPTH, 3, 1024, D])
            self.w_out = self.din("w_out", [DEPTH, D, D])
            self.router_w = self.din("router_w", [D, 16])
            self.router_b = self.din("router_bias", [1, 16])
            self.moe_g = self.din("moe_w_gate", [DEPTH, 16, D, 512])
            self.moe_u = self.din("moe_w_up", [DEPTH, 16, D, 512])
            self.moe_d = self.din("moe_w_down", [DEPTH, 16, 512, D])
            self.ldT = self.dscr("ldT_s", [2, 1024, TT])
            self.aT = self.dscr("aT_s", [2, 1024, TT])
            self.g2T = self.dscr("g2T_s", [1024, TT], BF16)
            self.ones_bf = self.sb(es, "ones_bf", [128, 128], BF16)
            self.vecs = [self.sb(es, f"vecs{l}", [128, NV]) for l in range(DEPTH)]
            self.mod = [self.sb(es, f"mod{l}", [128, 96, 2]) for l in range(DEPTH)]
            self.gm1 = [self.sb(es, f"gm1_{l}", [128, 16, 2]) for l in range(DEPTH)]
            self.gm2 = [self.sb(es, f"gm2_{l}", [128, 16, 2]) for l in range(DEPTH)]
            self.ps = [es.enter_context(nc.psum_tensor(f"ps{i}", [128, 512], F32)) for i in range(7)]
            self.psb = es.enter_context(nc.psum_tensor("psb", [128, 1024], BF16))
            psd = self.psb[:, 512:1024].bitcast(F32)
            P.e['pe']._dummy = lambda e: e.matmul(psd[:, 0:1], lhsT=self.ones_bf[:], rhs=self.ones_bf[:, 0:1], start=True, stop=True)
            cst = self.sb(es, "cst_f", [128, 4 * 128])
            self.cst_bf = self.sb(es, "cst_bf", [128, 4 * 128], BF16)
            P.dma(cst[:], self.consts[:, 0:512], [], ['cst_f'])
            P.op('dve', ['cst_f'], ['cst_bf'], lambda e: e.tensor_copy(out=self.cst_bf[:], in_=cst[:]))
            self.ident_bf = self.cst_bf[:, 0:128]
            self.perm_bf = self.cst_bf[:, 128:256]
            self.mask_f = cst[:, 256:384]
            self.mask_b = cst[:, 384:512]
            self.ident32 = cst[:, 0:128]
            cst2 = self.sb(es, "cst2", [128, 384])
            P.dma(cst2[:], self.consts[:, 512 + 2 * SEQ:512 + 2 * SEQ + 384], [], ['cst_f'])
            self.mask_fs = cst2[:, 0:128]
            self.mask_bs = cst2[:, 128:256]
            self.mask_bd = cst2[:, 256:384]
            P.op('pool', [], ['ones_bf'], lambda e: e.memset(self.ones_bf[:], 1.0))
            self.bd_bf = self.sb(es, "bd_bf", [128, 128], BF16)
            P.op('pool', [], ['bd_bf'], lambda e: e.memset(self.bd_bf[:], 0.0))
            P.op('pool', ['bd_bf'], ['bd_bf'], lambda e: e.memset(self.bd_bf[0:64, 0:64], 1.0))
            P.op('pool', ['bd_bf'], ['bd_bf'], lambda e: e.memset(self.bd_bf[64:128, 64:128], 1.0))
            self.eps_t = self.sb(es, "eps_t", [128, 1])
            self.selw_t = self.sb(es, "selw_t", [128, 2])
            P.dma(self.selw_t[:], self.selw, [], ['selw_t'])
            P.op('pool', [], ['eps_t'], lambda e: e.memset(self.eps_t[:], EPS))
            for l in range(DEPTH):
                P.dma(self.vecs[l][:], self.vecs_d[l], [], [f'vecs{l}'])

            with nc.named_scope('prologue'):
                self.prologue()
            for l in self.layers:
                self.layer(l)
                if self.upto != 'all':
                    break
            P.finish()
        return nc

    def dbg(self, name, src_ap_fn, shape, dt=F32, reads=()):
        o = self.dout("dbg_" + name, shape, dt)
        self.dbg_outs[name] = o
        src_ap_fn(o)

    def prologue(self):
        nc, P = self.nc, self.P
        with contextlib.ExitStack() as st:
            sc = self.sb(st, "sc", [128, 32])
            sc2 = self.sb(st, "sc2", [128, 32])
            awb = [self.sb(st, f"awb{i}", [128, 16, 512], BF16) for i in range(3)]
            sc2b = self.sb(st, "sc2b", [128, 32], BF16)
            P.dma(sc[:], self.cT, [], ['sc'])
            P.op('act', ['sc'], ['sc2'], lambda e: e.activation(out=sc2[:], in_=sc[:], func=AF.Silu))
            P.op('dve', ['sc2'], ['sc2'], lambda e: e.tensor_copy(out=sc2b[:], in_=sc2[:]))
            for l in range(DEPTH):
                aw = self.ada_w[l].rearrange("(kc p) c -> p kc c", p=128)
                adab = self.vec(l, 'adab')
                for g in range(24):
                    wt = awb[g % 3]
                    wk = f'awb{g % 3}'
                    P.dma(wt[:], aw[:, :, g * 512:(g + 1) * 512], [], [wk], q='pool')
                    pst = self.ps[g % 2]
                    pk = f'ps{g % 2}'
                    for f in range(4):
                        for kc in range(KC):
                            P.op('pe', [wk, 'sc2'], [pk], lambda e, f=f, kc=kc: e.matmul(
                                pst[:, f * 2:(f + 1) * 2], lhsT=wt[:, kc, f * 128:(f + 1) * 128],
                                rhs=sc2b[:, kc * 2:(kc + 1) * 2], start=(kc == 0), stop=(kc == KC - 1)))
                    P.op('dve', [pk, f'vecs{l}'], [f'mod{l}'], lambda e, g=g: e.tensor_tensor(
                        out=self.mod[l][:, g * 4:(g + 1) * 4, :],
                        in0=pst[:, 0:8].rearrange("p (f j) -> p f j", j=2),
                        in1=adab[:, g * 4:(g + 1) * 4].unsqueeze(2).to_broadcast([128, 4, 2]), op=ALU.add))
                for (gm, nm, sco, key) in ((self.gm1[l], 'n1g', 16, f'gm1_{l}'), (self.gm2[l], 'n2g', 64, f'gm2_{l}')):
                    P.op('dve', [f'mod{l}'], [key], lambda e, gm=gm, sco=sco: e.tensor_scalar(
                        out=gm[:], in0=self.mod[l][:, sco:sco + 16, :], scalar1=1.0, scalar2=None, op0=ALU.add))
                    P.op('dve', [key, f'vecs{l}'], [key], lambda e, gm=gm, nm=nm: e.tensor_tensor(
                        out=gm[:], in0=gm[:], in1=self.vec(l, nm).unsqueeze(2).to_broadcast([128, 16, 2]), op=ALU.mult))
            if 'mod' in self.debug:
                for l in range(DEPTH):
                    self.dbg(f'mod{l}', lambda o, l=l: P.dma(o, self.mod[l][:].rearrange("p a b -> p (a b)"), [f'mod{l}'], []), [128, 192])
            P.barrier()

    def norm_tile(self, st_bufs, x, xk, n, j, gm, gmk, shmod, shoff, modk, out_fn, outk, ps_i=6):
        P = self.P
        sq, rt = st_bufs
        pst = self.ps[ps_i]
        pk = f'ps{ps_i}'
        P.op('act', [xk], ['nsq'], lambda e: e.activation(out=sq[:, :, :n], in_=x, func=AF.Square))
        for kc in range(KC):
            P.op('pe', ['nsq', 'ones_bf'], [pk], lambda e, kc=kc: e.matmul(
                pst[:, :n], lhsT=self.ones_bf[:], rhs=sq[:, kc, :n], start=(kc == 0), stop=(kc == KC - 1)))
        P.op('act', [pk], ['nrt'], lambda e: e.activation(out=rt[:, :n], in_=pst[:, :n], func=AF.Sqrt,
                                                         scale=1.0 / D, bias=self.eps_t[:, 0:1]))
        P.op('dve', ['nrt'], ['nrt'], lambda e: e.reciprocal(out=rt[:, :n], in_=rt[:, :n]))
        P.op('dve', [xk, 'nrt'], [xk], lambda e: e.tensor_tensor(
            out=x, in0=x, in1=rt[:, :n].unsqueeze(1).to_broadcast([128, KC, n]), op=ALU.mult))
        for kc in range(KC):
            P.op('act', [xk, gmk, modk], [outk], lambda e, kc=kc: e.activation(
                out=out_fn(kc), in_=x[:, kc, :], func=AF.Identity,
                scale=gm[:, kc, j:j + 1], bias=shmod[:, shoff + kc, j:j + 1]))

    def layer(self, l):
        nc, P = self.nc, self.P
        src = self.xT0 if l == 0 else self.xT
        with contextlib.ExitStack() as st:
            hT = self.sb(st, "hT", [128, KC, TT], BF16)
            with contextlib.ExitStack() as st2:
                xb = [self.sb(st2, f"xb{i}", [128, KC, 512]) for i in range(2)]
                sq = self.sb(st2, "nsq", [128, KC, 512], BF16)
                rt = self.sb(st2, "nrt", [128, 512])
                for ti, (t0, n) in enumerate(TILES):
                    x = xb[ti % 2]
                    xk = f'xb{ti % 2}'
                    j = 1 if t0 < NCTX else 0
                    P.dma(x[:, :, :n], src.rearrange("(kc p) t -> p kc t", p=128)[:, :, t0:t0 + n], ['xT'], [xk])
                    self.norm_tile((sq, rt), x[:, :, :n], xk, n, j, self.gm1[l], f'gm1_{l}', self.mod[l], 0, f'mod{l}',
                                   lambda kc, t0=t0, n=n: hT[:, kc, t0:t0 + n], 'hT')
                if 'hT' in self.debug:
                    self.dbg(f'hT{l}', lambda o: P.dma(o.rearrange("(kc p) t -> p kc t", p=128), hT[:], ['hT'], []), [D, TT], BF16)
                P.barrier()
            if self.upto == 'norm':
                return
            with nc.named_scope(f'L{l}_inproj'):
                self.inproj(l, hT)
            P.barrier()
        if self.upto == 'inproj':
            return
        with nc.named_scope(f'L{l}_na'):
            self.na_mixer(l)
        P.barrier()
        if self.upto == 'na':
            return
        with nc.named_scope(f'L{l}_gla'):
            self.gla_mixer(l)
        P.barrier()
        if self.upto == 'gla':
            return
        with nc.named_scope(f'L{l}_rwpre'):
            self.rwkv_pre(l)
        P.barrier()
        if self.upto == 'rwpre':
            self.dbg(f'ld{l}', lambda o: P.dma(o, self.ldT, ['ldT'], []), [2, 1024, TT])
            self.dbg(f'a{l}', lambda o: P.dma(o, self.aT, ['aT'], []), [2, 1024, TT])
            self.dbg(f'g2{l}', lambda o: P.dma(o, self.g2T, ['g2T'], []), [1024, TT], BF16)
            return
        with nc.named_scope(f'L{l}_rwkv'):
            self.rwkv_mixer(l)
        P.halt = False
        P.barrier()
        if self.upto == 'rwkv':
            return
        with nc.named_scope(f'L{l}_mergemoe'):
            self.merge_moe(l)
        P.barrier()

    def inproj(self, l, hT):
        nc, P = self.nc, self.P
        w = self.w_in[l].rearrange("(kc p) c -> p kc c", p=128)
        with contextlib.ExitStack() as st:
            wsl = [self.sb(st, f"wsl{i}", [128, KC, 512], BF16) for i in range(3)]
            zst = [self.sb(st, f"zst{i}", [128, 512]) for i in range(4)]
            gst = [self.sb(st, f"gst{i}", [128, 512], BF16) for i in range(4)]
            last = l == DEPTH - 1
            if last:
                H_ = SEQ // 2
                hsel = self.sb(st, "hsel", [128, KC, H_], BF16)
                P.op('dve', ['hT', 'selw_t'], ['hsel'], lambda e: e.tensor_scalar(
                    out=hsel[:], in0=hT[:, :, NCTX:NCTX + H_], scalar1=self.selw_t[:, 0:1], scalar2=None, op0=ALU.mult))
                P.op('dve', ['hT', 'hsel', 'selw_t'], ['hsel'], lambda e: e.scalar_tensor_tensor(
                    out=hsel[:], in0=hT[:, :, NCTX + H_:NCTX + 2 * H_], scalar=self.selw_t[:, 1:2], in1=hsel[:], op0=ALU.mult, op1=ALU.add))
            segs = [(0, 2048), (C_GQ, C_GV), (C_GR, C_GGD), (C_GGD, C_GGD + 16), (C_GGD + 16, C_RW),
                    (C_RR, C_RWD), (C_RWD, C_RWD + 96), (C_RWD + 96, C_RAD), (C_RAD, C_RAD + 96), (C_RAD + 96, C_RGD),
                    (C_RGD, C_GATE), (C_GATE, D_IN)]
            nload = 0
            nev = 0
            npsum = 0
            for (s0, s1) in segs:
                for b0 in range(s0, s1, 512):
                    bw = min(512, s1 - b0)
                    si = nload % 3
                    nload += 1
                    wk = f'wsl{si}'
                    P.dma(wsl[si][:, :, :bw], w[:, :, b0:b0 + bw], [], [wk], q='pool')
                    for c0 in range(b0, b0 + bw, 128):
                        m = min(128, b0 + bw - c0)
                        gsel_blk = last and c0 >= C_GATE
                        hsrc, hkey = (hsel, 'hsel') if gsel_blk else (hT, 'hT')
                        for (t0, n) in ([(0, 512), (512, 512)] if gsel_blk else TILES):
                            pi = npsum % 4
                            npsum += 1
                            pst = self.ps[pi]
                            pk = f'ps{pi}'
                            for kc in range(KC):
                                P.op('pe', [wk, hkey], [pk], lambda e, kc=kc, c0=c0, m=m, t0=t0, n=n, si=si, pst=pst, hsrc=hsrc: e.matmul(
                                    pst[:m, :n], lhsT=wsl[si][:, kc, c0 - b0:c0 - b0 + m], rhs=hsrc[:, kc, t0:t0 + n],
                                    start=(kc == 0), stop=(kc == KC - 1)))
                            ei = nev % 4
                            eng = 'act' if nev % 2 == 0 else 'dve'
                            nev += 1
                            if c0 >= C_GATE:
                                P.op('act', [pk], [f'gst{ei}'], lambda e, m=m, n=n, ei=ei, pst=pst: e.activation(
                                    out=gst[ei][:m, :n], in_=pst[:m, :n], func=AF.Sigmoid))
                                gdst = self.gsel if gsel_blk else self.gT
                                P.dma(gdst[c0 - C_GATE:c0 - C_GATE + m, t0:t0 + n], gst[ei][:m, :n], [f'gst{ei}'], ['gT'])
                            else:
                                if eng == 'act':
                                    P.op('act', [pk], [f'zst{ei}'], lambda e, m=m, n=n, ei=ei, pst=pst: e.activation(
                                        out=zst[ei][:m, :n], in_=pst[:m, :n], func=AF.Copy))
                                else:
                                    P.op('dve', [pk], [f'zst{ei}'], lambda e, m=m, n=n, ei=ei, pst=pst: e.tensor_copy(
                                        out=zst[ei][:m, :n], in_=pst[:m, :n]))
                                P.dma(self.zT[c0:c0 + m, t0:t0 + n], zst[ei][:m, :n], [f'zst{ei}'], ['zT'])
            for (s0, vo) in ((C_NAV, 0), (C_GV, 1024)):
                for b0 in range(0, 1024, 512):
                    si = nload % 3
                    nload += 1
                    wk = f'wsl{si}'
                    P.dma(wsl[si][:, :, :], w[:, :, s0 + b0:s0 + b0 + 512], [], [wk], q='pool')
                    for tt in range(TT // 128):
                        pi = npsum % 4
                        npsum += 1
                        pst = self.ps[pi]
                        pk = f'ps{pi}'
                        for kc in range(KC):
                            P.op('pe', [wk, 'hT'], [pk], lambda e, kc=kc, tt=tt, si=si, pst=pst: e.matmul(
                                pst[:, :], lhsT=hT[:, kc, tt * 128:(tt + 1) * 128], rhs=wsl[si][:, kc, :],
                                start=(kc == 0), stop=(kc == KC - 1)))
                        ei = nev % 4
                        eng = 'act' if nev % 2 == 0 else 'dve'
                        nev += 1
                        if eng == 'act':
                            P.op('act', [pk], [f'gst{ei}'], lambda e, ei=ei, pst=pst: e.activation(
                                out=gst[ei][:, :], in_=pst[:, :], func=AF.Copy))
                        else:
                            P.op('dve', [pk], [f'gst{ei}'], lambda e, ei=ei, pst=pst: e.tensor_copy(
                                out=gst[ei][:, :], in_=pst[:, :]))
                        P.dma(self.vtm[tt * 128:(tt + 1) * 128, vo + b0:vo + b0 + 512], gst[ei][:, :], [f'gst{ei}'], ['vtm'])
            if 'z' in self.debug:
                self.dbg(f'z{l}', lambda o: P.dma(o, self.zT, ['zT'], []), [C_GATE, TT])
                self.dbg(f'g{l}', lambda o: P.dma(o, self.gT, ['gT'], []), [3 * D, TT], BF16)
                self.dbg(f'vtm{l}', lambda o: P.dma(o, self.vtm, ['vtm'], []), [TT, 2048], BF16)


    def na_mixer(self, l):
        nc, P = self.nc, self.P
        with_ctx = l < DEPTH - 1
        vt_all = self.vtm.rearrange("(tt p) c -> p tt c", p=128)
        vt_odd = self.vtm[64:64 + 17 * 128, :].rearrange("(tt p) c -> p tt c", p=128)
        with contextlib.ExitStack() as st:
            zq = [self.sb(st, f"naz{i}", [128, TT]) for i in range(2)]
            sqb = self.sb(st, "nasq", [128, 512], BF16)
            rtb = self.sb(st, "nart", [128, 512])
            qk = [self.sb(st, "qn", [128, TT], BF16), self.sb(st, "kn", [128, TT], BF16)]
            vte = self.sb(st, "vte", [128, 18, 128], BF16)
            vto = self.sb(st, "vto", [128, 17, 128], BF16)
            tbl = [self.sb(st, f"natbl{i}", [128, 14, 64]) for i in range(2)]
            sT = [self.sb(st, f"sT{i}", [128, 4, 64]) for i in range(2)]
            pT = [self.sb(st, f"pT{i}", [128, 6, 64], BF16) for i in range(2)]
            pTc = self.sb(st, "pTc", [128, 2, 256], BF16)
            rsb = [self.sb(st, f"nars{i}", [128, 256]) for i in range(2)]
            yna = [self.sb(st, f"yna{i}", [128, TT], BF16) for i in range(2)]
            g8 = self.sb(st, "g8", [128, 2])
            qm = [self.sb(st, f"qm{i}", [128, TT], BF16) for i in range(2)]
            P.op('dve', [f'vecs{l}'], ['g8'], lambda e: e.tensor_scalar(
                out=g8[:, 0:1], in0=self.vec(l, 'naq'), scalar1=0.125, scalar2=None, op0=ALU.mult))
            P.op('dve', [f'vecs{l}'], ['g8'], lambda e: e.tensor_copy(out=g8[:, 1:2], in_=self.vec(l, 'nak')))
            it = 0
            for hp in range(8):
                for w_, c0 in ((0, C_NAQ), (1, C_NAK)):
                    z = zq[w_]
                    zk = f'naz{w_}'
                    P.dma(z[:], self.zT[c0 + hp * 128:c0 + (hp + 1) * 128, :], ['zT'], [zk])
                    dst = qk[w_]
                    dk = 'qn' if w_ == 0 else 'kn'
                    for (t0, n) in TILES:
                        P.op('act', [zk], ['nasq'], lambda e, z=z, t0=t0, n=n: e.activation(
                            out=sqb[:, :n], in_=z[:, t0:t0 + n], func=AF.Square))
                        P.op('pe', ['nasq', 'bd_bf'], ['ps4'], lambda e, n=n: e.matmul(
                            self.ps[4][:, :n], lhsT=self.bd_bf[:], rhs=sqb[:, :n], start=True, stop=True))
                        P.op('act', ['ps4'], ['nart'], lambda e, n=n: e.activation(
                            out=rtb[:, :n], in_=self.ps[4][:, :n], func=AF.Sqrt, scale=1.0 / 64, bias=self.eps_t[:, 0:1]))
                        P.op('dve', ['nart'], ['nart'], lambda e, n=n: e.reciprocal(out=rtb[:, :n], in_=rtb[:, :n]))
                        P.op('dve', [zk, 'nart', 'g8'], [dk], lambda e, z=z, t0=t0, n=n, dst=dst, w_=w_: e.scalar_tensor_tensor(
                            out=dst[:, t0:t0 + n], in0=z[:, t0:t0 + n], scalar=g8[:, w_:w_ + 1], in1=rtb[:, :n],
                            op0=ALU.mult, op1=ALU.mult))
                for e_ in range(2):
                    P.op('pool', [], [f'qm{e_}'], lambda e, e_=e_: e.memset(qm[e_][:], 0.0))
                    pr = slice(e_ * 64, (e_ + 1) * 64)
                    P.op('act', ['qn', f'qm{e_}'], [f'qm{e_}'], lambda e, e_=e_, pr=pr: e.activation(out=qm[e_][pr, :], in_=qk[0][pr, :], func=AF.Copy))
                P.dma(vte[:], vt_all[:, :, hp * 128:(hp + 1) * 128], ['vtm'], ['vte'])
                P.dma(vto[:], vt_odd[:, :, hp * 128:(hp + 1) * 128], ['vtm'], ['vto'])
                qn, kn = qk
                y = yna[hp % 2]
                yk = f'yna{hp % 2}'
                stages = []
                for e_ in range(2):
                    h = 2 * hp + e_
                    tb = tbl[h % 2]
                    tk = f'natbl{h % 2}'
                    pr = slice(e_ * 64, (e_ + 1) * 64)
                    for r in range(32):
                        rs = min(max(r - 4, 0), 24)
                        dlt = rs - r
                        tq = NCTX + r * 64
                        b = it % 2
                        it += 1
                        psS, psSk = self.ps[b], f'ps{b}'
                        psO, psOk = self.ps[2 + b], f'ps{2 + b}'

                        def s1(e_=e_, h=h, tb=tb, tk=tk, pr=pr, r=r, rs=rs, dlt=dlt, tq=tq, b=b, psS=psS, psSk=psSk):
                            if r == 0:
                                P.dma(tb[:].rearrange("p a b -> p (a b)"), self.natab[l, h], [], [tk])
                            for j in range(6):
                                kt = NCTX + (rs + 2 * j) * 64 if j < 4 else (j - 4) * 128
                                P.op('pe', [f'qm{e_}', 'kn'], [psSk], lambda e, j=j, kt=kt: e.matmul(
                                    psS[:, j * 64:(j + 1) * 64], lhsT=kn[:, kt:kt + 128], rhs=qm[e_][:, tq:tq + 64],
                                    start=True, stop=True))
                            d0 = dlt + 7
                            tv = tb[:].rearrange("p (u two) c -> p u two c", two=2)[:, d0 // 2:d0 // 2 + 4, d0 % 2, :]
                            P.op('dve', [psSk, tk], [f'sT{b}'], lambda e: e.tensor_tensor(
                                out=sT[b][:], in0=psS[:, 0:256].rearrange("p (j c) -> p j c", c=64), in1=tv, op=ALU.add))
                            P.op('act', [f'sT{b}'], [f'pT{b}'], lambda e: e.activation(
                                out=pT[b][:, 0:4, :], in_=sT[b][:], func=AF.Exp))
                            P.op('act', [psSk], [f'pT{b}'], lambda e: e.activation(
                                out=pT[b][:, 4:6, :], in_=psS[:, 256:384].rearrange("p (j c) -> p j c", c=64), func=AF.Exp))

                        def s2(pr=pr, rs=rs, tq=tq, b=b, psO=psO, psOk=psOk):
                            for part in range(2):
                                for j in range(6):
                                    if part == 1:
                                        lhs = self.ones_bf[:, :]
                                        rk_ = 'ones_bf'
                                    elif j >= 4:
                                        lhs = vte[:, j - 4, :]
                                        rk_ = 'vte'
                                    elif rs % 2 == 0:
                                        lhs = vte[:, 2 + rs // 2 + j, :]
                                        rk_ = 'vte'
                                    else:
                                        lhs = vto[:, (rs + 1) // 2 + 1 + j, :]
                                        rk_ = 'vto'
                                    P.op('pe', [rk_, f'pT{b}'], [psOk], lambda e, lhs=lhs, j=j, part=part: e.matmul(
                                        psO[:, part * 64:(part + 1) * 64], lhsT=lhs, rhs=pT[b][:, j, :],
                                        start=(j == 0), stop=(j == 5)))
                            P.op('dve', [psOk], [f'nars{b}'], lambda e: e.reciprocal(
                                out=rsb[b][pr, 0:64], in_=psO[pr, 64:128]))
                            P.op('dve', [psOk, f'nars{b}'], [yk], lambda e: e.tensor_tensor(
                                out=y[pr, tq:tq + 64], in0=psO[pr, 0:64], in1=rsb[b][pr, 0:64], op=ALU.mult))
                        stages.append((s1, s2))
                    if with_ctx:
                        b = it % 2
                        it += 1
                        psS, psSk = self.ps[b], f'ps{b}'
                        psO, psOk = self.ps[2 + b], f'ps{2 + b}'

                        def s1(e_=e_, pr=pr, psS=psS, psSk=psSk):
                            for j in range(2):
                                P.op('pe', [f'qm{e_}', 'kn'], [psSk], lambda e, j=j: e.matmul(
                                    psS[:, j * 256:(j + 1) * 256], lhsT=kn[:, j * 128:(j + 1) * 128], rhs=qm[e_][:, 0:256],
                                    start=True, stop=True))
                            P.op('act', [psSk], ['pTc'], lambda e: e.activation(
                                out=pTc[:].rearrange("p j c -> p (j c)"), in_=psS[:, :], func=AF.Exp))

                        def s2(pr=pr, b=b, psO=psO, psOk=psOk):
                            for part in range(2):
                                for j in range(2):
                                    lhs = self.ones_bf[:, :] if part == 1 else vte[:, j, :]
                                    P.op('pe', ['vte', 'ones_bf', 'pTc'], [psOk], lambda e, lhs=lhs, j=j, part=part: e.matmul(
                                        psO[:, part * 256:(part + 1) * 256], lhsT=lhs, rhs=pTc[:, j, :],
                                        start=(j == 0), stop=(j == 1)))
                            P.op('dve', [psOk], [f'nars{b}'], lambda e: e.reciprocal(
                                out=rsb[b][pr, :], in_=psO[pr, 256:512]))
                            P.op('dve', [psOk, f'nars{b}'], [yk], lambda e: e.tensor_tensor(
                                out=y[pr, 0:256], in0=psO[pr, 0:256], in1=rsb[b][pr, :], op=ALU.mult))
                        stages.append((s1, s2))
                for k_ in range(len(stages) + 1):
                    if k_ < len(stages):
                        stages[k_][0]()
                    if k_ >= 1:
                        stages[k_ - 1][1]()
                t_lo = 0 if with_ctx else NCTX
                P.dma(self.yT[0, hp * 128:(hp + 1) * 128, t_lo:], y[:, t_lo:], [yk], ['yT'])
            if 'na' in self.debug:
                self.dbg(f'yna{l}', lambda o: P.dma(o, self.yT[0], ['yT'], []), [1024, TT], BF16)


    def gla_mixer(self, l):
        nc, P = self.nc, self.P
        with_ctx = l < DEPTH - 1
        NCH = TT // 64
        NT = TT // 128
        qscale = 128 ** -0.5
        vt_all = self.vtm.rearrange("(tt p) c -> p tt c", p=128)
        with contextlib.ExitStack() as st:
            cos = self.sb(st, "cos", [128, SEQ])
            sin = self.sb(st, "sin", [128, SEQ])
            msk = self.sb(st, "cmsk", [128, TT])
            qk32 = [self.sb(st, "gq32", [128, TT]), self.sb(st, "gk32", [128, TT])]
            zb = self.sb(st, "gzb", [128, 512], BF16)
            rz = self.sb(st, "grz", [128, 2, TT])
            gd = [self.sb(st, f"ggd{d}", [16, TT]) for d in range(2)]
            gw2 = self.sb(st, "gw2", [16, 2, 512])
            nb = self.sb(st, "gnb", [128, 8])
            T1 = self.sb(st, "gT1", [128, TT]); T2 = self.sb(st, "gT2", [128, TT]); T3 = self.sb(st, "gT3", [128, TT])
            qi = self.sb(st, "gqi", [128, TT], BF16); kj = self.sb(st, "gkj", [128, TT], BF16)
            kd = self.sb(st, "gkd", [128, TT], BF16); qb = self.sb(st, "gqb", [128, TT], BF16)
            dec = self.sb(st, "gdec", [128, NCH])
            Vt = self.sb(st, "gVt", [128, NT, 256], BF16)
            kdT = self.sb(st, "gkdT", [128, NT, 128], BF16)
            Abf = [self.sb(st, f"gA{i}", [128, 128], BF16) for i in range(2)]
            S32 = self.sb(st, "gS32", [128, 256])
            Sbf = [self.sb(st, f"gSbf{i}", [128, 256], BF16) for i in range(2)]
            yf = self.sb(st, "gyf", [128, 2, TT], BF16)
            yo = self.sb(st, "gyo", [128, 2, TT], BF16)
            yt = [self.sb(st, f"gyt{i}", [128, 2, 128]) for i in range(2)]
            ysq = self.sb(st, "gysq", [128, 2, 128], BF16)
            yrt = self.sb(st, "gyrt", [128, 128])
            P.dma(cos[:], self.consts[:, 512:512 + SEQ], [], ['cos'])
            P.dma(sin[:], self.consts[:, 512 + SEQ:512 + 2 * SEQ], [], ['sin'])
            P.op('pool', [], ['cmsk'], lambda e: e.memset(msk[:], 1.0))
            P.op('pool', ['cmsk'], ['cmsk'], lambda e: e.memset(msk[:].rearrange("p (c j) -> p c j", j=64)[:, :, 0:1], 0.0))
            for d in range(2):
                P.dma(gd[d][:], self.zT[C_GGD + 16 * d:C_GGD + 16 * (d + 1), :], ['zT'], [f'ggd{d}'])
            P.dma(gw2[:], self.gla_w2[l].rearrange("d k c -> k d c"), [], ['gw2'])
            P.op('dve', [f'vecs{l}'], ['gnb'], lambda e: e.tensor_scalar(
                out=nb[:], in0=self.vec(l, 'ggb'), scalar1=-1.0, scalar2=None, op0=ALU.mult))
            gng = self.vec(l, 'gng')
            c3 = lambda t: t[:].rearrange("p (c j) -> p c j", j=64)
            sbi = 0
            for h in range(4):
                for w_, c0 in ((0, C_GQ), (1, C_GK)):
                    z = qk32[w_]
                    zk = 'gq32' if w_ == 0 else 'gk32'
                    P.dma(z[:], self.zT[c0 + h * 128:c0 + (h + 1) * 128, :], ['zT'], [zk])
                    for ti in range(4):
                        t0 = NCTX + ti * 512
                        P.op('act', [zk], ['gzb'], lambda e, z=z, t0=t0: e.activation(out=zb[:], in_=z[:, t0:t0 + 512], func=AF.Copy))
                        P.op('pe', ['gzb', 'cst_bf'], ['ps4'], lambda e: e.matmul(
                            self.ps[4][:, :], lhsT=self.perm_bf, rhs=zb[:], start=True, stop=True))
                        P.op('dve', ['ps4', 'sin'], ['gT3'], lambda e, ti=ti: e.tensor_tensor(
                            out=T3[:, 0:512], in0=self.ps[4][:, :], in1=sin[:, ti * 512:(ti + 1) * 512], op=ALU.mult))
                        P.op('dve', [zk, 'cos'], [zk], lambda e, z=z, t0=t0, ti=ti: e.tensor_tensor(
                            out=z[:, t0:t0 + 512], in0=z[:, t0:t0 + 512], in1=cos[:, ti * 512:(ti + 1) * 512], op=ALU.mult))
                        P.op('dve', [zk, 'gT3'], [zk], lambda e, z=z, t0=t0: e.tensor_tensor(
                            out=z[:, t0:t0 + 512], in0=z[:, t0:t0 + 512], in1=T3[:, 0:512], op=ALU.add))
                q32, k32 = qk32
                P.dma(rz[:], self.zT[C_GR + h * 256:C_GR + (h + 1) * 256, :].rearrange("(c p) t -> p c t", p=128), ['zT'], ['grz'])
                P.op('act', ['grz'], ['grz'], lambda e: e.activation(out=rz[:], in_=rz[:], func=AF.Silu))
                P.dma(Vt[:], vt_all[:, :, 1024 + h * 256:1024 + (h + 1) * 256], ['vtm'], ['gVt'])
                for d in range(2):
                    for (t0, n) in TILES:
                        P.op('pe', ['gw2', f'ggd{d}'], ['ps4'], lambda e, t0=t0, n=n, d=d, h=h: e.matmul(
                            self.ps[4][:, :n], lhsT=gw2[:, d, h * 128:(h + 1) * 128], rhs=gd[d][:, t0:t0 + n], start=True, stop=True), rg=0)
                        P.op('act', ['ps4', 'gnb'], ['gT1'], lambda e, t0=t0, n=n, d=d, h=h: e.activation(
                            out=T1[:, t0:t0 + n], in_=self.ps[4][:, :n], func=AF.Exp, scale=-1.0, bias=nb[:, d * 4 + h:d * 4 + h + 1]))
                    P.op('act', ['gT1'], ['gT1'], lambda e: e.activation(out=T1[:], in_=T1[:], func=AF.Ln, bias=1.0, scale=1.0))
                    P.op('dve', ['cmsk', 'gT1'], ['gT2'], lambda e: e.tensor_tensor_scan(
                        out=T2[:], data0=msk[:], data1=T1[:], initial=0.0, op0=ALU.mult, op1=ALU.add))
                    if d == 1:
                        P.op('dve', ['gT2'], ['gT3'], lambda e: e.tensor_tensor(
                            out=c3(T3), in0=c3(T2)[:, :, 63:64].to_broadcast([128, NCH, 64]), in1=c3(T2), op=ALU.subtract))
                        P.op('dve', ['gT3', 'gT1'], ['gT2'], lambda e: e.tensor_tensor(out=T2[:], in0=T3[:], in1=T1[:], op=ALU.add))
                    tot = c3(T2)[:, :, 63:64] if d == 0 else c3(T2)[:, :, 0:1]
                    cref = c3(T2)[:, :, 32:33] if d == 0 else c3(T2)[:, :, 31:32]
                    P.op('act', ['gT2'], ['gdec'], lambda e, tot=tot: e.activation(
                        out=dec[:].unsqueeze(2), in_=tot, func=AF.Exp, scale=-1.0 / 16))
                    P.op('dve', ['gT2'], ['gT3'], lambda e, cref=cref: e.tensor_tensor(
                        out=c3(T3), in0=c3(T2), in1=cref.to_broadcast([128, NCH, 64]), op=ALU.subtract))
                    P.op('act', ['gT3'], ['gqi'], lambda e: e.activation(out=qi[:], in_=T3[:], func=AF.Exp, scale=-1.0 / 16))
                    P.op('act', ['gT3'], ['gkj'], lambda e: e.activation(out=kj[:], in_=T3[:], func=AF.Exp, scale=1.0 / 16))
                    P.op('act', ['gT2'], ['gqb'], lambda e: e.activation(out=qb[:], in_=T2[:], func=AF.Exp, scale=-1.0 / 16))
                    P.op('dve', ['gT2'], ['gT1'], lambda e, tot=tot: e.tensor_tensor(
                        out=c3(T1), in0=tot.to_broadcast([128, NCH, 64]), in1=c3(T2), op=ALU.subtract))
                    P.op('act', ['gT1'], ['gkd'], lambda e: e.activation(out=kd[:], in_=T1[:], func=AF.Exp, scale=-1.0 / 16))
                    P.op('dve', ['gq32', 'gqi'], ['gqi'], lambda e: e.scalar_tensor_tensor(
                        out=qi[:], in0=qi[:], scalar=qscale, in1=q32[:], op0=ALU.mult, op1=ALU.mult))
                    P.op('dve', ['gk32', 'gkj'], ['gkj'], lambda e: e.tensor_tensor(out=kj[:], in0=kj[:], in1=k32[:], op=ALU.mult))
                    P.op('dve', ['gq32', 'gqb'], ['gqb'], lambda e: e.scalar_tensor_tensor(
                        out=qb[:], in0=qb[:], scalar=qscale, in1=q32[:], op0=ALU.mult, op1=ALU.mult))
                    P.op('dve', ['gk32', 'gkd'], ['gkd'], lambda e: e.tensor_tensor(out=kd[:], in0=kd[:], in1=k32[:], op=ALU.mult))
                    for g0 in range(0, NT, 4):
                        g1 = min(NT, g0 + 4)
                        for tt in range(g0, g1):
                            P.op('pe', ['gkd', 'cst_bf'], ['psb'], lambda e, tt=tt, g0=g0: e.transpose(
                                self.psb[:, (tt - g0) * 128:(tt - g0 + 1) * 128], kd[:, tt * 128:(tt + 1) * 128], self.ident_bf))
                        P.op('act', ['psb'], ['gkdT'], lambda e, g0=g0, g1=g1: e.activation(
                            out=kdT[:, g0:g1, :].rearrange("p a b -> p (a b)"), in_=self.psb[:, 0:(g1 - g0) * 128], func=AF.Copy))
                    P.op('dve', [], ['gS32'], lambda e: e.memset(S32[:], 0.0))
                    P.op('dve', [], [f'gSbf{sbi % 2}'], lambda e, sbi=sbi: e.memset(Sbf[sbi % 2][:], 0.0))
                    order = list(range(NT)) if d == 0 else [1, 0] + list(range(NT - 1, 1, -1))
                    maskd = self.mask_f if d == 0 else self.mask_b
                    for it_, tt in enumerate(order):
                        tsl = slice(tt * 128, (tt + 1) * 128)
                        ab = it_ % 2
                        P.op('pe', ['gkj', 'gqi'], ['ps5'], lambda e, tsl=tsl: e.matmul(
                            self.ps[5][:, 0:128], lhsT=kj[:, tsl], rhs=qi[:, tsl], start=True, stop=True))
                        P.op('dve', ['ps5', 'cst_f'], [f'gA{ab}'], lambda e, ab=ab, maskd=maskd: e.tensor_tensor(
                            out=Abf[ab][:], in0=self.ps[5][:, 0:128], in1=maskd, op=ALU.mult))
                        halves = (0, 1) if d == 0 else (1, 0)
                        psy = [self.ps[0 + 2 * (it_ % 2)], self.ps[1 + 2 * (it_ % 2)]]
                        psyk = [f'ps{0 + 2 * (it_ % 2)}', f'ps{1 + 2 * (it_ % 2)}']
                        for hi, hf in enumerate(halves):
                            csl = slice(hf * 64, (hf + 1) * 64)
                            tok = slice(tt * 128 + hf * 64, tt * 128 + (hf + 1) * 64)
                            sk = f'gSbf{sbi % 2}'
                            Sb = Sbf[sbi % 2]
                            for vc in range(2):
                                if hi == 0:
                                    P.op('pe', ['gVt', f'gA{ab}'], [psyk[vc]], lambda e, vc=vc, tt=tt, ab=ab, psy=psy: e.matmul(
                                        psy[vc][:, 0:128], lhsT=Vt[:, tt, vc * 128:(vc + 1) * 128], rhs=Abf[ab][:], start=True, stop=False))
                                P.op('pe', [sk, 'gqb'], [psyk[vc]], lambda e, vc=vc, Sb=Sb, csl=csl, tok=tok, hi=hi, psy=psy: e.matmul(
                                    psy[vc][:, csl], lhsT=Sb[:, vc * 128:(vc + 1) * 128], rhs=qb[:, tok], start=False, stop=(hi == 1)))
                            ch = tt * 2 + hf
                            P.op('pe', ['gkdT', 'gVt'], ['ps6'], lambda e, csl=csl, tt=tt: e.matmul(
                                self.ps[6][:, 0:256], lhsT=kdT[csl, tt, :], rhs=Vt[csl, tt, :], start=True, stop=True), rg=csl.start)
                            P.op('dve', ['ps6', 'gS32', 'gdec'], ['gS32'], lambda e, ch=ch: e.scalar_tensor_tensor(
                                out=S32[:], in0=S32[:], scalar=dec[:, ch:ch + 1], in1=self.ps[6][:, 0:256], op0=ALU.mult, op1=ALU.add))
                            sbi += 1
                            P.op('act', ['gS32'], [f'gSbf{sbi % 2}'], lambda e, sbi=sbi: e.activation(
                                out=Sbf[sbi % 2][:], in_=S32[:], func=AF.Copy))
                        if d == 0:
                            for vc in range(2):
                                P.op('act', [psyk[vc]], ['gyf'], lambda e, vc=vc, tsl=tsl, psy=psy: e.activation(
                                    out=yf[:, vc, tsl], in_=psy[vc][:, 0:128], func=AF.Copy))
                        elif with_ctx or tt >= 2:
                            ytb = yt[it_ % 2]
                            ytk = f'gyt{it_ % 2}'
                            for vc in range(2):
                                P.op('dve', [psyk[vc], 'gyf'], [ytk], lambda e, vc=vc, tsl=tsl, psy=psy, ytb=ytb: e.tensor_tensor(
                                    out=ytb[:, vc, :], in0=psy[vc][:, 0:128], in1=yf[:, vc, tsl], op=ALU.add))
                            P.op('act', [ytk], ['gysq'], lambda e, ytb=ytb: e.activation(out=ysq[:], in_=ytb[:], func=AF.Square))
                            for vc in range(2):
                                P.op('pe', ['gysq', 'ones_bf'], ['ps4'], lambda e, vc=vc: e.matmul(
                                    self.ps[4][:, 0:128], lhsT=self.ones_bf[:], rhs=ysq[:, vc, :], start=(vc == 0), stop=(vc == 1)))
                            P.op('act', ['ps4'], ['gyrt'], lambda e: e.activation(
                                out=yrt[:], in_=self.ps[4][:, 0:128], func=AF.Sqrt, scale=1.0 / 256, bias=self.eps_t[:, 0:1]))
                            P.op('dve', ['gyrt'], ['gyrt'], lambda e: e.reciprocal(out=yrt[:], in_=yrt[:]))
                            for vc in range(2):
                                P.op('dve', [ytk, 'gyrt', f'vecs{l}'], [ytk], lambda e, vc=vc, ytb=ytb: e.scalar_tensor_tensor(
                                    out=ytb[:, vc, :], in0=ytb[:, vc, :], scalar=gng[:, vc:vc + 1], in1=yrt[:], op0=ALU.mult, op1=ALU.mult))
                                P.op('dve', [ytk, 'grz'], ['gyo'], lambda e, vc=vc, ytb=ytb, tsl=tsl: e.tensor_tensor(
                                    out=yo[:, vc, tsl], in0=ytb[:, vc, :], in1=rz[:, vc, tsl], op=ALU.mult))
                t_lo = 0 if with_ctx else NCTX
                P.dma(self.yT[1, h * 256:(h + 1) * 256, t_lo:].rearrange("(c p) t -> p c t", p=128), yo[:, :, t_lo:], ['gyo'], ['yT'])
            if 'gla' in self.debug:
                self.dbg(f'ygla{l}', lambda o: P.dma(o, self.yT[1], ['yT'], []), [1024, TT], BF16)


    def _shift(self, z, zk, tmp, tmpk, om, hm, m):
        P = self.P
        P.op('dve', [zk], [tmpk], lambda e: e.tensor_tensor(out=tmp[:m, 1:TT - 1], in0=z[:m, 0:TT - 2], in1=z[:m, 2:TT], op=ALU.add))
        for (dst, src_) in ((0, 1), (NCTX - 1, NCTX - 2), (NCTX, NCTX + 1), (TT - 1, TT - 2)):
            P.op('dve', [zk, tmpk], [tmpk], lambda e, dst=dst, src_=src_: e.tensor_copy(out=tmp[:m, dst:dst + 1], in_=z[:m, src_:src_ + 1]))
        P.op('dve', [tmpk], [tmpk], lambda e: e.tensor_scalar(out=tmp[:m, :], in0=tmp[:m, :], scalar1=hm, scalar2=None, op0=ALU.mult))
        P.op('dve', [zk, tmpk], [zk], lambda e: e.scalar_tensor_tensor(out=z[:m, :], in0=z[:m, :], scalar=om, in1=tmp[:m, :], op0=ALU.mult, op1=ALU.add))

    def _mu_prep(self, st, l):
        P = self.P
        o0, _ = VEC_SLOTS['mu_r']
        mu = self.vecs[l][:, o0:o0 + 30]
        om = self.sb(st, "rw_om", [128, 30]); hm = self.sb(st, "rw_hm", [128, 30])
        P.op('dve', [f'vecs{l}'], ['rw_om'], lambda e: e.tensor_scalar(out=om[:], in0=mu, scalar1=-1.0, scalar2=1.0, op0=ALU.mult, op1=ALU.add))
        P.op('dve', [f'vecs{l}'], ['rw_hm'], lambda e: e.tensor_scalar(out=hm[:], in0=mu, scalar1=0.5, scalar2=None, op0=ALU.mult))
        return om, hm

    def rwkv_pre(self, l):
        nc, P = self.nc, self.P
        with contextlib.ExitStack() as st:
            om, hm = self._mu_prep(st, l)
            zt = self.sb(st, "rp_z", [128, TT]); tmp = self.sb(st, "rp_tmp", [128, TT])
            twd = [self.sb(st, f"rp_twd{d}", [96, TT], BF16) for d in range(2)]
            adb = [self.sb(st, f"rp_adb{d}", [96, TT], BF16) for d in range(2)]
            sgd = self.sb(st, "rp_sgd", [128, 2, TT], BF16)
            w2 = self.sb(st, "rp_w2", [96, 2, 1024], BF16); a2 = self.sb(st, "rp_a2", [96, 2, 1024], BF16)
            g2 = self.sb(st, "rp_g2", [128, 2, 1024], BF16)
            stg = [self.sb(st, f"rp_st{i}", [128, 512]) for i in range(4)]
            stb = [self.sb(st, f"rp_sb{i}", [128, 512], BF16) for i in range(2)]
            P.dma(w2[:], self.rw_w2[l].rearrange("d k c -> k d c"), [], ['rp_w2'], q='pool')
            P.dma(a2[:], self.rw_a2[l].rearrange("d k c -> k d c"), [], ['rp_a2'], q='pool')
            P.dma(g2[:], self.rw_g2[l].rearrange("(c p) n -> p c n", p=128), [], ['rp_g2'], q='pool')
            for d in range(2):
                P.dma(zt[:96, :], self.zT[C_RWD + 96 * d:C_RWD + 96 * (d + 1), :], ['zT'], ['rp_z'])
                self._shift(zt, 'rp_z', tmp, 'rp_tmp', om[:96, 24 + d:25 + d], hm[:96, 24 + d:25 + d], 96)
                P.op('act', ['rp_z'], [f'rp_twd{d}'], lambda e, d=d: e.activation(out=twd[d][:], in_=zt[:96, :], func=AF.Tanh))
                P.dma(zt[:96, :], self.zT[C_RAD + 96 * d:C_RAD + 96 * (d + 1), :], ['zT'], ['rp_z'])
                self._shift(zt, 'rp_z', tmp, 'rp_tmp', om[:96, 26 + d:27 + d], hm[:96, 26 + d:27 + d], 96)
                P.op('act', ['rp_z'], [f'rp_adb{d}'], lambda e, d=d: e.activation(out=adb[d][:], in_=zt[:96, :], func=AF.Copy))
            for c in range(2):
                P.dma(zt[:, :], self.zT[C_RGD + 128 * c:C_RGD + 128 * (c + 1), :], ['zT'], ['rp_z'])
                self._shift(zt, 'rp_z', tmp, 'rp_tmp', om[:, 28 + c:29 + c], hm[:, 28 + c:29 + c], 128)
                P.op('act', ['rp_z'], ['rp_sgd'], lambda e, c=c: e.activation(out=sgd[:, c, :], in_=zt[:, :], func=AF.Sigmoid))
            w0 = self.vec(l, 'w0'); a0 = self.vec(l, 'a0')
            n_ = 0
            for hp in range(8):
                cs = slice(hp * 128, (hp + 1) * 128)
                for (t0, n) in TILES:
                    for d in range(2):
                        for which in range(2):
                            pi = n_ % 4; n_ += 1
                            pst, pk = self.ps[pi], f'ps{pi}'
                            wm, src_, bias_ = ((w2, twd[d], w0), (a2, adb[d], a0))[which]
                            rk_ = [('rp_w2', f'rp_twd{d}'), ('rp_a2', f'rp_adb{d}')][which]
                            P.op('pe', list(rk_), [pk], lambda e, wm=wm, src_=src_, d=d, t0=t0, n=n, pst=pst, cs=cs: e.matmul(
                                pst[:, :n], lhsT=wm[:, d, cs], rhs=src_[:, t0:t0 + n], start=True, stop=True), rg=0)
                            sg = stg[pi]
                            P.op('act', [pk, f'vecs{l}'], [f'rp_st{pi}'], lambda e, pst=pst, sg=sg, n=n, bias_=bias_, d=d, hp=hp: e.activation(
                                out=sg[:, :n], in_=pst[:, :n], func=AF.Sigmoid, bias=bias_[:, d * 8 + hp:d * 8 + hp + 1], scale=1.0))
                            if which == 0:
                                P.op('dve', [f'rp_st{pi}'], [f'rp_st{pi}'], lambda e, sg=sg, n=n: e.tensor_scalar(
                                    out=sg[:, :n], in0=sg[:, :n], scalar1=-0.6065306597126334, scalar2=None, op0=ALU.mult))
                                P.dma(self.ldT[d, cs, t0:t0 + n], sg[:, :n], [f'rp_st{pi}'], ['ldT'])
                            else:
                                P.dma(self.aT[d, cs, t0:t0 + n], sg[:, :n], [f'rp_st{pi}'], ['aT'])
                    pi = n_ % 4; n_ += 1
                    pst, pk = self.ps[pi], f'ps{pi}'
                    for c in range(2):
                        P.op('pe', ['rp_g2', 'rp_sgd'], [pk], lambda e, c=c, t0=t0, n=n, pst=pst, cs=cs: e.matmul(
                            pst[:, :n], lhsT=g2[:, c, cs], rhs=sgd[:, c, t0:t0 + n], start=(c == 0), stop=(c == 1)))
                    bi = n_ % 2
                    P.op('act', [pk], [f'rp_sb{bi}'], lambda e, pst=pst, n=n, bi=bi: e.activation(out=stb[bi][:, :n], in_=pst[:, :n], func=AF.Copy))
                    P.dma(self.g2T[cs, t0:t0 + n], stb[bi][:, :n], [f'rp_sb{bi}'], ['g2T'])

    def rwkv_mixer(self, l):
        nc, P = self.nc, self.P
        with_ctx = l < DEPTH - 1
        NCH = TT // 64
        NT = TT // 128
        with contextlib.ExitStack() as st:
            om, hm = self._mu_prep(st, l)
            A = [self.sb(st, f"rwA{i}", [128, TT]) for i in range(7)]
            Ak = [f'rwA{i}' for i in range(7)]
            msk = self.sb(st, "rmsk", [128, TT])
            vb = self.sb(st, "rw_vb", [128, TT], BF16)
            bon = self.sb(st, "rw_bon", [128, TT], BF16)
            gsb = self.sb(st, "rw_g", [128, 512], BF16)
            al = self.sb(st, "rw_al", [128, TT], BF16); rho = self.sb(st, "rw_rho", [128, TT], BF16)
            be = self.sb(st, "rw_be", [128, TT], BF16); ka = self.sb(st, "rw_ka", [128, TT], BF16)
            Bp = self.sb(st, "rw_Bp", [128, TT], BF16); Kp = self.sb(st, "rw_Kp", [128, TT], BF16)
            al_tm = self.sb(st, "rw_altm", [128, NT, 128], BF16); Bp_tm = self.sb(st, "rw_Bptm", [128, NT, 128], BF16)
            Kp_tm = self.sb(st, "rw_Kptm", [128, NT, 128], BF16); V_tm = self.sb(st, "rw_Vtm", [128, NT, 128], BF16)
            etot = self.sb(st, "rw_etot", [128, NCH])
            slots = []
            s0 = dict(XN=[[self.sb(st, f"rw_X{i}", [128, 512], BF16), self.sb(st, f"rw_N{i}", [128, 512], BF16)] for i in range(2)],
                      Q=[self.sb(st, f"rw_Q{i}", [128, 512], BF16) for i in range(2)],
                      LkT=self.sb(st, "rw_LkT", [128, 512], BF16), MbT=self.sb(st, "rw_MbT", [128, 512], BF16), MkT=self.sb(st, "rw_MkT", [128, 512], BF16),
                      H=self.sb(st, "rw_H", [128, 256], BF16)[:], P1n=self.sb(st, "rw_P1n", [128, 256], BF16)[:], G=self.sb(st, "rw_G", [128, 256], BF16)[:],
                      ptmp=self.sb(st, "rw_ptmp", [128, 128])[:], banks=(self.ps[0], self.ps[1], self.ps[2]), bank_ids=(0, 1, 2))
            slots.append(s0)
            a2b = A[2][:].bitcast(BF16)
            cut = lambda i: a2b[:, i * 512:(i + 1) * 512]
            rt = self.sb(st, "rw_rt", [128, 512]); t5 = self.sb(st, "rw_t5", [128, 512])
            t5b = t5[:].bitcast(BF16)
            s1_ = dict(XN=[[cut(0), cut(1)], [cut(2), cut(3)]], Q=[cut(4), cut(5)], LkT=cut(6), MbT=cut(7), MkT=cut(8),
                       H=t5b[:, 0:256], P1n=t5b[:, 256:512], G=t5b[:, 512:768], ptmp=rt[:, 0:128],
                       banks=(self.ps[3], self.ps[5], self.ps[6]), bank_ids=(3, 5, 6))
            slots.append(s1_)
            for nm in ('rw_X0', 'rw_N0', 'rw_X1', 'rw_N1', 'rw_Q0', 'rw_Q1', 'rw_LkT', 'rw_MbT', 'rw_MkT'):
                P.set_parent(nm + '_s1', Ak[2])
            for nm in ('rw_H', 'rw_P1n', 'rw_G'):
                P.set_parent(nm + '_s1', 'rw_t5')
            P.set_parent('rw_ptmp_s1', 'rw_rt')
            a5b = A[5][:].bitcast(BF16); a6b = A[6][:].bitcast(BF16)
            al_m = [a5b[:, 0:TT], a5b[:, TT:2 * TT]]
            rho_m = [a6b[:, 0:TT], a6b[:, TT:2 * TT]]
            P.set_parent('rw_alm', Ak[5]); P.set_parent('rw_rhom', Ak[6])
            R32 = self.sb(st, "rw_R32", [128, TT]); yloc = self.sb(st, "rw_yloc", [128, TT]); yacc = self.sb(st, "rw_yacc", [128, TT])
            Phi = self.sb(st, "rw_Phi", [128, NCH, 128]); Dd = self.sb(st, "rw_D", [128, NCH, 128], BF16)
            Sbd = [self.sb(st, f"rw_S{i}", [128, 128]) for i in range(2)]
            ka1 = self.sb(st, "rw_ka1", [128, 8])
            sq5 = self.sb(st, "rw_sq5", [128, 512], BF16)
            yo = vb
            geps = self.sb(st, "rw_geps", [128, 1])
            P.op('pool', [], ['rw_geps'], lambda e: e.memset(geps[:], 64e-5))
            P.op('pool', [], ['rmsk'], lambda e: e.memset(msk[:], 1.0))
            P.op('pool', ['rmsk'], ['rmsk'], lambda e: e.memset(msk[:].rearrange("p (c j) -> p c j", j=64)[:, :, 0:1], 0.0))
            P.op('dve', [f'vecs{l}'], ['rw_ka1'], lambda e: e.tensor_scalar(
                out=ka1[:], in0=self.vec(l, 'ka'), scalar1=-1.0, scalar2=1.0, op0=ALU.mult, op1=ALU.add))
            c3 = lambda t: t[:].rearrange("p (c j) -> p c j", j=64)
            kkv = self.vec(l, 'kk'); kav = self.vec(l, 'ka'); rkv = self.vec(l, 'rk')
            gng = self.vec(l, 'gng_rw'); gnb = self.vec(l, 'gnb_rw')
            si = 0
            for hp in range(8):
                cs = slice(hp * 128, (hp + 1) * 128)
                self.sc_in('rw_load', hp == 0 and l == 0)
                r32, k32, v32, kk32 = A[0], A[1], A[2], A[3]
                for i_, c0 in enumerate((C_RR, C_RK, C_RV)):
                    P.dma(A[i_][:], self.zT[c0 + hp * 128:c0 + (hp + 1) * 128, :], ['zT'], [Ak[i_]])
                    ci = i_ * 8 + hp
                    self._shift(A[i_], Ak[i_], A[6], Ak[6], om[:, ci:ci + 1], hm[:, ci:ci + 1], 128)
                P.op('act', [Ak[2]], ['rw_vb'], lambda e: e.activation(out=vb[:], in_=v32[:], func=AF.Copy))
                for (srcb, srck, dst, dstk) in ((vb, 'rw_vb', V_tm, 'rw_Vtm'),):
                    for g0 in range(0, NT, 4):
                        g1 = min(NT, g0 + 4)
                        for tt in range(g0, g1):
                            P.op('pe', [srck, 'cst_bf'], ['psb'], lambda e, tt=tt, g0=g0, srcb=srcb: e.transpose(
                                self.psb[:, (tt - g0) * 128:(tt - g0 + 1) * 128], srcb[:, tt * 128:(tt + 1) * 128], self.ident_bf))
                        P.op('act', ['psb'], [dstk], lambda e, g0=g0, g1=g1, dst=dst: e.activation(
                            out=dst[:, g0:g1, :].rearrange("p a b -> p (a b)"), in_=self.psb[:, 0:(g1 - g0) * 128], func=AF.Copy))
                P.op('dve', [Ak[1], f'vecs{l}'], [Ak[3]], lambda e: e.tensor_scalar(
                    out=kk32[:], in0=k32[:], scalar1=kkv[:, hp:hp + 1], scalar2=None, op0=ALU.mult))
                P.op('dve', [Ak[0], Ak[1], f'vecs{l}'], [Ak[6]], lambda e: e.scalar_tensor_tensor(
                    out=A[6][:], in0=r32[:], scalar=rkv[:, hp:hp + 1], in1=k32[:], op0=ALU.mult, op1=ALU.mult))
                for (t0, n) in TILES:
                    P.op('act', [Ak[3]], ['rw_sq5'], lambda e, t0=t0, n=n: e.activation(out=sq5[:, :n], in_=kk32[:, t0:t0 + n], func=AF.Square))
                    P.op('pe', ['rw_sq5', 'bd_bf'], ['ps4'], lambda e, n=n: e.matmul(self.ps[4][:, :n], lhsT=self.bd_bf[:], rhs=sq5[:, :n], start=True, stop=True))
                    P.op('act', ['ps4'], ['rw_rt'], lambda e, n=n: e.activation(out=rt[:, :n], in_=self.ps[4][:, :n], func=AF.Sqrt, scale=1.0, bias=self.eps_t[:, 0:1]))
                    P.op('dve', ['rw_rt'], ['rw_rt'], lambda e, n=n: e.reciprocal(out=rt[:, :n], in_=rt[:, :n]))
                    P.op('dve', [Ak[3], 'rw_rt'], [Ak[3]], lambda e, t0=t0, n=n: e.tensor_tensor(out=kk32[:, t0:t0 + n], in0=kk32[:, t0:t0 + n], in1=rt[:, :n], op=ALU.mult))
                    P.op('act', [Ak[6]], ['rw_sq5'], lambda e, t0=t0, n=n: e.activation(out=sq5[:, :n], in_=A[6][:, t0:t0 + n], func=AF.Copy))
                    P.op('pe', ['rw_sq5', 'bd_bf'], ['ps5'], lambda e, n=n: e.matmul(self.ps[5][:, :n], lhsT=self.bd_bf[:], rhs=sq5[:, :n], start=True, stop=True))
                    P.op('dve', ['ps5', Ak[2]], ['rw_bon'], lambda e, t0=t0, n=n: e.tensor_tensor(out=bon[:, t0:t0 + n], in0=self.ps[5][:, :n], in1=v32[:, t0:t0 + n], op=ALU.mult))
                for d in range(2):
                    self.sc_in('rw_prep', hp == 0 and d == 0 and l == 0)
                    ld, a_, cin, E = A[4], A[5], A[2], A[6]
                    ldk, ak_, cink, Ek = Ak[4], Ak[5], Ak[2], Ak[6]
                    P.dma(ld[:], self.ldT[d, cs, :], ['ldT'], [ldk])
                    P.dma(a_[:], self.aT[d, cs, :], ['aT'], [ak_])
                    P.op('dve', ['rmsk', ldk], [cink], lambda e: e.tensor_tensor_scan(
                        out=cin[:], data0=msk[:], data1=ld[:], initial=0.0, op0=ALU.mult, op1=ALU.add))
                    if d == 1:
                        P.op('dve', [cink], [Ek], lambda e: e.tensor_tensor(
                            out=c3(E), in0=c3(cin)[:, :, 63:64].to_broadcast([128, NCH, 64]), in1=c3(cin), op=ALU.subtract))
                        P.op('dve', [Ek, ldk], [cink], lambda e: e.tensor_tensor(out=cin[:], in0=E[:], in1=ld[:], op=ALU.add))
                    tot = c3(cin)[:, :, 63:64] if d == 0 else c3(cin)[:, :, 0:1]
                    P.op('act', [cink], ['rw_etot'], lambda e, tot=tot: e.activation(out=etot[:].unsqueeze(2), in_=tot, func=AF.Exp))
                    P.op('dve', [cink, ldk], [ldk], lambda e: e.tensor_tensor(out=ld[:], in0=cin[:], in1=ld[:], op=ALU.subtract))
                    P.op('act', [ldk], ['rw_al'], lambda e: e.activation(out=al[:], in_=ld[:], func=AF.Exp))
                    P.op('act', [cink], [Ek], lambda e: e.activation(out=E[:], in_=cin[:], func=AF.Exp))
                    P.op('act', [cink], ['rw_be'], lambda e: e.activation(out=be[:], in_=cin[:], func=AF.Exp, scale=-1.0))
                    P.op('act', [cink], ['rw_ka'], lambda e: e.activation(out=ka[:], in_=cin[:], func=AF.Exp, scale=-1.0))
                    P.op('dve', [Ak[3], 'rw_al'], ['rw_al'], lambda e: e.tensor_tensor(out=al[:], in0=al[:], in1=kk32[:], op=ALU.mult))
                    rho32 = ld
                    P.op('dve', [Ak[0], Ek], [ldk], lambda e: e.tensor_tensor(out=rho32[:], in0=r32[:], in1=E[:], op=ALU.mult))
                    P.op('act', [ldk], ['rw_rho'], lambda e: e.activation(out=rho[:], in_=rho32[:], func=AF.Copy))
                    P.op('dve', ['rw_be', ak_], ['rw_be'], lambda e: e.tensor_tensor(out=be[:], in0=be[:], in1=a_[:], op=ALU.mult))
                    P.op('dve', ['rw_be', Ak[3]], ['rw_be'], lambda e: e.tensor_tensor(out=be[:], in0=be[:], in1=kk32[:], op=ALU.mult))
                    P.op('dve', [ak_, f'vecs{l}', 'rw_ka1'], [ak_], lambda e: e.tensor_scalar(
                        out=a_[:], in0=a_[:], scalar1=kav[:, hp:hp + 1], scalar2=ka1[:, hp:hp + 1], op0=ALU.mult, op1=ALU.add))
                    P.op('dve', [ak_, Ak[1]], [ak_], lambda e: e.tensor_tensor(out=a_[:], in0=a_[:], in1=k32[:], op=ALU.mult))
                    P.op('dve', [ak_, 'rw_ka'], ['rw_ka'], lambda e: e.tensor_tensor(out=ka[:], in0=ka[:], in1=a_[:], op=ALU.mult))
                    eb = etot[:].unsqueeze(2).to_broadcast([128, NCH, 64])
                    P.op('dve', ['rw_be', 'rw_etot'], ['rw_Bp'], lambda e: e.tensor_tensor(out=c3(Bp), in0=c3(be), in1=eb, op=ALU.mult))
                    P.op('dve', ['rw_ka', 'rw_etot'], ['rw_Kp'], lambda e: e.tensor_tensor(out=c3(Kp), in0=c3(ka), in1=eb, op=ALU.mult))
                    self.stopat(1)
                    self.sc_in('rw_tm', hp == 0 and d == 0 and l == 0)
                    for (srcb, srck, dst, dstk) in ((al, 'rw_al', al_tm, 'rw_altm'), (Bp, 'rw_Bp', Bp_tm, 'rw_Bptm'), (Kp, 'rw_Kp', Kp_tm, 'rw_Kptm')):
                        for g0 in range(0, NT, 4):
                            g1 = min(NT, g0 + 4)
                            for tt in range(g0, g1):
                                P.op('pe', [srck, 'cst_bf'], ['psb'], lambda e, tt=tt, g0=g0, srcb=srcb: e.transpose(
                                    self.psb[:, (tt - g0) * 128:(tt - g0 + 1) * 128], srcb[:, tt * 128:(tt + 1) * 128], self.ident_bf))
                            P.op('act', ['psb'], [dstk], lambda e, g0=g0, g1=g1, dst=dst: e.activation(
                                out=dst[:, g0:g1, :].rearrange("p a b -> p (a b)"), in_=self.psb[:, 0:(g1 - g0) * 128], func=AF.Copy))
                    P.op('pool', [], ['rw_alm'], lambda e: e.memset(a5b, 0.0))
                    P.op('pool', [], ['rw_rhom'], lambda e: e.memset(a6b, 0.0))
                    for e_ in range(2):
                        pr = slice(e_ * 64, (e_ + 1) * 64)
                        P.op('act', ['rw_al'], ['rw_alm'], lambda e, e_=e_, pr=pr: e.activation(out=al_m[e_][pr, :], in_=al[pr, :], func=AF.Copy))
                        P.op('act', ['rw_rho'], ['rw_rhom'], lambda e, e_=e_, pr=pr: e.activation(out=rho_m[e_][pr, :], in_=rho[pr, :], func=AF.Copy))
                    self.stopat(2)
                    m_st = self.mask_fs if d == 0 else self.mask_bs
                    m_in = self.mask_f if d == 0 else self.mask_b
                    m_ts = self.mask_bs if d == 0 else self.mask_fs
                    bc4 = lambda m: m.unsqueeze(1).to_broadcast([128, 4, 128])
                    v4 = lambda t: t[:].rearrange("p (a b) -> p a b", b=128)
                    def grp(g0, sl):
                        B_ = slots[sl]
                        pa, pb, pc = B_['banks']
                        pak, pbk, pck = (f'ps{i}' for i in B_['bank_ids'])
                        XNs, Qs, LkT, MbT, MkT, Hh, P1n, Gt, ptmp = B_['XN'], B_['Q'], B_['LkT'], B_['MbT'], B_['MkT'], B_['H'], B_['P1n'], B_['G'], B_['ptmp']
                        kx = lambda nm: f'{nm}_s{sl}'
                        probs = [(tt, e_) for tt in (g0, g0 + 1) for e_ in range(2)]
                        X, N_ = XNs[0]

                        def gram(specs):
                            for pi_, (tt, e_) in enumerate(probs):
                                tsl = slice(tt * 128, (tt + 1) * 128); osl = slice(pi_ * 128, (pi_ + 1) * 128)
                                for (pst, pk, lh, rh, rks) in specs:
                                    lh_ = lh[e_] if isinstance(lh, list) else lh
                                    rh_ = rh[e_] if isinstance(rh, list) else rh
                                    P.op('pe', rks, [pk], lambda e, pst=pst, lh_=lh_, rh_=rh_, tsl=tsl, osl=osl: e.matmul(
                                        pst[:, osl], lhsT=lh_[:, tsl], rhs=rh_[:, tsl], start=True, stop=True))
                        gram(((pa, pak, be, al_m, ['rw_be', 'rw_alm']), (pb, pbk, al_m, be, ['rw_alm', 'rw_be']), (pc, pck, ka, al_m, ['rw_ka', 'rw_alm'])))
                        P.op('dve', [pak, 'cst_f'], [kx('rw_X0')], lambda e: e.scalar_tensor_tensor(
                            out=v4(X), in0=v4(pa), scalar=-1.0, in1=bc4(m_st), op0=ALU.mult, op1=ALU.mult))
                        P.op('dve', [pbk, 'cst_f'], [kx('rw_N0')], lambda e: e.scalar_tensor_tensor(
                            out=v4(N_), in0=v4(pb), scalar=-1.0, in1=bc4(m_ts), op0=ALU.mult, op1=ALU.mult))
                        P.op('dve', [pck, 'cst_f'], [kx('rw_LkT')], lambda e: e.tensor_tensor(out=v4(LkT), in0=v4(pc), in1=bc4(m_st), op=ALU.mult))
                        yield
                        pa2, pa2k = pa, pak
                        gram(((pa2, pa2k, be, rho_m, ['rw_be', 'rw_rhom']), (pb, pbk, ka, rho_m, ['rw_ka', 'rw_rhom'])))
                        P.op('dve', [pa2k, 'cst_f'], [kx('rw_MbT')], lambda e: e.tensor_tensor(out=v4(MbT), in0=v4(pa2), in1=bc4(m_in), op=ALU.mult))
                        P.op('dve', [pbk, 'cst_f'], [kx('rw_MkT')], lambda e: e.tensor_tensor(out=v4(MkT), in0=v4(pb), in1=bc4(m_in), op=ALU.mult))
                        P.op('dve', [kx('rw_X0'), 'cst_bf'], [kx('rw_Q0')], lambda e: e.tensor_tensor(
                            out=v4(Qs[0]), in0=v4(X), in1=self.ident_bf.unsqueeze(1).to_broadcast([128, 4, 128]), op=ALU.add))
                        yield
                        qi_ = 0
                        for j in range(1, 6):
                            Xo, No = XNs[(j - 1) % 2]
                            Xn, Nn = XNs[j % 2]
                            xo_k, no_k = kx(f'rw_X{(j - 1) % 2}'), kx(f'rw_N{(j - 1) % 2}')
                            xn_k, nn_k = kx(f'rw_X{j % 2}'), kx(f'rw_N{j % 2}')
                            for pi_ in range(4):
                                osl = slice(pi_ * 128, (pi_ + 1) * 128)
                                if j < 5:
                                    P.op('pe', [xo_k, no_k], [pak], lambda e, osl=osl, Xo=Xo, No=No: e.matmul(
                                        pa[:, osl], lhsT=No[:, osl], rhs=Xo[:, osl], start=True, stop=True))
                                P.op('pe', [xo_k, no_k], [pbk], lambda e, osl=osl, Xo=Xo, No=No: e.matmul(
                                    pb[:, osl], lhsT=Xo[:, osl], rhs=No[:, osl], start=True, stop=True))
                            if j < 5:
                                P.op('act', [pak], [xn_k], lambda e, Xn=Xn: e.activation(out=Xn[:], in_=pa[:], func=AF.Copy))
                            P.op('act', [pbk], [nn_k], lambda e, Nn=Nn: e.activation(out=Nn[:], in_=pb[:], func=AF.Copy))
                            yield
                            Qo, Qn = Qs[qi_ % 2], Qs[(qi_ + 1) % 2]
                            qo_k, qn_k = kx(f'rw_Q{qi_ % 2}'), kx(f'rw_Q{(qi_ + 1) % 2}')
                            for pi_ in range(4):
                                osl = slice(pi_ * 128, (pi_ + 1) * 128)
                                P.op('pe', [nn_k, qo_k], [pck], lambda e, osl=osl, Nn=Nn, Qo=Qo: e.matmul(
                                    pc[:, osl], lhsT=Nn[:, osl], rhs=Qo[:, osl], start=True, stop=True))
                            P.op('dve', [pck, qo_k], [qn_k], lambda e, Qo=Qo, Qn=Qn: e.tensor_tensor(out=Qn[:], in0=pc[:], in1=Qo[:], op=ALU.add))
                            qi_ += 1
                            yield
                        Qf = Qs[qi_ % 2]; qf_k = kx(f'rw_Q{qi_ % 2}')
                        for pi_, (tt, e_) in enumerate(probs):
                            pr = slice(e_ * 64, (e_ + 1) * 64); osl = slice(pi_ * 128, (pi_ + 1) * 128)
                            P.op('pe', [kx('rw_LkT'), 'rw_Vtm'], [pak], lambda e, osl=osl, tt=tt, pr=pr, pi_=pi_: e.matmul(
                                pa[:, pi_ * 64:(pi_ + 1) * 64], lhsT=LkT[:, osl], rhs=V_tm[:, tt, pr], start=True, stop=True))
                            P.op('pe', [qf_k, 'rw_altm'], [pak], lambda e, osl=osl, tt=tt, pr=pr, pi_=pi_, Qf=Qf: e.matmul(
                                pa[:, 256 + pi_ * 64:256 + (pi_ + 1) * 64], lhsT=Qf[:, osl], rhs=al_tm[:, tt, pr], start=True, stop=True))
                        P.op('act', [pak], [kx('rw_H')], lambda e: e.activation(out=Hh, in_=pa[:, 0:256], func=AF.Copy))
                        P.op('act', [pak], [kx('rw_G')], lambda e: e.activation(out=Gt, in_=pa[:, 256:512], func=AF.Copy))
                        yield
                        H3 = Hh.rearrange("p (a b) -> p a b", b=64); G3 = Gt.rearrange("p (a b) -> p a b", b=64); P3 = P1n.rearrange("p (a b) -> p a b", b=64)
                        for pi_, (tt, e_) in enumerate(probs):
                            osl = slice(pi_ * 128, (pi_ + 1) * 128)
                            P.op('pe', [qf_k, kx('rw_H')], [pbk], lambda e, osl=osl, pi_=pi_, Qf=Qf: e.matmul(
                                pb[:, pi_ * 64:(pi_ + 1) * 64], lhsT=Qf[:, osl], rhs=H3[:, pi_, :], start=True, stop=True))
                        P.op('act', [pbk], [kx('rw_P1n')], lambda e: e.activation(out=P1n, in_=pb[:, 0:256], func=AF.Identity, scale=-1.0))
                        yield
                        for ti_, tt in enumerate((g0, g0 + 1)):
                            for e_ in range(2):
                                pi_ = ti_ * 2 + e_
                                pr = slice(e_ * 64, (e_ + 1) * 64); osl = slice(pi_ * 128, (pi_ + 1) * 128)
                                P.op('pe', [kx('rw_G'), kx('rw_MbT')], [pak], lambda e, pr=pr, osl=osl, pi_=pi_, ti_=ti_: e.matmul(
                                    pa[pr, ti_ * 128:(ti_ + 1) * 128], lhsT=G3[:, pi_, :], rhs=MbT[:, osl], start=True, stop=True))
                                P.op('pe', ['rw_Vtm', kx('rw_MkT')], [pbk], lambda e, pr=pr, osl=osl, tt=tt, ti_=ti_: e.matmul(
                                    pb[pr, ti_ * 128:(ti_ + 1) * 128], lhsT=V_tm[:, tt, pr], rhs=MkT[:, osl], start=True, stop=False))
                                P.op('pe', [kx('rw_P1n'), kx('rw_MbT')], [pbk], lambda e, pr=pr, osl=osl, pi_=pi_, ti_=ti_: e.matmul(
                                    pb[pr, ti_ * 128:(ti_ + 1) * 128], lhsT=P3[:, pi_, :], rhs=MbT[:, osl], start=False, stop=True))
                        t2 = slice(g0 * 128, (g0 + 2) * 128)
                        P.op('dve', [pak, ldk], ['rw_R32'], lambda e, t2=t2: e.tensor_tensor(out=R32[:, t2], in0=rho32[:, t2], in1=pa[:, 0:256], op=ALU.subtract))
                        P.op('act', [pbk], ['rw_yloc'], lambda e, t2=t2: e.activation(out=yloc[:, t2], in_=pb[:, 0:256], func=AF.Copy))
                        yield
                        for ti_, tt in enumerate((g0, g0 + 1)):
                            for hf in range(2):
                                csl = slice(hf * 64, (hf + 1) * 64)
                                gsl = G3[csl, ti_ * 2:ti_ * 2 + 2, :].rearrange("p a b -> p (a b)")
                                p1sl = P3[csl, ti_ * 2:ti_ * 2 + 2, :].rearrange("p a b -> p (a b)")
                                col = slice((ti_ * 2 + hf) * 128, (ti_ * 2 + hf + 1) * 128)
                                P.op('pe', [kx('rw_G'), 'rw_Bptm'], [pak], lambda e, gsl=gsl, csl=csl, tt=tt, col=col: e.matmul(
                                    pa[:, col], lhsT=gsl, rhs=Bp_tm[csl, tt, :], start=True, stop=True))
                                P.op('pe', ['rw_Kptm', 'rw_Vtm'], [pck], lambda e, csl=csl, tt=tt, col=col: e.matmul(
                                    pc[:, col], lhsT=Kp_tm[csl, tt, :], rhs=V_tm[csl, tt, :], start=True, stop=False))
                                P.op('pe', ['rw_Bptm', kx('rw_P1n')], [pck], lambda e, csl=csl, tt=tt, col=col, p1sl=p1sl: e.matmul(
                                    pc[:, col], lhsT=Bp_tm[csl, tt, :], rhs=p1sl, start=False, stop=True))
                        for q_ in range(4):
                            ch = g0 * 2 + q_
                            col = slice(q_ * 128, (q_ + 1) * 128)
                            P.op('dve', [pak, 'cst_f'], [kx('rw_ptmp')], lambda e, col=col: e.tensor_tensor(out=ptmp, in0=pa[:, col], in1=self.mask_bd, op=ALU.mult))
                            P.op('dve', [kx('rw_ptmp'), 'rw_etot', 'cst_f'], ['rw_Phi'], lambda e, ch=ch: e.scalar_tensor_tensor(
                                out=Phi[:, ch, :], in0=self.ident32, scalar=etot[:, ch:ch + 1], in1=ptmp, op0=ALU.mult, op1=ALU.subtract))
                        P.op('dve', [pck, 'cst_f'], ['rw_D'], lambda e, g0=g0: e.tensor_tensor(
                            out=Dd[:, g0 * 2:g0 * 2 + 4, :], in0=v4(pc), in1=bc4(self.mask_bd), op=ALU.mult))

                    self.sc_in('rw_grp', hp == 0 and d == 0 and l == 0)
                    pending = list(range(0, NT, 2))
                    active = {}
                    while pending or active:
                        for sl in range(self.rw_slots):
                            if sl not in active and pending:
                                active[sl] = grp(pending.pop(0), sl)
                        for sl in list(active):
                            try:
                                self._steps = getattr(self, '_steps', 0) + 1
                                if self._steps == self.rw_stop:
                                    P.halt = True
                                next(active[sl])
                            except StopIteration:
                                del active[sl]
                    self.stopat(7)
                    self.sc_in('rw_rec', hp == 0 and d == 0 and l == 0)
                    P.op('dve', [], [f'rw_S{si % 2}'], lambda e, si=si: e.memset(Sbd[si % 2][:], 0.0))
                    order = list(range(NCH)) if d == 0 else [3, 2, 1, 0] + list(range(NCH - 1, 3, -1))
                    for n_i, ch in enumerate(order):
                        S_ = Sbd[si % 2]; sk = f'rw_S{si % 2}'
                        tok = slice(ch * 64, (ch + 1) * 64)
                        yb = n_i % 2
                        need_y = with_ctx or ch >= 4
                        if need_y:
                            P.op('pe', [sk, 'rw_R32'], [f'ps{3 + yb}'], lambda e, S_=S_, tok=tok, yb=yb: e.matmul(
                                self.ps[3 + yb][:, 0:64], lhsT=S_[:], rhs=R32[:, tok], start=True, stop=True))
                            if d == 0:
                                P.op('dve', [f'ps{3 + yb}', 'rw_yloc'], ['rw_yacc'], lambda e, tok=tok, yb=yb: e.tensor_tensor(
                                    out=yacc[:, tok], in0=self.ps[3 + yb][:, 0:64], in1=yloc[:, tok], op=ALU.add))
                            else:
                                P.op('dve', [f'ps{3 + yb}', 'rw_yloc'], ['rw_yloc'], lambda e, tok=tok, yb=yb: e.tensor_tensor(
                                    out=yloc[:, tok], in0=self.ps[3 + yb][:, 0:64], in1=yloc[:, tok], op=ALU.add))
                                P.op('pool', ['rw_yloc', 'rw_yacc'], ['rw_yacc'], lambda e, tok=tok: e.tensor_tensor(
                                    out=yacc[:, tok], in0=yacc[:, tok], in1=yloc[:, tok], op=ALU.add))
                        P.op('pe', [sk, 'rw_Phi'], [f'ps{5 + yb}'], lambda e, S_=S_, ch=ch, yb=yb: e.matmul(
                            self.ps[5 + yb][:, 0:128], lhsT=Phi[:, ch, :], rhs=S_[:], start=True, stop=True))
                        si += 1
                        P.op('dve', [f'ps{5 + yb}', 'rw_D'], [f'rw_S{si % 2}'], lambda e, ch=ch, yb=yb, si=si: e.tensor_tensor(
                            out=Sbd[si % 2][:], in0=self.ps[5 + yb][:, 0:128], in1=Dd[:, ch, :], op=ALU.add))
                    self.stopat(8)
                self.sc_in('rw_fin', hp == 0 and l == 0)
                for (t0, n) in TILES:
                    if not with_ctx and t0 < NCTX:
                        continue
                    ya = yacc[:, t0:t0 + n]
                    P.op('act', ['rw_yacc'], ['rw_sq5'], lambda e, ya=ya, n=n: e.activation(out=sq5[:, :n], in_=ya, func=AF.Copy))
                    P.op('pe', ['rw_sq5', 'bd_bf'], ['ps4'], lambda e, n=n: e.matmul(self.ps[4][:, :n], lhsT=self.bd_bf[:], rhs=sq5[:, :n], start=True, stop=True))
                    P.op('dve', ['ps4', 'rw_yacc'], ['rw_t5'], lambda e, ya=ya, n=n: e.scalar_tensor_tensor(
                        out=t5[:, :n], in0=self.ps[4][:, :n], scalar=-1.0 / 64, in1=ya, op0=ALU.mult, op1=ALU.add))
                    P.op('act', ['rw_t5'], ['rw_sq5'], lambda e, n=n: e.activation(out=sq5[:, :n], in_=t5[:, :n], func=AF.Square))
                    P.op('pe', ['rw_sq5', 'bd_bf'], ['ps4'], lambda e, n=n: e.matmul(self.ps[4][:, :n], lhsT=self.bd_bf[:], rhs=sq5[:, :n], start=True, stop=True))
                    P.op('act', ['ps4'], ['rw_rt'], lambda e, n=n: e.activation(out=rt[:, :n], in_=self.ps[4][:, :n], func=AF.Sqrt, scale=1.0 / 64, bias=geps[:, 0:1]))
                    P.op('dve', ['rw_rt'], ['rw_rt'], lambda e, n=n: e.reciprocal(out=rt[:, :n], in_=rt[:, :n]))
                    P.op('dve', ['rw_t5', 'rw_rt'], ['rw_t5'], lambda e, n=n: e.tensor_tensor(out=t5[:, :n], in0=t5[:, :n], in1=rt[:, :n], op=ALU.mult))
                    P.op('dve', ['rw_t5', f'vecs{l}'], ['rw_t5'], lambda e, n=n: e.tensor_scalar(
                        out=t5[:, :n], in0=t5[:, :n], scalar1=gng[:, hp:hp + 1], scalar2=gnb[:, hp:hp + 1], op0=ALU.mult, op1=ALU.add))
                    P.op('dve', ['rw_t5', 'rw_bon'], ['rw_t5'], lambda e, t0=t0, n=n: e.tensor_tensor(out=t5[:, :n], in0=t5[:, :n], in1=bon[:, t0:t0 + n], op=ALU.add))
                    P.dma(gsb[:, :n], self.g2T[cs, t0:t0 + n], ['g2T'], ['rw_g'])
                    P.op('dve', ['rw_t5', 'rw_g'], ['rw_vb'], lambda e, t0=t0, n=n: e.tensor_tensor(out=yo[:, t0:t0 + n], in0=t5[:, :n], in1=gsb[:, :n], op=ALU.mult))
                self.sc_out()
                t_lo = 0 if with_ctx else NCTX
                P.dma(self.yT[2, cs, t_lo:], yo[:, t_lo:], ['rw_vb'], ['yT'])
            if 'rwkv' in self.debug:
                self.dbg(f'yrw{l}', lambda o: P.dma(o, self.yT[2], ['yT'], []), [1024, TT], BF16)


    def select_pass(self):
        nc, P = self.nc, self.P
        H = SEQ // 2
        with contextlib.ExitStack() as st:
            sw = self.sb(st, "sp_w", [128, 2])
            P.dma(sw[:], self.selw, [], ['sp_w'])
            fa = [self.sb(st, f"sp_fa{i}", [128, 4, H]) for i in range(2)]
            fb = [self.sb(st, f"sp_fb{i}", [128, 4, H]) for i in range(2)]
            ha = [self.sb(st, f"sp_ha{i}", [128, 8, H], BF16) for i in range(2)]
            hb = [self.sb(st, f"sp_hb{i}", [128, 8, H], BF16) for i in range(2)]
            jobs = []
            xv = self.xT.rearrange("(c p) t -> p c t", p=128)
            xo = self.xsel.rearrange("(c p) t -> p c t", p=128)
            for c0 in range(0, KC, 4):
                jobs.append((fa, fb, 'f', xv[:, c0:c0 + 4, :], xo[:, c0:c0 + 4, :], 'xT'))
            yv = self.yT.rearrange("i (c p) t -> p (i c) t", p=128)
            yo = self.ysel.rearrange("i (c p) t -> p (i c) t", p=128)
            for c0 in range(0, 24, 8):
                jobs.append((ha, hb, 'h', yv[:, c0:c0 + 8, :], yo[:, c0:c0 + 8, :], 'yT'))
            cnt = {'f': 0, 'h': 0}
            for (ta, tb, kind, sv, dv, rk_) in jobs:
                i = cnt[kind] % 2
                cnt[kind] += 1
                a, b = ta[i], tb[i]
                ak, bk = f'sp_{kind}a{i}', f'sp_{kind}b{i}'
                P.dma(a[:], sv[:, :, NCTX:NCTX + H], [rk_], [ak])
                P.dma(b[:], sv[:, :, NCTX + H:NCTX + 2 * H], [rk_], [bk], q='act')
                P.op('dve', [ak, 'sp_w'], [ak], lambda e, a=a: e.tensor_scalar(
                    out=a[:], in0=a[:], scalar1=sw[:, 0:1], scalar2=None, op0=ALU.mult))
                P.op('dve', [ak, bk, 'sp_w'], [ak], lambda e, a=a, b=b: e.scalar_tensor_tensor(
                    out=a[:], in0=b[:], scalar=sw[:, 1:2], in1=a[:], op0=ALU.mult, op1=ALU.add))
                P.dma(dv, a[:], [ak], ['sel_out'])

    def merge_moe(self, l):
        nc, P = self.nc, self.P
        with_ctx = l < DEPTH - 1
        last = l == DEPTH - 1
        src = (self.xT0 if l == 0 else self.xT).rearrange("(kc p) t -> p kc t", p=128)
        dstx = self.xT.rearrange("(kc p) t -> p kc t", p=128)
        dsto = self.outT.rearrange("(kc p) t -> p kc t", p=128)
        gTv = self.gT.rearrange("(i c p) t -> p i c t", i=3, p=128)
        yTv = self.yT.rearrange("i (c p) t -> p i c t", p=128)
        tiles = TILES
        if last:
            self.select_pass()
            P.barrier()
            src = self.xsel.rearrange("(kc p) t -> p kc t", p=128)
            gTv = self.gsel.rearrange("(i c p) t -> p i c t", i=3, p=128)
            yTv = self.ysel.rearrange("i (c p) t -> p i c t", p=128)
            tiles = [(0, 512), (512, 512)]
        wbv = self.w_branch[l].rearrange("i (kc p) c -> p i kc c", p=128)
        wov = self.w_out[l].rearrange("(kc p) c -> p kc c", p=128)
        BIG = 1.0e4
        with contextlib.ExitStack() as st:
            xg = self.sb(st, "mm_xg", [128, KC, 512])
            rw32 = self.sb(st, "mm_rw32", [128, KC, 16])
            rbias = self.sb(st, "mm_rbias", [128, 16])
            sel = self.sb(st, "mm_sel", [16, 16, 128])
            P.dma(rw32[:], self.router_w.rearrange("(kc p) e -> p kc e", p=128), [], ['mm_rw32'])
            P.dma(rbias[:], self.router_b[0:1, :].partition_broadcast(128), [], ['mm_rbias'])
            P.op('dve', ['cst_f'], ['mm_sel'], lambda e: e.tensor_copy(
                out=sel[:], in_=self.ident32[0:16, 0:16].unsqueeze(2).to_broadcast([16, 16, 128])))
            wld = 0
            for (t0, n) in tiles:
                if not last and t0 < NCTX and not with_ctx:
                    continue
                j = 1 if (t0 < NCTX and not last) else 0
                nb = n // 128
                P.dma(xg[:, :, :n], src[:, :, t0:t0 + n], ['xT'], ['mm_xg'])
                with contextlib.ExitStack() as s2:
                    yb = self.sb(s2, "mm_y", [128, 3, 8, 512], BF16)
                    mg = self.sb(s2, "mm_mg", [128, KC, 512], BF16)
                    gb = [self.sb(s2, f"mm_g{i}", [128, 3, 512], BF16) for i in range(2)]
                    wb = [self.sb(s2, f"mm_wb{i}", [128, 3, 8, 512], BF16) for i in range(2)]
                    wo = [self.sb(s2, f"mm_wo{i}", [128, KC, 512], BF16) for i in range(2)]
                    m32 = self.sb(s2, "mm_m32", [128, 512])
                    tm = self.sb(s2, "mm_tm", [128, 512])
                    for i in range(3):
                        P.dma(yb[:, i, :, :n], yTv[:, i, :, t0:t0 + n], ['yT'], ['mm_y'])
                    pi = 0
                    for cb in range(4):
                        wk = f'mm_wb{cb % 2}'
                        for i in range(3):
                            P.dma(wb[cb % 2][:, i], wbv[:, i, :, cb * 512:(cb + 1) * 512], [], [wk], q='pool')
                        for dcl in range(4):
                            dc = cb * 4 + dcl
                            gk = f'mm_g{dc % 2}'
                            P.dma(gb[dc % 2][:, :, :n], gTv[:, :, dc, t0:t0 + n], ['gT'], [gk])
                            for i in range(3):
                                pst, pk = self.ps[pi % 4], f'ps{pi % 4}'
                                pi += 1
                                for kc in range(8):
                                    P.op('pe', [wk, 'mm_y'], [pk], lambda e, i=i, kc=kc, cb=cb, dcl=dcl, pst=pst: e.matmul(
                                        pst[:, :n], lhsT=wb[cb % 2][:, i, kc, dcl * 128:(dcl + 1) * 128], rhs=yb[:, i, kc, :n],
                                        start=(kc == 0), stop=(kc == 7)))
                                if i == 0:
                                    P.op('dve', [pk, gk], ['mm_m32'], lambda e, pst=pst, dc=dc: e.tensor_tensor(
                                        out=m32[:, :n], in0=pst[:, :n], in1=gb[dc % 2][:, 0, :n], op=ALU.mult))
                                else:
                                    P.op('dve', [pk, gk], ['mm_tm'], lambda e, pst=pst, dc=dc, i=i: e.tensor_tensor(
                                        out=tm[:, :n], in0=pst[:, :n], in1=gb[dc % 2][:, i, :n], op=ALU.mult))
                                    if i == 1:
                                        P.op('dve', ['mm_tm', 'mm_m32'], ['mm_m32'], lambda e: e.tensor_tensor(
                                            out=m32[:, :n], in0=m32[:, :n], in1=tm[:, :n], op=ALU.add))
                                    else:
                                        P.op('dve', ['mm_tm', 'mm_m32'], ['mm_mg'], lambda e, dc=dc: e.tensor_tensor(
                                            out=mg[:, dc, :n], in0=m32[:, :n], in1=tm[:, :n], op=ALU.add))
                    for cb in range(4):
                        wk = f'mm_wo{cb % 2}'
                        P.dma(wo[cb % 2][:], wov[:, :, cb * 512:(cb + 1) * 512], [], [wk], q='pool')
                        for dcl in range(4):
                            dc = cb * 4 + dcl
                            pst, pk = self.ps[pi % 4], f'ps{pi % 4}'
                            pi += 1
                            for kc in range(KC):
                                P.op('pe', [wk, 'mm_mg'], [pk], lambda e, kc=kc, cb=cb, dcl=dcl, pst=pst: e.matmul(
                                    pst[:, :n], lhsT=wo[cb % 2][:, kc, dcl * 128:(dcl + 1) * 128], rhs=mg[:, kc, :n],
                                    start=(kc == 0), stop=(kc == KC - 1)))
                            P.op('dve', [pk, 'mm_xg', f'mod{l}'], ['mm_xg'], lambda e, pst=pst, dc=dc: e.scalar_tensor_tensor(
                                out=xg[:, dc, :n], in0=pst[:, :n], scalar=self.mod[l][:, 32 + dc, j:j + 1], in1=xg[:, dc, :n],
                                op0=ALU.mult, op1=ALU.add))
                    if 'x1' in self.debug:
                        self.dbg(f'x1_{l}_{t0}', lambda o: P.dma(o.rearrange("(kc p) t -> p kc t", p=128), xg[:, :, :n], ['mm_xg'], []), [D, n])
                    P.barrier()
                with contextlib.ExitStack() as s2:
                    h2 = self.sb(s2, "mo_h2", [128, KC, 512], BF16)
                    sq = h2
                    rt = self.sb(s2, "mo_rt", [128, 512])
                    lg = self.sb(s2, "mo_lg", [16, 512])
                    R = {nm: self.sb(s2, "mo_" + nm, [128, 4, 16]) for nm in ('s', 'bz', 'eq', 'b2', 'mb', 'e1', 'w')}
                    r4 = {nm: self.sb(s2, "mo_" + nm, [128, 4, 4]) for nm in ('m1', 'm2', 'gsel')}
                    r1 = {nm: self.sb(s2, "mo_" + nm, [128, 4]) for nm in ('gmax', 't1', 't2', 'ws')}
                    cT = self.sb(s2, "mo_cT", [16, 512])
                    bce = [self.sb(s2, f"mo_bce{i}", [128, 512]) for i in range(2)]
                    sg = [self.sb(s2, f"mo_sg{i}", [128, 512]) for i in range(2)]
                    act = [self.sb(s2, f"mo_act{i}", [128, 4, 512], BF16) for i in range(2)]
                    wg = [self.sb(s2, f"mo_wg{i}", [128, KC, 512], BF16) for i in range(2)]
                    wu = [self.sb(s2, f"mo_wu{i}", [128, KC, 512], BF16) for i in range(2)]
                    s3 = contextlib.ExitStack()
                    xn = self.sb(s3, "mo_xn", [128, KC, 512])
                    P.dma(wg[0][:], self.moe_g[l, 0].rearrange("(kc p) f -> p kc f", p=128), [], ['mo_wg0'], q='pool')
                    P.dma(wu[0][:], self.moe_u[l, 0].rearrange("(kc p) f -> p kc f", p=128), [], ['mo_wu0'], q='pool')
                    P.op('act', ['mm_xg'], ['mo_h2'], lambda e: e.activation(out=sq[:, :, :n], in_=xg[:, :, :n], func=AF.Square))
                    for kc in range(KC):
                        P.op('pe', ['mo_h2', 'ones_bf'], ['ps6'], lambda e, kc=kc: e.matmul(
                            self.ps[6][:, :n], lhsT=self.ones_bf[:], rhs=sq[:, kc, :n], start=(kc == 0), stop=(kc == KC - 1)))
                    P.op('act', ['ps6'], ['mo_rt'], lambda e: e.activation(out=rt[:, :n], in_=self.ps[6][:, :n], func=AF.Sqrt,
                                                                      scale=1.0 / D, bias=self.eps_t[:, 0:1]))
                    P.op('dve', ['mo_rt'], ['mo_rt'], lambda e: e.reciprocal(out=rt[:, :n], in_=rt[:, :n]))
                    P.op('dve', ['mm_xg', 'mo_rt'], ['mo_xn'], lambda e: e.tensor_tensor(
                        out=xn[:, :, :n], in0=xg[:, :, :n], in1=rt[:, :n].unsqueeze(1).to_broadcast([128, KC, n]), op=ALU.mult))
                    for kc in range(KC):
                        P.op('act', ['mo_xn', f'gm2_{l}', f'mod{l}'], ['mo_xn'], lambda e, kc=kc: e.activation(
                            out=xn[:, kc, :n], in_=xn[:, kc, :n], func=AF.Identity,
                            scale=self.gm2[l][:, kc, j:j + 1], bias=self.mod[l][:, 48 + kc, j:j + 1]))
                    P.op('dve', ['mo_xn'], ['mo_h2'], lambda e: e.tensor_copy(out=h2[:, :, :n], in_=xn[:, :, :n]))
                    for kc in range(KC):
                        P.op('pe', ['mo_xn', 'mm_rw32'], ['ps5'], lambda e, kc=kc: e.matmul(
                            self.ps[5][0:16, :n], lhsT=rw32[:, kc, :], rhs=xn[:, kc, :n], start=(kc == 0), stop=(kc == KC - 1)))
                    P.op('act', ['ps5'], ['mo_lg'], lambda e: e.activation(out=lg[:, :n], in_=self.ps[5][0:16, :n], func=AF.Copy))
                    for b_ in range(nb):
                        P.op('pe', ['mo_lg', 'cst_f'], ['ps4'], lambda e, b_=b_: e.transpose(
                            self.ps[4][:, b_ * 16:(b_ + 1) * 16], lg[0:16, b_ * 128:(b_ + 1) * 128], self.ident32[0:16, 0:16]))
                    s_, bz, eq, b2, mb, e1, w_ = (R[k][:, :nb, :] for k in ('s', 'bz', 'eq', 'b2', 'mb', 'e1', 'w'))
                    m1, m2, gsel = (r4[k][:, :nb, :] for k in ('m1', 'm2', 'gsel'))
                    gmax, t1, t2, ws = (r1[k][:, :nb] for k in ('gmax', 't1', 't2', 'ws'))
                    g4 = lambda a: a.rearrange("p b (g k) -> p b g k", k=4)
                    V = lambda reads, writes, fn: P.op('dve', reads, writes, fn)
                    P.op('act', ['ps4'], ['mo_s'], lambda e: e.activation(
                        out=s_, in_=self.ps[4][:, 0:nb * 16].rearrange("p (b k) -> p b k", k=16), func=AF.Sigmoid))
                    V(['mo_s', 'mm_rbias'], ['mo_bz'], lambda e: e.tensor_tensor(out=bz, in0=s_, in1=rbias[:].unsqueeze(1).to_broadcast([128, nb, 16]), op=ALU.add))
                    V(['mo_bz'], ['mo_m1'], lambda e: e.tensor_reduce(out=m1, in_=g4(bz), axis=AX.X, op=ALU.max))
                    V(['mo_bz', 'mo_m1'], ['mo_eq'], lambda e: e.tensor_tensor(out=g4(eq), in0=g4(bz), in1=m1.unsqueeze(3).to_broadcast([128, nb, 4, 4]), op=ALU.is_equal))
                    V(['mo_eq', 'mo_bz'], ['mo_b2'], lambda e: e.scalar_tensor_tensor(out=b2, in0=eq, scalar=-BIG, in1=bz, op0=ALU.mult, op1=ALU.add))
                    V(['mo_b2'], ['mo_m2'], lambda e: e.tensor_reduce(out=m2, in_=g4(b2), axis=AX.X, op=ALU.max))
                    V(['mo_m1', 'mo_m2'], ['mo_m1'], lambda e: e.tensor_tensor(out=m1, in0=m1, in1=m2, op=ALU.add))
                    V(['mo_m1'], ['mo_gmax'], lambda e: e.tensor_reduce(out=gmax, in_=m1, axis=AX.X, op=ALU.max))
                    V(['mo_m1', 'mo_gmax'], ['mo_gsel'], lambda e: e.tensor_tensor(out=gsel, in0=m1, in1=gmax.unsqueeze(2).to_broadcast([128, nb, 4]), op=ALU.is_equal))
                    V(['mo_gsel'], ['mo_gsel'], lambda e: e.tensor_scalar(out=gsel, in0=gsel, scalar1=BIG, scalar2=-BIG, op0=ALU.mult, op1=ALU.add))
                    V(['mo_bz', 'mo_gsel'], ['mo_mb'], lambda e: e.tensor_tensor(out=g4(mb), in0=g4(bz), in1=gsel.unsqueeze(3).to_broadcast([128, nb, 4, 4]), op=ALU.add))
                    V(['mo_mb'], ['mo_t1'], lambda e: e.tensor_reduce(out=t1, in_=mb, axis=AX.X, op=ALU.max))
                    V(['mo_mb', 'mo_t1'], ['mo_e1'], lambda e: e.tensor_tensor(out=e1, in0=mb, in1=t1.unsqueeze(2).to_broadcast([128, nb, 16]), op=ALU.is_equal))
                    V(['mo_e1', 'mo_mb'], ['mo_b2'], lambda e: e.scalar_tensor_tensor(out=b2, in0=e1, scalar=-BIG, in1=mb, op0=ALU.mult, op1=ALU.add))
                    V(['mo_b2'], ['mo_t2'], lambda e: e.tensor_reduce(out=t2, in_=b2, axis=AX.X, op=ALU.max))
                    V(['mo_b2', 'mo_t2'], ['mo_eq'], lambda e: e.tensor_tensor(out=eq, in0=b2, in1=t2.unsqueeze(2).to_broadcast([128, nb, 16]), op=ALU.is_equal))
                    V(['mo_eq', 'mo_e1'], ['mo_e1'], lambda e: e.tensor_tensor(out=e1, in0=e1, in1=eq, op=ALU.add))
                    V(['mo_e1', 'mo_s'], ['mo_w'], lambda e: e.tensor_tensor(out=w_, in0=e1, in1=s_, op=ALU.mult))
                    V(['mo_w'], ['mo_ws'], lambda e: e.tensor_reduce(out=ws, in_=w_, axis=AX.X, op=ALU.add))
                    V(['mo_ws'], ['mo_ws'], lambda e: e.reciprocal(out=ws, in_=ws))
                    V(['mo_w', 'mo_ws'], ['mo_w'], lambda e: e.tensor_tensor(out=w_, in0=w_, in1=ws.unsqueeze(2).to_broadcast([128, nb, 16]), op=ALU.mult))
                    for b_ in range(nb):
                        P.op('pe', ['mo_w', 'cst_f'], ['ps5'], lambda e, b_=b_: e.transpose(
                            self.ps[5][0:16, b_ * 128:(b_ + 1) * 128], R['w'][:, b_, :], self.ident32))
                    P.op('act', ['ps5'], ['mo_cT'], lambda e: e.activation(out=cT[:, :n], in_=self.ps[5][0:16, :n], func=AF.Copy))
                    if 'comb' in self.debug:
                        self.dbg(f'comb_{l}_{t0}', lambda o: P.dma(o, cT[:, :n], ['mo_cT'], []), [16, n])
                    P.barrier()
                    s3.close()
                    s3 = contextlib.ExitStack()
                    wd = [self.sb(s3, f"mo_wd{i}", [128, 4, D], BF16) for i in range(2)]
                    pi = 0
                    for ex in range(16):
                        b = ex % 2
                        if ex > 0:
                            P.dma(wg[b][:], self.moe_g[l, ex].rearrange("(kc p) f -> p kc f", p=128), [], [f'mo_wg{b}'], q='pool')
                            P.dma(wu[b][:], self.moe_u[l, ex].rearrange("(kc p) f -> p kc f", p=128), [], [f'mo_wu{b}'], q='pool')
                        P.dma(wd[b][:], self.moe_d[l, ex].rearrange("(fc p) d -> p fc d", p=128), [], [f'mo_wd{b}'], q='pool')
                        P.op('pe', ['mm_sel', 'mo_cT'], ['ps6'], lambda e, ex=ex: e.matmul(
                            self.ps[6][:, :n], lhsT=sel[:, ex, :], rhs=cT[:, :n], start=True, stop=True), rg=0)
                        P.op('act', ['ps6'], [f'mo_bce{b}'], lambda e, b=b: e.activation(out=bce[b][:, :n], in_=self.ps[6][:, :n], func=AF.Copy))
                        for fc in range(4):
                            pg, pgk = self.ps[pi % 4], f'ps{pi % 4}'
                            pu, puk = self.ps[(pi + 1) % 4], f'ps{(pi + 1) % 4}'
                            pi += 2
                            for kc in range(KC):
                                P.op('pe', [f'mo_wg{b}', 'mo_h2'], [pgk], lambda e, kc=kc, fc=fc, b=b, pg=pg: e.matmul(
                                    pg[:, :n], lhsT=wg[b][:, kc, fc * 128:(fc + 1) * 128], rhs=h2[:, kc, :n], start=(kc == 0), stop=(kc == KC - 1)))
                            for kc in range(KC):
                                P.op('pe', [f'mo_wu{b}', 'mo_h2'], [puk], lambda e, kc=kc, fc=fc, b=b, pu=pu: e.matmul(
                                    pu[:, :n], lhsT=wu[b][:, kc, fc * 128:(fc + 1) * 128], rhs=h2[:, kc, :n], start=(kc == 0), stop=(kc == KC - 1)))
                            sb_ = fc % 2
                            P.op('act', [pgk], [f'mo_sg{sb_}'], lambda e, pg=pg, sb_=sb_: e.activation(out=sg[sb_][:, :n], in_=pg[:, :n], func=AF.Silu))
                            P.op('dve', [puk, f'mo_sg{sb_}'], [f'mo_sg{sb_}'], lambda e, pu=pu, sb_=sb_: e.tensor_tensor(
                                out=sg[sb_][:, :n], in0=pu[:, :n], in1=sg[sb_][:, :n], op=ALU.mult))
                            P.op('dve', [f'mo_sg{sb_}', f'mo_bce{b}'], [f'mo_act{b}'], lambda e, sb_=sb_, b=b, fc=fc: e.tensor_tensor(
                                out=act[b][:, fc, :n], in0=sg[sb_][:, :n], in1=bce[b][:, :n], op=ALU.mult))
                        for dc in range(KC):
                            pd, pdk = self.ps[pi % 4], f'ps{pi % 4}'
                            pi += 1
                            for fc in range(4):
                                P.op('pe', [f'mo_wd{b}', f'mo_act{b}'], [pdk], lambda e, fc=fc, dc=dc, b=b, pd=pd: e.matmul(
                                    pd[:, :n], lhsT=wd[b][:, fc, dc * 128:(dc + 1) * 128], rhs=act[b][:, fc, :n], start=(fc == 0), stop=(fc == 3)))
                            P.op('dve', [pdk, 'mm_xg', f'mod{l}'], ['mm_xg'], lambda e, pd=pd, dc=dc: e.scalar_tensor_tensor(
                                out=xg[:, dc, :n], in0=pd[:, :n], scalar=self.mod[l][:, 80 + dc, j:j + 1], in1=xg[:, dc, :n],
                                op0=ALU.mult, op1=ALU.add))
                    if last:
                        P.dma(dsto[:, :, t0:t0 + n], xg[:, :, :n], ['mm_xg'], ['outT'])
                    else:
                        P.dma(dstx[:, :, t0:t0 + n], xg[:, :, :n], ['mm_xg'], ['xT'])
                    if 'x2' in self.debug:
                        self.dbg(f'x2_{l}_{t0}', lambda o: P.dma(o.rearrange("(kc p) t -> p kc t", p=128), xg[:, :, :n], ['mm_xg'], []), [D, n])
                    P.barrier()
                    s3.close()

    def final_out(self):
        P = self.P
        P.dma(self.outT, self.xT[:, NCTX:], ['xT'], [])


def na_tables(rpb):
    c = np.arange(64)
    cs = np.clip(c - 8, 0, 48)
    kc = np.arange(64)
    inwin = (kc[:, None] >= cs[None, :]) & (kc[:, None] < cs[None, :] + 16)
    dc = np.clip(kc[:, None] - c[None, :] + 15, 0, 30)
    out = np.full((16, 2, 64, 14, 64), NEG, np.float32)
    for jj in range(2):
        for dr in range(14):
            g = rpb[:, dr + jj][:, dc]
            out[:, jj, :, dr, :] = np.where(inwin[None], g, np.float32(NEG))
    return out.reshape(16, 128, 14 * 64)


NCONST = 512 + 2 * SEQ + 384


def make_consts():
    c = np.zeros((128, NCONST), np.float32)
    p = np.arange(128)
    c[p, p] = 1.0
    c[p, 128 + (p ^ 32)] = 1.0
    j = p[:, None]; i = p[None, :]
    same = (j // 64) == (i // 64)
    c[:, 256:384] = (same & (j <= i)).astype(np.float32)
    c[:, 384:512] = (same & (j >= i)).astype(np.float32)
    t = np.arange(SEQ)
    pos = np.where(p[:, None] < 64, (t // 64)[None, :], (t % 64)[None, :]).astype(np.float32)
    inv = (10000.0 ** (-np.arange(0, 64, 2, dtype=np.float32) / 64)).astype(np.float32)
    ang = pos * inv[(p % 32)][:, None]
    c[:, 512:512 + SEQ] = np.cos(ang)
    sgn = np.where((p % 64) < 32, -1.0, 1.0).astype(np.float32)
    c[:, 512 + SEQ:512 + 2 * SEQ] = np.sin(ang) * sgn[:, None]
    o = 512 + 2 * SEQ
    c[:, o:o + 128] = (same & (j < i)).astype(np.float32)
    c[:, o + 128:o + 256] = (same & (j > i)).astype(np.float32)
    c[:, o + 256:o + 384] = same.astype(np.float32)
    return c


def host_inputs(inp, b, half=0):
    xT0 = np.ascontiguousarray(np.concatenate([inp['ctx'][b], inp['x'][b]], axis=0).T)
    cT = np.stack([fm(inp['c'][b]), fm(inp['c_ctx'])], axis=-1).reshape(128, 32)
    return {
        'xT0': xT0, 'cT': np.ascontiguousarray(cT),
        'ada_w': inp['ada_w'], 'w_in': inp['w_in'],
        'vecs': np.stack([pack_vecs(inp, l) for l in range(DEPTH)]),
        'natab': np.stack([na_tables(inp['na_rpb'][l]) for l in range(DEPTH)]),
        'consts': make_consts(), 'gla_gate_w2': inp['gla_gate_w2'],
        'rw_w2': inp['rw_w2'], 'rw_a2': inp['rw_a2'], 'rw_g2': inp['rw_g2'],
        'w_branch': inp['w_branch'], 'w_out': inp['w_out'], 'router_w': inp['router_w'],
        'router_bias': inp['router_bias'].reshape(1, 16),
        'moe_w_gate': inp['moe_w_gate'], 'moe_w_up': inp['moe_w_up'], 'moe_w_down': inp['moe_w_down'],
        'selw': np.tile(np.asarray([[1.0, 0.0]] if half == 0 else [[0.0, 1.0]], np.float32), (128, 1)),
    }


def kernel(**inputs):
    inp = {k: np.asarray(v) for k, v in inputs.items()}
    bld = Builder()
    nc = bld.build()
    in_maps = [host_inputs(inp, c % 4, c // 4) for c in range(8)]
    res = run_bass_kernel_spmd(nc, in_maps, core_ids=list(range(8)))
    out = np.stack([np.concatenate([res.results[b]["outT"].T, res.results[b + 4]["outT"].T], axis=0) for b in range(4)], axis=0)
    return np.ascontiguousarray(out).astype(np.float32)
```
